# Optimizing a Trainium2 kernel written in Bass

```python
import math, functools
import jax, jax.numpy as jnp
from jax import lax
import numpy as np

D_MODEL = 2048
BATCH = 4
SEQ = 4096
DEPTH = 2

GRID_W = 64
CTX_LEN = 256
N_BRANCH = 4
BRANCH_W = 512
NORM_EPS = 1e-6
Q_BLOCK = 128
ROPE_THETA = 10000.0
SHORT_CONV = 3

GDN_HEADS = 4
GDN_HEAD_DIM = 128
GDN_CHUNK = 64

MLA_HEADS = 4
MLA_Q_LORA = 512
MLA_KV_LORA = 256
MLA_NOPE = 128
MLA_ROPE = 64
MLA_V = 128

GQA_HEADS = 8
GQA_KV_HEADS = 2
GQA_HEAD_DIM = 64

HY_WIDTH = 512
HY_ORDER = 2
HY_EMB = 33
HY_HIDDEN = 64
HY_DECAY_TARGET = 1e-2
HY_FAST_DECAY = 0.3
HY_SLOW_DECAY = 1.5

N_EXPERTS = 16
N_GROUPS = 4
EXPERTS_PER_GROUP = N_EXPERTS // N_GROUPS
TOP_K = 2
D_EXPERT = 512

GDN_W = GDN_HEADS * GDN_HEAD_DIM
IN_WIDTHS = (GDN_W, GDN_W, GDN_W, GDN_W, 2 * GDN_HEADS, 2 * GDN_HEADS,
             MLA_Q_LORA, MLA_KV_LORA, MLA_ROPE,
             GQA_HEADS * GQA_HEAD_DIM, GQA_KV_HEADS * GQA_HEAD_DIM, GQA_KV_HEADS * GQA_HEAD_DIM,
             (HY_ORDER + 1) * HY_WIDTH)
MIX_IN = sum(IN_WIDTHS)
IN_DIM = MIX_IN + N_BRANCH * D_MODEL

kernel_name = 'hybrid_diffusion_trunk'


def rms_norm(x, w):
    xf = x.astype(jnp.float32)
    y = xf * lax.rsqrt(jnp.mean(xf * xf, axis=-1, keepdims=True) + NORM_EPS)
    return (y * w.astype(jnp.float32)).astype(x.dtype)


def l2_normalize(x):
    xf = x.astype(jnp.float32)
    return xf * lax.rsqrt(jnp.sum(xf * xf, axis=-1, keepdims=True) + NORM_EPS)


def depthwise_conv_centred(u, w):
    k = w.shape[0]
    return lax.conv_general_dilated(u, w[:, None, :].astype(u.dtype), window_strides=(1,),
                                    padding=[(k // 2, k // 2)],
                                    dimension_numbers=('NWC', 'WIO', 'NWC'),
                                    feature_group_count=u.shape[-1])


def axial_rope_tables(rows, rot_dim):
    n_freq = rot_dim // 4
    freqs = ROPE_THETA ** (-jnp.arange(n_freq, dtype=jnp.float32) / n_freq)
    row = jnp.repeat(jnp.arange(rows, dtype=jnp.float32), GRID_W)
    col = jnp.tile(jnp.arange(GRID_W, dtype=jnp.float32), rows)
    ang = jnp.concatenate([row[:, None] * freqs, col[:, None] * freqs], axis=-1)
    return jnp.cos(ang), jnp.sin(ang)


def apply_rope(x, cos, sin):
    half = x.shape[-1] // 2
    x1, x2 = x[..., :half], x[..., half:]
    cos, sin = cos[None, :, None, :], sin[None, :, None, :]
    return jnp.concatenate([x1 * cos - x2 * sin, x2 * cos + x1 * sin], axis=-1).astype(x.dtype)


def block_attention(q, k, v):
    b, sq = q.shape[:2]
    scale = q.shape[-1] ** -0.5
    qb = q.reshape(b, sq // Q_BLOCK, Q_BLOCK, *q.shape[2:]).swapaxes(0, 1)

    def attend(qi):
        s = jnp.einsum('bqhgd,bkhd->bhgqk', qi, k).astype(jnp.float32) * scale
        p = jax.nn.softmax(s, axis=-1).astype(v.dtype)
        return jnp.einsum('bhgqk,bkhe->bqhge', p, v)

    ob = lax.map(attend, qb)
    return ob.swapaxes(0, 1).reshape(b, sq, *ob.shape[3:])


def gdn_prep(q, k, v, a, bt, conv_w, a_log, dt_bias):
    b, l = q.shape[:2]
    qkv = jax.nn.silu(depthwise_conv_centred(jnp.concatenate([q, k, v], axis=-1), conv_w)).astype(jnp.float32)
    q, k, v = jnp.split(qkv, 3, axis=-1)
    hd = (b, l, GDN_HEADS, GDN_HEAD_DIM)
    q = l2_normalize(q.reshape(hd)) * GDN_HEAD_DIM ** -0.5
    k = l2_normalize(k.reshape(hd))
    v = v.reshape(hd)
    a = a.astype(jnp.float32).reshape(b, l, 2, GDN_HEADS)
    g = -jnp.exp(a_log.astype(jnp.float32)) * jax.nn.softplus(a + dt_bias.astype(jnp.float32))
    beta = jax.nn.sigmoid(bt.astype(jnp.float32).reshape(b, l, 2, GDN_HEADS))
    return q, k, v, g, beta


def gated_delta_rule(q, k, v, g, beta, state, with_out):
    b, l, h, _ = q.shape
    dv = v.shape[-1]
    c = GDN_CHUNK
    n = l // c

    def to_chunks(t):
        t = t.reshape(b, n, c, h, *t.shape[3:])
        return jnp.moveaxis(t, (1, 3), (0, 2))

    qc, kc, vc, bc = to_chunks(q), to_chunks(k), to_chunks(v), to_chunks(beta)
    gc = jnp.cumsum(to_chunks(g), axis=-1)
    idx = jnp.arange(c)
    lower = idx[:, None] >= idx[None, :]
    strict = idx[:, None] > idx[None, :]
    diff = gc[..., :, None] - gc[..., None, :]
    decay = jnp.where(lower, jnp.exp(jnp.where(lower, diff, 0.0)), 0.0)
    kb = kc * bc[..., None]
    a = jnp.where(strict, jnp.einsum('nbhid,nbhjd->nbhij', kb, kc) * decay, 0.0)
    solve = functools.partial(lax.linalg.triangular_solve, left_side=True, lower=True, unit_diagonal=True)
    u = solve(a, vc * bc[..., None])
    w = solve(a, kb * jnp.exp(gc)[..., None])
    g_last = gc[..., -1]
    k_dec = kc * jnp.exp(g_last[..., None] - gc)[..., None]
    xs = (u, w, k_dec, g_last)
    if with_out:
        qk = jnp.where(lower, jnp.einsum('nbhid,nbhjd->nbhij', qc, kc) * decay, 0.0)
        xs = xs + (qc * jnp.exp(gc)[..., None], qk)

    def step(s, inp):
        u_i, w_i, kd_i, gl_i = inp[:4]
        v_new = u_i - jnp.einsum('bhck,bhkv->bhcv', w_i, s)
        s_new = s * jnp.exp(gl_i)[..., None, None] + jnp.einsum('bhck,bhcv->bhkv', kd_i, v_new)
        if not with_out:
            return s_new, None
        qd_i, qk_i = inp[4:]
        o = jnp.einsum('bhck,bhkv->bhcv', qd_i, s) + jnp.einsum('bhij,bhjv->bhiv', qk_i, v_new)
        return s_new, o

    state, o = lax.scan(step, state, xs)
    if not with_out:
        return None, state
    o = jnp.moveaxis(o, (0, 2), (1, 3)).reshape(b, l, h, dv)
    return o, state


def gdn_output(o, z, norm_w):
    b, l = z.shape[:2]
    zh = z.reshape(b, l, GDN_HEADS, GDN_HEAD_DIM).astype(jnp.float32)
    y = rms_norm(o, norm_w) * jax.nn.silu(zh)
    return y.reshape(b, l, GDN_W).astype(z.dtype)


def gdn_mixer(p_lat, p_ctx, conv_w, a_log, dt_bias, norm_w, ctx_out):
    lat = gdn_prep(p_lat[0], p_lat[1], p_lat[2], p_lat[4], p_lat[5], conv_w, a_log, dt_bias)
    ctx = gdn_prep(p_ctx[0], p_ctx[1], p_ctx[2], p_ctx[4], p_ctx[5], conv_w, a_log, dt_bias)
    b = p_lat[0].shape[0]
    s0 = jnp.zeros((b, GDN_HEADS, GDN_HEAD_DIM, GDN_HEAD_DIM), jnp.float32)
    o_lat, o_ctx = 0.0, 0.0
    for direction in range(2):
        flip = (lambda t: t[:, ::-1]) if direction else (lambda t: t)

        def seq_args(s):
            q, k, v, g, beta = s
            return flip(q), flip(k), flip(v), flip(g[:, :, direction]), flip(beta[:, :, direction])

        oc, s_ctx = gated_delta_rule(*seq_args(ctx), s0, ctx_out)
        ol, _ = gated_delta_rule(*seq_args(lat), s_ctx, True)
        o_lat = o_lat + flip(ol)
        if ctx_out:
            o_ctx = o_ctx + flip(oc)
    out_lat = gdn_output(o_lat, p_lat[3], norm_w)
    out_ctx = gdn_output(o_ctx, p_ctx[3], norm_w) if ctx_out else None
    return out_lat, out_ctx


def mla_q(cq, q_norm_w, w_uq, rope):
    q = jnp.einsum('blr,rhe->blhe', rms_norm(cq, q_norm_w), w_uq)
    if rope is not None:
        q = jnp.concatenate([q[..., :MLA_NOPE], apply_rope(q[..., MLA_NOPE:], *rope)], axis=-1)
    return q[:, :, :, None, :]


def mla_kv(ckv, k_rope, kv_norm_w, w_ukv, rope):
    kv = jnp.einsum('blr,rhe->blhe', rms_norm(ckv, kv_norm_w), w_ukv)
    k_nope, v = kv[..., :MLA_NOPE], kv[..., MLA_NOPE:]
    k_rope = k_rope[:, :, None, :]
    if rope is not None:
        k_rope = apply_rope(k_rope, *rope)
    k_rope = jnp.broadcast_to(k_rope, k_nope.shape[:3] + (MLA_ROPE,)).astype(k_nope.dtype)
    return jnp.concatenate([k_nope, k_rope], axis=-1), v


def gqa_q(q, q_norm_w, rope):
    b, l = q.shape[:2]
    q = rms_norm(q.reshape(b, l, GQA_HEADS, GQA_HEAD_DIM), q_norm_w)
    if rope is not None:
        q = apply_rope(q, *rope)
    return q.reshape(b, l, GQA_KV_HEADS, GQA_HEADS // GQA_KV_HEADS, GQA_HEAD_DIM)


def gqa_kv(k, v, k_norm_w, rope):
    b, l = k.shape[:2]
    k = rms_norm(k.reshape(b, l, GQA_KV_HEADS, GQA_HEAD_DIM), k_norm_w)
    if rope is not None:
        k = apply_rope(k, *rope)
    return k, v.reshape(b, l, GQA_KV_HEADS, GQA_HEAD_DIM)


def hyena_filters(length, w1, b1, w2, b2, w3, sin_freq):
    t = jnp.arange(length, dtype=jnp.float32)
    bands = (HY_EMB - 1) // 2
    f = jnp.linspace(1e-4, bands - 1, bands, dtype=jnp.float32)
    phase = (2.0 * math.pi / length) * t[:, None] * f[None, :]
    feats = jnp.concatenate([t[:, None] / (length - 1), jnp.cos(phase), -jnp.sin(phase)], axis=-1)
    hid = jnp.sin(sin_freq[0] * (feats @ w1 + b1))
    hid = jnp.sin(sin_freq[1] * (hid @ w2 + b2))
    filt = (hid @ w3).astype(jnp.float32)
    centre = length // 2
    dist = jnp.abs(t - centre) / centre
    deltas = jnp.abs(jnp.linspace(math.log(HY_DECAY_TARGET) / HY_SLOW_DECAY,
                                  math.log(HY_DECAY_TARGET) / HY_FAST_DECAY,
                                  HY_ORDER * HY_WIDTH, dtype=jnp.float32))
    filt = filt * jnp.exp(-dist[:, None] * deltas[None, :])
    filt = filt / jnp.sum(jnp.abs(filt), axis=0, keepdims=True)
    return filt.reshape(length, HY_ORDER, HY_WIDTH)


def fft_conv_centred(u, h):
    l = u.shape[1]
    n = 2 * l
    uf = jnp.fft.rfft(u.astype(jnp.float32), n=n, axis=1)
    hf = jnp.fft.rfft(h.astype(jnp.float32), n=n, axis=0)
    y = jnp.fft.irfft(uf * hf[None], n=n, axis=1)
    return y[:, l // 2: l // 2 + l]


def hyena_mixer(u, conv_w, filt, bias):
    parts = jnp.split(depthwise_conv_centred(u, conv_w).astype(jnp.float32), HY_ORDER + 1, axis=-1)
    z = parts[0]
    for o in range(HY_ORDER):
        z = parts[o + 1] * (fft_conv_centred(z, filt[:, o]) + bias[o] * z)
    return z


def merge_branches(branches, gate_logits, w_branch, w_out):
    stacked = jnp.stack(branches, axis=-2)
    gates = jax.nn.sigmoid(gate_logits.reshape(*gate_logits.shape[:-1], N_BRANCH, D_MODEL))
    merged = jnp.einsum('blnd,blnd->bld', gates, jnp.einsum('blnw,nwd->blnd', stacked, w_branch))
    return merged @ w_out


def moe_ffn(h, w_router, router_bias, w_gate, w_up, w_down):
    scores = jax.nn.sigmoid(jnp.einsum('bld,de->ble', h, w_router).astype(jnp.float32))
    sel = scores + router_bias.astype(jnp.float32)
    grp = sel.reshape(*sel.shape[:-1], N_GROUPS, EXPERTS_PER_GROUP)
    grp_score = jnp.sum(lax.top_k(grp, TOP_K)[0], axis=-1)
    grp_mask = jnp.argmax(grp_score, axis=-1)[..., None] == jnp.arange(N_GROUPS)
    expert_mask = jnp.repeat(grp_mask, EXPERTS_PER_GROUP, axis=-1)
    _, e_idx = lax.top_k(jnp.where(expert_mask, sel, -jnp.inf), TOP_K)
    wts = jnp.take_along_axis(scores, e_idx, axis=-1)
    wts = wts / jnp.sum(wts, axis=-1, keepdims=True)
    gate = jnp.sum(jax.nn.one_hot(e_idx, N_EXPERTS, dtype=jnp.float32) * wts[..., None], axis=-2)
    hg = jnp.einsum('bld,edf->blef', h, w_gate)
    hu = jnp.einsum('bld,edf->blef', h, w_up)
    act = jax.nn.silu(hg) * hu * gate[..., None].astype(h.dtype)
    return jnp.einsum('blef,efd->bld', act, w_down)


def setup_inputs(seed: int = 0) -> dict:
    key = jax.random.key(seed)
    keys = iter(jax.random.split(key, 48))

    def normal(shape, scale):
        return jax.random.normal(next(keys), shape, jnp.float32) * scale

    def gain(shape):
        return 1.0 + normal(shape, 0.05)

    a_log = jnp.log(jax.random.uniform(next(keys), (DEPTH, 2, GDN_HEADS), jnp.float32, 1.0, 16.0))
    dt = jnp.exp(jax.random.uniform(next(keys), (DEPTH, 2, GDN_HEADS), jnp.float32,
                                    math.log(1e-3), math.log(1e-1)))
    dt_bias = dt + jnp.log(-jnp.expm1(-dt))
    d = D_MODEL
    return {
        'x': normal((BATCH, SEQ, d), 1.0),
        'c': normal((BATCH, d), 1.0),
        'ctx': normal((BATCH, CTX_LEN, d), 1.0),
        'c_ctx': normal((d,), 1.0),
        'w_ada': normal((DEPTH, d, 6 * d), 0.5 * d ** -0.5),
        'b_ada': normal((DEPTH, 6 * d), 0.02),
        'norm1_w': gain((DEPTH, d)),
        'norm2_w': gain((DEPTH, d)),
        'w_in': normal((DEPTH, d, IN_DIM), d ** -0.5),
        'gdn_conv_w': normal((DEPTH, SHORT_CONV, 3 * GDN_W), SHORT_CONV ** -0.5),
        'gdn_a_log': a_log,
        'gdn_dt_bias': dt_bias,
        'gdn_norm_w': gain((DEPTH, GDN_HEAD_DIM)),
        'mla_q_norm_w': gain((DEPTH, MLA_Q_LORA)),
        'mla_kv_norm_w': gain((DEPTH, MLA_KV_LORA)),
        'mla_w_uq': normal((DEPTH, MLA_Q_LORA, MLA_HEADS, MLA_NOPE + MLA_ROPE), MLA_Q_LORA ** -0.5),
        'mla_w_ukv': normal((DEPTH, MLA_KV_LORA, MLA_HEADS, MLA_NOPE + MLA_V), MLA_KV_LORA ** -0.5),
        'gqa_q_norm_w': gain((DEPTH, GQA_HEAD_DIM)),
        'gqa_k_norm_w': gain((DEPTH, GQA_HEAD_DIM)),
        'hy_conv_w': normal((DEPTH, SHORT_CONV, (HY_ORDER + 1) * HY_WIDTH), SHORT_CONV ** -0.5),
        'hy_w1': normal((DEPTH, HY_EMB, HY_HIDDEN), HY_EMB ** -0.5),
        'hy_b1': normal((DEPTH, HY_HIDDEN), 0.1),
        'hy_w2': normal((DEPTH, HY_HIDDEN, HY_HIDDEN), HY_HIDDEN ** -0.5),
        'hy_b2': normal((DEPTH, HY_HIDDEN), 0.1),
        'hy_w3': normal((DEPTH, HY_HIDDEN, HY_ORDER * HY_WIDTH), HY_HIDDEN ** -0.5),
        'hy_sin_freq': gain((DEPTH, 2, HY_HIDDEN)),
        'hy_bias': normal((DEPTH, HY_ORDER, HY_WIDTH), 0.5),
        'w_branch': normal((DEPTH, N_BRANCH, BRANCH_W, d), BRANCH_W ** -0.5),
        'w_out': normal((DEPTH, d, d), d ** -0.5),
        'w_router': normal((d, N_EXPERTS), d ** -0.5),
        'router_bias': normal((N_EXPERTS,), 0.01),
        'moe_w_gate': normal((DEPTH, N_EXPERTS, d, D_EXPERT), d ** -0.5),
        'moe_w_up': normal((DEPTH, N_EXPERTS, d, D_EXPERT), d ** -0.5),
        'moe_w_down': normal((DEPTH, N_EXPERTS, D_EXPERT, d), D_EXPERT ** -0.5),
        'final_norm_w': gain((d,)),
    }


def reference(x, c, ctx, c_ctx, w_ada, b_ada, norm1_w, norm2_w, w_in,
              gdn_conv_w, gdn_a_log, gdn_dt_bias, gdn_norm_w,
              mla_q_norm_w, mla_kv_norm_w, mla_w_uq, mla_w_ukv,
              gqa_q_norm_w, gqa_k_norm_w,
              hy_conv_w, hy_w1, hy_b1, hy_w2, hy_b2, hy_w3, hy_sin_freq, hy_bias,
              w_branch, w_out, w_router, router_bias,
              moe_w_gate, moe_w_up, moe_w_down, final_norm_w):
    b, seq, d = x.shape
    ctx_len = ctx.shape[1]
    rows = seq // GRID_W
    rope_mla = axial_rope_tables(rows, MLA_ROPE)
    rope_gqa = axial_rope_tables(rows, GQA_HEAD_DIM)
    split_at = np.cumsum(IN_WIDTHS)[:-1].tolist()
    xl, xc = x, ctx
    for layer in range(DEPTH):
        ctx_out = layer < DEPTH - 1
        mod_l = (jax.nn.silu(c) @ w_ada[layer] + b_ada[layer]).reshape(b, 6, 1, d)
        mod_c = (jax.nn.silu(c_ctx) @ w_ada[layer] + b_ada[layer]).reshape(6, 1, d)

        hl = rms_norm(xl, norm1_w[layer]) * (1 + mod_l[:, 1]) + mod_l[:, 0]
        hc = rms_norm(xc, norm1_w[layer]) * (1 + mod_c[1]) + mod_c[0]
        pl = hl @ w_in[layer]
        pc = hc @ (w_in[layer] if ctx_out else w_in[layer][:, :MIX_IN])
        sl = jnp.split(pl[..., :MIX_IN], split_at, axis=-1)
        sc = jnp.split(pc[..., :MIX_IN], split_at, axis=-1)

        gdn_l, gdn_c = gdn_mixer(sl[0:6], sc[0:6], gdn_conv_w[layer], gdn_a_log[layer],
                                 gdn_dt_bias[layer], gdn_norm_w[layer], ctx_out)
        mk_c, mv_c = mla_kv(sc[7], sc[8], mla_kv_norm_w[layer], mla_w_ukv[layer], None)
        mk_l, mv_l = mla_kv(sl[7], sl[8], mla_kv_norm_w[layer], mla_w_ukv[layer], rope_mla)
        mq_l = mla_q(sl[6], mla_q_norm_w[layer], mla_w_uq[layer], rope_mla)
        mla_l = block_attention(mq_l, jnp.concatenate([mk_l, mk_c], axis=1),
                                jnp.concatenate([mv_l, mv_c], axis=1)).reshape(b, seq, BRANCH_W)
        gk_c, gv_c = gqa_kv(sc[10], sc[11], gqa_k_norm_w[layer], None)
        gk_l, gv_l = gqa_kv(sl[10], sl[11], gqa_k_norm_w[layer], rope_gqa)
        gq_l = gqa_q(sl[9], gqa_q_norm_w[layer], rope_gqa)
        gqa_l = block_attention(gq_l, jnp.concatenate([gk_l, gk_c], axis=1),
                                jnp.concatenate([gv_l, gv_c], axis=1)).reshape(b, seq, BRANCH_W)
        hy_params = (hy_w1[layer], hy_b1[layer], hy_w2[layer], hy_b2[layer], hy_w3[layer], hy_sin_freq[layer])
        hy_l = hyena_mixer(sl[12], hy_conv_w[layer], hyena_filters(seq, *hy_params), hy_bias[layer])

        branches_l = [t.astype(xl.dtype) for t in (gdn_l, mla_l, gqa_l, hy_l)]
        mix_l = merge_branches(branches_l, pl[..., MIX_IN:], w_branch[layer], w_out[layer])

        if ctx_out:
            mla_c = block_attention(mla_q(sc[6], mla_q_norm_w[layer], mla_w_uq[layer], None),
                                    mk_c, mv_c).reshape(b, ctx_len, BRANCH_W)
            gqa_c = block_attention(gqa_q(sc[9], gqa_q_norm_w[layer], None),
                                    gk_c, gv_c).reshape(b, ctx_len, BRANCH_W)
            hy_c = hyena_mixer(sc[12], hy_conv_w[layer], hyena_filters(ctx_len, *hy_params), hy_bias[layer])
            branches_c = [t.astype(xc.dtype) for t in (gdn_c, mla_c, gqa_c, hy_c)]
            xc = xc + mod_c[2] * merge_branches(branches_c, pc[..., MIX_IN:], w_branch[layer], w_out[layer])
            hc = rms_norm(xc, norm2_w[layer]) * (1 + mod_c[4]) + mod_c[3]
            xc = xc + mod_c[5] * moe_ffn(hc, w_router, router_bias, moe_w_gate[layer],
                                         moe_w_up[layer], moe_w_down[layer])

        xl = xl + mod_l[:, 2] * mix_l
        hl = rms_norm(xl, norm2_w[layer]) * (1 + mod_l[:, 4]) + mod_l[:, 3]
        xl = xl + mod_l[:, 5] * moe_ffn(hl, w_router, router_bias, moe_w_gate[layer],
                                        moe_w_up[layer], moe_w_down[layer])
    return rms_norm(xl, final_norm_w)
```

```python
import math
from contextlib import ExitStack
import types
import numpy as np
import concourse.bass as bass
import concourse.mybir as mybir
from concourse.bass_utils import run_bass_kernel_spmd

F32 = mybir.dt.float32
BF16 = mybir.dt.bfloat16
I32 = mybir.dt.int32
AF = mybir.ActivationFunctionType
ALU = mybir.AluOpType
AX = mybir.AxisListType

COMPUTE = ('tensor', 'vector', 'scalar', 'gpsimd')
NDMASEM = 24


def _freeze(fn):
    if not getattr(fn, '__closure__', None):
        return fn
    cells = []
    for c in fn.__closure__:
        try:
            cells.append(types.CellType(c.cell_contents))
        except ValueError:
            cells.append(c)
    g = types.FunctionType(fn.__code__, fn.__globals__, fn.__name__, fn.__defaults__, tuple(cells))
    g.__kwdefaults__ = fn.__kwdefaults__
    return g


class Prog:
    def __init__(self, name='k'):
        self.nc = bass.Bass('TRN2', target_bir_lowering=False)
        self.ops = {e: [] for e in COMPUTE + ('sync',)}
        self.cnt = {e: 0 for e in COMPUTE}
        self.waited = {e: {} for e in COMPUTE + ('sync',)}
        self.last_w = {}
        self.readers = {}
        self.dma_cnt = [0] * NDMASEM
        self.dma_rr = 0
        self.ctx = []
        self.sems = {}
        self.n_ops = 0
        self._stack = None
        self.safe = False
        self.pe_pending = {}

    def _enter(self, cm):
        return self._stack.enter_context(cm)

    def dram(self, name, shape, dt, kind):
        return self.nc.dram_tensor(name, list(shape), dt, kind=kind)

    def sb(self, name, shape, dt=F32):
        return self._enter(self.nc.sbuf_tensor(name, list(shape), dt))

    def ps(self, name, shape, dt=F32):
        return self._enter(self.nc.psum_tensor(name, list(shape), dt))

    @staticmethod
    def _key(t):
        if isinstance(t, str):
            return t
        if hasattr(t, 'tensor'):
            t = t.tensor
        return t.name

    def _deps(self, reads, writes):
        deps = []
        for r in reads:
            k = self._key(r)
            if k in self.last_w:
                deps.append(self.last_w[k])
        for w in writes:
            k = self._key(w)
            if k in self.last_w:
                deps.append(self.last_w[k])
            deps.extend(self.readers.get(k, []))
        return deps

    def _commit(self, ticket, reads, writes):
        for r in reads:
            self.readers.setdefault(self._key(r), []).append(ticket)
        for w in writes:
            k = self._key(w)
            self.last_w[k] = ticket
            self.readers[k] = []

    def _wait_list(self, eng, deps):
        need = {}
        for (kind, idx, val) in deps:
            if kind == 'eng' and idx == 'tensor' and eng == 'tensor':
                continue
            sk = (kind, idx)
            if self.waited[eng].get(sk, 0) >= val:
                continue
            if need.get(sk, 0) < val:
                need[sk] = val
        for sk, val in need.items():
            self.waited[eng][sk] = val
        return list(need.items())

    def op(self, eng, fn, reads=(), writes=()):
        deps = self._deps(reads, writes)
        waits = self._wait_list(eng, deps)
        self.cnt[eng] += 1
        ticket = ('eng', eng, self.cnt[eng])
        self._commit(ticket, reads, writes)
        if eng == 'tensor':
            for w in writes:
                self.pe_pending.setdefault(self._key(w), set()).update(self._key(r) for r in reads)
        else:
            for r in reads:
                pk = self._key(r)
                if pk in self.pe_pending:
                    for k in self.pe_pending.pop(pk):
                        self.readers.setdefault(k, []).append(ticket)
        self.ops[eng].append(('c', waits, _freeze(fn)))
        self.n_ops += 1
        return ticket

    def dma(self, out, in_, reads=(), writes=(), q='sync', **kw):
        deps = self._deps(reads, writes)
        si = self.dma_rr
        self.dma_rr = (self.dma_rr + 1) % NDMASEM
        if self.dma_cnt[si] > 0:
            deps.append(('dma', si, self.dma_cnt[si]))
        waits = self._wait_list(q, deps)
        self.dma_cnt[si] += 16
        ticket = ('dma', si, self.dma_cnt[si])
        self._commit(ticket, reads, writes)
        self.ops[q].append(('d', waits, (out, in_, si, kw)))
        self.n_ops += 1
        return ticket

    def finish(self, eng='sync'):
        deps = [('dma', i, c) for i, c in enumerate(self.dma_cnt) if c > 0]
        waits = self._wait_list(eng, deps)
        self.ops[eng].append(('w', waits, None))

    def emit(self):
        nc = self.nc
        esem = {e: self._enter(nc.semaphore('s_' + e)) for e in COMPUTE}
        dsem = [self._enter(nc.semaphore('d_%d' % i)) for i in range(NDMASEM)]

        def semof(sk):
            return esem[sk[1]] if sk[0] == 'eng' else dsem[sk[1]]

        ops = self.ops

        def run(engname):
            def body(e):
                for (kind, waits, payload) in ops[engname]:
                    for sk, val in waits:
                        e.wait_ge(semof(sk), val)
                    if kind == 'c':
                        ins = payload(e)
                        if self.safe and engname != 'tensor':
                            e.drain().then_inc(esem[engname], 1)
                        else:
                            ins.then_inc(esem[engname], 1)
                    elif kind == 'd':
                        out, in_, si, kw = payload
                        e.dma_start(out=out, in_=in_, **kw).then_inc(dsem[si], 16)
            return body

        with nc.Block() as block:
            block.sync(run('sync'))
            block.tensor(run('tensor'))
            block.vector(run('vector'))
            block.scalar(run('scalar'))
            block.gpsimd(run('gpsimd'))
        return nc


D = 2048
NKC = 16
IN_DIM = 13392
T1 = 2176


def AP(t, off, dims):
    return bass.AP(t, off, [list(d) for d in dims])


def build_l0():
    P = Prog()
    with ExitStack() as st:
        P._stack = st
        cT = P.dram('cT', [128, 16, 5], F32, 'ExternalInput')
        w = P.dram('w', [2, D, 1536], F32, 'ExternalInput')
        b = P.dram('b', [2, 1536], F32, 'ExternalInput')
        out = P.dram('mod', [2, 5, 1536], F32, 'ExternalOutput')
        ct = P.sb('ct', [128, 16, 5])
        cs = P.sb('cs', [128, 16, 5])
        P.dma(ct[:], cT.ap(), reads=[cT], writes=[ct])
        P.op('scalar', lambda e: e.activation(cs[:], ct[:], AF.Silu), reads=[ct], writes=[cs])
        wt = [P.sb('wt%d' % i, [128, 4, 512]) for i in range(2)]
        bt = P.sb('bt', [5, 2, 1536])
        P.dma(bt[:], AP(b, 0, [[0, 5], [1536, 2], [1, 1536]]), reads=[b], writes=[bt])
        ps = [P.ps('ps%d' % i, [5, 512]) for i in range(2)]
        ot = P.sb('ot', [5, 2, 1536])
        n = 0
        pi = 0
        for l in range(2):
            for cb in range(3):
                pst = ps[pi % 2]
                pi += 1
                for kg in range(4):
                    wtile = wt[n % 2]
                    n += 1
                    src = AP(w, l * D * 1536 + kg * 512 * 1536 + cb * 512, [[1536, 128], [128 * 1536, 4], [1, 512]])
                    P.dma(wtile[:], src, reads=[w], writes=[wtile])
                    for kk in range(4):
                        k = kg * 4 + kk
                        P.op('tensor', lambda e, k=k, kk=kk, wtile=wtile, pst=pst: e.matmul(
                            pst[:], cs[:, k, :], wtile[:, kk, :], start=(k == 0), stop=(k == 15)),
                            reads=[cs, wtile], writes=[pst])
                P.op('vector', lambda e, l=l, cb=cb, pst=pst: e.tensor_tensor(
                    ot[:, l, cb * 512:(cb + 1) * 512], pst[:], bt[:, l, cb * 512:(cb + 1) * 512], ALU.add),
                    reads=[pst, bt], writes=[ot])
        P.dma(AP(out, 0, [[1536, 5], [5 * 1536, 2], [1, 1536]]), ot[:], reads=[ot], writes=[out])
        P.finish()
        nc = P.emit()
    return nc


def run_l0(c, c_ctx, w_ada, b_ada):
    c5 = np.concatenate([c, c_ctx[None]], 0)
    cT = np.ascontiguousarray(c5.T.reshape(16, 128, 5).transpose(1, 0, 2))
    nc = build_l0()
    in_maps = []
    for i in range(8):
        sl = slice(i * 1536, (i + 1) * 1536)
        in_maps.append({'cT': cT, 'w': np.ascontiguousarray(w_ada[:, :, sl]), 'b': np.ascontiguousarray(b_ada[:, sl])})
    res = run_bass_kernel_spmd(nc, in_maps, core_ids=list(range(8)))
    mod = np.concatenate([r['mod'] for r in res.results], axis=2)
    return mod


def vecT(v):
    return np.ascontiguousarray(v.reshape(16, 128).T)


TOKB = [(0, 512), (512, 512), (1024, 512), (1536, 512), (2048, 128)]


def AP_slice(mvt, r):
    return mvt[:, r, :]


def norm_phase(P, xT, hT, gl, sl_, gc, sc_, ones_bf, eps_t, T, n_lat, x_reads=None, out_key='hT', SUB=256):
    xs = [P.sb('xs%d' % i, [128, NKC, SUB]) for i in range(2)]
    sq = P.sb('sq', [128, NKC, SUB], BF16)
    pss = P.ps('ps_ss', [128, SUB])
    rs = P.sb('rs', [128, SUB])
    tmp = P.sb('ntmp', [128, NKC, SUB])
    nsub = (T + SUB - 1) // SUB
    for s in range(nsub):
        t0 = s * SUB
        n = min(SUB, T - t0)
        x_ = xs[s % 2]
        P.dma(x_[:, :, 0:n], AP(xT, t0, [[NKC * T, 128], [T, NKC], [1, n]]), reads=(x_reads if x_reads is not None else [xT]), writes=[x_])
        P.op('scalar', lambda e, x_=x_, n=n: e.activation(sq[:, :, 0:n], x_[:, :, 0:n], AF.Square),
             reads=[x_], writes=[sq])
        for c in range(NKC):
            P.op('tensor', lambda e, c=c, n=n: e.matmul(pss[:, 0:n], ones_bf[:], sq[:, c, 0:n],
                                                          start=(c == 0), stop=(c == NKC - 1)),
                 reads=[sq, ones_bf], writes=[pss])
        P.op('scalar', lambda e, n=n: e.activation(rs[:, 0:n], pss[:, 0:n], AF.Sqrt, bias=eps_t[:], scale=1.0 / D),
             reads=[pss, eps_t], writes=[rs])
        P.op('vector', lambda e, n=n: e.reciprocal(rs[:, 0:n], rs[:, 0:n]), reads=[rs], writes=[rs])
        P.op('vector', lambda e, x_=x_, n=n: e.tensor_tensor(
            tmp[:, :, 0:n], x_[:, :, 0:n], AP(rs, 0, [[SUB, 128], [0, NKC], [1, n]]), ALU.mult),
            reads=[x_, rs], writes=[tmp])
        g_, s_ = (gl, sl_) if t0 < n_lat else (gc, sc_)
        for c in range(NKC):
            eng = 'vector' if c % 2 == 0 else 'gpsimd'
            P.op(eng, lambda e, c=c, n=n, g_=g_, s_=s_, t0=t0: e.tensor_scalar(
                hT[:, c, t0:t0 + n], tmp[:, c, 0:n], g_[:, c:c + 1], s_[:, c:c + 1], ALU.mult, ALU.add),
                reads=[tmp, g_, s_], writes=['%s:%d' % (out_key, c)])


def linear_fm(P, W, w_off, w_rs, K_chunks, n_out, rhs_fn, tokb, evac_fn, rhs_reads, tag='lin', grp=256):
    wst = [P.sb('%s_wst%d' % (tag, i), [128, K_chunks, grp]) for i in range(2)]
    wbf = [P.sb('%s_wbf%d' % (tag, i), [128, K_chunks, grp], BF16) for i in range(2)]
    pst = [P.ps('%s_ps%d' % (tag, i), [128, 512]) for i in range(2)]
    ng = (n_out + grp - 1) // grp
    pi = 0
    for g in range(ng):
        o0 = g * grp
        gw = min(grp, n_out - o0)
        ws, wb = wst[g % 2], wbf[g % 2]
        P.dma(ws[:, :, 0:gw], AP(W, w_off + o0, [[w_rs, 128], [128 * w_rs, K_chunks], [1, gw]]),
              reads=[W], writes=[ws], q='sync' if g % 2 == 0 else 'gpsimd')
        P.op('gpsimd', lambda e, ws=ws, wb=wb, gw=gw: e.tensor_copy(wb[:, :, 0:gw], ws[:, :, 0:gw]),
             reads=[ws], writes=[wb])
        for m0 in range(0, gw, 128):
            m = min(128, gw - m0)
            for (t0, n) in tokb:
                ps_ = pst[pi % 2]
                pi += 1
                for kc in range(K_chunks):
                    P.op('tensor', lambda e, ps_=ps_, wb=wb, kc=kc, m0=m0, m=m, t0=t0, n=n: e.matmul(
                        ps_[0:m, 0:n], wb[:, kc, m0:m0 + m], rhs_fn(kc, t0, n),
                        start=(kc == 0), stop=(kc == K_chunks - 1)),
                        reads=[wb] + rhs_reads(kc), writes=[ps_])
                evac_fn(o0 + m0, m, t0, n, ps_)


def build_l1():
    P = Prog()
    with ExitStack() as st:
        P._stack = st
        xT = P.dram('xT', [128, NKC, T1], F32, 'ExternalInput')
        mv = P.dram('mv', [5, 128, NKC], F32, 'ExternalInput')
        W = P.dram('W', [D, IN_DIM], F32, 'ExternalInput')
        pT = P.dram('pT', [IN_DIM, T1], F32, 'ExternalOutput')
        mvt = P.sb('mvt', [128, 5, NKC])
        P.dma(mvt[:], AP(mv, 0, [[NKC, 128], [128 * NKC, 5], [1, NKC]]), reads=[mv], writes=[mvt])
        gl = P.sb('gl', [128, NKC]); gc = P.sb('gc', [128, NKC])
        sl_ = P.sb('sl', [128, NKC]); sc_ = P.sb('sc', [128, NKC])
        P.op('vector', lambda e: e.scalar_tensor_tensor(gl[:], mvt[:, 1, :], 1.0, mvt[:, 0, :], ALU.add, ALU.mult),
             reads=[mvt], writes=[gl])
        P.op('vector', lambda e: e.scalar_tensor_tensor(gc[:], mvt[:, 3, :], 1.0, mvt[:, 0, :], ALU.add, ALU.mult),
             reads=[mvt], writes=[gc])
        P.op('vector', lambda e: e.tensor_copy(sl_[:], mvt[:, 2, :]), reads=[mvt], writes=[sl_])
        P.op('vector', lambda e: e.tensor_copy(sc_[:], mvt[:, 4, :]), reads=[mvt], writes=[sc_])
        ones_bf = P.sb('ones_bf', [128, 128], BF16)
        P.op('vector', lambda e: e.memset(ones_bf[:], 1.0), writes=[ones_bf])
        eps_t = P.sb('eps_t', [128, 1])
        P.op('vector', lambda e: e.memset(eps_t[:], 1e-6), writes=[eps_t])
        hT = P.sb('hT', [128, NKC, T1], BF16)
        norm_phase(P, xT, hT, gl, sl_, gc, sc_, ones_bf, eps_t, T1, 2048)
        osb = [P.sb('osb%d' % i, [128, 512]) for i in range(3)]
        cnt = [0]

        def evac(o0, m, t0, n, ps_):
            o_ = osb[cnt[0] % 3]
            eng = 'vector' if cnt[0] % 2 == 0 else 'scalar'
            cnt[0] += 1
            if eng == 'vector':
                P.op('vector', lambda e: e.tensor_copy(o_[0:m, 0:n], ps_[0:m, 0:n]), reads=[ps_], writes=[o_])
            else:
                P.op('scalar', lambda e: e.copy(o_[0:m, 0:n], ps_[0:m, 0:n]), reads=[ps_], writes=[o_])
            P.dma(AP(pT, o0 * T1 + t0, [[T1, m], [1, n]]), o_[0:m, 0:n], reads=[o_], writes=[])

        linear_fm(P, W, 0, IN_DIM, NKC, IN_DIM, lambda kc, t0, n: hT[:, kc, t0:t0 + n], TOKB, evac, lambda kc: ['hT:%d' % kc])
        P.finish()
        nc = P.emit()
    return nc


MIX_IN = 5200
T3 = 1088
TOKB3 = [(0, 512), (512, 512), (1024, 64)]
NLAT3 = 1024
BIG = 1.0e4


class Lin:
    def __init__(self, P, tag, kmax=16, grp=256, nps=2):
        self.P = P
        self.grp = grp
        self.wst = [P.sb('%s_wst%d' % (tag, i), [128, kmax, grp]) for i in range(2)]
        self.wbf = [P.sb('%s_wbf%d' % (tag, i), [128, kmax, grp], BF16) for i in range(2)]
        self.pst = [P.ps('%s_ps%d' % (tag, i), [128, 512]) for i in range(nps)]
        self.nps = nps
        self.g = 0
        self.pi = 0

    def run(self, W, w_off, w_rs, K_chunks, n_out, rhs_fn, tokb, evac_fn, rhs_reads, split=1):
        P = self.P
        grp = self.grp
        ng = (n_out + grp - 1) // grp
        kper = K_chunks // split
        for g in range(ng):
            o0 = g * grp
            gw = min(grp, n_out - o0)
            ws, wb = self.wst[self.g % 2], self.wbf[self.g % 2]
            P.dma(ws[:, 0:K_chunks, 0:gw], AP(W, w_off + o0, [[w_rs, 128], [128 * w_rs, K_chunks], [1, gw]]),
                  reads=[W], writes=[ws], q='sync' if self.g % 2 == 0 else 'gpsimd')
            self.g += 1
            P.op('gpsimd', lambda e, ws=ws, wb=wb, gw=gw: e.tensor_copy(wb[:, 0:K_chunks, 0:gw], ws[:, 0:K_chunks, 0:gw]),
                 reads=[ws], writes=[wb])
            for m0 in range(0, gw, 128):
                m = min(128, gw - m0)
                for (t0, n) in tokb:
                    tiles = []
                    for sp in range(split):
                        ps_ = self.pst[self.pi % self.nps]
                        self.pi += 1
                        tiles.append(ps_)
                        for kk in range(kper):
                            kc = sp * kper + kk
                            P.op('tensor', lambda e, ps_=ps_, wb=wb, kc=kc, kk=kk, m0=m0, m=m, t0=t0, n=n: e.matmul(
                                ps_[0:m, 0:n], wb[:, kc, m0:m0 + m], rhs_fn(kc, t0, n),
                                start=(kk == 0), stop=(kk == kper - 1)),
                                reads=[wb] + rhs_reads(kc), writes=[ps_])
                    evac_fn(o0 + m0, m, t0, n, tiles if split > 1 else tiles[0])


def build_l3():
    P = Prog()
    with ExitStack() as st:
        P._stack = st
        xT = P.dram('xT', [128, NKC, T3], F32, 'ExternalInput')
        brT = P.dram('brT', [2048, T3], F32, 'ExternalInput')
        gT = P.dram('gT', [8192, T3], F32, 'ExternalInput')
        mv = P.dram('mv', [9, 128, NKC], F32, 'ExternalInput')
        wbr = P.dram('wbr', [2048, D], F32, 'ExternalInput')
        wo = P.dram('wo', [D, D], F32, 'ExternalInput')
        wr = P.dram('wr', [128, NKC, 16], F32, 'ExternalInput')
        rb = P.dram('rb', [16], F32, 'ExternalInput')
        wg = P.dram('wg', [16, D, 512], F32, 'ExternalInput')
        wu = P.dram('wu', [16, D, 512], F32, 'ExternalInput')
        wd = P.dram('wd', [16, 512, D], F32, 'ExternalInput')
        ident_d = P.dram('ident', [128, 128], F32, 'ExternalInput')
        x1T = P.dram('x1T', [128, NKC, T3], F32, 'Internal')
        x2T = P.dram('x2T', [128, NKC, T3], F32, 'ExternalOutput')

        mvt = P.sb('mvt', [128, 9, NKC])
        P.dma(mvt[:], AP(mv, 0, [[NKC, 128], [128 * NKC, 9], [1, NKC]]), reads=[mv], writes=[mvt])
        gl = P.sb('gl', [128, NKC]); gc = P.sb('gc', [128, NKC])
        P.op('vector', lambda e: e.scalar_tensor_tensor(gl[:], mvt[:, 3, :], 1.0, mvt[:, 0, :], ALU.add, ALU.mult),
             reads=[mvt], writes=[gl])
        P.op('vector', lambda e: e.scalar_tensor_tensor(gc[:], mvt[:, 7, :], 1.0, mvt[:, 0, :], ALU.add, ALU.mult),
             reads=[mvt], writes=[gc])
        ones_bf = P.sb('ones_bf', [128, 128], BF16)
        P.op('vector', lambda e: e.memset(ones_bf[:], 1.0), writes=[ones_bf])
        eps_t = P.sb('eps_t', [128, 1])
        P.op('vector', lambda e: e.memset(eps_t[:], 1e-6), writes=[eps_t])
        ident = P.sb('identsb', [128, 128])
        P.dma(ident[:], ident_d.ap(), reads=[ident_d], writes=[ident])
        wrt = P.sb('wrt', [128, NKC, 16])
        P.dma(wrt[:], wr.ap(), reads=[wr], writes=[wrt])
        wrb = P.sb('wrb', [128, NKC, 16], BF16)
        P.op('vector', lambda e: e.tensor_copy(wrb[:], wrt[:]), reads=[wrt], writes=[wrb])
        rbt = P.sb('rbt', [128, 16])
        P.dma(rbt[:], AP(rb, 0, [[0, 128], [1, 16]]), reads=[rb], writes=[rbt])

        lin = Lin(P, 'lin', grp=128, nps=4)
        bigT = P.sb('bigT', [128, NKC, T3], BF16)
        RY = P.sb('RY', [128, NKC, T3])
        mrgT = AP(RY, 0, [[NKC * T3, 128], [1, NKC * T3 * 2]]).bitcast(BF16) if False else RY.bitcast(BF16)
        stg = [P.sb('stg%d' % i, [128, 512]) for i in range(3)]
        si = [0]

        def nxt():
            s = stg[si[0] % 3]
            si[0] += 1
            return s
        for c in range(NKC):
            for (t0, n) in TOKB3:
                s = nxt()
                P.dma(s[:, 0:n], AP(brT, c * 128 * T3 + t0, [[T3, 128], [1, n]]), reads=[brT], writes=[s])
                P.op('gpsimd', lambda e, s=s, c=c, t0=t0, n=n: e.tensor_copy(bigT[:, c, t0:t0 + n], s[:, 0:n]),
                     reads=[s], writes=['bigT:%d' % c])
        gst = [P.sb('gst%d' % i, [128, 4, 512]) for i in range(1)]
        acc = P.sb('macc', [128, 512])
        acc2 = P.sb('macc2', [128, 512])
        gi = [0]

        def evacA(o0, m, t0, n, tiles):
            c = o0 // 128
            g_ = gst[0]
            gi[0] += 1
            P.dma(g_[:, :, 0:n], AP(gT, o0 * T3 + t0, [[T3, 128], [2048 * T3, 4], [1, n]]), reads=[gT], writes=[g_])
            P.op('scalar', lambda e: e.activation(g_[:, :, 0:n], g_[:, :, 0:n], AF.Sigmoid), reads=[g_], writes=[g_])
            P.op('vector', lambda e: e.tensor_tensor(acc[:, 0:n], tiles[0][:, 0:n], g_[:, 0, 0:n], ALU.mult),
                 reads=[tiles[0], g_], writes=[acc])
            for j in range(1, 4):
                P.op('vector', lambda e, j=j: e.tensor_tensor(acc2[:, 0:n], tiles[j][:, 0:n], g_[:, j, 0:n], ALU.mult),
                     reads=[tiles[j], g_], writes=[acc2])
                if j < 3:
                    P.op('gpsimd', lambda e: e.tensor_tensor(acc[:, 0:n], acc[:, 0:n], acc2[:, 0:n], ALU.add),
                         reads=[acc, acc2], writes=[acc])
                else:
                    P.op('gpsimd', lambda e: e.tensor_tensor(mrgT[:, c, t0:t0 + n], acc[:, 0:n], acc2[:, 0:n], ALU.add),
                         reads=[acc, acc2], writes=['mrgT:%d' % c])
        lin.run(wbr, 0, D, 16, D, lambda kc, t0, n: bigT[:, kc, t0:t0 + n], TOKB3, evacA,
                lambda kc: ['bigT:%d' % kc], split=4)

        xo = [P.sb('xo%d' % i, [128, 512]) for i in range(2)]
        bi = [0]

        def evacB(o0, m, t0, n, ps_):
            c = o0 // 128
            s = nxt()
            P.dma(s[:, 0:n], AP(xT, c * T3 + t0, [[NKC * T3, 128], [1, n]]), reads=[xT], writes=[s])
            o_ = xo[bi[0] % 2]
            bi[0] += 1
            gm = mvt[:, 1, c:c + 1] if t0 < NLAT3 else mvt[:, 5, c:c + 1]
            P.op('vector', lambda e: e.scalar_tensor_tensor(o_[:, 0:n], ps_[:, 0:n], gm, s[:, 0:n], ALU.mult, ALU.add),
                 reads=[ps_, s, mvt], writes=[o_])
            P.dma(AP(x1T, c * T3 + t0, [[NKC * T3, 128], [1, n]]), o_[:, 0:n], reads=[o_], writes=['x1T:%d' % (t0 // 512)])
        lin.run(wo, 0, D, 16, D, lambda kc, t0, n: mrgT[:, kc, t0:t0 + n], TOKB3, evacB, lambda kc: ['mrgT:%d' % kc])

        class X1:
            pass
        norm_phase(P, x1T, bigT, gl, AP_slice(mvt, 2), gc, AP_slice(mvt, 6), ones_bf, eps_t, T3, NLAT3,
                   x_reads=['x1T:%d' % i for i in range(3)], out_key='h2T', SUB=64)

        psb = P.ps('psb', [128, 512])
        psr = psb[0:16, :]
        pssm = P.ps('pssm', [128, 128])
        pst_ = pssm[:, 0:16]
        lg = P.sb('lg', [16, 1152])
        P.op('vector', lambda e: e.memset(lg[:], 0.0), writes=[lg])
        gateT = P.sb('gateT', [16, T3], BF16)
        for (t0, n) in TOKB3:
            for kc in range(NKC):
                P.op('tensor', lambda e, kc=kc, t0=t0, n=n: e.matmul(psr[:, 0:n], wrb[:, kc, :], bigT[:, kc, t0:t0 + n],
                                                                    start=(kc == 0), stop=(kc == NKC - 1)),
                     reads=[wrb, 'h2T:%d' % kc], writes=[psr])
            P.op('scalar', lambda e, t0=t0, n=n: e.activation(lg[:, t0:t0 + n], psr[:, 0:n], AF.Sigmoid),
                 reads=[psr], writes=[lg])
        sc = P.sb('r_sc', [128, 16]); sel = P.sb('r_sel', [128, 16]); t4 = P.sb('r_t4', [128, 4]); t4b = P.sb('r_t4b', [128, 4])
        e16 = P.sb('r_e16', [128, 16]); s2 = P.sb('r_s2', [128, 16]); gs = P.sb('r_gs', [128, 4]); t1 = P.sb('r_t1', [128, 1])
        gm_ = P.sb('r_gm', [128, 4]); oh = P.sb('r_oh', [128, 16]); gt = P.sb('r_gt', [128, 16])
        psg = pssm[0:16, :]

        def V(fn, reads, writes):
            P.op('vector', fn, reads=reads, writes=writes)

        def bc4(t):
            return AP(t, 0, [[4, 128], [1, 4], [0, 4]])

        def v44(t):
            return AP(t, 0, [[16, 128], [4, 4], [1, 4]])
        for tt in range((T3 + 127) // 128):
            t0 = tt * 128
            nt = min(128, T3 - t0)
            P.op('tensor', lambda e, t0=t0: e.transpose(pst_[:], lg[:, t0:t0 + 128], ident[0:16, 0:16]),
                 reads=[lg, ident], writes=[pst_])
            V(lambda e: e.tensor_copy(sc[:], pst_[:]), [pst_], [sc])
            V(lambda e: e.tensor_tensor(sel[:], sc[:], rbt[:], ALU.add), [sc, rbt], [sel])
            V(lambda e: e.tensor_reduce(t4[:], v44(sel), AX.X, ALU.max), [sel], [t4])
            V(lambda e: e.tensor_tensor(v44(e16), v44(sel), bc4(t4), ALU.is_equal), [sel, t4], [e16])
            V(lambda e: e.scalar_tensor_tensor(s2[:], e16[:], -BIG, sel[:], ALU.mult, ALU.add), [e16, sel], [s2])
            V(lambda e: e.tensor_reduce(t4b[:], v44(s2), AX.X, ALU.max), [s2], [t4b])
            V(lambda e: e.tensor_tensor(gs[:], t4[:], t4b[:], ALU.add), [t4, t4b], [gs])
            V(lambda e: e.tensor_reduce(t1[:], gs[:], AX.X, ALU.max), [gs], [t1])
            V(lambda e: e.tensor_scalar(gm_[:], gs[:], t1[:, 0:1], None, ALU.is_equal), [gs, t1], [gm_])
            V(lambda e: e.tensor_scalar(gm_[:], gm_[:], -1.0, BIG, ALU.add, ALU.mult), [gm_], [gm_])
            V(lambda e: e.tensor_tensor(v44(s2), v44(sel), bc4(gm_), ALU.add), [sel, gm_], [s2])
            V(lambda e: e.tensor_reduce(t1[:], s2[:], AX.X, ALU.max), [s2], [t1])
            V(lambda e: e.tensor_scalar(oh[:], s2[:], t1[:, 0:1], None, ALU.is_equal), [s2, t1], [oh])
            V(lambda e: e.scalar_tensor_tensor(s2[:], oh[:], -4 * BIG, s2[:], ALU.mult, ALU.add), [oh, s2], [s2])
            V(lambda e: e.tensor_reduce(t1[:], s2[:], AX.X, ALU.max), [s2], [t1])
            V(lambda e: e.scalar_tensor_tensor(oh[:], s2[:], t1[:, 0:1], oh[:], ALU.is_equal, ALU.add), [s2, t1, oh], [oh])
            V(lambda e: e.tensor_tensor(gt[:], sc[:], oh[:], ALU.mult), [sc, oh], [gt])
            V(lambda e: e.tensor_reduce(t1[:], gt[:], AX.X, ALU.add), [gt], [t1])
            V(lambda e: e.reciprocal(t1[:], t1[:]), [t1], [t1])
            V(lambda e: e.tensor_scalar(gt[:], gt[:], t1[:, 0:1], None, ALU.mult), [gt, t1], [gt])
            P.op('tensor', lambda e: e.transpose(psg[:], gt[:], ident[:]), reads=[gt, ident], writes=[psg])
            P.op('scalar', lambda e, t0=t0, nt=nt: e.copy(gateT[:, t0:t0 + nt], psg[:, 0:nt]), reads=[psg], writes=[gateT])
        selm = P.sb('selm', [16, 16, 128], BF16)
        V(lambda e: e.tensor_copy(selm[:], AP(ident, 0, [[128, 16], [1, 16], [0, 128]])), [ident], [selm])

        yacc = RY
        gact = P.sb('gact', [128, 4, T3], BF16)
        actT = P.sb('actT', [128, 4, T3], BF16)
        gb = P.sb('gb', [128, T3])
        tmpm = P.sb('tmpm', [128, 512])
        for h in range(1):
            tokb = TOKB3
            hb = 0
            for ex in range(16):
                for (t0, n) in tokb:
                    P.op('tensor', lambda e, ex=ex, t0=t0, n=n: e.matmul(psb[:, 0:n], selm[:, ex, :], gateT[:, t0:t0 + n],
                                                                        start=True, stop=True),
                         reads=[selm, gateT], writes=[psb])
                    P.op('scalar', lambda e, t0=t0, n=n: e.copy(gb[:, t0 - hb:t0 - hb + n], psb[:, 0:n]),
                         reads=[psb], writes=[gb])

                def evG(o0, m, t0, n, ps_):
                    fc = o0 // 128
                    P.op('scalar', lambda e: e.activation(gact[:, fc, t0 - hb:t0 - hb + n], ps_[:, 0:n], AF.Silu),
                         reads=[ps_], writes=[gact])
                lin.run(wg, ex * D * 512, 512, 16, 512, lambda kc, t0, n: bigT[:, kc, t0:t0 + n], tokb, evG,
                        lambda kc: ['h2T:%d' % kc])

                def evU(o0, m, t0, n, ps_):
                    fc = o0 // 128
                    P.op('vector', lambda e: e.tensor_tensor(tmpm[:, 0:n], ps_[:, 0:n], gact[:, fc, t0 - hb:t0 - hb + n], ALU.mult),
                         reads=[ps_, gact], writes=[tmpm])
                    P.op('gpsimd', lambda e: e.tensor_tensor(actT[:, fc, t0 - hb:t0 - hb + n], tmpm[:, 0:n],
                                                             gb[:, t0 - hb:t0 - hb + n], ALU.mult),
                         reads=[tmpm, gb], writes=[actT])
                lin.run(wu, ex * D * 512, 512, 16, 512, lambda kc, t0, n: bigT[:, kc, t0:t0 + n], tokb, evU,
                        lambda kc: ['h2T:%d' % kc])

                def evD(o0, m, t0, n, ps_, ex=ex):
                    c = o0 // 128
                    if ex == 0:
                        P.op('scalar', lambda e: e.copy(yacc[:, c, t0 - hb:t0 - hb + n], ps_[:, 0:n]),
                             reads=[ps_], writes=['yacc:%d' % c])
                    else:
                        P.op('vector', lambda e: e.tensor_tensor(yacc[:, c, t0 - hb:t0 - hb + n], ps_[:, 0:n],
                                                                 yacc[:, c, t0 - hb:t0 - hb + n], ALU.add),
                             reads=[ps_, 'yacc:%d' % c], writes=['yacc:%d' % c])
                lin.run(wd, ex * 512 * D, D, 4, D, lambda kc, t0, n: actT[:, kc, t0 - hb:t0 - hb + n], tokb, evD,
                        lambda kc: [actT])
            for c in range(NKC):
                for (t0, n) in tokb:
                    s = nxt()
                    P.dma(s[:, 0:n], AP(x1T, c * T3 + t0, [[NKC * T3, 128], [1, n]]), reads=['x1T:%d' % (t0 // 512)], writes=[s])
                    o_ = xo[bi[0] % 2]
                    bi[0] += 1
                    gm = mvt[:, 4, c:c + 1] if t0 < NLAT3 else mvt[:, 8, c:c + 1]
                    P.op('vector', lambda e, s=s, o_=o_, gm=gm, c=c, t0=t0, n=n: e.scalar_tensor_tensor(
                        o_[:, 0:n], yacc[:, c, t0 - hb:t0 - hb + n], gm, s[:, 0:n], ALU.mult, ALU.add),
                        reads=['yacc:%d' % c, s, mvt], writes=[o_])
                    P.dma(AP(x2T, c * T3 + t0, [[NKC * T3, 128], [1, n]]), o_[:, 0:n], reads=[o_], writes=[])
        P.finish()
        nc = P.emit()
    return nc


DEBUG = False
TA = 4352
NKT = 34


def build_l2a(with_ctx=True):
    P = Prog()
    with ExitStack() as st:
        P._stack = st
        gq = P.dram('gq', [256, TA], F32, 'ExternalInput')
        gk = P.dram('gk', [64, TA], F32, 'ExternalInput')
        gv = P.dram('gv', [TA, 64], F32, 'ExternalInput')
        cq = P.dram('cq', [512, TA], F32, 'ExternalInput')
        ckv = P.dram('ckv', [256, TA], F32, 'ExternalInput')
        kr = P.dram('kr', [64, TA], F32, 'ExternalInput')
        wuq = P.dram('wuq', [512, 384], F32, 'ExternalInput')
        wukv = P.dram('wukv', [256, 512], F32, 'ExternalInput')
        nw = P.dram('nw', [128, 8], F32, 'ExternalInput')
        cosd = P.dram('cosd', [64, TA], F32, 'ExternalInput')
        sind = P.dram('sind', [64, TA], F32, 'ExternalInput')
        rmd = P.dram('rmd', [64, 64], F32, 'ExternalInput')
        og = P.dram('og', [TA, 256], F32, 'ExternalOutput')
        om = P.dram('om', [TA, 256], F32, 'ExternalOutput')

        pp = [P.ps('pp%d' % i, [128, 512]) for i in range(8)]
        ones = P.sb('ones', [128, 128])
        P.op('vector', lambda e: e.memset(ones[:], 1.0), writes=[ones])
        nwt = P.sb('nwt', [128, 8])
        P.dma(nwt[:], nw.ap(), reads=[nw], writes=[nwt])
        rm = P.sb('rm', [64, 64])
        P.dma(rm[:], rmd.ap(), reads=[rmd], writes=[rm])
        eps64 = P.sb('eps64', [128, 1])
        P.op('vector', lambda e: e.memset(eps64[:], 1e-6), writes=[eps64])
        wq_s = P.sb('wq_s', [128, 4, 384]); wq_b = P.sb('wq_b', [128, 4, 384], BF16)
        wqr_b = P.sb('wqr_b', [128, 4, 128], BF16)
        wkv_s = P.sb('wkv_s', [128, 2, 512]); wkv_b = P.sb('wkv_b', [128, 2, 512], BF16)
        P.dma(wq_s[:], AP(wuq, 0, [[384, 128], [128 * 384, 4], [1, 384]]), reads=[wuq], writes=[wq_s])
        P.dma(wkv_s[:], AP(wukv, 0, [[512, 128], [128 * 512, 2], [1, 512]]), reads=[wukv], writes=[wkv_s])
        P.op('vector', lambda e: e.tensor_copy(wq_b[:], wq_s[:]), reads=[wq_s], writes=[wq_b])
        P.op('vector', lambda e: e.tensor_copy(wkv_b[:], wkv_s[:]), reads=[wkv_s], writes=[wkv_b])
        for h in range(2):
            P.op('vector', lambda e, h=h: e.tensor_copy(wqr_b[:, :, h * 64:h * 64 + 32], wq_s[:, :, h * 192 + 160:h * 192 + 192]),
                 reads=[wq_s], writes=[wqr_b])
            P.op('vector', lambda e, h=h: e.tensor_copy(wqr_b[:, :, h * 64 + 32:h * 64 + 64], wq_s[:, :, h * 192 + 128:h * 192 + 160]),
                 reads=[wq_s], writes=[wqr_b])

        GQ = P.sb('GQ', [64, 4, TA], BF16)
        GK = P.sb('GK', [64, TA], BF16)
        GV = P.sb('GV', [128, NKT, 65], BF16)
        MQN = P.sb('MQN', [128, 2, TA], BF16)
        MQR = P.sb('MQR', [64, 2, TA], BF16)
        MKN = P.sb('MKN', [128, 2, TA], BF16)
        MKR = P.sb('MKR', [64, TA], BF16)
        MV = P.sb('MV', [128, NKT, 2, 129], BF16)
        P.op('vector', lambda e: e.memset(GV[:], 1.0), writes=[GV])
        P.op('vector', lambda e: e.memset(MV[:], 1.0), writes=[MV])
        gvs = P.sb('gvs', [128, NKT, 64])
        P.dma(gvs[:], AP(gv, 0, [[64, 128], [128 * 64, NKT], [1, 64]]), reads=[gv], writes=[gvs])
        P.op('vector', lambda e: e.tensor_copy(GV[:, :, 0:64], gvs[:]), reads=[gvs], writes=[GV])

        xs = [P.sb('xa%d' % i, [128, 4, 512]) for i in range(2)]
        sq = P.sb('sqa', [128, 4, 512])
        rs = P.sb('rsa', [128, 512])
        xn = P.sb('xna', [128, 512])
        xnb = P.sb('xnb', [128, 4, 512], BF16)
        xnk = P.sb('xnk', [128, 2, 512], BF16)
        cs = P.sb('csa', [64, 512]); sn = P.sb('sna', [64, 512])
        t1 = P.sb('t1a', [128, 512]); t2 = P.sb('t2a', [128, 512])
        ppi = [0]
        fence = P.sb('fence', [128, 8], BF16)

        def nps():
            p_ = pp[ppi[0] % 8]
            ppi[0] += 1
            return p_

        def rstd_of(x_, rows, nch, n, dim):
            P.op('scalar', lambda e: e.activation(sq[0:rows, 0:nch, 0:n], x_[0:rows, 0:nch, 0:n], AF.Square), reads=[x_], writes=[sq])
            ps_ = nps()
            for c in range(nch):
                P.op('tensor', lambda e, c=c: e.matmul(ps_[0:rows, 0:n], ones[0:rows, 0:rows], sq[0:rows, c, 0:n],
                                                       start=(c == 0), stop=(c == nch - 1)), reads=[ones, sq], writes=[ps_])
            P.op('scalar', lambda e: e.activation(rs[0:rows, 0:n], ps_[0:rows, 0:n], AF.Sqrt, bias=eps64[0:rows, :], scale=1.0 / dim),
                 reads=[ps_, eps64], writes=[rs])
            P.op('vector', lambda e: e.reciprocal(rs[0:rows, 0:n], rs[0:rows, 0:n]), reads=[rs], writes=[rs])

        def rope_to(dst, src, n):
            ps_ = nps()
            P.op('tensor', lambda e: e.matmul(ps_[0:64, 0:n], rm[:], src, start=True, stop=True), reads=[rm, src], writes=[ps_])
            P.op('vector', lambda e: e.tensor_tensor(t1[0:64, 0:n], ps_[0:64, 0:n], sn[:, 0:n], ALU.mult), reads=[ps_, sn], writes=[t1])
            P.op('gpsimd', lambda e: e.tensor_tensor(t2[0:64, 0:n], src, cs[:, 0:n], ALU.mult), reads=[src, cs], writes=[t2])
            P.op('vector', lambda e: e.tensor_tensor(dst, t1[0:64, 0:n], t2[0:64, 0:n], ALU.add), reads=[t1, t2], writes=[dst])

        for blk in range(17):
            t0 = blk * 256
            n = 256
            P.dma(cs[:, 0:n], AP(cosd, t0, [[TA, 64], [1, n]]), reads=[cosd], writes=[cs])
            P.dma(sn[:, 0:n], AP(sind, t0, [[TA, 64], [1, n]]), reads=[sind], writes=[sn])
            x_ = xs[0]
            P.dma(x_[0:64, :, 0:n], AP(gq, t0, [[TA, 64], [64 * TA, 4], [1, n]]), reads=[gq], writes=[x_])
            for h in range(4):
                P.op('scalar', lambda e, h=h: e.activation(sq[0:64, 0, 0:n], x_[0:64, h, 0:n], AF.Square), reads=[x_], writes=[sq])
                ps_ = nps()
                P.op('tensor', lambda e, ps_=ps_: e.matmul(ps_[0:64, 0:n], ones[0:64, 0:64], sq[0:64, 0, 0:n], start=True, stop=True),
                     reads=[ones, sq], writes=[ps_])
                P.op('scalar', lambda e, ps_=ps_: e.activation(rs[0:64, 0:n], ps_[0:64, 0:n], AF.Sqrt, bias=eps64[0:64, :], scale=1.0 / 64),
                     reads=[ps_, eps64], writes=[rs])
                P.op('vector', lambda e: e.reciprocal(rs[0:64, 0:n], rs[0:64, 0:n]), reads=[rs], writes=[rs])
                P.op('vector', lambda e, h=h: e.scalar_tensor_tensor(xn[0:64, 0:n], x_[0:64, h, 0:n], nwt[0:64, 6:7], rs[0:64, 0:n],
                                                                     ALU.mult, ALU.mult), reads=[x_, nwt, rs], writes=[xn])
                rope_to(GQ[:, h, t0:t0 + n], xn[0:64, 0:n], n)
            x2 = xs[1]
            P.dma(x2[0:64, 0, 0:n], AP(gk, t0, [[TA, 64], [1, n]]), reads=[gk], writes=[x2])
            rstd_of(x2, 64, 1, n, 64)
            P.op('vector', lambda e: e.scalar_tensor_tensor(xn[0:64, 0:n], x2[0:64, 0, 0:n], nwt[0:64, 7:8], rs[0:64, 0:n],
                                                            ALU.mult, ALU.mult), reads=[x2, nwt, rs], writes=[xn])
            rope_to(GK[:, t0:t0 + n], xn[0:64, 0:n], n)
            P.dma(x2[0:64, 1, 0:n], AP(kr, t0, [[TA, 64], [1, n]]), reads=[kr], writes=[x2])
            rope_to(MKR[:, t0:t0 + n], x2[0:64, 1, 0:n], n)
            P.dma(x_[:, :, 0:n], AP(cq, t0, [[TA, 128], [128 * TA, 4], [1, n]]), reads=[cq], writes=[x_])
            rstd_of(x_, 128, 4, n, 512)
            for c in range(4):
                P.op('vector', lambda e, c=c: e.scalar_tensor_tensor(xnb[:, c, 0:n], x_[:, c, 0:n], nwt[:, c:c + 1], rs[:, 0:n],
                                                                     ALU.mult, ALU.mult), reads=[x_, nwt, rs], writes=[xnb])
            P.op('vector', lambda e: e.drain(), reads=[xnb], writes=[xnb])
            if DEBUG and blk == 0:
                d1 = P.dram('d_xnb', [128, 4 * 512], BF16, 'ExternalOutput'); d2 = P.dram('d_rs', [128, 512], F32, 'ExternalOutput'); d3 = P.dram('d_x', [128, 4*512], F32, 'ExternalOutput')
                P.dma(d1.ap(), xnb[:], reads=[xnb], writes=[]); P.dma(d2.ap(), rs[:], reads=[rs], writes=[]); P.dma(d3.ap(), x_[:], reads=[x_], writes=[])
            for h in (0, 1):
                ps_ = nps()
                for c in range(4):
                    P.op('tensor', lambda e, ps_=ps_, c=c, h=h: e.matmul(ps_[:, 0:n], wq_b[:, c, h * 192:h * 192 + 128], xnb[:, c, 0:n],
                                                                         start=(c == 0), stop=(c == 3)), reads=[wq_b, xnb], writes=[ps_])
                P.op('vector', lambda e, t0=t0, ps_=ps_, h=h: e.tensor_copy(MQN[:, h, t0:t0 + n], ps_[:, 0:n]), reads=[ps_], writes=[MQN])
                pa = nps(); pb = nps()
                for c in range(4):
                    P.op('tensor', lambda e, pa=pa, c=c, h=h: e.matmul(pa[0:64, 0:n], wq_b[:, c, h * 192 + 128:h * 192 + 192], xnb[:, c, 0:n],
                                                                       start=(c == 0), stop=(c == 3)), reads=[wq_b, xnb], writes=[pa])
                for c in range(4):
                    P.op('tensor', lambda e, pb=pb, c=c, h=h: e.matmul(pb[0:64, 0:n], wqr_b[:, c, h * 64:h * 64 + 64], xnb[:, c, 0:n],
                                                                       start=(c == 0), stop=(c == 3)), reads=[wqr_b, xnb], writes=[pb])
                P.op('vector', lambda e, pa=pa: e.tensor_tensor(t1[0:64, 0:n], pa[0:64, 0:n], cs[:, 0:n], ALU.mult), reads=[pa, cs], writes=[t1])
                P.op('vector', lambda e, pb=pb: e.tensor_tensor(t2[0:64, 0:n], pb[0:64, 0:n], sn[:, 0:n], ALU.mult), reads=[pb, sn], writes=[t2])
                P.op('gpsimd', lambda e, t0=t0, h=h: e.tensor_tensor(MQR[:, h, t0:t0 + n], t1[0:64, 0:n], t2[0:64, 0:n], ALU.add), reads=[t1, t2], writes=[MQR])
            P.dma(x2[:, 2:4, 0:n], AP(ckv, t0, [[TA, 128], [128 * TA, 2], [1, n]]), reads=[ckv], writes=[x2])
            P.op('scalar', lambda e: e.activation(sq[:, 0:2, 0:n], x2[:, 2:4, 0:n], AF.Square), reads=[x2], writes=[sq])
            ps_ = nps()
            for c in range(2):
                P.op('tensor', lambda e, c=c, ps_=ps_: e.matmul(ps_[:, 0:n], ones[:], sq[:, c, 0:n], start=(c == 0), stop=(c == 1)),
                     reads=[ones, sq], writes=[ps_])
            P.op('scalar', lambda e, ps_=ps_: e.activation(rs[:, 0:n], ps_[:, 0:n], AF.Sqrt, bias=eps64[:], scale=1.0 / 256),
                 reads=[ps_, eps64], writes=[rs])
            P.op('vector', lambda e: e.reciprocal(rs[:, 0:n], rs[:, 0:n]), reads=[rs], writes=[rs])
            for c in range(2):
                P.op('vector', lambda e, c=c: e.scalar_tensor_tensor(xnk[:, c, 0:n], x2[:, 2 + c, 0:n], nwt[:, 4 + c:5 + c], rs[:, 0:n],
                                                                     ALU.mult, ALU.mult), reads=[x2, nwt, rs], writes=[xnk])
            P.op('vector', lambda e: e.drain(), reads=[xnk], writes=[xnk])
            for h in range(2):
                ps_ = nps()
                for c in range(2):
                    P.op('tensor', lambda e, ps_=ps_, c=c, h=h: e.matmul(ps_[:, 0:n], wkv_b[:, c, h * 256:h * 256 + 128], xnk[:, c, 0:n],
                                                                         start=(c == 0), stop=(c == 1)), reads=[wkv_b, xnk], writes=[ps_])
                P.op('scalar', lambda e, t0=t0, ps_=ps_, h=h: e.copy(MKN[:, h, t0:t0 + n], ps_[:, 0:n]), reads=[ps_], writes=[MKN])
                for j in range(n // 128):
                    ps2 = nps()
                    kt = (t0 + j * 128) // 128
                    for c in range(2):
                        P.op('tensor', lambda e, ps2=ps2, c=c, h=h, j=j: e.matmul(ps2[:, 0:128], xnk[:, c, j * 128:(j + 1) * 128],
                                                                                 wkv_b[:, c, h * 256 + 128:h * 256 + 256],
                                                                                 start=(c == 0), stop=(c == 1)), reads=[wkv_b, xnk], writes=[ps2])
                    P.op('vector', lambda e, ps2=ps2, h=h, kt=kt: e.tensor_copy(MV[:, kt, h, 0:128], ps2[:, 0:128]), reads=[ps2], writes=[MV])

        pT = [P.sb('pT%d' % i, [128, 512], BF16) for i in range(2)]
        osb = [P.sb('osb%d' % i, [128, 128]) for i in range(2)]
        rec = P.sb('rec', [128, 1])
        cnt = [0]

        def attn(qparts, kparts, vfn, dv, scale, t0, n, kts, out, ocol, ow):
            sps = [pp[0], pp[1]]
            ops_ = pp[2:6]
            nq = n // 128
            for ki, kt in enumerate(kts):
                s_ = sps[cnt[0] % 2]
                p_ = pT[cnt[0] % 2]
                cnt[0] += 1
                for i, (qf, kf) in enumerate(zip(qparts, kparts)):
                    P.op('tensor', lambda e, s_=s_, qf=qf, kf=kf, i=i, kt=kt: e.matmul(
                        s_[:, 0:n], kf(kt), qf(t0, n), start=(i == 0), stop=(i == len(qparts) - 1)),
                        reads=[kf(kt), qf(t0, n)], writes=[s_])
                P.op('scalar', lambda e, s_=s_, p_=p_: e.activation(p_[:, 0:n], s_[:, 0:n], AF.Exp, scale=scale), reads=[s_], writes=[p_])
                for qs in range(nq):
                    P.op('tensor', lambda e, p_=p_, qs=qs, kt=kt, ki=ki: e.matmul(
                        ops_[qs][:, 0:dv + 1], p_[:, qs * 128:(qs + 1) * 128], vfn(kt), start=(ki == 0), stop=(ki == len(kts) - 1)),
                        reads=[p_, vfn(kt)], writes=[ops_[qs]])
            for qs in range(nq):
                o_ = osb[qs % 2]
                P.op('vector', lambda e, qs=qs: e.reciprocal(rec[:], ops_[qs][:, dv:dv + 1]), reads=[ops_[qs]], writes=[rec])
                P.op('vector', lambda e, qs=qs, o_=o_: e.tensor_scalar(o_[:, 0:dv], ops_[qs][:, 0:dv], rec[:, 0:1], None, ALU.mult),
                     reads=[ops_[qs], rec], writes=[o_])
                P.dma(AP(out, (t0 + qs * 128) * ow + ocol, [[ow, 128], [1, dv]]), o_[:, 0:dv], reads=[o_], writes=[])

        all_k = list(range(NKT))
        ctx_k = [32, 33]
        qblocks = [(i * 512, 512, all_k) for i in range(8)]
        if with_ctx:
            qblocks.append((4096, 256, ctx_k))
        for (t0, n, kts) in qblocks:
            for h in range(4):
                attn([lambda a, b, h=h: GQ[:, h, a:a + b]], [lambda kt: GK[:, kt * 128:(kt + 1) * 128]],
                     lambda kt: GV[:, kt, :], 64, 0.125, t0, n, kts, og, h * 64, 256)
            for h in range(2):
                attn([lambda a, b, h=h: MQN[:, h, a:a + b], lambda a, b, h=h: MQR[:, h, a:a + b]],
                     [lambda kt, h=h: MKN[:, h, kt * 128:(kt + 1) * 128], lambda kt: MKR[:, kt * 128:(kt + 1) * 128]],
                     lambda kt, h=h: MV[:, kt, h, :], 128, 192 ** -0.5, t0, n, kts, om, h * 128, 256)
        if DEBUG:
            for nm, t_, shp in [('d_GQ', GQ, [64, 4 * TA]), ('d_GK', GK, [64, TA]), ('d_MQR', MQR, [64, 2 * TA]), ('d_MKR', MKR, [64, TA]), ('d_MQN', MQN, [128, 2 * TA]), ('d_MKN', MKN, [128, 2*TA])]:
                dd = P.dram(nm, shp, BF16, 'ExternalOutput')
                P.dma(dd.ap(), t_[:], reads=[t_], writes=[])
        P.finish()
        nc = P.emit()
    return nc


HC = 64
TWO_PI = 2.0 * math.pi


def build_l2h():
    P = Prog()
    with ExitStack() as st:
        P._stack = st
        seqs = []
        for nm, L in (('L', 4096), ('C', 256)):
            nb = L // 128
            seqs.append(dict(
                nm=nm, L=L, nb=nb, NQ=128 * (2 * nb - 1), HPLEN=256 * nb, pad=L // 2 - 1,
                U=P.dram('U' + nm, [3, 3, 128, HC * 4 * nb], F32, 'ExternalInput'),
                feats=P.dram('feats' + nm, [33, L], F32, 'ExternalInput'),
                dec=P.dram('dec' + nm, [2, HC, L], F32, 'ExternalInput'),
                hp=P.dram('hp' + nm, [2, HC, 256 * nb], BF16, 'Internal'),
                out=P.dram('y' + nm, [128, HC * 4 * nb], F32, 'ExternalOutput')))
        cw = P.dram('cw', [3, 3, HC], F32, 'ExternalInput')
        w1 = P.dram('w1', [33, 64], F32, 'ExternalInput')
        w2 = P.dram('w2', [64, 64], F32, 'ExternalInput')
        w3 = P.dram('w3', [64, 2, HC], F32, 'ExternalInput')
        pv = P.dram('pv', [64, 4], F32, 'ExternalInput')
        hb = P.dram('hb', [2, HC], F32, 'ExternalInput')
        jm = P.dram('jm', [128, 128], F32, 'ExternalInput')

        F = 8192
        A1 = P.sb('A1', [128, F]); A2 = P.sb('A2', [128, F]); vN = P.sb('vN', [128, F]); xN = P.sb('xN', [128, F])
        zR = P.sb('zR', [128, F], BF16)
        toep = [P.sb('toep%d' % i, [128, 8064], BF16) for i in range(2)]
        wbc = P.sb('wbc', [128, 9 * HC]); hbc = P.sb('hbc', [128, 2 * HC])
        P.dma(wbc[:], AP(cw, 0, [[0, 128], [1, 9 * HC]]), reads=[cw], writes=[wbc])
        P.dma(hbc[:], AP(hb, 0, [[0, 128], [1, 2 * HC]]), reads=[hb], writes=[hbc])
        w1t = P.sb('w1t', [33, 64]); w2t = P.sb('w2t', [64, 64]); w3t = P.sb('w3t', [64, 2 * HC]); pvt = P.sb('pvt', [64, 4])
        jt = P.sb('jt', [128, 128])
        P.dma(w1t[:], w1.ap(), reads=[w1], writes=[w1t]); P.dma(w2t[:], w2.ap(), reads=[w2], writes=[w2t])
        P.dma(w3t[:], AP(w3, 0, [[2 * HC, 64], [1, 2 * HC]]), reads=[w3], writes=[w3t]); P.dma(pvt[:], pv.ap(), reads=[pv], writes=[pvt])
        P.dma(jt[:], jm.ap(), reads=[jm], writes=[jt])
        zero_bf = P.sb('zero_bf', [64, 2304], BF16)
        P.op('vector', lambda e: e.memset(zero_bf[:], 0.0), writes=[zero_bf])
        pp = [P.ps('pp%d' % i, [128, 512]) for i in range(6)]
        ppi = [0]

        def nps():
            p_ = pp[ppi[0] % 6]
            ppi[0] += 1
            return p_
        ft = [P.sb('ft%d' % i, [33, 256]) for i in range(2)]
        dct = [P.sb('dct%d' % i, [64, 2, 256]) for i in range(2)]
        arg = P.sb('arg', [64, 256]); ki = P.sb('ki', [64, 256], I32); kf = P.sb('kf', [64, 256])
        rsum = P.sb('rsum', [64, 2])

        def sin_to(dst, ps_, bcol, fcol):
            P.op('vector', lambda e: e.tensor_scalar(arg[:], ps_, pvt[:, bcol:bcol + 1], pvt[:, fcol:fcol + 1], ALU.add, ALU.mult),
                 reads=[ps_, pvt], writes=[arg])
            P.op('vector', lambda e: e.tensor_scalar(ki[:], arg[:], 1.0 / TWO_PI, None, ALU.mult), reads=[arg], writes=[ki])
            P.op('vector', lambda e: e.tensor_copy(kf[:], ki[:]), reads=[ki], writes=[kf])
            P.op('vector', lambda e: e.scalar_tensor_tensor(arg[:], kf[:], -TWO_PI, arg[:], ALU.mult, ALU.add), reads=[kf, arg], writes=[arg])
            P.op('vector', lambda e: e.tensor_scalar(kf[:], arg[:], math.pi, -TWO_PI, ALU.is_gt, ALU.mult), reads=[arg], writes=[kf])
            P.op('vector', lambda e: e.tensor_tensor(arg[:], arg[:], kf[:], ALU.add), reads=[arg, kf], writes=[arg])
            P.op('vector', lambda e: e.tensor_scalar(kf[:], arg[:], -math.pi, TWO_PI, ALU.is_lt, ALU.mult), reads=[arg], writes=[kf])
            P.op('vector', lambda e: e.tensor_tensor(arg[:], arg[:], kf[:], ALU.add), reads=[arg, kf], writes=[arg])
            P.op('vector', lambda e: e.tensor_scalar(arg[:], arg[:], 3.14159, -3.14159, ALU.min, ALU.max), reads=[arg], writes=[arg])
            P.op('scalar', lambda e: e.activation(dst, arg[:], AF.Sin), reads=[arg], writes=[dst])

        def filter_gen(S):
            L, hp, pad, HPLEN = S['L'], S['hp'], S['pad'], S['HPLEN']
            hid1 = A1[0:64, 0:L]; hid2 = A1[0:64, 4096:4096 + L]
            fr = [A2[0:64, 0:L], A2[0:64, 4096:4096 + L]]
            for bi, t0 in enumerate(range(0, L, 256)):
                f_ = ft[bi % 2]; d_ = dct[bi % 2]
                P.dma(f_[:], AP(S['feats'], t0, [[L, 33], [1, 256]]), reads=[S['feats']], writes=[f_])
                P.dma(d_[:], AP(S['dec'], t0, [[L, 64], [HC * L, 2], [1, 256]]), reads=[S['dec']], writes=[d_])
                ps_ = nps()
                P.op('tensor', lambda e, ps_=ps_, f_=f_: e.matmul(ps_[0:64, 0:256], w1t[:], f_[:], start=True, stop=True),
                     reads=[w1t, f_], writes=[ps_])
                sin_to(hid1[:, t0:t0 + 256], ps_[0:64, 0:256], 0, 2)
                ps_ = nps()
                P.op('tensor', lambda e, ps_=ps_, t0=t0: e.matmul(ps_[0:64, 0:256], w2t[:], hid1[:, t0:t0 + 256], start=True, stop=True),
                     reads=[w2t, A1], writes=[ps_])
                sin_to(hid2[:, t0:t0 + 256], ps_[0:64, 0:256], 1, 3)
                for o in range(2):
                    ps_ = nps()
                    P.op('tensor', lambda e, ps_=ps_, t0=t0, o=o: e.matmul(ps_[0:64, 0:256], w3t[:, o * HC:(o + 1) * HC], hid2[:, t0:t0 + 256],
                                                                          start=True, stop=True), reads=[w3t, A1], writes=[ps_])
                    P.op('vector', lambda e, ps_=ps_, t0=t0, o=o, d_=d_: e.tensor_tensor(fr[o][:, t0:t0 + 256], ps_[0:64, 0:256], d_[:, o, :], ALU.mult),
                         reads=[ps_, d_], writes=[A2])
            for o in range(2):
                P.op('vector', lambda e, o=o: e.tensor_reduce(rsum[:, o:o + 1], fr[o], AX.X, ALU.add, apply_absolute_value=True),
                     reads=[A2], writes=[rsum])
            P.op('vector', lambda e: e.reciprocal(rsum[:], rsum[:]), reads=[rsum], writes=[rsum])
            for o in range(2):
                fb = zR[0:64, o * 4096:o * 4096 + L]
                P.op('vector', lambda e, o=o, fb=fb: e.tensor_scalar(fb, fr[o], rsum[:, o:o + 1], None, ALU.mult), reads=[A2, rsum], writes=[zR])
                P.dma(AP(hp, o * HC * HPLEN + pad, [[HPLEN, 64], [1, L]]), fb, reads=[zR], writes=[hp])
                n1 = pad
                n2 = HPLEN - pad - L
                P.dma(AP(hp, o * HC * HPLEN, [[HPLEN, 64], [1, n1]]), zero_bf[:, 0:n1], reads=[zero_bf], writes=[hp])
                P.dma(AP(hp, o * HC * HPLEN + pad + L, [[HPLEN, 64], [1, n2]]), zero_bf[:, 0:n2], reads=[zero_bf], writes=[hp])

        def conv3(S, g, dest):
            n = HC * 4 * S['nb']
            for k in range(3):
                P.dma(A1[:, 0:n], AP(S['U'], (g * 3 + k) * 128 * n, [[n, 128], [1, n]]), reads=[S['U']], writes=[A1])
                wv = AP(wbc, (k * 3 + g) * HC, [[9 * HC, 128], [1, HC], [0, 4 * S['nb']]])
                a1v = AP(A1, 0, [[F, 128], [4 * S['nb'], HC], [1, 4 * S['nb']]])
                dv = AP(dest, 0, [[F, 128], [4 * S['nb'], HC], [1, 4 * S['nb']]])
                if k == 0:
                    P.op('vector', lambda e, a1v=a1v, wv=wv, dv=dv: e.tensor_tensor(dv, a1v, wv, ALU.mult), reads=[A1, wbc], writes=[dest])
                else:
                    P.op('vector', lambda e, a1v=a1v, wv=wv: e.tensor_tensor(a1v, a1v, wv, ALU.mult), reads=[A1, wbc], writes=[A1])
                    P.op('gpsimd', lambda e, n=n: e.tensor_tensor(dest[:, 0:n], dest[:, 0:n], A1[:, 0:n], ALU.add), reads=[dest, A1], writes=[dest])

        def reverse_to_zR(S, src):
            n = HC * 4 * S['nb']
            for i, c0 in enumerate(range(0, n, 256)):
                ps_ = nps()
                P.op('tensor', lambda e, ps_=ps_, c0=c0: e.matmul(ps_[:, 0:256], jt[:], src[:, c0:c0 + 256], start=True, stop=True),
                     reads=[jt, src], writes=[ps_])
                if i % 2 == 0:
                    P.op('vector', lambda e, ps_=ps_, c0=c0: e.tensor_copy(zR[:, c0:c0 + 256], ps_[:, 0:256]), reads=[ps_], writes=[zR])
                else:
                    P.op('scalar', lambda e, ps_=ps_, c0=c0: e.copy(zR[:, c0:c0 + 256], ps_[:, 0:256]), reads=[ps_], writes=[zR])

        tcnt = [0]

        def longconv(S, o):
            nb, NQ, HPLEN = S['nb'], S['NQ'], S['HPLEN']
            for c in range(HC):
                tp = toep[tcnt[0] % 2]
                q = 'sync' if tcnt[0] % 2 == 0 else 'gpsimd'
                tcnt[0] += 1
                P.dma(tp[:, 0:NQ], AP(S['hp'], (o * HC + c) * HPLEN, [[1, 128], [1, NQ]]), reads=[S['hp']], writes=[tp], q=q)
                ps_ = nps()
                ms = [0] + [m for m in range(-(nb - 1), nb) if m != 0]
                for mi, m in enumerate(ms):
                    a_lo, a_hi = max(0, m), min(nb, nb + m)
                    cnt = a_hi - a_lo
                    outv = AP(ps_, a_lo, [[512, 128], [nb, 4], [1, cnt]])
                    rhs = AP(zR, c * 4 * nb + (a_lo - m), [[F, 128], [nb, 4], [1, cnt]])
                    P.op('tensor', lambda e, outv=outv, rhs=rhs, tp=tp, m=m, mi=mi: e.matmul(
                        outv, tp[:, 128 * (m + nb - 1):128 * (m + nb)], rhs, start=(mi == 0), stop=(mi == len(ms) - 1), skip_group_check=True),
                        reads=[tp, zR], writes=[ps_])
                dst = A2[:, c * 4 * nb:(c + 1) * 4 * nb]
                if c % 2 == 0:
                    P.op('vector', lambda e, ps_=ps_, dst=dst: e.tensor_copy(dst, ps_[:, 0:4 * nb]), reads=[ps_], writes=[A2])
                else:
                    P.op('scalar', lambda e, ps_=ps_, dst=dst: e.copy(dst, ps_[:, 0:4 * nb]), reads=[ps_], writes=[A2])

        def gate(S, o, z, x):
            nb = S['nb']
            n = HC * 4 * nb
            bv = AP(hbc, o * HC, [[2 * HC, 128], [1, HC], [0, 4 * nb]])
            zv = AP(z, 0, [[F, 128], [4 * nb, HC], [1, 4 * nb]])
            P.op('vector', lambda e: e.tensor_tensor(zv, zv, bv, ALU.mult), reads=[z, hbc], writes=[z])
            P.op('gpsimd', lambda e: e.tensor_tensor(z[:, 0:n], z[:, 0:n], A2[:, 0:n], ALU.add), reads=[z, A2], writes=[z])
            P.op('vector', lambda e: e.tensor_tensor(z[:, 0:n], z[:, 0:n], x[:, 0:n], ALU.mult), reads=[z, x], writes=[z])

        for S in seqs:
            filter_gen(S)
            n = HC * 4 * S['nb']
            conv3(S, 0, vN)
            conv3(S, 1, xN)
            reverse_to_zR(S, vN)
            longconv(S, 0)
            gate(S, 0, vN, xN)
            conv3(S, 2, xN)
            reverse_to_zR(S, vN)
            longconv(S, 1)
            gate(S, 1, vN, xN)
            P.dma(S['out'].ap(), vN[:, 0:n], reads=[vN], writes=[])
        P.finish()
        nc = P.emit()
    return nc


def hy_consts(L):
    t = np.arange(L, dtype=np.float32)
    bands = 16
    f = np.linspace(1e-4, bands - 1, bands, dtype=np.float32)
    phase = (np.float32(2.0 * math.pi / L) * t[:, None] * f[None, :]).astype(np.float32)
    feats = np.concatenate([t[:, None] / np.float32(L - 1), np.cos(phase), -np.sin(phase)], -1).astype(np.float32)
    centre = L // 2
    dist = (np.abs(t - centre) / centre).astype(np.float32)
    deltas = np.abs(np.linspace(math.log(1e-2) / 1.5, math.log(1e-2) / 0.3, 1024, dtype=np.float32))
    dec = np.exp(-dist[:, None] * deltas[None, :]).astype(np.float32)
    return np.ascontiguousarray(feats.T), dec


def hy_inputs(core, pl_h, pc_h, inp, L_):
    c0 = core * HC
    d = {}
    for nm, u, L in (('L', pl_h, 4096), ('C', pc_h, 256)):
        nb = L // 128
        feats, dec = hy_consts(L)
        d['feats' + nm] = feats
        d['dec' + nm] = np.ascontiguousarray(dec.reshape(L, 2, 512)[:, :, c0:c0 + HC].transpose(1, 2, 0))
        ug = u.reshape(4, L, 3, 512)[:, :, :, c0:c0 + HC]
        up = np.pad(ug, ((0, 0), (1, 1), (0, 0), (0, 0)))
        U = np.empty((3, 3, 128, HC, 4, nb), np.float32)
        for k in range(3):
            s = up[:, k:k + L]
            U[:, k] = s.reshape(4, nb, 128, 3, HC).transpose(3, 2, 4, 0, 1)
        d['U' + nm] = U.reshape(3, 3, 128, HC * 4 * nb)
    d['cw'] = np.ascontiguousarray(inp['hy_conv_w'][L_].reshape(3, 3, 512)[:, :, c0:c0 + HC])
    d['w1'] = inp['hy_w1'][L_]; d['w2'] = inp['hy_w2'][L_]
    d['w3'] = np.ascontiguousarray(inp['hy_w3'][L_].reshape(64, 2, 512)[:, :, c0:c0 + HC])
    d['pv'] = np.ascontiguousarray(np.stack([inp['hy_b1'][L_], inp['hy_b2'][L_], inp['hy_sin_freq'][L_][0], inp['hy_sin_freq'][L_][1]], 1))
    d['hb'] = np.ascontiguousarray(inp['hy_bias'][L_][:, c0:c0 + HC])
    d['jm'] = np.ascontiguousarray(np.eye(128, dtype=np.float32)[::-1])
    return d


def hy_unpack(y, L):
    nb = L // 128
    return y.reshape(128, HC, 4, nb).transpose(2, 3, 0, 1).reshape(4, L, HC)


TG = 4352
NCH = 68


def gdn_groups(d):
    ctx = [0, 1, 2, 3]
    lat = [list(range(4 + 8 * i, 12 + 8 * i)) for i in range(8)]
    if d == 0:
        return [ctx] + lat
    return [ctx[::-1]] + [g[::-1] for g in lat[::-1]]


def build_l2g(NB=2):
    P = Prog()
    with ExitStack() as st:
        P._stack = st
        X = P.dram('X', [NB, 3, 128, 3, TG], F32, 'ExternalInput')
        Z = P.dram('Z', [NB, 64, NCH, 128], F32, 'ExternalInput')
        AB = P.dram('AB', [NB, 64, 2, 2, NCH], F32, 'ExternalInput')
        cwd = P.dram('cw', [128, 3, 3], F32, 'ExternalInput')
        scd = P.dram('sc', [64, 2, 2], F32, 'ExternalInput')
        nwd = P.dram('nw', [128], F32, 'ExternalInput')
        Md = P.dram('M', [64, 2, 5, 64], F32, 'ExternalInput')
        identd = P.dram('ident', [128, 128], F32, 'ExternalInput')
        Y = P.dram('Y', [NB, 64, NCH, 128], F32, 'ExternalOutput')

        ident = P.sb('identsb', [128, 128]); P.dma(ident[:], identd.ap(), reads=[identd], writes=[ident])
        Mt = P.sb('Mt', [64, 2, 5, 64]); P.dma(Mt[:], Md.ap(), reads=[Md], writes=[Mt])
        cw = P.sb('cwt', [128, 9]); P.dma(cw[:], AP(cwd, 0, [[9, 128], [1, 9]]), reads=[cwd], writes=[cw])
        sc = P.sb('sct', [64, 4]); P.dma(sc[:], AP(scd, 0, [[4, 64], [1, 4]]), reads=[scd], writes=[sc])
        nwb = P.sb('nwb', [64, 128]); P.dma(nwb[:], AP(nwd, 0, [[0, 64], [1, 128]]), reads=[nwd], writes=[nwb])
        ones = P.sb('ones', [128, 128]); P.op('vector', lambda e: e.memset(ones[:], 1.0), writes=[ones])
        one1 = P.sb('one1', [128, 1]); P.op('vector', lambda e: e.memset(one1[:], 1.0), writes=[one1])
        eps1 = P.sb('eps1', [128, 1]); P.op('vector', lambda e: e.memset(eps1[:], 1e-6), writes=[eps1])
        nea = P.sb('nea', [64, 2])
        for d in range(2):
            P.op('scalar', lambda e, d=d: e.activation(nea[:, d:d + 1], sc[:, 2 * d:2 * d + 1], AF.Exp), reads=[sc], writes=[nea])
        P.op('vector', lambda e: e.tensor_scalar(nea[:], nea[:], -1.0, None, ALU.mult), reads=[nea], writes=[nea])

        pp = [P.ps('pp%d' % i, [128, 512]) for i in range(5)]
        pr = [P.ps('pr%d' % i, [128, 128]) for i in range(3)]
        ppi = [0]

        def nps():
            p_ = pp[ppi[0] % 5]
            ppi[0] += 1
            return p_

        Qf = P.sb('Qf', [128, TG]); Kf = P.sb('Kf', [128, TG]); Vf = P.sb('Vf', [128, TG])
        Oacc = P.sb('Oacc', [64, NCH, 128])
        Zt = P.sb('Zt', [64, NCH, 128])
        stg = P.sb('stg', [128, 3, 256]); cacc = P.sb('cacc', [128, 256]); sq = P.sb('sqg', [128, 256]); rsb = P.sb('rsb', [128, 256])
        abt = P.sb('abt', [64, 2, 2, NCH])
        gt = P.sb('gt', [64, 2, NCH]); bet = P.sb('bet', [64, 2, NCH])
        S = P.sb('S', [128, 128])
        W = 512
        gc = P.sb('gc', [64, 8]); egc = P.sb('egc', [64, 8]); ckb = P.sb('ckb', [64, 8]); ckd = P.sb('ckd', [64, 8])
        E3a = P.sb('E3a', [64, W]); E3b = P.sb('E3b', [64, W])
        Dm = P.sb('Dm', [64, W]); CA = P.sb('CA', [64, W]); CAT = P.sb('CAT', [64, W]); tmpw = P.sb('tmpw', [64, W])
        Pm = P.sb('Pm', [64, W]); PT = P.sb('PTm', [64, W]); TT = P.sb('TTm', [64, W])
        KB = P.sb('KB', [64, 8, 128]); BV = P.sb('BV', [64, 8, 128])
        dbl = []
        for i in range(2):
            dbl.append(dict(U=P.sb('U%d' % i, [64, 8, 128]), KD=P.sb('KD%d' % i, [64, 8, 128]), WT=P.sb('WT%d' % i, [128, W]),
                            QD=P.sb('QD%d' % i, [128, W]), QKT=P.sb('QKT%d' % i, [64, W]), egl=P.sb('egl%d' % i, [128, 8])))
        vnew = P.sb('vnew', [64, 128])
        otmp = P.sb('otmp', [64, 128])

        def V(fn, reads, writes):
            P.op('vector', fn, reads=reads, writes=writes)

        def G_(fn, reads, writes):
            P.op('gpsimd', fn, reads=reads, writes=writes)

        def A_(fn, reads, writes):
            P.op('scalar', fn, reads=reads, writes=writes)

        def T_(fn, reads, writes):
            P.op('tensor', fn, reads=reads, writes=writes)

        def mm256(out_fn, lhsT, rhs_fn, n, reads, ps_):
            for c0 in range(0, n, 256):
                m = min(256, n - c0)
                T_(lambda e, c0=c0, m=m: e.matmul(out_fn(c0, m), lhsT, rhs_fn(c0, m), start=True, stop=True), reads, [ps_])

        gcount = [0]
        for bb in range(NB):
            for s, dst in enumerate((Qf, Kf, Vf)):
                for t0 in range(0, TG, 256):
                    P.dma(stg[:], AP(X, ((bb * 3 + s) * 128) * 3 * TG + t0, [[3 * TG, 128], [TG, 3], [1, 256]]), reads=[X], writes=[stg])
                    V(lambda e, s=s: e.tensor_scalar(cacc[:], stg[:, 0, :], cw[:, s * 3:s * 3 + 1], None, ALU.mult), [stg, cw], [cacc])
                    V(lambda e, s=s: e.scalar_tensor_tensor(cacc[:], stg[:, 1, :], cw[:, s * 3 + 1:s * 3 + 2], cacc[:], ALU.mult, ALU.add), [stg, cw, cacc], [cacc])
                    V(lambda e, s=s: e.scalar_tensor_tensor(cacc[:], stg[:, 2, :], cw[:, s * 3 + 2:s * 3 + 3], cacc[:], ALU.mult, ALU.add), [stg, cw, cacc], [cacc])
                    if s == 2:
                        A_(lambda e, t0=t0, dst=dst: e.activation(dst[:, t0:t0 + 256], cacc[:], AF.Silu), [cacc], [dst])
                    else:
                        A_(lambda e: e.activation(cacc[:], cacc[:], AF.Silu), [cacc], [cacc])
                        A_(lambda e: e.activation(sq[:], cacc[:], AF.Square), [cacc], [sq])
                        ps_ = nps()
                        T_(lambda e, ps_=ps_: e.matmul(ps_[:, 0:256], ones[:], sq[:], start=True, stop=True), [ones, sq], [ps_])
                        A_(lambda e, ps_=ps_: e.activation(rsb[:], ps_[:, 0:256], AF.Sqrt, bias=eps1[:], scale=1.0), [ps_, eps1], [rsb])
                        V(lambda e: e.reciprocal(rsb[:], rsb[:]), [rsb], [rsb])
                        scl = 128 ** -0.5 if s == 0 else 1.0
                        V(lambda e, t0=t0, dst=dst, scl=scl: e.scalar_tensor_tensor(dst[:, t0:t0 + 256], cacc[:], scl, rsb[:], ALU.mult, ALU.mult),
                          [cacc, rsb], [dst])
            P.dma(abt[:], AP(AB, bb * 64 * 4 * NCH, [[4 * NCH, 64], [1, 4 * NCH]]), reads=[AB], writes=[abt])
            for d in range(2):
                V(lambda e, d=d: e.tensor_scalar(gt[:, d, :], abt[:, 0, d, :], sc[:, 2 * d + 1:2 * d + 2], None, ALU.add), [abt, sc], [gt])
                A_(lambda e, d=d: e.activation(gt[:, d, :], gt[:, d, :], AF.Exp), [gt], [gt])
                A_(lambda e, d=d: e.activation(gt[:, d, :], gt[:, d, :], AF.Ln, bias=one1[0:64, :], scale=1.0), [gt, one1], [gt])
                V(lambda e, d=d: e.tensor_scalar(gt[:, d, :], gt[:, d, :], nea[:, d:d + 1], None, ALU.mult), [gt, nea], [gt])
                A_(lambda e, d=d: e.activation(bet[:, d, :], abt[:, 1, d, :], AF.Sigmoid), [abt], [bet])
            for d in range(2):
                V(lambda e: e.memset(S[:], 0.0), [], [S])
                tri = Mt[:, d, 0, :]

                def mask(i, G, d=d):
                    return AP(Mt, (d * 5 + i) * 64, [[640, 64], [0, G], [1, 64]])
                for grp in gdn_groups(d):
                    G = len(grp)
                    c_lo = min(grp)
                    Wg = 64 * G
                    col0 = 64 * c_lo
                    B_ = dbl[gcount[0] % 2]
                    gcount[0] += 1
                    U, KD, WT, QD, QKT, egl = B_['U'], B_['KD'], B_['WT'], B_['QD'], B_['QKT'], B_['egl']
                    gsl = gt[:, d, c_lo:c_lo + G]
                    bsl = bet[:, d, c_lo:c_lo + G]
                    ps_ = nps()
                    T_(lambda e, ps_=ps_, gsl=gsl, tri=tri: e.matmul(ps_[0:64, 0:G], tri, gsl, start=True, stop=True), [Mt, gt], [ps_])
                    T_(lambda e, ps_=ps_, gsl=gsl: e.matmul(ps_[:, 8:8 + G], ones[0:64, :], gsl, start=True, stop=True), [ones, gt], [ps_])
                    V(lambda e, ps_=ps_, G=G: e.tensor_copy(gc[:, 0:G], ps_[0:64, 0:G]), [ps_], [gc])
                    A_(lambda e, ps_=ps_, G=G, egl=egl: e.activation(egl[:, 0:G], ps_[:, 8:8 + G], AF.Exp), [ps_], [egl])
                    A_(lambda e, G=G: e.activation(egc[:, 0:G], gc[:, 0:G], AF.Exp), [gc], [egc])
                    V(lambda e, G=G, bsl=bsl: e.tensor_tensor(ckb[:, 0:G], egc[:, 0:G], bsl, ALU.mult), [egc, bet], [ckb])
                    V(lambda e, ps_=ps_, G=G: e.tensor_tensor(ckd[:, 0:G], ps_[0:64, 8:8 + G], gc[:, 0:G], ALU.subtract), [ps_, gc], [ckd])
                    A_(lambda e, G=G: e.activation(ckd[:, 0:G], ckd[:, 0:G], AF.Exp), [ckd], [ckd])
                    for (src, outs) in ((Kf, ((KB, ckb), (KD, ckd))), (Vf, ((BV, None),))):
                        for h0 in range(0, G, 4):
                            gh = min(4, G - h0)
                            ps_ = nps()
                            for j in range(gh):
                                c = c_lo + h0 + j
                                T_(lambda e, ps_=ps_, j=j, c=c, src=src: e.transpose(ps_[0:64, j * 128:(j + 1) * 128], src[:, 64 * c:64 * c + 64], ident[:]),
                                   [src, ident], [ps_])
                            pv = AP(ps_, 0, [[512, 64], [128, gh], [1, 128]])
                            for (dst, coef) in outs:
                                dv = dst[:, h0:h0 + gh, :]
                                if coef is None:
                                    cf = AP(bet, d * NCH + c_lo + h0, [[2 * NCH, 64], [1, gh], [0, 128]])
                                    V(lambda e, dv=dv, pv=pv, cf=cf: e.tensor_tensor(dv, pv, cf, ALU.mult), [ps_, bet], [dst])
                                else:
                                    cf = AP(coef, h0, [[8, 64], [1, gh], [0, 128]])
                                    V(lambda e, dv=dv, pv=pv, cf=cf: e.tensor_tensor(dv, pv, cf, ALU.mult), [ps_, coef], [dst])
                    idb = AP(ident, 0, [[128, 64], [0, G], [1, 64]])
                    e3a = AP(E3a, 0, [[W, 64], [64, G], [1, 64]]); e3b = AP(E3b, 0, [[W, 64], [64, G], [1, 64]])
                    V(lambda e, e3a=e3a, idb=idb, G=G: e.tensor_tensor(e3a, idb, AP(gc, 0, [[8, 64], [1, G], [0, 64]]), ALU.mult), [ident, gc], [E3a])
                    V(lambda e, e3b=e3b, idb=idb, G=G, c_lo=c_lo: e.tensor_tensor(e3b, idb, AP(bet, d * NCH + c_lo, [[2 * NCH, 64], [1, G], [0, 64]]), ALU.mult),
                      [ident, bet], [E3b])
                    psA = nps(); psB = nps(); psC = nps()
                    mm256(lambda c0, m, psA=psA: psA[0:64, c0:c0 + m], ones[0:64, 0:64], lambda c0, m: E3a[:, c0:c0 + m], Wg, [ones, E3a], psA)
                    mm256(lambda c0, m, psB=psB: psB[0:64, c0:c0 + m], ones[0:64, 0:64], lambda c0, m: E3b[:, c0:c0 + m], Wg, [ones, E3b], psB)
                    mm256(lambda c0, m, psC=psC: psC[:, c0:c0 + m], ones[0:64, :], lambda c0, m: E3a[:, c0:c0 + m], Wg, [ones, E3a], psC)
                    A_(lambda e, psC=psC, Wg=Wg, QD=QD: e.activation(QD[:, 0:Wg], psC[:, 0:Wg], AF.Exp), [psC], [QD])
                    V(lambda e, Wg=Wg, QD=QD, col0=col0: e.tensor_tensor(QD[:, 0:Wg], QD[:, 0:Wg], Qf[:, col0:col0 + Wg], ALU.mult), [QD, Qf], [QD])
                    dm3 = AP(Dm, 0, [[W, 64], [64, G], [1, 64]])
                    V(lambda e, psA=psA, dm3=dm3, G=G: e.scalar_tensor_tensor(dm3, AP(psA, 0, [[512, 64], [64, G], [1, 64]]), -1.0,
                                                                              AP(gc, 0, [[8, 64], [1, G], [0, 64]]), ALU.mult, ALU.add), [psA, gc], [Dm])
                    V(lambda e, Wg=Wg: e.tensor_scalar(tmpw[:, 0:Wg], Dm[:, 0:Wg], 0.0, None, ALU.min), [Dm], [tmpw])
                    A_(lambda e, Wg=Wg: e.activation(tmpw[:, 0:Wg], tmpw[:, 0:Wg], AF.Exp), [tmpw], [tmpw])
                    ca3 = AP(CA, 0, [[W, 64], [64, G], [1, 64]]); tw3 = AP(tmpw, 0, [[W, 64], [64, G], [1, 64]])
                    V(lambda e, ca3=ca3, tw3=tw3, G=G, c_lo=c_lo: e.tensor_tensor(ca3, tw3, AP(bet, d * NCH + c_lo, [[2 * NCH, 64], [1, G], [0, 64]]), ALU.mult),
                      [tmpw, bet], [CA])
                    G_(lambda e, ca3=ca3, G=G: e.tensor_tensor(ca3, ca3, mask(2, G), ALU.mult), [CA, Mt], [CA])
                    V(lambda e, Wg=Wg: e.tensor_scalar(tmpw[:, 0:Wg], Dm[:, 0:Wg], -1.0, 0.0, ALU.mult, ALU.min), [Dm], [tmpw])
                    A_(lambda e, Wg=Wg: e.activation(tmpw[:, 0:Wg], tmpw[:, 0:Wg], AF.Exp), [tmpw], [tmpw])
                    cat3 = AP(CAT, 0, [[W, 64], [64, G], [1, 64]])
                    V(lambda e, psB=psB, Wg=Wg: e.tensor_tensor(CAT[:, 0:Wg], tmpw[:, 0:Wg], psB[0:64, 0:Wg], ALU.mult), [tmpw, psB], [CAT])
                    G_(lambda e, cat3=cat3, G=G: e.tensor_tensor(cat3, cat3, mask(4, G), ALU.mult), [CAT, Mt], [CAT])
                    G_(lambda e, dm3=dm3, tw3=tw3, G=G: e.tensor_tensor(dm3, tw3, mask(3, G), ALU.mult), [tmpw, Mt], [Dm])
                    psK = nps(); psQ = nps()
                    for j in range(G):
                        c = c_lo + j
                        T_(lambda e, psK=psK, j=j, c=c: e.matmul(psK[0:64, 64 * j:64 * j + 64], Kf[:, 64 * c:64 * c + 64], Kf[:, 64 * c:64 * c + 64], start=True, stop=True),
                           [Kf], [psK])
                    for j in range(G):
                        c = c_lo + j
                        T_(lambda e, psQ=psQ, j=j, c=c: e.matmul(psQ[0:64, 64 * j:64 * j + 64], Kf[:, 64 * c:64 * c + 64], Qf[:, 64 * c:64 * c + 64], start=True, stop=True),
                           [Kf, Qf], [psQ])
                    V(lambda e, psK=psK, Wg=Wg: e.tensor_tensor(Pm[:, 0:Wg], psK[0:64, 0:Wg], CA[:, 0:Wg], ALU.mult), [psK, CA], [Pm])
                    V(lambda e, psK=psK, Wg=Wg: e.tensor_tensor(PT[:, 0:Wg], psK[0:64, 0:Wg], CAT[:, 0:Wg], ALU.mult), [psK, CAT], [PT])
                    V(lambda e, psQ=psQ, Wg=Wg, QKT=QKT: e.tensor_tensor(QKT[:, 0:Wg], psQ[0:64, 0:Wg], Dm[:, 0:Wg], ALU.mult), [psQ, Dm], [QKT])
                    tt3 = AP(TT, 0, [[W, 64], [64, G], [1, 64]]); pt3 = AP(PT, 0, [[W, 64], [64, G], [1, 64]])
                    V(lambda e, tt3=tt3, pt3=pt3, idb=idb: e.tensor_tensor(tt3, idb, pt3, ALU.subtract), [ident, PT], [TT])
                    for lev in range(5):
                        p1 = nps(); p2 = nps(); p3 = nps()
                        for j in range(G):
                            sl = slice(64 * j, 64 * j + 64)
                            T_(lambda e, p1=p1, sl=sl: e.matmul(p1[0:64, sl], PT[:, sl], Pm[:, sl], start=True, stop=True), [PT, Pm], [p1])
                        for j in range(G):
                            sl = slice(64 * j, 64 * j + 64)
                            T_(lambda e, p2=p2, sl=sl: e.matmul(p2[0:64, sl], Pm[:, sl], PT[:, sl], start=True, stop=True), [PT, Pm], [p2])
                        V(lambda e, p1=p1, Wg=Wg: e.tensor_copy(Pm[:, 0:Wg], p1[0:64, 0:Wg]), [p1], [Pm])
                        A_(lambda e, p2=p2, Wg=Wg: e.copy(PT[:, 0:Wg], p2[0:64, 0:Wg]), [p2], [PT])
                        for j in range(G):
                            sl = slice(64 * j, 64 * j + 64)
                            T_(lambda e, p3=p3, sl=sl: e.matmul(p3[0:64, sl], Pm[:, sl], TT[:, sl], start=True, stop=True), [Pm, TT], [p3])
                        V(lambda e, p3=p3, Wg=Wg: e.tensor_tensor(TT[:, 0:Wg], TT[:, 0:Wg], p3[0:64, 0:Wg], ALU.add), [p3, TT], [TT])
                    for h0 in range(0, G, 4):
                        gh = min(4, G - h0)
                        ps_ = nps()
                        for j in range(gh):
                            jj = h0 + j
                            T_(lambda e, ps_=ps_, j=j, jj=jj: e.matmul(ps_[0:64, j * 128:(j + 1) * 128], TT[:, 64 * jj:64 * jj + 64], BV[:, jj, :], start=True, stop=True),
                               [TT, BV], [ps_])
                        A_(lambda e, ps_=ps_, h0=h0, gh=gh, U=U: e.copy(U[:, h0:h0 + gh, :], AP(ps_, 0, [[512, 64], [128, gh], [1, 128]])), [ps_], [U])
                    ps_ = nps()
                    for j in range(G):
                        T_(lambda e, ps_=ps_, j=j: e.matmul(ps_[:, 64 * j:64 * j + 64], KB[:, j, :], TT[:, 64 * j:64 * j + 64], start=True, stop=True), [KB, TT], [ps_])
                    V(lambda e, ps_=ps_, Wg=Wg, WT=WT: e.tensor_copy(WT[:, 0:Wg], ps_[:, 0:Wg]), [ps_], [WT])
                    for c in grp:
                        j = c - c_lo
                        sl = slice(64 * j, 64 * j + 64)
                        p1, p2, p3 = pr
                        T_(lambda e, p1=p1, sl=sl, WT=WT: e.matmul(p1[0:64, :], WT[:, sl], S[:], start=True, stop=True), [WT, S], [p1])
                        V(lambda e, p1=p1, j=j, U=U: e.tensor_tensor(vnew[:], U[:, j, :], p1[0:64, :], ALU.subtract), [U, p1], [vnew])
                        T_(lambda e, p2=p2, sl=sl, QD=QD: e.matmul(p2[0:64, :], QD[:, sl], S[:], start=True, stop=False), [QD, S], [p2])
                        T_(lambda e, p2=p2, sl=sl, QKT=QKT: e.matmul(p2[0:64, :], QKT[:, sl], vnew[:], start=False, stop=True), [QKT, vnew], [p2])
                        T_(lambda e, p3=p3, j=j, KD=KD: e.matmul(p3[:, :], KD[:, j, :], vnew[:], start=True, stop=True), [KD, vnew], [p3])
                        V(lambda e, p3=p3, j=j, egl=egl: e.scalar_tensor_tensor(S[:], S[:], egl[:, j:j + 1], p3[:, :], ALU.mult, ALU.add), [S, egl, p3], [S])
                        if d == 0:
                            A_(lambda e, p2=p2, c=c: e.copy(Oacc[:, c, :], p2[0:64, :]), [p2], [Oacc])
                        else:
                            G_(lambda e: None, [], []) if False else None
                            V(lambda e, p2=p2, c=c: e.tensor_tensor(Oacc[:, c, :], Oacc[:, c, :], p2[0:64, :], ALU.add), [p2, Oacc], [Oacc])
            P.dma(Zt[:], AP(Z, bb * 64 * NCH * 128, [[NCH * 128, 64], [1, NCH * 128]]), reads=[Z], writes=[Zt])
            A_(lambda e: e.activation(Zt[:], Zt[:], AF.Silu), [Zt], [Zt])
            ssq = gt
            for h0 in range(0, NCH, 4):
                V(lambda e, h0=h0: e.tensor_tensor(KB[:, 0:4, :], Oacc[:, h0:h0 + 4, :], Oacc[:, h0:h0 + 4, :], ALU.mult), [Oacc], [KB])
                V(lambda e, h0=h0: e.tensor_reduce(ssq[:, 0, h0:h0 + 4], KB[:, 0:4, :], AX.X, ALU.add), [KB], [gt])
            A_(lambda e: e.activation(ssq[:, 0, :], ssq[:, 0, :], AF.Sqrt, bias=eps1[0:64, :], scale=1.0 / 128), [gt, eps1], [gt])
            V(lambda e: e.reciprocal(ssq[:, 0, :], ssq[:, 0, :]), [gt], [gt])
            V(lambda e: e.tensor_tensor(Oacc[:], Oacc[:], AP(gt, 0, [[2 * NCH, 64], [1, NCH], [0, 128]]), ALU.mult), [Oacc, gt], [Oacc])
            G_(lambda e: e.tensor_tensor(Oacc[:], Oacc[:], AP(nwb, 0, [[128, 64], [0, NCH], [1, 128]]), ALU.mult), [Oacc, nwb], [Oacc])
            V(lambda e: e.tensor_tensor(Oacc[:], Oacc[:], Zt[:], ALU.mult), [Oacc, Zt], [Oacc])
            P.dma(AP(Y, bb * 64 * NCH * 128, [[NCH * 128, 64], [1, NCH * 128]]), Oacc[:], reads=[Oacc], writes=[])
        P.finish()
        nc = P.emit()
    return nc


def gdn_masks():
    i = np.arange(64)
    M = np.zeros((64, 2, 5, 64), np.float32)
    for d in range(2):
        le = (i[:, None] <= i[None, :]) if d == 0 else (i[:, None] >= i[None, :])
        incl = (i[:, None] >= i[None, :]) if d == 0 else (i[:, None] <= i[None, :])
        strict = (i[:, None] > i[None, :]) if d == 0 else (i[:, None] < i[None, :])
        M[:, d, 0] = le; M[:, d, 1] = incl; M[:, d, 2] = strict; M[:, d, 3] = incl.T; M[:, d, 4] = strict.T
    return M


def gdn_inputs(h, pb_list, inp, L_):
    NB = len(pb_list)
    X = np.zeros((NB, 3, 128, 3, TG), np.float32)
    Zz = np.empty((NB, 64, NCH, 128), np.float32)
    AB = np.empty((NB, 64, 2, 2, NCH), np.float32)
    for bi, pb in enumerate(pb_list):
        seqs = [(pb[4096:4352], 0, 256), (pb[0:4096], 256, 4096)]
        for (ps_, off, L) in seqs:
            for s in range(3):
                x = ps_[:, s * 512 + h * 128:s * 512 + (h + 1) * 128]
                xp = np.pad(x, ((1, 1), (0, 0)))
                for k in range(3):
                    X[bi, s, :, k, off:off + L] = xp[k:k + L].T
        tok = np.concatenate([pb[4096:4352], pb[0:4096]], 0)
        z = tok[:, 1536 + h * 128:1536 + (h + 1) * 128]
        Zz[bi] = z.reshape(NCH, 64, 128).transpose(1, 0, 2)
        a = tok[:, 2048:2056].reshape(TG, 2, 4)[:, :, h]
        bt = tok[:, 2056:2064].reshape(TG, 2, 4)[:, :, h]
        AB[bi, :, 0] = a.reshape(NCH, 64, 2).transpose(1, 2, 0)
        AB[bi, :, 1] = bt.reshape(NCH, 64, 2).transpose(1, 2, 0)
    cw = np.ascontiguousarray(inp['gdn_conv_w'][L_].reshape(3, 3, 4, 128)[:, :, h, :].transpose(2, 1, 0))
    sc = np.empty((64, 2, 2), np.float32)
    sc[:, :, 0] = inp['gdn_a_log'][L_][:, h][None, :]
    sc[:, :, 1] = inp['gdn_dt_bias'][L_][:, h][None, :]
    return {'X': X, 'Z': Zz, 'AB': AB, 'cw': cw, 'sc': sc, 'nw': inp['gdn_norm_w'][L_], 'M': gdn_masks(), 'ident': np.eye(128, dtype=np.float32)}


def gdn_unpack(Y):
    t = Y.transpose(1, 0, 2).reshape(TG, 128)
    return t[256:], t[:256]


def build_l4():
    P = Prog()
    with ExitStack() as st:
        P._stack = st
        xT = P.dram('xT', [128, NKC, T3], F32, 'ExternalInput')
        fw = P.dram('fw', [128, NKC], F32, 'ExternalInput')
        oT = P.dram('oT', [128, NKC, T3], F32, 'ExternalOutput')
        fwt = P.sb('fwt', [128, NKC])
        P.dma(fwt[:], fw.ap(), reads=[fw], writes=[fwt])
        ones_bf = P.sb('ones_bf', [128, 128], BF16)
        P.op('vector', lambda e: e.memset(ones_bf[:], 1.0), writes=[ones_bf])
        eps_t = P.sb('eps_t', [128, 1])
        P.op('vector', lambda e: e.memset(eps_t[:], 1e-6), writes=[eps_t])
        SUB = 256
        xs = [P.sb('xs%d' % i, [128, NKC, SUB]) for i in range(2)]
        sq = P.sb('sq', [128, NKC, SUB], BF16)
        pss = P.ps('ps_ss', [128, SUB])
        rs = P.sb('rs', [128, SUB])
        tmp = P.sb('ntmp', [128, NKC, SUB])
        ot = [P.sb('ot%d' % i, [128, NKC, SUB]) for i in range(2)]
        nsub = (T3 + SUB - 1) // SUB
        for s in range(nsub):
            t0 = s * SUB
            n = min(SUB, T3 - t0)
            x_ = xs[s % 2]
            o_ = ot[s % 2]
            P.dma(x_[:, :, 0:n], AP(xT, t0, [[NKC * T3, 128], [T3, NKC], [1, n]]), reads=[xT], writes=[x_])
            P.op('scalar', lambda e, x_=x_, n=n: e.activation(sq[:, :, 0:n], x_[:, :, 0:n], AF.Square), reads=[x_], writes=[sq])
            for c in range(NKC):
                P.op('tensor', lambda e, c=c, n=n: e.matmul(pss[:, 0:n], ones_bf[:], sq[:, c, 0:n], start=(c == 0), stop=(c == NKC - 1)),
                     reads=[sq, ones_bf], writes=[pss])
            P.op('scalar', lambda e, n=n: e.activation(rs[:, 0:n], pss[:, 0:n], AF.Sqrt, bias=eps_t[:], scale=1.0 / D),
                 reads=[pss, eps_t], writes=[rs])
            P.op('vector', lambda e, n=n: e.reciprocal(rs[:, 0:n], rs[:, 0:n]), reads=[rs], writes=[rs])
            P.op('vector', lambda e, x_=x_, n=n: e.tensor_tensor(
                tmp[:, :, 0:n], x_[:, :, 0:n], AP(rs, 0, [[SUB, 128], [0, NKC], [1, n]]), ALU.mult), reads=[x_, rs], writes=[tmp])
            P.op('vector', lambda e, o_=o_, n=n: e.tensor_tensor(
                o_[:, :, 0:n], tmp[:, :, 0:n], AP(fwt, 0, [[NKC, 128], [1, NKC], [0, n]]), ALU.mult), reads=[tmp, fwt], writes=[o_])
            P.dma(AP(oT, t0, [[NKC * T3, 128], [T3, NKC], [1, n]]), o_[:, :, 0:n], reads=[o_], writes=[])
        P.finish()
        nc = P.emit()
    return nc


def _fm(a):
    return np.ascontiguousarray(a.T.reshape(16, 128, a.shape[0]).transpose(1, 0, 2))


def _unfm(a):
    return np.ascontiguousarray(a.transpose(2, 1, 0).reshape(a.shape[2], 2048))


def _rope_tables():
    n_freq = 16
    freqs = (10000.0 ** (-np.arange(n_freq, dtype=np.float32) / n_freq)).astype(np.float32)
    row = np.repeat(np.arange(64, dtype=np.float32), 64)
    col = np.tile(np.arange(64, dtype=np.float32), 64)
    ang = np.concatenate([row[:, None] * freqs, col[:, None] * freqs], -1)
    c = np.cos(ang).T.astype(np.float32)
    s = np.sin(ang).T.astype(np.float32)
    C = np.ones((64, TA), np.float32)
    S = np.zeros((64, TA), np.float32)
    C[:32, :4096] = c; C[32:, :4096] = c; S[:32, :4096] = -s; S[32:, :4096] = s
    rm = np.zeros((64, 64), np.float32)
    for m in range(64):
        rm[(m + 32) % 64, m] = 1
    return C, S, rm


def _run(nc, in_maps):
    res = run_bass_kernel_spmd(nc, in_maps, core_ids=list(range(8)))
    return res.results


def kernel(**inp):
    inp = {k: np.asarray(v) for k, v in inp.items()}
    x = inp['x']; ctx = inp['ctx']
    B = 4
    mod = run_l0(inp['c'], inp['c_ctx'], inp['w_ada'], inp['b_ada'])
    C, S, rm = _rope_tables()
    ident = np.eye(128, dtype=np.float32)
    nc1 = build_l1(); nc2 = build_l2a(); nc3 = build_l3(); nc4 = build_l4(); ncg = build_l2g(2); nch = build_l2h()
    xl = x.astype(np.float32); xc = ctx.astype(np.float32)
    for L in range(2):
        m = mod[L]

        def mrow(r, i):
            return vecT(m[r, i * 2048:(i + 1) * 2048])
        ims = []
        for core in range(8):
            b, hh = core // 2, core % 2
            xcat = np.concatenate([xl[b, hh * 2048:(hh + 1) * 2048], xc[b, hh * 128:(hh + 1) * 128]], 0)
            mv = np.stack([vecT(inp['norm1_w'][L]), mrow(b, 1), mrow(b, 0), mrow(4, 1), mrow(4, 0)])
            ims.append({'xT': _fm(xcat), 'mv': mv, 'W': inp['w_in'][L]})
        r1 = _run(nc1, ims)
        p = np.empty((B, TA, IN_DIM), np.float32)
        for core in range(8):
            b, hh = core // 2, core % 2
            pT = r1[core]['pT']
            p[b, hh * 2048:(hh + 1) * 2048] = pT[:, :2048].T
            p[b, 4096 + hh * 128:4096 + (hh + 1) * 128] = pT[:, 2048:].T
        del r1
        o = 2048 + 16
        nw = np.zeros((128, 8), np.float32)
        nw[:, 0:4] = inp['mla_q_norm_w'][L].reshape(4, 128).T
        nw[:, 4:6] = inp['mla_kv_norm_w'][L].reshape(2, 128).T
        nw[:64, 6] = inp['gqa_q_norm_w'][L]; nw[:64, 7] = inp['gqa_k_norm_w'][L]
        ims = []
        for core in range(8):
            b, hh = core // 2, core % 2
            pb = p[b]
            o2 = o + 832
            ims.append({'gq': np.ascontiguousarray(pb[:, o2 + hh * 256:o2 + (hh + 1) * 256].T),
                        'gk': np.ascontiguousarray(pb[:, o2 + 512 + hh * 64:o2 + 512 + (hh + 1) * 64].T),
                        'gv': np.ascontiguousarray(pb[:, o2 + 640 + hh * 64:o2 + 640 + (hh + 1) * 64]),
                        'cq': np.ascontiguousarray(pb[:, o:o + 512].T), 'ckv': np.ascontiguousarray(pb[:, o + 512:o + 768].T),
                        'kr': np.ascontiguousarray(pb[:, o + 768:o + 832].T),
                        'wuq': np.ascontiguousarray(inp['mla_w_uq'][L][:, 2 * hh:2 * hh + 2].reshape(512, 384)),
                        'wukv': np.ascontiguousarray(inp['mla_w_ukv'][L][:, 2 * hh:2 * hh + 2].reshape(256, 512)),
                        'nw': nw, 'cosd': C, 'sind': S, 'rmd': rm})
        r2 = _run(nc2, ims)
        br = np.zeros((B, TA, 4, 512), np.float32)
        for core in range(8):
            b, hh = core // 2, core % 2
            br[b, :, 1, hh * 256:(hh + 1) * 256] = r2[core]['om']
            br[b, :, 2, hh * 256:(hh + 1) * 256] = r2[core]['og']
        del r2
        ims = []
        for core in range(8):
            h, bp = core // 2, core % 2
            ims.append(gdn_inputs(h, [p[2 * bp], p[2 * bp + 1]], inp, L))
        rg = _run(ncg, ims)
        for core in range(8):
            h, bp = core // 2, core % 2
            for bi in range(2):
                lat, cx = gdn_unpack(rg[core]['Y'][bi])
                br[2 * bp + bi, :4096, 0, h * 128:(h + 1) * 128] = lat
                br[2 * bp + bi, 4096:, 0, h * 128:(h + 1) * 128] = cx
        del rg, ims
        pl_h = np.ascontiguousarray(p[:, :4096, 3664:5200]); pc_h = np.ascontiguousarray(p[:, 4096:, 3664:5200])
        ims = [hy_inputs(core, pl_h, pc_h, inp, L) for core in range(8)]
        rh = _run(nch, ims)
        for core in range(8):
            br[:, :4096, 3, core * 64:(core + 1) * 64] = hy_unpack(rh[core]['yL'], 4096)
            br[:, 4096:, 3, core * 64:(core + 1) * 64] = hy_unpack(rh[core]['yC'], 256)
        del rh, ims
        wbr = np.ascontiguousarray(inp['w_branch'][L].reshape(2048, 2048))
        wr = np.ascontiguousarray(inp['w_router'].reshape(16, 128, 16).transpose(1, 0, 2))
        nxl = np.empty_like(xl); nxc = np.empty_like(xc)
        for half in range(2):
            ims = []
            for core in range(8):
                sh = half * 8 + core
                b, q = sh // 4, sh % 4
                ls = slice(q * 1024, (q + 1) * 1024); cs_ = slice(q * 64, (q + 1) * 64)
                cs2 = slice(4096 + q * 64, 4096 + (q + 1) * 64)
                xcat = np.concatenate([xl[b, ls], xc[b, cs_]], 0)
                brc = np.concatenate([br[b, ls], br[b, cs2]], 0).reshape(T3, 2048)
                g = np.concatenate([p[b, ls, MIX_IN:], p[b, cs2, MIX_IN:]], 0)
                mv = np.stack([vecT(inp['norm2_w'][L]), mrow(b, 2), mrow(b, 3), mrow(b, 4), mrow(b, 5),
                               mrow(4, 2), mrow(4, 3), mrow(4, 4), mrow(4, 5)])
                ims.append({'xT': _fm(xcat), 'brT': np.ascontiguousarray(brc.T), 'gT': np.ascontiguousarray(g.T), 'mv': mv,
                            'wbr': wbr, 'wo': inp['w_out'][L], 'wr': wr, 'rb': inp['router_bias'],
                            'wg': inp['moe_w_gate'][L], 'wu': inp['moe_w_up'][L], 'wd': inp['moe_w_down'][L], 'ident': ident})
            r3 = _run(nc3, ims)
            for core in range(8):
                sh = half * 8 + core
                b, q = sh // 4, sh % 4
                x2 = _unfm(r3[core]['x2T'])
                nxl[b, q * 1024:(q + 1) * 1024] = x2[:1024]
                nxc[b, q * 64:(q + 1) * 64] = x2[1024:]
            del r3
        xl, xc = nxl, nxc
        del p, br
    out = np.empty_like(xl)
    fw = vecT(inp['final_norm_w'])
    for half in range(2):
        ims = []
        for core in range(8):
            sh = half * 8 + core
            b, q = sh // 4, sh % 4
            xcat = np.concatenate([xl[b, q * 1024:(q + 1) * 1024], xc[b, q * 64:(q + 1) * 64]], 0)
            ims.append({'xT': _fm(xcat), 'fw': fw})
        r4 = _run(nc4, ims)
        for core in range(8):
            sh = half * 8 + core
            b, q = sh // 4, sh % 4
            out[b, q * 1024:(q + 1) * 1024] = _unfm(r4[core]['oT'])[:1024]
    return out.astype(np.float32)
```

```python
import math
from contextlib import ExitStack
import types
import numpy as np
import concourse.bass as bass
import concourse.mybir as mybir
from concourse.bass_utils import run_bass_kernel_spmd

F32 = mybir.dt.float32
BF16 = mybir.dt.bfloat16
I32 = mybir.dt.int32
AF = mybir.ActivationFunctionType
ALU = mybir.AluOpType
AX = mybir.AxisListType

COMPUTE = ('tensor', 'vector', 'scalar', 'gpsimd')
NDMASEM = 24


def _freeze(fn):
    if not getattr(fn, '__closure__', None):
        return fn
    cells = []
    for c in fn.__closure__:
        try:
            cells.append(types.CellType(c.cell_contents))
        except ValueError:
            cells.append(c)
    g = types.FunctionType(fn.__code__, fn.__globals__, fn.__name__, fn.__defaults__, tuple(cells))
    g.__kwdefaults__ = fn.__kwdefaults__
    return g


class Prog:
    def __init__(self, name='k'):
        self.nc = bass.Bass('TRN2', target_bir_lowering=False)
        self.ops = {e: [] for e in COMPUTE + ('sync',)}
        self.cnt = {e: 0 for e in COMPUTE}
        self.waited = {e: {} for e in COMPUTE + ('sync',)}
        self.last_w = {}
        self.readers = {}
        self.dma_cnt = [0] * NDMASEM
        self.dma_rr = 0
        self.ctx = []
        self.sems = {}
        self.n_ops = 0
        self._stack = None
        self.safe = False
        self.pe_pending = {}

    def _enter(self, cm):
        return self._stack.enter_context(cm)

    def dram(self, name, shape, dt, kind):
        return self.nc.dram_tensor(name, list(shape), dt, kind=kind)

    def sb(self, name, shape, dt=F32):
        return self._enter(self.nc.sbuf_tensor(name, list(shape), dt))

    def ps(self, name, shape, dt=F32):
        return self._enter(self.nc.psum_tensor(name, list(shape), dt))

    @staticmethod
    def _key(t):
        if isinstance(t, str):
            return t
        if hasattr(t, 'tensor'):
            t = t.tensor
        return t.name

    def _deps(self, reads, writes):
        deps = []
        for r in reads:
            k = self._key(r)
            if k in self.last_w:
                deps.append(self.last_w[k])
        for w in writes:
            k = self._key(w)
            if k in self.last_w:
                deps.append(self.last_w[k])
            deps.extend(self.readers.get(k, []))
        return deps

    def _commit(self, ticket, reads, writes):
        for r in reads:
            self.readers.setdefault(self._key(r), []).append(ticket)
        for w in writes:
            k = self._key(w)
            self.last_w[k] = ticket
            self.readers[k] = []

    def _wait_list(self, eng, deps):
        need = {}
        for (kind, idx, val) in deps:
            if kind == 'eng' and idx == 'tensor' and eng == 'tensor':
                continue
            sk = (kind, idx)
            if self.waited[eng].get(sk, 0) >= val:
                continue
            if need.get(sk, 0) < val:
                need[sk] = val
        for sk, val in need.items():
            self.waited[eng][sk] = val
        return list(need.items())

    def op(self, eng, fn, reads=(), writes=()):
        deps = self._deps(reads, writes)
        waits = self._wait_list(eng, deps)
        self.cnt[eng] += 1
        ticket = ('eng', eng, self.cnt[eng])
        self._commit(ticket, reads, writes)
        if eng == 'tensor':
            for w in writes:
                self.pe_pending.setdefault(self._key(w), set()).update(self._key(r) for r in reads)
        else:
            for r in reads:
                pk = self._key(r)
                if pk in self.pe_pending:
                    for k in self.pe_pending.pop(pk):
                        self.readers.setdefault(k, []).append(ticket)
        self.ops[eng].append(('c', waits, _freeze(fn)))
        self.n_ops += 1
        return ticket

    def dma(self, out, in_, reads=(), writes=(), q='sync', **kw):
        deps = self._deps(reads, writes)
        si = self.dma_rr
        self.dma_rr = (self.dma_rr + 1) % NDMASEM
        if self.dma_cnt[si] > 0:
            deps.append(('dma', si, self.dma_cnt[si]))
        waits = self._wait_list(q, deps)
        self.dma_cnt[si] += 16
        ticket = ('dma', si, self.dma_cnt[si])
        self._commit(ticket, reads, writes)
        self.ops[q].append(('d', waits, (out, in_, si, kw)))
        self.n_ops += 1
        return ticket

    def finish(self, eng='sync'):
        deps = [('dma', i, c) for i, c in enumerate(self.dma_cnt) if c > 0]
        waits = self._wait_list(eng, deps)
        self.ops[eng].append(('w', waits, None))

    def emit(self):
        nc = self.nc
        esem = {e: self._enter(nc.semaphore('s_' + e)) for e in COMPUTE}
        dsem = [self._enter(nc.semaphore('d_%d' % i)) for i in range(NDMASEM)]

        def semof(sk):
            return esem[sk[1]] if sk[0] == 'eng' else dsem[sk[1]]

        ops = self.ops

        def run(engname):
            def body(e):
                for (kind, waits, payload) in ops[engname]:
                    for sk, val in waits:
                        e.wait_ge(semof(sk), val)
                    if kind == 'c':
                        ins = payload(e)
                        if self.safe and engname != 'tensor':
                            e.drain().then_inc(esem[engname], 1)
                        else:
                            ins.then_inc(esem[engname], 1)
                    elif kind == 'd':
                        out, in_, si, kw = payload
                        e.dma_start(out=out, in_=in_, **kw).then_inc(dsem[si], 16)
            return body

        with nc.Block() as block:
            block.sync(run('sync'))
            block.tensor(run('tensor'))
            block.vector(run('vector'))
            block.scalar(run('scalar'))
            block.gpsimd(run('gpsimd'))
        return nc


D = 2048
NKC = 16
IN_DIM = 13392
T1 = 2176


def AP(t, off, dims):
    return bass.AP(t, off, [list(d) for d in dims])


def build_l0():
    P = Prog()
    with ExitStack() as st:
        P._stack = st
        cT = P.dram('cT', [128, 16, 5], F32, 'ExternalInput')
        w = P.dram('w', [2, D, 1536], F32, 'ExternalInput')
        b = P.dram('b', [2, 1536], F32, 'ExternalInput')
        out = P.dram('mod', [2, 5, 1536], F32, 'ExternalOutput')
        ct = P.sb('ct', [128, 16, 5])
        cs = P.sb('cs', [128, 16, 5])
        P.dma(ct[:], cT.ap(), reads=[cT], writes=[ct])
        P.op('scalar', lambda e: e.activation(cs[:], ct[:], AF.Silu), reads=[ct], writes=[cs])
        wt = [P.sb('wt%d' % i, [128, 4, 512]) for i in range(2)]
        bt = P.sb('bt', [5, 2, 1536])
        P.dma(bt[:], AP(b, 0, [[0, 5], [1536, 2], [1, 1536]]), reads=[b], writes=[bt])
        ps = [P.ps('ps%d' % i, [5, 512]) for i in range(2)]
        ot = P.sb('ot', [5, 2, 1536])
        n = 0
        pi = 0
        for l in range(2):
            for cb in range(3):
                pst = ps[pi % 2]
                pi += 1
                for kg in range(4):
                    wtile = wt[n % 2]
                    n += 1
                    src = AP(w, l * D * 1536 + kg * 512 * 1536 + cb * 512, [[1536, 128], [128 * 1536, 4], [1, 512]])
                    P.dma(wtile[:], src, reads=[w], writes=[wtile])
                    for kk in range(4):
                        k = kg * 4 + kk
                        P.op('tensor', lambda e, k=k, kk=kk, wtile=wtile, pst=pst: e.matmul(
                            pst[:], cs[:, k, :], wtile[:, kk, :], start=(k == 0), stop=(k == 15)),
                            reads=[cs, wtile], writes=[pst])
                P.op('vector', lambda e, l=l, cb=cb, pst=pst: e.tensor_tensor(
                    ot[:, l, cb * 512:(cb + 1) * 512], pst[:], bt[:, l, cb * 512:(cb + 1) * 512], ALU.add),
                    reads=[pst, bt], writes=[ot])
        P.dma(AP(out, 0, [[1536, 5], [5 * 1536, 2], [1, 1536]]), ot[:], reads=[ot], writes=[out])
        P.finish()
        nc = P.emit()
    return nc


def run_l0(c, c_ctx, w_ada, b_ada):
    c5 = np.concatenate([c, c_ctx[None]], 0)
    cT = np.ascontiguousarray(c5.T.reshape(16, 128, 5).transpose(1, 0, 2))
    nc = build_l0()
    in_maps = []
    for i in range(8):
        sl = slice(i * 1536, (i + 1) * 1536)
        in_maps.append({'cT': cT, 'w': np.ascontiguousarray(w_ada[:, :, sl]), 'b': np.ascontiguousarray(b_ada[:, sl])})
    res = run_bass_kernel_spmd(nc, in_maps, core_ids=list(range(8)))
    mod = np.concatenate([r['mod'] for r in res.results], axis=2)
    return mod


def vecT(v):
    return np.ascontiguousarray(v.reshape(16, 128).T)


TOKB = [(0, 512), (512, 512), (1024, 512), (1536, 512), (2048, 128)]


def AP_slice(mvt, r):
    return mvt[:, r, :]


def norm_phase(P, xT, hT, gl, sl_, gc, sc_, ones_bf, eps_t, T, n_lat, x_reads=None, out_key='hT', SUB=256):
    xs = [P.sb('xs%d' % i, [128, NKC, SUB]) for i in range(2)]
    sq = P.sb('sq', [128, NKC, SUB], BF16)
    pss = P.ps('ps_ss', [128, SUB])
    rs = P.sb('rs', [128, SUB])
    tmp = P.sb('ntmp', [128, NKC, SUB])
    nsub = (T + SUB - 1) // SUB
    for s in range(nsub):
        t0 = s * SUB
        n = min(SUB, T - t0)
        x_ = xs[s % 2]
        P.dma(x_[:, :, 0:n], AP(xT, t0, [[NKC * T, 128], [T, NKC], [1, n]]), reads=(x_reads if x_reads is not None else [xT]), writes=[x_])
        P.op('scalar', lambda e, x_=x_, n=n: e.activation(sq[:, :, 0:n], x_[:, :, 0:n], AF.Square),
             reads=[x_], writes=[sq])
        for c in range(NKC):
            P.op('tensor', lambda e, c=c, n=n: e.matmul(pss[:, 0:n], ones_bf[:], sq[:, c, 0:n],
                                                          start=(c == 0), stop=(c == NKC - 1)),
                 reads=[sq, ones_bf], writes=[pss])
        P.op('scalar', lambda e, n=n: e.activation(rs[:, 0:n], pss[:, 0:n], AF.Sqrt, bias=eps_t[:], scale=1.0 / D),
             reads=[pss, eps_t], writes=[rs])
        P.op('vector', lambda e, n=n: e.reciprocal(rs[:, 0:n], rs[:, 0:n]), reads=[rs], writes=[rs])
        P.op('vector', lambda e, x_=x_, n=n: e.tensor_tensor(
            tmp[:, :, 0:n], x_[:, :, 0:n], AP(rs, 0, [[SUB, 128], [0, NKC], [1, n]]), ALU.mult),
            reads=[x_, rs], writes=[tmp])
        g_, s_ = (gl, sl_) if t0 < n_lat else (gc, sc_)
        for c in range(NKC):
            eng = 'vector' if c % 2 == 0 else 'gpsimd'
            P.op(eng, lambda e, c=c, n=n, g_=g_, s_=s_, t0=t0: e.tensor_scalar(
                hT[:, c, t0:t0 + n], tmp[:, c, 0:n], g_[:, c:c + 1], s_[:, c:c + 1], ALU.mult, ALU.add),
                reads=[tmp, g_, s_], writes=['%s:%d' % (out_key, c)])


def linear_fm(P, W, w_off, w_rs, K_chunks, n_out, rhs_fn, tokb, evac_fn, rhs_reads, tag='lin', grp=256):
    wst = [P.sb('%s_wst%d' % (tag, i), [128, K_chunks, grp]) for i in range(2)]
    wbf = [P.sb('%s_wbf%d' % (tag, i), [128, K_chunks, grp], BF16) for i in range(2)]
    pst = [P.ps('%s_ps%d' % (tag, i), [128, 512]) for i in range(2)]
    ng = (n_out + grp - 1) // grp
    pi = 0
    for g in range(ng):
        o0 = g * grp
        gw = min(grp, n_out - o0)
        ws, wb = wst[g % 2], wbf[g % 2]
        P.dma(ws[:, :, 0:gw], AP(W, w_off + o0, [[w_rs, 128], [128 * w_rs, K_chunks], [1, gw]]),
              reads=[W], writes=[ws])
        P.op('scalar', lambda e, ws=ws, wb=wb, gw=gw: e.copy(wb[:, :, 0:gw], ws[:, :, 0:gw]),
             reads=[ws], writes=[wb])
        for m0 in range(0, gw, 128):
            m = min(128, gw - m0)
            for (t0, n) in tokb:
                ps_ = pst[pi % 2]
                pi += 1
                for kc in range(K_chunks):
                    P.op('tensor', lambda e, ps_=ps_, wb=wb, kc=kc, m0=m0, m=m, t0=t0, n=n: e.matmul(
                        ps_[0:m, 0:n], wb[:, kc, m0:m0 + m], rhs_fn(kc, t0, n),
                        start=(kc == 0), stop=(kc == K_chunks - 1)),
                        reads=[wb] + rhs_reads(kc), writes=[ps_])
                evac_fn(o0 + m0, m, t0, n, ps_)


def build_l1():
    P = Prog()
    with ExitStack() as st:
        P._stack = st
        xT = P.dram('xT', [128, NKC, T1], F32, 'ExternalInput')
        mv = P.dram('mv', [5, 128, NKC], F32, 'ExternalInput')
        W = P.dram('W', [D, IN_DIM], F32, 'ExternalInput')
        pT = P.dram('pT', [IN_DIM, T1], F32, 'ExternalOutput')
        mvt = P.sb('mvt', [128, 5, NKC])
        P.dma(mvt[:], AP(mv, 0, [[NKC, 128], [128 * NKC, 5], [1, NKC]]), reads=[mv], writes=[mvt])
        gl = P.sb('gl', [128, NKC]); gc = P.sb('gc', [128, NKC])
        sl_ = P.sb('sl', [128, NKC]); sc_ = P.sb('sc', [128, NKC])
        P.op('vector', lambda e: e.scalar_tensor_tensor(gl[:], mvt[:, 1, :], 1.0, mvt[:, 0, :], ALU.add, ALU.mult),
             reads=[mvt], writes=[gl])
        P.op('vector', lambda e: e.scalar_tensor_tensor(gc[:], mvt[:, 3, :], 1.0, mvt[:, 0, :], ALU.add, ALU.mult),
             reads=[mvt], writes=[gc])
        P.op('vector', lambda e: e.tensor_copy(sl_[:], mvt[:, 2, :]), reads=[mvt], writes=[sl_])
        P.op('vector', lambda e: e.tensor_copy(sc_[:], mvt[:, 4, :]), reads=[mvt], writes=[sc_])
        ones_bf = P.sb('ones_bf', [128, 128], BF16)
        P.op('vector', lambda e: e.memset(ones_bf[:], 1.0), writes=[ones_bf])
        eps_t = P.sb('eps_t', [128, 1])
        P.op('vector', lambda e: e.memset(eps_t[:], 1e-6), writes=[eps_t])
        hT = P.sb('hT', [128, NKC, T1], BF16)
        norm_phase(P, xT, hT, gl, sl_, gc, sc_, ones_bf, eps_t, T1, 2048)
        osb = [P.sb('osb%d' % i, [128, 512]) for i in range(3)]
        cnt = [0]

        def evac(o0, m, t0, n, ps_):
            o_ = osb[cnt[0] % 3]
            eng = 'vector' if cnt[0] % 2 == 0 else 'scalar'
            cnt[0] += 1
            if eng == 'vector':
                P.op('vector', lambda e: e.tensor_copy(o_[0:m, 0:n], ps_[0:m, 0:n]), reads=[ps_], writes=[o_])
            else:
                P.op('scalar', lambda e: e.copy(o_[0:m, 0:n], ps_[0:m, 0:n]), reads=[ps_], writes=[o_])
            P.dma(AP(pT, o0 * T1 + t0, [[T1, m], [1, n]]), o_[0:m, 0:n], reads=[o_], writes=[])

        linear_fm(P, W, 0, IN_DIM, NKC, IN_DIM, lambda kc, t0, n: hT[:, kc, t0:t0 + n], TOKB, evac, lambda kc: ['hT:%d' % kc])
        P.finish()
        nc = P.emit()
    return nc


MIX_IN = 5200
T3 = 1088
TOKB3 = [(0, 512), (512, 512), (1024, 64)]
NLAT3 = 1024
BIG = 1.0e4


class Lin:
    def __init__(self, P, tag, kmax=16, grp=256, nps=2):
        self.P = P
        self.grp = grp
        self.wst = [P.sb('%s_wst%d' % (tag, i), [128, kmax, grp]) for i in range(2)]
        self.wbf = [P.sb('%s_wbf%d' % (tag, i), [128, kmax, grp], BF16) for i in range(2)]
        self.pst = [P.ps('%s_ps%d' % (tag, i), [128, 512]) for i in range(nps)]
        self.nps = nps
        self.g = 0
        self.pi = 0

    def run(self, W, w_off, w_rs, K_chunks, n_out, rhs_fn, tokb, evac_fn, rhs_reads, split=1):
        P = self.P
        grp = self.grp
        ng = (n_out + grp - 1) // grp
        kper = K_chunks // split
        for g in range(ng):
            o0 = g * grp
            gw = min(grp, n_out - o0)
            ws, wb = self.wst[self.g % 2], self.wbf[self.g % 2]
            P.dma(ws[:, 0:K_chunks, 0:gw], AP(W, w_off + o0, [[w_rs, 128], [128 * w_rs, K_chunks], [1, gw]]),
                  reads=[W], writes=[ws])
            self.g += 1
            P.op('scalar', lambda e, ws=ws, wb=wb, gw=gw: e.copy(wb[:, 0:K_chunks, 0:gw], ws[:, 0:K_chunks, 0:gw]),
                 reads=[ws], writes=[wb])
            for m0 in range(0, gw, 128):
                m = min(128, gw - m0)
                for (t0, n) in tokb:
                    tiles = []
                    for sp in range(split):
                        ps_ = self.pst[self.pi % self.nps]
                        self.pi += 1
                        tiles.append(ps_)
                        for kk in range(kper):
                            kc = sp * kper + kk
                            P.op('tensor', lambda e, ps_=ps_, wb=wb, kc=kc, kk=kk, m0=m0, m=m, t0=t0, n=n: e.matmul(
                                ps_[0:m, 0:n], wb[:, kc, m0:m0 + m], rhs_fn(kc, t0, n),
                                start=(kk == 0), stop=(kk == kper - 1)),
                                reads=[wb] + rhs_reads(kc), writes=[ps_])
                    evac_fn(o0 + m0, m, t0, n, tiles if split > 1 else tiles[0])


def build_l3():
    P = Prog()
    with ExitStack() as st:
        P._stack = st
        xT = P.dram('xT', [128, NKC, T3], F32, 'ExternalInput')
        brT = P.dram('brT', [2048, T3], F32, 'ExternalInput')
        gT = P.dram('gT', [8192, T3], F32, 'ExternalInput')
        mv = P.dram('mv', [9, 128, NKC], F32, 'ExternalInput')
        wbr = P.dram('wbr', [2048, D], F32, 'ExternalInput')
        wo = P.dram('wo', [D, D], F32, 'ExternalInput')
        wr = P.dram('wr', [128, NKC, 16], F32, 'ExternalInput')
        rb = P.dram('rb', [16], F32, 'ExternalInput')
        wg = P.dram('wg', [16, D, 512], F32, 'ExternalInput')
        wu = P.dram('wu', [16, D, 512], F32, 'ExternalInput')
        wd = P.dram('wd', [16, 512, D], F32, 'ExternalInput')
        ident_d = P.dram('ident', [128, 128], F32, 'ExternalInput')
        x1T = P.dram('x1T', [128, NKC, T3], F32, 'Internal')
        x2T = P.dram('x2T', [128, NKC, T3], F32, 'ExternalOutput')

        mvt = P.sb('mvt', [128, 9, NKC])
        P.dma(mvt[:], AP(mv, 0, [[NKC, 128], [128 * NKC, 9], [1, NKC]]), reads=[mv], writes=[mvt])
        gl = P.sb('gl', [128, NKC]); gc = P.sb('gc', [128, NKC])
        P.op('vector', lambda e: e.scalar_tensor_tensor(gl[:], mvt[:, 3, :], 1.0, mvt[:, 0, :], ALU.add, ALU.mult),
             reads=[mvt], writes=[gl])
        P.op('vector', lambda e: e.scalar_tensor_tensor(gc[:], mvt[:, 7, :], 1.0, mvt[:, 0, :], ALU.add, ALU.mult),
             reads=[mvt], writes=[gc])
        ones_bf = P.sb('ones_bf', [128, 128], BF16)
        P.op('vector', lambda e: e.memset(ones_bf[:], 1.0), writes=[ones_bf])
        eps_t = P.sb('eps_t', [128, 1])
        P.op('vector', lambda e: e.memset(eps_t[:], 1e-6), writes=[eps_t])
        ident = P.sb('identsb', [128, 128])
        P.dma(ident[:], ident_d.ap(), reads=[ident_d], writes=[ident])
        wrt = P.sb('wrt', [128, NKC, 16])
        P.dma(wrt[:], wr.ap(), reads=[wr], writes=[wrt])
        wrb = P.sb('wrb', [128, NKC, 16], BF16)
        P.op('vector', lambda e: e.tensor_copy(wrb[:], wrt[:]), reads=[wrt], writes=[wrb])
        rbt = P.sb('rbt', [128, 16])
        P.dma(rbt[:], AP(rb, 0, [[0, 128], [1, 16]]), reads=[rb], writes=[rbt])

        lin = Lin(P, 'lin', grp=128, nps=4)
        bigT = P.sb('bigT', [128, NKC, T3], BF16)
        RY = P.sb('RY', [128, NKC, T3])
        mrgT = AP(RY, 0, [[NKC * T3, 128], [1, NKC * T3 * 2]]).bitcast(BF16) if False else RY.bitcast(BF16)
        stg = [P.sb('stg%d' % i, [128, 512]) for i in range(3)]
        si = [0]

        def nxt():
            s = stg[si[0] % 3]
            si[0] += 1
            return s
        for c in range(NKC):
            for (t0, n) in TOKB3:
                s = nxt()
                P.dma(s[:, 0:n], AP(brT, c * 128 * T3 + t0, [[T3, 128], [1, n]]), reads=[brT], writes=[s])
                P.op('gpsimd', lambda e, s=s, c=c, t0=t0, n=n: e.tensor_copy(bigT[:, c, t0:t0 + n], s[:, 0:n]),
                     reads=[s], writes=['bigT:%d' % c])
        gst = [P.sb('gst%d' % i, [128, 4, 512]) for i in range(1)]
        acc = P.sb('macc', [128, 512])
        acc2 = P.sb('macc2', [128, 512])
        gi = [0]

        def evacA(o0, m, t0, n, tiles):
            c = o0 // 128
            g_ = gst[0]
            gi[0] += 1
            P.dma(g_[:, :, 0:n], AP(gT, o0 * T3 + t0, [[T3, 128], [2048 * T3, 4], [1, n]]), reads=[gT], writes=[g_])
            P.op('scalar', lambda e: e.activation(g_[:, :, 0:n], g_[:, :, 0:n], AF.Sigmoid), reads=[g_], writes=[g_])
            P.op('vector', lambda e: e.tensor_tensor(acc[:, 0:n], tiles[0][:, 0:n], g_[:, 0, 0:n], ALU.mult),
                 reads=[tiles[0], g_], writes=[acc])
            for j in range(1, 4):
                P.op('vector', lambda e, j=j: e.tensor_tensor(acc2[:, 0:n], tiles[j][:, 0:n], g_[:, j, 0:n], ALU.mult),
                     reads=[tiles[j], g_], writes=[acc2])
                if j < 3:
                    P.op('gpsimd', lambda e: e.tensor_tensor(acc[:, 0:n], acc[:, 0:n], acc2[:, 0:n], ALU.add),
                         reads=[acc, acc2], writes=[acc])
                else:
                    P.op('gpsimd', lambda e: e.tensor_tensor(mrgT[:, c, t0:t0 + n], acc[:, 0:n], acc2[:, 0:n], ALU.add),
                         reads=[acc, acc2], writes=['mrgT:%d' % c])
        lin.run(wbr, 0, D, 16, D, lambda kc, t0, n: bigT[:, kc, t0:t0 + n], TOKB3, evacA,
                lambda kc: ['bigT:%d' % kc], split=4)

        xo = [P.sb('xo%d' % i, [128, 512]) for i in range(2)]
        bi = [0]

        def evacB(o0, m, t0, n, ps_):
            c = o0 // 128
            s = nxt()
            P.dma(s[:, 0:n], AP(xT, c * T3 + t0, [[NKC * T3, 128], [1, n]]), reads=[xT], writes=[s])
            o_ = xo[bi[0] % 2]
            bi[0] += 1
            gm = mvt[:, 1, c:c + 1] if t0 < NLAT3 else mvt[:, 5, c:c + 1]
            P.op('vector', lambda e: e.scalar_tensor_tensor(o_[:, 0:n], ps_[:, 0:n], gm, s[:, 0:n], ALU.mult, ALU.add),
                 reads=[ps_, s, mvt], writes=[o_])
            P.dma(AP(x1T, c * T3 + t0, [[NKC * T3, 128], [1, n]]), o_[:, 0:n], reads=[o_], writes=['x1T:%d' % (t0 // 512)])
        lin.run(wo, 0, D, 16, D, lambda kc, t0, n: mrgT[:, kc, t0:t0 + n], TOKB3, evacB, lambda kc: ['mrgT:%d' % kc])

        class X1:
            pass
        norm_phase(P, x1T, bigT, gl, AP_slice(mvt, 2), gc, AP_slice(mvt, 6), ones_bf, eps_t, T3, NLAT3,
                   x_reads=['x1T:%d' % i for i in range(3)], out_key='h2T', SUB=64)

        psb = P.ps('psb', [128, 512])
        psr = psb[0:16, :]
        pssm = P.ps('pssm', [128, 128])
        pst_ = pssm[:, 0:16]
        lg = P.sb('lg', [16, 1152])
        P.op('vector', lambda e: e.memset(lg[:], 0.0), writes=[lg])
        gateT = P.sb('gateT', [16, T3], BF16)
        for (t0, n) in TOKB3:
            for kc in range(NKC):
                P.op('tensor', lambda e, kc=kc, t0=t0, n=n: e.matmul(psr[:, 0:n], wrb[:, kc, :], bigT[:, kc, t0:t0 + n],
                                                                    start=(kc == 0), stop=(kc == NKC - 1)),
                     reads=[wrb, 'h2T:%d' % kc], writes=[psr])
            P.op('scalar', lambda e, t0=t0, n=n: e.activation(lg[:, t0:t0 + n], psr[:, 0:n], AF.Sigmoid),
                 reads=[psr], writes=[lg])
        sc = P.sb('r_sc', [128, 16]); sel = P.sb('r_sel', [128, 16]); t4 = P.sb('r_t4', [128, 4]); t4b = P.sb('r_t4b', [128, 4])
        e16 = P.sb('r_e16', [128, 16]); s2 = P.sb('r_s2', [128, 16]); gs = P.sb('r_gs', [128, 4]); t1 = P.sb('r_t1', [128, 1])
        gm_ = P.sb('r_gm', [128, 4]); oh = P.sb('r_oh', [128, 16]); gt = P.sb('r_gt', [128, 16])
        psg = pssm[0:16, :]

        def V(fn, reads, writes):
            P.op('vector', fn, reads=reads, writes=writes)

        def bc4(t):
            return AP(t, 0, [[4, 128], [1, 4], [0, 4]])

        def v44(t):
            return AP(t, 0, [[16, 128], [4, 4], [1, 4]])
        for tt in range((T3 + 127) // 128):
            t0 = tt * 128
            nt = min(128, T3 - t0)
            P.op('tensor', lambda e, t0=t0: e.transpose(pst_[:], lg[:, t0:t0 + 128], ident[0:16, 0:16]),
                 reads=[lg, ident], writes=[pst_])
            V(lambda e: e.tensor_copy(sc[:], pst_[:]), [pst_], [sc])
            V(lambda e: e.tensor_tensor(sel[:], sc[:], rbt[:], ALU.add), [sc, rbt], [sel])
            V(lambda e: e.tensor_reduce(t4[:], v44(sel), AX.X, ALU.max), [sel], [t4])
            V(lambda e: e.tensor_tensor(v44(e16), v44(sel), bc4(t4), ALU.is_equal), [sel, t4], [e16])
            V(lambda e: e.scalar_tensor_tensor(s2[:], e16[:], -BIG, sel[:], ALU.mult, ALU.add), [e16, sel], [s2])
            V(lambda e: e.tensor_reduce(t4b[:], v44(s2), AX.X, ALU.max), [s2], [t4b])
            V(lambda e: e.tensor_tensor(gs[:], t4[:], t4b[:], ALU.add), [t4, t4b], [gs])
            V(lambda e: e.tensor_reduce(t1[:], gs[:], AX.X, ALU.max), [gs], [t1])
            V(lambda e: e.tensor_scalar(gm_[:], gs[:], t1[:, 0:1], None, ALU.is_equal), [gs, t1], [gm_])
            V(lambda e: e.tensor_scalar(gm_[:], gm_[:], -1.0, BIG, ALU.add, ALU.mult), [gm_], [gm_])
            V(lambda e: e.tensor_tensor(v44(s2), v44(sel), bc4(gm_), ALU.add), [sel, gm_], [s2])
            V(lambda e: e.tensor_reduce(t1[:], s2[:], AX.X, ALU.max), [s2], [t1])
            V(lambda e: e.tensor_scalar(oh[:], s2[:], t1[:, 0:1], None, ALU.is_equal), [s2, t1], [oh])
            V(lambda e: e.scalar_tensor_tensor(s2[:], oh[:], -4 * BIG, s2[:], ALU.mult, ALU.add), [oh, s2], [s2])
            V(lambda e: e.tensor_reduce(t1[:], s2[:], AX.X, ALU.max), [s2], [t1])
            V(lambda e: e.scalar_tensor_tensor(oh[:], s2[:], t1[:, 0:1], oh[:], ALU.is_equal, ALU.add), [s2, t1, oh], [oh])
            V(lambda e: e.tensor_tensor(gt[:], sc[:], oh[:], ALU.mult), [sc, oh], [gt])
            V(lambda e: e.tensor_reduce(t1[:], gt[:], AX.X, ALU.add), [gt], [t1])
            V(lambda e: e.reciprocal(t1[:], t1[:]), [t1], [t1])
            V(lambda e: e.tensor_scalar(gt[:], gt[:], t1[:, 0:1], None, ALU.mult), [gt, t1], [gt])
            P.op('tensor', lambda e: e.transpose(psg[:], gt[:], ident[:]), reads=[gt, ident], writes=[psg])
            P.op('scalar', lambda e, t0=t0, nt=nt: e.copy(gateT[:, t0:t0 + nt], psg[:, 0:nt]), reads=[psg], writes=[gateT])
        selm = P.sb('selm', [16, 16, 128], BF16)
        V(lambda e: e.tensor_copy(selm[:], AP(ident, 0, [[128, 16], [1, 16], [0, 128]])), [ident], [selm])

        yacc = RY
        gact = P.sb('gact', [128, 4, T3], BF16)
        actT = P.sb('actT', [128, 4, T3], BF16)
        gb = P.sb('gb', [128, T3])
        tmpm = P.sb('tmpm', [128, 512])
        for h in range(1):
            tokb = TOKB3
            hb = 0
            for ex in range(16):
                for (t0, n) in tokb:
                    P.op('tensor', lambda e, ex=ex, t0=t0, n=n: e.matmul(psb[:, 0:n], selm[:, ex, :], gateT[:, t0:t0 + n],
                                                                        start=True, stop=True),
                         reads=[selm, gateT], writes=[psb])
                    P.op('scalar', lambda e, t0=t0, n=n: e.copy(gb[:, t0 - hb:t0 - hb + n], psb[:, 0:n]),
                         reads=[psb], writes=[gb])

                def evG(o0, m, t0, n, ps_):
                    fc = o0 // 128
                    P.op('scalar', lambda e: e.activation(gact[:, fc, t0 - hb:t0 - hb + n], ps_[:, 0:n], AF.Silu),
                         reads=[ps_], writes=[gact])
                lin.run(wg, ex * D * 512, 512, 16, 512, lambda kc, t0, n: bigT[:, kc, t0:t0 + n], tokb, evG,
                        lambda kc: ['h2T:%d' % kc])

                def evU(o0, m, t0, n, ps_):
                    fc = o0 // 128
                    P.op('vector', lambda e: e.tensor_tensor(tmpm[:, 0:n], ps_[:, 0:n], gact[:, fc, t0 - hb:t0 - hb + n], ALU.mult),
                         reads=[ps_, gact], writes=[tmpm])
                    P.op('gpsimd', lambda e: e.tensor_tensor(actT[:, fc, t0 - hb:t0 - hb + n], tmpm[:, 0:n],
                                                             gb[:, t0 - hb:t0 - hb + n], ALU.mult),
                         reads=[tmpm, gb], writes=[actT])
                lin.run(wu, ex * D * 512, 512, 16, 512, lambda kc, t0, n: bigT[:, kc, t0:t0 + n], tokb, evU,
                        lambda kc: ['h2T:%d' % kc])

                def evD(o0, m, t0, n, ps_, ex=ex):
                    c = o0 // 128
                    if ex == 0:
                        P.op('scalar', lambda e: e.copy(yacc[:, c, t0 - hb:t0 - hb + n], ps_[:, 0:n]),
                             reads=[ps_], writes=['yacc:%d' % c])
                    else:
                        P.op('vector', lambda e: e.tensor_tensor(yacc[:, c, t0 - hb:t0 - hb + n], ps_[:, 0:n],
                                                                 yacc[:, c, t0 - hb:t0 - hb + n], ALU.add),
                             reads=[ps_, 'yacc:%d' % c], writes=['yacc:%d' % c])
                lin.run(wd, ex * 512 * D, D, 4, D, lambda kc, t0, n: actT[:, kc, t0 - hb:t0 - hb + n], tokb, evD,
                        lambda kc: [actT])
            for c in range(NKC):
                for (t0, n) in tokb:
                    s = nxt()
                    P.dma(s[:, 0:n], AP(x1T, c * T3 + t0, [[NKC * T3, 128], [1, n]]), reads=['x1T:%d' % (t0 // 512)], writes=[s])
                    o_ = xo[bi[0] % 2]
                    bi[0] += 1
                    gm = mvt[:, 4, c:c + 1] if t0 < NLAT3 else mvt[:, 8, c:c + 1]
                    P.op('vector', lambda e, s=s, o_=o_, gm=gm, c=c, t0=t0, n=n: e.scalar_tensor_tensor(
                        o_[:, 0:n], yacc[:, c, t0 - hb:t0 - hb + n], gm, s[:, 0:n], ALU.mult, ALU.add),
                        reads=['yacc:%d' % c, s, mvt], writes=[o_])
                    P.dma(AP(x2T, c * T3 + t0, [[NKC * T3, 128], [1, n]]), o_[:, 0:n], reads=[o_], writes=[])
        P.finish()
        nc = P.emit()
    return nc


DEBUG = False
TA = 4352
NKT = 34


def build_l2a(with_ctx=True):
    P = Prog()
    with ExitStack() as st:
        P._stack = st
        gq = P.dram('gq', [256, TA], F32, 'ExternalInput')
        gk = P.dram('gk', [64, TA], F32, 'ExternalInput')
        gv = P.dram('gv', [TA, 64], F32, 'ExternalInput')
        cq = P.dram('cq', [512, TA], F32, 'ExternalInput')
        ckv = P.dram('ckv', [256, TA], F32, 'ExternalInput')
        kr = P.dram('kr', [64, TA], F32, 'ExternalInput')
        wuq = P.dram('wuq', [512, 384], F32, 'ExternalInput')
        wukv = P.dram('wukv', [256, 512], F32, 'ExternalInput')
        nw = P.dram('nw', [128, 8], F32, 'ExternalInput')
        cosd = P.dram('cosd', [64, TA], F32, 'ExternalInput')
        sind = P.dram('sind', [64, TA], F32, 'ExternalInput')
        rmd = P.dram('rmd', [64, 64], F32, 'ExternalInput')
        og = P.dram('og', [TA, 256], F32, 'ExternalOutput')
        om = P.dram('om', [TA, 256], F32, 'ExternalOutput')

        pp = [P.ps('pp%d' % i, [128, 512]) for i in range(8)]
        ones = P.sb('ones', [128, 128])
        P.op('vector', lambda e: e.memset(ones[:], 1.0), writes=[ones])
        nwt = P.sb('nwt', [128, 8])
        P.dma(nwt[:], nw.ap(), reads=[nw], writes=[nwt])
        rm = P.sb('rm', [64, 64])
        P.dma(rm[:], rmd.ap(), reads=[rmd], writes=[rm])
        eps64 = P.sb('eps64', [128, 1])
        P.op('vector', lambda e: e.memset(eps64[:], 1e-6), writes=[eps64])
        wq_s = P.sb('wq_s', [128, 4, 384]); wq_b = P.sb('wq_b', [128, 4, 384], BF16)
        wqr_b = P.sb('wqr_b', [128, 4, 128], BF16)
        wkv_s = P.sb('wkv_s', [128, 2, 512]); wkv_b = P.sb('wkv_b', [128, 2, 512], BF16)
        P.dma(wq_s[:], AP(wuq, 0, [[384, 128], [128 * 384, 4], [1, 384]]), reads=[wuq], writes=[wq_s])
        P.dma(wkv_s[:], AP(wukv, 0, [[512, 128], [128 * 512, 2], [1, 512]]), reads=[wukv], writes=[wkv_s])
        P.op('vector', lambda e: e.tensor_copy(wq_b[:], wq_s[:]), reads=[wq_s], writes=[wq_b])
        P.op('vector', lambda e: e.tensor_copy(wkv_b[:], wkv_s[:]), reads=[wkv_s], writes=[wkv_b])
        for h in range(2):
            P.op('vector', lambda e, h=h: e.tensor_copy(wqr_b[:, :, h * 64:h * 64 + 32], wq_s[:, :, h * 192 + 160:h * 192 + 192]),
                 reads=[wq_s], writes=[wqr_b])
            P.op('vector', lambda e, h=h: e.tensor_copy(wqr_b[:, :, h * 64 + 32:h * 64 + 64], wq_s[:, :, h * 192 + 128:h * 192 + 160]),
                 reads=[wq_s], writes=[wqr_b])

        GQ = P.sb('GQ', [64, 4, TA], BF16)
        GK = P.sb('GK', [64, TA], BF16)
        GV = P.sb('GV', [128, NKT, 65], BF16)
        MQN = P.sb('MQN', [128, 2, TA], BF16)
        MQR = P.sb('MQR', [64, 2, TA], BF16)
        MKN = P.sb('MKN', [128, 2, TA], BF16)
        MKR = P.sb('MKR', [64, TA], BF16)
        MV = P.sb('MV', [128, NKT, 2, 129], BF16)
        P.op('vector', lambda e: e.memset(GV[:], 1.0), writes=[GV])
        P.op('vector', lambda e: e.memset(MV[:], 1.0), writes=[MV])
        gvs = P.sb('gvs', [128, NKT, 64])
        P.dma(gvs[:], AP(gv, 0, [[64, 128], [128 * 64, NKT], [1, 64]]), reads=[gv], writes=[gvs])
        P.op('vector', lambda e: e.tensor_copy(GV[:, :, 0:64], gvs[:]), reads=[gvs], writes=[GV])

        xs = [P.sb('xa%d' % i, [128, 4, 512]) for i in range(2)]
        sq = P.sb('sqa', [128, 4, 512])
        rs = P.sb('rsa', [128, 512])
        xn = P.sb('xna', [128, 512])
        xnb = P.sb('xnb', [128, 4, 512], BF16)
        xnk = P.sb('xnk', [128, 2, 512], BF16)
        cs = P.sb('csa', [64, 512]); sn = P.sb('sna', [64, 512])
        t1 = P.sb('t1a', [128, 512]); t2 = P.sb('t2a', [128, 512])
        ppi = [0]
        fence = P.sb('fence', [128, 8], BF16)

        def nps():
            p_ = pp[ppi[0] % 8]
            ppi[0] += 1
            return p_

        def rstd_of(x_, rows, nch, n, dim):
            P.op('scalar', lambda e: e.activation(sq[0:rows, 0:nch, 0:n], x_[0:rows, 0:nch, 0:n], AF.Square), reads=[x_], writes=[sq])
            ps_ = nps()
            for c in range(nch):
                P.op('tensor', lambda e, c=c: e.matmul(ps_[0:rows, 0:n], ones[0:rows, 0:rows], sq[0:rows, c, 0:n],
                                                       start=(c == 0), stop=(c == nch - 1)), reads=[ones, sq], writes=[ps_])
            P.op('scalar', lambda e: e.activation(rs[0:rows, 0:n], ps_[0:rows, 0:n], AF.Sqrt, bias=eps64[0:rows, :], scale=1.0 / dim),
                 reads=[ps_, eps64], writes=[rs])
            P.op('vector', lambda e: e.reciprocal(rs[0:rows, 0:n], rs[0:rows, 0:n]), reads=[rs], writes=[rs])

        def rope_to(dst, src, n):
            ps_ = nps()
            P.op('tensor', lambda e: e.matmul(ps_[0:64, 0:n], rm[:], src, start=True, stop=True), reads=[rm, src], writes=[ps_])
            P.op('vector', lambda e: e.tensor_tensor(t1[0:64, 0:n], ps_[0:64, 0:n], sn[:, 0:n], ALU.mult), reads=[ps_, sn], writes=[t1])
            P.op('gpsimd', lambda e: e.tensor_tensor(t2[0:64, 0:n], src, cs[:, 0:n], ALU.mult), reads=[src, cs], writes=[t2])
            P.op('vector', lambda e: e.tensor_tensor(dst, t1[0:64, 0:n], t2[0:64, 0:n], ALU.add), reads=[t1, t2], writes=[dst])

        for blk in range(9):
            t0 = blk * 512
            n = min(512, TA - t0)
            P.dma(cs[:, 0:n], AP(cosd, t0, [[TA, 64], [1, n]]), reads=[cosd], writes=[cs])
            P.dma(sn[:, 0:n], AP(sind, t0, [[TA, 64], [1, n]]), reads=[sind], writes=[sn])
            x_ = xs[0]
            P.dma(x_[0:64, :, 0:n], AP(gq, t0, [[TA, 64], [64 * TA, 4], [1, n]]), reads=[gq], writes=[x_])
            for h in range(4):
                P.op('scalar', lambda e, h=h: e.activation(sq[0:64, 0, 0:n], x_[0:64, h, 0:n], AF.Square), reads=[x_], writes=[sq])
                ps_ = nps()
                P.op('tensor', lambda e, ps_=ps_: e.matmul(ps_[0:64, 0:n], ones[0:64, 0:64], sq[0:64, 0, 0:n], start=True, stop=True),
                     reads=[ones, sq], writes=[ps_])
                P.op('scalar', lambda e, ps_=ps_: e.activation(rs[0:64, 0:n], ps_[0:64, 0:n], AF.Sqrt, bias=eps64[0:64, :], scale=1.0 / 64),
                     reads=[ps_, eps64], writes=[rs])
                P.op('vector', lambda e: e.reciprocal(rs[0:64, 0:n], rs[0:64, 0:n]), reads=[rs], writes=[rs])
                P.op('vector', lambda e, h=h: e.scalar_tensor_tensor(xn[0:64, 0:n], x_[0:64, h, 0:n], nwt[0:64, 6:7], rs[0:64, 0:n],
                                                                     ALU.mult, ALU.mult), reads=[x_, nwt, rs], writes=[xn])
                rope_to(GQ[:, h, t0:t0 + n], xn[0:64, 0:n], n)
            x2 = xs[1]
            P.dma(x2[0:64, 0, 0:n], AP(gk, t0, [[TA, 64], [1, n]]), reads=[gk], writes=[x2])
            rstd_of(x2, 64, 1, n, 64)
            P.op('vector', lambda e: e.scalar_tensor_tensor(xn[0:64, 0:n], x2[0:64, 0, 0:n], nwt[0:64, 7:8], rs[0:64, 0:n],
                                                            ALU.mult, ALU.mult), reads=[x2, nwt, rs], writes=[xn])
            rope_to(GK[:, t0:t0 + n], xn[0:64, 0:n], n)
            P.dma(x2[0:64, 1, 0:n], AP(kr, t0, [[TA, 64], [1, n]]), reads=[kr], writes=[x2])
            rope_to(MKR[:, t0:t0 + n], x2[0:64, 1, 0:n], n)
            P.dma(x_[:, :, 0:n], AP(cq, t0, [[TA, 128], [128 * TA, 4], [1, n]]), reads=[cq], writes=[x_])
            rstd_of(x_, 128, 4, n, 512)
            for c in range(4):
                P.op('vector', lambda e, c=c: e.scalar_tensor_tensor(xnb[:, c, 0:n], x_[:, c, 0:n], nwt[:, c:c + 1], rs[:, 0:n],
                                                                     ALU.mult, ALU.mult), reads=[x_, nwt, rs], writes=[xnb])
            P.op('vector', lambda e: e.drain(), reads=[xnb], writes=[xnb])
            if DEBUG and blk == 0:
                d1 = P.dram('d_xnb', [128, 4 * 512], BF16, 'ExternalOutput'); d2 = P.dram('d_rs', [128, 512], F32, 'ExternalOutput'); d3 = P.dram('d_x', [128, 4*512], F32, 'ExternalOutput')
                P.dma(d1.ap(), xnb[:], reads=[xnb], writes=[]); P.dma(d2.ap(), rs[:], reads=[rs], writes=[]); P.dma(d3.ap(), x_[:], reads=[x_], writes=[])
            for h in (0, 1):
                ps_ = nps()
                for c in range(4):
                    P.op('tensor', lambda e, ps_=ps_, c=c, h=h: e.matmul(ps_[:, 0:n], wq_b[:, c, h * 192:h * 192 + 128], xnb[:, c, 0:n],
                                                                         start=(c == 0), stop=(c == 3)), reads=[wq_b, xnb], writes=[ps_])
                P.op('vector', lambda e, t0=t0, ps_=ps_, h=h: e.tensor_copy(MQN[:, h, t0:t0 + n], ps_[:, 0:n]), reads=[ps_], writes=[MQN])
                pa = nps(); pb = nps()
                for c in range(4):
                    P.op('tensor', lambda e, pa=pa, c=c, h=h: e.matmul(pa[0:64, 0:n], wq_b[:, c, h * 192 + 128:h * 192 + 192], xnb[:, c, 0:n],
                                                                       start=(c == 0), stop=(c == 3)), reads=[wq_b, xnb], writes=[pa])
                for c in range(4):
                    P.op('tensor', lambda e, pb=pb, c=c, h=h: e.matmul(pb[0:64, 0:n], wqr_b[:, c, h * 64:h * 64 + 64], xnb[:, c, 0:n],
                                                                       start=(c == 0), stop=(c == 3)), reads=[wqr_b, xnb], writes=[pb])
                P.op('vector', lambda e, pa=pa: e.tensor_tensor(t1[0:64, 0:n], pa[0:64, 0:n], cs[:, 0:n], ALU.mult), reads=[pa, cs], writes=[t1])
                P.op('vector', lambda e, pb=pb: e.tensor_tensor(t2[0:64, 0:n], pb[0:64, 0:n], sn[:, 0:n], ALU.mult), reads=[pb, sn], writes=[t2])
                P.op('gpsimd', lambda e, t0=t0, h=h: e.tensor_tensor(MQR[:, h, t0:t0 + n], t1[0:64, 0:n], t2[0:64, 0:n], ALU.add), reads=[t1, t2], writes=[MQR])
            P.dma(x2[:, 2:4, 0:n], AP(ckv, t0, [[TA, 128], [128 * TA, 2], [1, n]]), reads=[ckv], writes=[x2])
            P.op('scalar', lambda e: e.activation(sq[:, 0:2, 0:n], x2[:, 2:4, 0:n], AF.Square), reads=[x2], writes=[sq])
            ps_ = nps()
            for c in range(2):
                P.op('tensor', lambda e, c=c, ps_=ps_: e.matmul(ps_[:, 0:n], ones[:], sq[:, c, 0:n], start=(c == 0), stop=(c == 1)),
                     reads=[ones, sq], writes=[ps_])
            P.op('scalar', lambda e, ps_=ps_: e.activation(rs[:, 0:n], ps_[:, 0:n], AF.Sqrt, bias=eps64[:], scale=1.0 / 256),
                 reads=[ps_, eps64], writes=[rs])
            P.op('vector', lambda e: e.reciprocal(rs[:, 0:n], rs[:, 0:n]), reads=[rs], writes=[rs])
            for c in range(2):
                P.op('vector', lambda e, c=c: e.scalar_tensor_tensor(xnk[:, c, 0:n], x2[:, 2 + c, 0:n], nwt[:, 4 + c:5 + c], rs[:, 0:n],
                                                                     ALU.mult, ALU.mult), reads=[x2, nwt, rs], writes=[xnk])
            P.op('vector', lambda e: e.drain(), reads=[xnk], writes=[xnk])
            for h in range(2):
                ps_ = nps()
                for c in range(2):
                    P.op('tensor', lambda e, ps_=ps_, c=c, h=h: e.matmul(ps_[:, 0:n], wkv_b[:, c, h * 256:h * 256 + 128], xnk[:, c, 0:n],
                                                                         start=(c == 0), stop=(c == 1)), reads=[wkv_b, xnk], writes=[ps_])
                P.op('scalar', lambda e, t0=t0, ps_=ps_, h=h: e.copy(MKN[:, h, t0:t0 + n], ps_[:, 0:n]), reads=[ps_], writes=[MKN])
                for j in range(n // 128):
                    ps2 = nps()
                    kt = (t0 + j * 128) // 128
                    for c in range(2):
                        P.op('tensor', lambda e, ps2=ps2, c=c, h=h, j=j: e.matmul(ps2[:, 0:128], xnk[:, c, j * 128:(j + 1) * 128],
                                                                                 wkv_b[:, c, h * 256 + 128:h * 256 + 256],
                                                                                 start=(c == 0), stop=(c == 1)), reads=[wkv_b, xnk], writes=[ps2])
                    P.op('vector', lambda e, ps2=ps2, h=h, kt=kt: e.tensor_copy(MV[:, kt, h, 0:128], ps2[:, 0:128]), reads=[ps2], writes=[MV])

        pT = [P.sb('pT%d' % i, [128, 512], BF16) for i in range(4)]
        osb = [P.sb('osb%d' % i, [128, 128]) for i in range(2)]
        rec = P.sb('rec', [128, 1])
        cnt = [0]
        DEPTH = 2

        def attn(qparts, kparts, vfn, dv, scale, t0, n, kts, out, ocol, ow):
            sps = [pp[0], pp[1], pp[6], pp[7]]
            ops_ = pp[2:6]
            nq = n // 128
            bufs = []
            nk = len(kts)
            for step in range(nk + DEPTH):
                if step < nk:
                    kt = kts[step]
                    s_ = sps[cnt[0] % 4]
                    p_ = pT[cnt[0] % 4]
                    cnt[0] += 1
                    bufs.append(p_)
                    for i, (qf, kf) in enumerate(zip(qparts, kparts)):
                        P.op('tensor', lambda e, s_=s_, qf=qf, kf=kf, i=i, kt=kt: e.matmul(
                            s_[:, 0:n], kf(kt), qf(t0, n), start=(i == 0), stop=(i == len(qparts) - 1)),
                            reads=[kf(kt), qf(t0, n)], writes=[s_])
                    P.op('scalar', lambda e, s_=s_, p_=p_: e.activation(p_[:, 0:n], s_[:, 0:n], AF.Exp, scale=scale), reads=[s_], writes=[p_])
                if step >= DEPTH:
                    ki = step - DEPTH
                    kt = kts[ki]
                    p_ = bufs[ki]
                    for qs in range(nq):
                        P.op('tensor', lambda e, p_=p_, qs=qs, kt=kt, ki=ki: e.matmul(
                            ops_[qs][:, 0:dv + 1], p_[:, qs * 128:(qs + 1) * 128], vfn(kt), start=(ki == 0), stop=(ki == nk - 1)),
                            reads=[p_, vfn(kt)], writes=[ops_[qs]])
            for qs in range(nq):
                o_ = osb[qs % 2]
                P.op('vector', lambda e, qs=qs: e.reciprocal(rec[:], ops_[qs][:, dv:dv + 1]), reads=[ops_[qs]], writes=[rec])
                P.op('vector', lambda e, qs=qs, o_=o_: e.tensor_scalar(o_[:, 0:dv], ops_[qs][:, 0:dv], rec[:, 0:1], None, ALU.mult),
                     reads=[ops_[qs], rec], writes=[o_])
                P.dma(AP(out, (t0 + qs * 128) * ow + ocol, [[ow, 128], [1, dv]]), o_[:, 0:dv], reads=[o_], writes=[])

        all_k = list(range(NKT))
        ctx_k = [32, 33]
        qblocks = [(i * 512, 512, all_k) for i in range(8)]
        if with_ctx:
            qblocks.append((4096, 256, ctx_k))
        for (t0, n, kts) in qblocks:
            for h in range(4):
                attn([lambda a, b, h=h: GQ[:, h, a:a + b]], [lambda kt: GK[:, kt * 128:(kt + 1) * 128]],
                     lambda kt: GV[:, kt, :], 64, 0.125, t0, n, kts, og, h * 64, 256)
            for h in range(2):
                attn([lambda a, b, h=h: MQN[:, h, a:a + b], lambda a, b, h=h: MQR[:, h, a:a + b]],
                     [lambda kt, h=h: MKN[:, h, kt * 128:(kt + 1) * 128], lambda kt: MKR[:, kt * 128:(kt + 1) * 128]],
                     lambda kt, h=h: MV[:, kt, h, :], 128, 192 ** -0.5, t0, n, kts, om, h * 128, 256)
        if DEBUG:
            for nm, t_, shp in [('d_GQ', GQ, [64, 4 * TA]), ('d_GK', GK, [64, TA]), ('d_MQR', MQR, [64, 2 * TA]), ('d_MKR', MKR, [64, TA]), ('d_MQN', MQN, [128, 2 * TA]), ('d_MKN', MKN, [128, 2*TA])]:
                dd = P.dram(nm, shp, BF16, 'ExternalOutput')
                P.dma(dd.ap(), t_[:], reads=[t_], writes=[])
        P.finish()
        nc = P.emit()
    return nc


HC = 64
TWO_PI = 2.0 * math.pi


def build_l2h():
    P = Prog()
    with ExitStack() as st:
        P._stack = st
        seqs = []
        for nm, L in (('L', 4096), ('C', 256)):
            nb = L // 128
            seqs.append(dict(
                nm=nm, L=L, nb=nb, NQ=128 * (2 * nb - 1), HPLEN=256 * nb, pad=L // 2 - 1,
                U=P.dram('U' + nm, [3, 3, 128, HC * 4 * nb], F32, 'ExternalInput'),
                feats=P.dram('feats' + nm, [33, L], F32, 'ExternalInput'),
                dec=P.dram('dec' + nm, [2, HC, L], F32, 'ExternalInput'),
                hp=P.dram('hp' + nm, [2, HC, 256 * nb], BF16, 'Internal'),
                out=P.dram('y' + nm, [128, HC * 4 * nb], F32, 'ExternalOutput')))
        cw = P.dram('cw', [3, 3, HC], F32, 'ExternalInput')
        w1 = P.dram('w1', [33, 64], F32, 'ExternalInput')
        w2 = P.dram('w2', [64, 64], F32, 'ExternalInput')
        w3 = P.dram('w3', [64, 2, HC], F32, 'ExternalInput')
        pv = P.dram('pv', [64, 4], F32, 'ExternalInput')
        hb = P.dram('hb', [2, HC], F32, 'ExternalInput')
        jm = P.dram('jm', [128, 128], F32, 'ExternalInput')

        F = 8192
        A1 = P.sb('A1', [128, F]); A2 = P.sb('A2', [128, F]); vN = P.sb('vN', [128, F]); xN = P.sb('xN', [128, F])
        zR = P.sb('zR', [128, F], BF16)
        toep = [P.sb('toep%d' % i, [128, 8064], BF16) for i in range(2)]
        wbc = P.sb('wbc', [128, 9 * HC]); hbc = P.sb('hbc', [128, 2 * HC])
        P.dma(wbc[:], AP(cw, 0, [[0, 128], [1, 9 * HC]]), reads=[cw], writes=[wbc])
        P.dma(hbc[:], AP(hb, 0, [[0, 128], [1, 2 * HC]]), reads=[hb], writes=[hbc])
        w1t = P.sb('w1t', [33, 64]); w2t = P.sb('w2t', [64, 64]); w3t = P.sb('w3t', [64, 2 * HC]); pvt = P.sb('pvt', [64, 4])
        jt = P.sb('jt', [128, 128])
        P.dma(w1t[:], w1.ap(), reads=[w1], writes=[w1t]); P.dma(w2t[:], w2.ap(), reads=[w2], writes=[w2t])
        P.dma(w3t[:], AP(w3, 0, [[2 * HC, 64], [1, 2 * HC]]), reads=[w3], writes=[w3t]); P.dma(pvt[:], pv.ap(), reads=[pv], writes=[pvt])
        P.dma(jt[:], jm.ap(), reads=[jm], writes=[jt])
        zero_bf = P.sb('zero_bf', [64, 2304], BF16)
        P.op('vector', lambda e: e.memset(zero_bf[:], 0.0), writes=[zero_bf])
        pp = [P.ps('pp%d' % i, [128, 512]) for i in range(6)]
        ppi = [0]

        def nps():
            p_ = pp[ppi[0] % 6]
            ppi[0] += 1
            return p_
        ft = [P.sb('ft%d' % i, [33, 256]) for i in range(2)]
        dct = [P.sb('dct%d' % i, [64, 2, 256]) for i in range(2)]
        arg = P.sb('arg', [64, 256]); ki = P.sb('ki', [64, 256], I32); kf = P.sb('kf', [64, 256])
        rsum = P.sb('rsum', [64, 2])

        def sin_to(dst, ps_, bcol, fcol):
            P.op('vector', lambda e: e.tensor_scalar(arg[:], ps_, pvt[:, bcol:bcol + 1], pvt[:, fcol:fcol + 1], ALU.add, ALU.mult),
                 reads=[ps_, pvt], writes=[arg])
            P.op('vector', lambda e: e.tensor_scalar(ki[:], arg[:], 1.0 / TWO_PI, None, ALU.mult), reads=[arg], writes=[ki])
            P.op('vector', lambda e: e.tensor_copy(kf[:], ki[:]), reads=[ki], writes=[kf])
            P.op('vector', lambda e: e.scalar_tensor_tensor(arg[:], kf[:], -TWO_PI, arg[:], ALU.mult, ALU.add), reads=[kf, arg], writes=[arg])
            P.op('vector', lambda e: e.tensor_scalar(kf[:], arg[:], math.pi, -TWO_PI, ALU.is_gt, ALU.mult), reads=[arg], writes=[kf])
            P.op('vector', lambda e: e.tensor_tensor(arg[:], arg[:], kf[:], ALU.add), reads=[arg, kf], writes=[arg])
            P.op('vector', lambda e: e.tensor_scalar(kf[:], arg[:], -math.pi, TWO_PI, ALU.is_lt, ALU.mult), reads=[arg], writes=[kf])
            P.op('vector', lambda e: e.tensor_tensor(arg[:], arg[:], kf[:], ALU.add), reads=[arg, kf], writes=[arg])
            P.op('vector', lambda e: e.tensor_scalar(arg[:], arg[:], 3.14159, -3.14159, ALU.min, ALU.max), reads=[arg], writes=[arg])
            P.op('scalar', lambda e: e.activation(dst, arg[:], AF.Sin), reads=[arg], writes=[dst])

        def filter_gen(S):
            L, hp, pad, HPLEN = S['L'], S['hp'], S['pad'], S['HPLEN']
            hid1 = A1[0:64, 0:L]; hid2 = A1[0:64, 4096:4096 + L]
            fr = [A2[0:64, 0:L], A2[0:64, 4096:4096 + L]]
            for bi, t0 in enumerate(range(0, L, 256)):
                f_ = ft[bi % 2]; d_ = dct[bi % 2]
                P.dma(f_[:], AP(S['feats'], t0, [[L, 33], [1, 256]]), reads=[S['feats']], writes=[f_])
                P.dma(d_[:], AP(S['dec'], t0, [[L, 64], [HC * L, 2], [1, 256]]), reads=[S['dec']], writes=[d_])
                ps_ = nps()
                P.op('tensor', lambda e, ps_=ps_, f_=f_: e.matmul(ps_[0:64, 0:256], w1t[:], f_[:], start=True, stop=True),
                     reads=[w1t, f_], writes=[ps_])
                sin_to(hid1[:, t0:t0 + 256], ps_[0:64, 0:256], 0, 2)
                ps_ = nps()
                P.op('tensor', lambda e, ps_=ps_, t0=t0: e.matmul(ps_[0:64, 0:256], w2t[:], hid1[:, t0:t0 + 256], start=True, stop=True),
                     reads=[w2t, A1], writes=[ps_])
                sin_to(hid2[:, t0:t0 + 256], ps_[0:64, 0:256], 1, 3)
                for o in range(2):
                    ps_ = nps()
                    P.op('tensor', lambda e, ps_=ps_, t0=t0, o=o: e.matmul(ps_[0:64, 0:256], w3t[:, o * HC:(o + 1) * HC], hid2[:, t0:t0 + 256],
                                                                          start=True, stop=True), reads=[w3t, A1], writes=[ps_])
                    P.op('vector', lambda e, ps_=ps_, t0=t0, o=o, d_=d_: e.tensor_tensor(fr[o][:, t0:t0 + 256], ps_[0:64, 0:256], d_[:, o, :], ALU.mult),
                         reads=[ps_, d_], writes=[A2])
            for o in range(2):
                P.op('vector', lambda e, o=o: e.tensor_reduce(rsum[:, o:o + 1], fr[o], AX.X, ALU.add, apply_absolute_value=True),
                     reads=[A2], writes=[rsum])
            P.op('vector', lambda e: e.reciprocal(rsum[:], rsum[:]), reads=[rsum], writes=[rsum])
            for o in range(2):
                fb = zR[0:64, o * 4096:o * 4096 + L]
                P.op('vector', lambda e, o=o, fb=fb: e.tensor_scalar(fb, fr[o], rsum[:, o:o + 1], None, ALU.mult), reads=[A2, rsum], writes=[zR])
                P.dma(AP(hp, o * HC * HPLEN + pad, [[HPLEN, 64], [1, L]]), fb, reads=[zR], writes=[hp])
                n1 = pad
                n2 = HPLEN - pad - L
                P.dma(AP(hp, o * HC * HPLEN, [[HPLEN, 64], [1, n1]]), zero_bf[:, 0:n1], reads=[zero_bf], writes=[hp])
                P.dma(AP(hp, o * HC * HPLEN + pad + L, [[HPLEN, 64], [1, n2]]), zero_bf[:, 0:n2], reads=[zero_bf], writes=[hp])

        def conv3(S, g, dest):
            n = HC * 4 * S['nb']
            for k in range(3):
                P.dma(A1[:, 0:n], AP(S['U'], (g * 3 + k) * 128 * n, [[n, 128], [1, n]]), reads=[S['U']], writes=[A1])
                wv = AP(wbc, (k * 3 + g) * HC, [[9 * HC, 128], [1, HC], [0, 4 * S['nb']]])
                a1v = AP(A1, 0, [[F, 128], [4 * S['nb'], HC], [1, 4 * S['nb']]])
                dv = AP(dest, 0, [[F, 128], [4 * S['nb'], HC], [1, 4 * S['nb']]])
                if k == 0:
                    P.op('vector', lambda e, a1v=a1v, wv=wv, dv=dv: e.tensor_tensor(dv, a1v, wv, ALU.mult), reads=[A1, wbc], writes=[dest])
                else:
                    P.op('vector', lambda e, a1v=a1v, wv=wv: e.tensor_tensor(a1v, a1v, wv, ALU.mult), reads=[A1, wbc], writes=[A1])
                    P.op('gpsimd', lambda e, n=n: e.tensor_tensor(dest[:, 0:n], dest[:, 0:n], A1[:, 0:n], ALU.add), reads=[dest, A1], writes=[dest])

        def reverse_to_zR(S, src):
            n = HC * 4 * S['nb']
            for i, c0 in enumerate(range(0, n, 256)):
                ps_ = nps()
                P.op('tensor', lambda e, ps_=ps_, c0=c0: e.matmul(ps_[:, 0:256], jt[:], src[:, c0:c0 + 256], start=True, stop=True),
                     reads=[jt, src], writes=[ps_])
                if i % 2 == 0:
                    P.op('vector', lambda e, ps_=ps_, c0=c0: e.tensor_copy(zR[:, c0:c0 + 256], ps_[:, 0:256]), reads=[ps_], writes=[zR])
                else:
                    P.op('scalar', lambda e, ps_=ps_, c0=c0: e.copy(zR[:, c0:c0 + 256], ps_[:, 0:256]), reads=[ps_], writes=[zR])

        tcnt = [0]

        def longconv(S, o):
            nb, NQ, HPLEN = S['nb'], S['NQ'], S['HPLEN']
            for c in range(HC):
                tp = toep[tcnt[0] % 2]
                q = 'sync' if tcnt[0] % 2 == 0 else 'gpsimd'
                tcnt[0] += 1
                P.dma(tp[:, 0:NQ], AP(S['hp'], (o * HC + c) * HPLEN, [[1, 128], [1, NQ]]), reads=[S['hp']], writes=[tp], q=q)
                ps_ = nps()
                ms = [0] + [m for m in range(-(nb - 1), nb) if m != 0]
                for mi, m in enumerate(ms):
                    a_lo, a_hi = max(0, m), min(nb, nb + m)
                    cnt = a_hi - a_lo
                    outv = AP(ps_, a_lo, [[512, 128], [nb, 4], [1, cnt]])
                    rhs = AP(zR, c * 4 * nb + (a_lo - m), [[F, 128], [nb, 4], [1, cnt]])
                    P.op('tensor', lambda e, outv=outv, rhs=rhs, tp=tp, m=m, mi=mi: e.matmul(
                        outv, tp[:, 128 * (m + nb - 1):128 * (m + nb)], rhs, start=(mi == 0), stop=(mi == len(ms) - 1), skip_group_check=True),
                        reads=[tp, zR], writes=[ps_])
                dst = A2[:, c * 4 * nb:(c + 1) * 4 * nb]
                if c % 2 == 0:
                    P.op('vector', lambda e, ps_=ps_, dst=dst: e.tensor_copy(dst, ps_[:, 0:4 * nb]), reads=[ps_], writes=[A2])
                else:
                    P.op('scalar', lambda e, ps_=ps_, dst=dst: e.copy(dst, ps_[:, 0:4 * nb]), reads=[ps_], writes=[A2])

        def gate(S, o, z, x):
            nb = S['nb']
            n = HC * 4 * nb
            bv = AP(hbc, o * HC, [[2 * HC, 128], [1, HC], [0, 4 * nb]])
            zv = AP(z, 0, [[F, 128], [4 * nb, HC], [1, 4 * nb]])
            P.op('vector', lambda e: e.tensor_tensor(zv, zv, bv, ALU.mult), reads=[z, hbc], writes=[z])
            P.op('gpsimd', lambda e: e.tensor_tensor(z[:, 0:n], z[:, 0:n], A2[:, 0:n], ALU.add), reads=[z, A2], writes=[z])
            P.op('vector', lambda e: e.tensor_tensor(z[:, 0:n], z[:, 0:n], x[:, 0:n], ALU.mult), reads=[z, x], writes=[z])

        for S in seqs:
            filter_gen(S)
            n = HC * 4 * S['nb']
            conv3(S, 0, vN)
            conv3(S, 1, xN)
            reverse_to_zR(S, vN)
            longconv(S, 0)
            gate(S, 0, vN, xN)
            conv3(S, 2, xN)
            reverse_to_zR(S, vN)
            longconv(S, 1)
            gate(S, 1, vN, xN)
            P.dma(S['out'].ap(), vN[:, 0:n], reads=[vN], writes=[])
        P.finish()
        nc = P.emit()
    return nc


def hy_consts(L):
    t = np.arange(L, dtype=np.float32)
    bands = 16
    f = np.linspace(1e-4, bands - 1, bands, dtype=np.float32)
    phase = (np.float32(2.0 * math.pi / L) * t[:, None] * f[None, :]).astype(np.float32)
    feats = np.concatenate([t[:, None] / np.float32(L - 1), np.cos(phase), -np.sin(phase)], -1).astype(np.float32)
    centre = L // 2
    dist = (np.abs(t - centre) / centre).astype(np.float32)
    deltas = np.abs(np.linspace(math.log(1e-2) / 1.5, math.log(1e-2) / 0.3, 1024, dtype=np.float32))
    dec = np.exp(-dist[:, None] * deltas[None, :]).astype(np.float32)
    return np.ascontiguousarray(feats.T), dec


def hy_inputs(core, pl_h, pc_h, inp, L_):
    c0 = core * HC
    d = {}
    for nm, u, L in (('L', pl_h, 4096), ('C', pc_h, 256)):
        nb = L // 128
        feats, dec = hy_consts(L)
        d['feats' + nm] = feats
        d['dec' + nm] = np.ascontiguousarray(dec.reshape(L, 2, 512)[:, :, c0:c0 + HC].transpose(1, 2, 0))
        ug = u.reshape(4, L, 3, 512)[:, :, :, c0:c0 + HC]
        up = np.pad(ug, ((0, 0), (1, 1), (0, 0), (0, 0)))
        U = np.empty((3, 3, 128, HC, 4, nb), np.float32)
        for k in range(3):
            s = up[:, k:k + L]
            U[:, k] = s.reshape(4, nb, 128, 3, HC).transpose(3, 2, 4, 0, 1)
        d['U' + nm] = U.reshape(3, 3, 128, HC * 4 * nb)
    d['cw'] = np.ascontiguousarray(inp['hy_conv_w'][L_].reshape(3, 3, 512)[:, :, c0:c0 + HC])
    d['w1'] = inp['hy_w1'][L_]; d['w2'] = inp['hy_w2'][L_]
    d['w3'] = np.ascontiguousarray(inp['hy_w3'][L_].reshape(64, 2, 512)[:, :, c0:c0 + HC])
    d['pv'] = np.ascontiguousarray(np.stack([inp['hy_b1'][L_], inp['hy_b2'][L_], inp['hy_sin_freq'][L_][0], inp['hy_sin_freq'][L_][1]], 1))
    d['hb'] = np.ascontiguousarray(inp['hy_bias'][L_][:, c0:c0 + HC])
    d['jm'] = np.ascontiguousarray(np.eye(128, dtype=np.float32)[::-1])
    return d


def hy_unpack(y, L):
    nb = L // 128
    return y.reshape(128, HC, 4, nb).transpose(2, 3, 0, 1).reshape(4, L, HC)


TG = 4352
NCH = 68


def gdn_groups(d):
    ctx = [0, 1, 2, 3]
    lat = [list(range(4 + 8 * i, 12 + 8 * i)) for i in range(8)]
    if d == 0:
        return [ctx] + lat
    return [ctx[::-1]] + [g[::-1] for g in lat[::-1]]


def build_l2g(NB=2):
    P = Prog()
    with ExitStack() as st:
        P._stack = st
        X = P.dram('X', [NB, 3, 128, 3, TG], F32, 'ExternalInput')
        Z = P.dram('Z', [NB, 64, NCH, 128], F32, 'ExternalInput')
        AB = P.dram('AB', [NB, 64, 2, 2, NCH], F32, 'ExternalInput')
        cwd = P.dram('cw', [128, 3, 3], F32, 'ExternalInput')
        scd = P.dram('sc', [64, 2, 2], F32, 'ExternalInput')
        nwd = P.dram('nw', [128], F32, 'ExternalInput')
        Md = P.dram('M', [64, 2, 5, 64], F32, 'ExternalInput')
        identd = P.dram('ident', [128, 128], F32, 'ExternalInput')
        Y = P.dram('Y', [NB, 64, NCH, 128], F32, 'ExternalOutput')

        ident = P.sb('identsb', [128, 128]); P.dma(ident[:], identd.ap(), reads=[identd], writes=[ident])
        Mt = P.sb('Mt', [64, 2, 5, 64]); P.dma(Mt[:], Md.ap(), reads=[Md], writes=[Mt])
        cw = P.sb('cwt', [128, 9]); P.dma(cw[:], AP(cwd, 0, [[9, 128], [1, 9]]), reads=[cwd], writes=[cw])
        sc = P.sb('sct', [64, 4]); P.dma(sc[:], AP(scd, 0, [[4, 64], [1, 4]]), reads=[scd], writes=[sc])
        nwb = P.sb('nwb', [64, 128]); P.dma(nwb[:], AP(nwd, 0, [[0, 64], [1, 128]]), reads=[nwd], writes=[nwb])
        ones = P.sb('ones', [128, 128]); P.op('vector', lambda e: e.memset(ones[:], 1.0), writes=[ones])
        one1 = P.sb('one1', [128, 1]); P.op('vector', lambda e: e.memset(one1[:], 1.0), writes=[one1])
        eps1 = P.sb('eps1', [128, 1]); P.op('vector', lambda e: e.memset(eps1[:], 1e-6), writes=[eps1])
        nea = P.sb('nea', [64, 2])
        for d in range(2):
            P.op('scalar', lambda e, d=d: e.activation(nea[:, d:d + 1], sc[:, 2 * d:2 * d + 1], AF.Exp), reads=[sc], writes=[nea])
        P.op('vector', lambda e: e.tensor_scalar(nea[:], nea[:], -1.0, None, ALU.mult), reads=[nea], writes=[nea])

        pp = [P.ps('pp%d' % i, [128, 512]) for i in range(5)]
        pr = [P.ps('pr%d' % i, [128, 128]) for i in range(3)]
        ppi = [0]

        def nps():
            p_ = pp[ppi[0] % 5]
            ppi[0] += 1
            return p_

        Qf = P.sb('Qf', [128, TG]); Kf = P.sb('Kf', [128, TG]); Vf = P.sb('Vf', [128, TG])
        Oacc = P.sb('Oacc', [64, NCH, 128])
        Zt = P.sb('Zt', [64, NCH, 128])
        stg = P.sb('stg', [128, 3, 256]); cacc = P.sb('cacc', [128, 256]); sq = P.sb('sqg', [128, 256]); rsb = P.sb('rsb', [128, 256])
        abt = P.sb('abt', [64, 2, 2, NCH])
        gt = P.sb('gt', [64, 2, NCH]); bet = P.sb('bet', [64, 2, NCH])
        S = P.sb('S', [128, 128])
        W = 512
        gc = P.sb('gc', [64, 8]); egc = P.sb('egc', [64, 8]); ckb = P.sb('ckb', [64, 8]); ckd = P.sb('ckd', [64, 8])
        E3a = P.sb('E3a', [64, W]); E3b = P.sb('E3b', [64, W])
        Dm = P.sb('Dm', [64, W]); CA = P.sb('CA', [64, W]); CAT = P.sb('CAT', [64, W]); tmpw = P.sb('tmpw', [64, W])
        Pm = P.sb('Pm', [64, W]); PT = P.sb('PTm', [64, W]); TT = P.sb('TTm', [64, W])
        KB = P.sb('KB', [64, 8, 128]); BV = P.sb('BV', [64, 8, 128])
        dbl = []
        for i in range(2):
            dbl.append(dict(U=P.sb('U%d' % i, [64, 8, 128]), KD=P.sb('KD%d' % i, [64, 8, 128]), WT=P.sb('WT%d' % i, [128, W]),
                            QD=P.sb('QD%d' % i, [128, W]), QKT=P.sb('QKT%d' % i, [64, W]), egl=P.sb('egl%d' % i, [128, 8])))
        vnew = P.sb('vnew', [64, 128])
        otmp = P.sb('otmp', [64, 128])

        def V(fn, reads, writes):
            P.op('vector', fn, reads=reads, writes=writes)

        def G_(fn, reads, writes):
            P.op('gpsimd', fn, reads=reads, writes=writes)

        def A_(fn, reads, writes):
            P.op('scalar', fn, reads=reads, writes=writes)

        def T_(fn, reads, writes):
            P.op('tensor', fn, reads=reads, writes=writes)

        def mm256(out_fn, lhsT, rhs_fn, n, reads, ps_):
            for c0 in range(0, n, 256):
                m = min(256, n - c0)
                T_(lambda e, c0=c0, m=m: e.matmul(out_fn(c0, m), lhsT, rhs_fn(c0, m), start=True, stop=True), reads, [ps_])

        gcount = [0]
        for bb in range(NB):
            for s, dst in enumerate((Qf, Kf, Vf)):
                for t0 in range(0, TG, 256):
                    P.dma(stg[:], AP(X, ((bb * 3 + s) * 128) * 3 * TG + t0, [[3 * TG, 128], [TG, 3], [1, 256]]), reads=[X], writes=[stg])
                    V(lambda e, s=s: e.tensor_scalar(cacc[:], stg[:, 0, :], cw[:, s * 3:s * 3 + 1], None, ALU.mult), [stg, cw], [cacc])
                    V(lambda e, s=s: e.scalar_tensor_tensor(cacc[:], stg[:, 1, :], cw[:, s * 3 + 1:s * 3 + 2], cacc[:], ALU.mult, ALU.add), [stg, cw, cacc], [cacc])
                    V(lambda e, s=s: e.scalar_tensor_tensor(cacc[:], stg[:, 2, :], cw[:, s * 3 + 2:s * 3 + 3], cacc[:], ALU.mult, ALU.add), [stg, cw, cacc], [cacc])
                    if s == 2:
                        A_(lambda e, t0=t0, dst=dst: e.activation(dst[:, t0:t0 + 256], cacc[:], AF.Silu), [cacc], [dst])
                    else:
                        A_(lambda e: e.activation(cacc[:], cacc[:], AF.Silu), [cacc], [cacc])
                        A_(lambda e: e.activation(sq[:], cacc[:], AF.Square), [cacc], [sq])
                        ps_ = nps()
                        T_(lambda e, ps_=ps_: e.matmul(ps_[:, 0:256], ones[:], sq[:], start=True, stop=True), [ones, sq], [ps_])
                        A_(lambda e, ps_=ps_: e.activation(rsb[:], ps_[:, 0:256], AF.Sqrt, bias=eps1[:], scale=1.0), [ps_, eps1], [rsb])
                        V(lambda e: e.reciprocal(rsb[:], rsb[:]), [rsb], [rsb])
                        scl = 128 ** -0.5 if s == 0 else 1.0
                        V(lambda e, t0=t0, dst=dst, scl=scl: e.scalar_tensor_tensor(dst[:, t0:t0 + 256], cacc[:], scl, rsb[:], ALU.mult, ALU.mult),
                          [cacc, rsb], [dst])
            P.dma(abt[:], AP(AB, bb * 64 * 4 * NCH, [[4 * NCH, 64], [1, 4 * NCH]]), reads=[AB], writes=[abt])
            for d in range(2):
                V(lambda e, d=d: e.tensor_scalar(gt[:, d, :], abt[:, 0, d, :], sc[:, 2 * d + 1:2 * d + 2], None, ALU.add), [abt, sc], [gt])
                A_(lambda e, d=d: e.activation(gt[:, d, :], gt[:, d, :], AF.Exp), [gt], [gt])
                A_(lambda e, d=d: e.activation(gt[:, d, :], gt[:, d, :], AF.Ln, bias=one1[0:64, :], scale=1.0), [gt, one1], [gt])
                V(lambda e, d=d: e.tensor_scalar(gt[:, d, :], gt[:, d, :], nea[:, d:d + 1], None, ALU.mult), [gt, nea], [gt])
                A_(lambda e, d=d: e.activation(bet[:, d, :], abt[:, 1, d, :], AF.Sigmoid), [abt], [bet])
            for d in range(2):
                V(lambda e: e.memset(S[:], 0.0), [], [S])
                tri = Mt[:, d, 0, :]

                def mask(i, G, d=d):
                    return AP(Mt, (d * 5 + i) * 64, [[640, 64], [0, G], [1, 64]])
                for grp in gdn_groups(d):
                    G = len(grp)
                    c_lo = min(grp)
                    Wg = 64 * G
                    col0 = 64 * c_lo
                    B_ = dbl[gcount[0] % 2]
                    gcount[0] += 1
                    U, KD, WT, QD, QKT, egl = B_['U'], B_['KD'], B_['WT'], B_['QD'], B_['QKT'], B_['egl']
                    gsl = gt[:, d, c_lo:c_lo + G]
                    bsl = bet[:, d, c_lo:c_lo + G]
                    ps_ = nps()
                    T_(lambda e, ps_=ps_, gsl=gsl, tri=tri: e.matmul(ps_[0:64, 0:G], tri, gsl, start=True, stop=True), [Mt, gt], [ps_])
                    T_(lambda e, ps_=ps_, gsl=gsl: e.matmul(ps_[:, 8:8 + G], ones[0:64, :], gsl, start=True, stop=True), [ones, gt], [ps_])
                    V(lambda e, ps_=ps_, G=G: e.tensor_copy(gc[:, 0:G], ps_[0:64, 0:G]), [ps_], [gc])
                    A_(lambda e, ps_=ps_, G=G, egl=egl: e.activation(egl[:, 0:G], ps_[:, 8:8 + G], AF.Exp), [ps_], [egl])
                    A_(lambda e, G=G: e.activation(egc[:, 0:G], gc[:, 0:G], AF.Exp), [gc], [egc])
                    V(lambda e, G=G, bsl=bsl: e.tensor_tensor(ckb[:, 0:G], egc[:, 0:G], bsl, ALU.mult), [egc, bet], [ckb])
                    V(lambda e, ps_=ps_, G=G: e.tensor_tensor(ckd[:, 0:G], ps_[0:64, 8:8 + G], gc[:, 0:G], ALU.subtract), [ps_, gc], [ckd])
                    A_(lambda e, G=G: e.activation(ckd[:, 0:G], ckd[:, 0:G], AF.Exp), [ckd], [ckd])
                    for (src, outs) in ((Kf, ((KB, ckb), (KD, ckd))), (Vf, ((BV, None),))):
                        for h0 in range(0, G, 4):
                            gh = min(4, G - h0)
                            ps_ = nps()
                            for j in range(gh):
                                c = c_lo + h0 + j
                                T_(lambda e, ps_=ps_, j=j, c=c, src=src: e.transpose(ps_[0:64, j * 128:(j + 1) * 128], src[:, 64 * c:64 * c + 64], ident[:]),
                                   [src, ident], [ps_])
                            pv = AP(ps_, 0, [[512, 64], [128, gh], [1, 128]])
                            for (dst, coef) in outs:
                                dv = dst[:, h0:h0 + gh, :]
                                if coef is None:
                                    cf = AP(bet, d * NCH + c_lo + h0, [[2 * NCH, 64], [1, gh], [0, 128]])
                                    V(lambda e, dv=dv, pv=pv, cf=cf: e.tensor_tensor(dv, pv, cf, ALU.mult), [ps_, bet], [dst])
                                else:
                                    cf = AP(coef, h0, [[8, 64], [1, gh], [0, 128]])
                                    V(lambda e, dv=dv, pv=pv, cf=cf: e.tensor_tensor(dv, pv, cf, ALU.mult), [ps_, coef], [dst])
                    idb = AP(ident, 0, [[128, 64], [0, G], [1, 64]])
                    e3a = AP(E3a, 0, [[W, 64], [64, G], [1, 64]]); e3b = AP(E3b, 0, [[W, 64], [64, G], [1, 64]])
                    V(lambda e, e3a=e3a, idb=idb, G=G: e.tensor_tensor(e3a, idb, AP(gc, 0, [[8, 64], [1, G], [0, 64]]), ALU.mult), [ident, gc], [E3a])
                    V(lambda e, e3b=e3b, idb=idb, G=G, c_lo=c_lo: e.tensor_tensor(e3b, idb, AP(bet, d * NCH + c_lo, [[2 * NCH, 64], [1, G], [0, 64]]), ALU.mult),
                      [ident, bet], [E3b])
                    psA = nps(); psB = nps(); psC = nps()
                    mm256(lambda c0, m, psA=psA: psA[0:64, c0:c0 + m], ones[0:64, 0:64], lambda c0, m: E3a[:, c0:c0 + m], Wg, [ones, E3a], psA)
                    mm256(lambda c0, m, psB=psB: psB[0:64, c0:c0 + m], ones[0:64, 0:64], lambda c0, m: E3b[:, c0:c0 + m], Wg, [ones, E3b], psB)
                    mm256(lambda c0, m, psC=psC: psC[:, c0:c0 + m], ones[0:64, :], lambda c0, m: E3a[:, c0:c0 + m], Wg, [ones, E3a], psC)
                    A_(lambda e, psC=psC, Wg=Wg, QD=QD: e.activation(QD[:, 0:Wg], psC[:, 0:Wg], AF.Exp), [psC], [QD])
                    V(lambda e, Wg=Wg, QD=QD, col0=col0: e.tensor_tensor(QD[:, 0:Wg], QD[:, 0:Wg], Qf[:, col0:col0 + Wg], ALU.mult), [QD, Qf], [QD])
                    dm3 = AP(Dm, 0, [[W, 64], [64, G], [1, 64]])
                    V(lambda e, psA=psA, dm3=dm3, G=G: e.scalar_tensor_tensor(dm3, AP(psA, 0, [[512, 64], [64, G], [1, 64]]), -1.0,
                                                                              AP(gc, 0, [[8, 64], [1, G], [0, 64]]), ALU.mult, ALU.add), [psA, gc], [Dm])
                    V(lambda e, Wg=Wg: e.tensor_scalar(tmpw[:, 0:Wg], Dm[:, 0:Wg], 0.0, None, ALU.min), [Dm], [tmpw])
                    A_(lambda e, Wg=Wg: e.activation(tmpw[:, 0:Wg], tmpw[:, 0:Wg], AF.Exp), [tmpw], [tmpw])
                    ca3 = AP(CA, 0, [[W, 64], [64, G], [1, 64]]); tw3 = AP(tmpw, 0, [[W, 64], [64, G], [1, 64]])
                    V(lambda e, ca3=ca3, tw3=tw3, G=G, c_lo=c_lo: e.tensor_tensor(ca3, tw3, AP(bet, d * NCH + c_lo, [[2 * NCH, 64], [1, G], [0, 64]]), ALU.mult),
                      [tmpw, bet], [CA])
                    G_(lambda e, ca3=ca3, G=G: e.tensor_tensor(ca3, ca3, mask(2, G), ALU.mult), [CA, Mt], [CA])
                    V(lambda e, Wg=Wg: e.tensor_scalar(tmpw[:, 0:Wg], Dm[:, 0:Wg], -1.0, 0.0, ALU.mult, ALU.min), [Dm], [tmpw])
                    A_(lambda e, Wg=Wg: e.activation(tmpw[:, 0:Wg], tmpw[:, 0:Wg], AF.Exp), [tmpw], [tmpw])
                    cat3 = AP(CAT, 0, [[W, 64], [64, G], [1, 64]])
                    V(lambda e, psB=psB, Wg=Wg: e.tensor_tensor(CAT[:, 0:Wg], tmpw[:, 0:Wg], psB[0:64, 0:Wg], ALU.mult), [tmpw, psB], [CAT])
                    G_(lambda e, cat3=cat3, G=G: e.tensor_tensor(cat3, cat3, mask(4, G), ALU.mult), [CAT, Mt], [CAT])
                    G_(lambda e, dm3=dm3, tw3=tw3, G=G: e.tensor_tensor(dm3, tw3, mask(3, G), ALU.mult), [tmpw, Mt], [Dm])
                    psK = nps(); psQ = nps()
                    for j in range(G):
                        c = c_lo + j
                        T_(lambda e, psK=psK, j=j, c=c: e.matmul(psK[0:64, 64 * j:64 * j + 64], Kf[:, 64 * c:64 * c + 64], Kf[:, 64 * c:64 * c + 64], start=True, stop=True),
                           [Kf], [psK])
                    for j in range(G):
                        c = c_lo + j
                        T_(lambda e, psQ=psQ, j=j, c=c: e.matmul(psQ[0:64, 64 * j:64 * j + 64], Kf[:, 64 * c:64 * c + 64], Qf[:, 64 * c:64 * c + 64], start=True, stop=True),
                           [Kf, Qf], [psQ])
                    V(lambda e, psK=psK, Wg=Wg: e.tensor_tensor(Pm[:, 0:Wg], psK[0:64, 0:Wg], CA[:, 0:Wg], ALU.mult), [psK, CA], [Pm])
                    V(lambda e, psK=psK, Wg=Wg: e.tensor_tensor(PT[:, 0:Wg], psK[0:64, 0:Wg], CAT[:, 0:Wg], ALU.mult), [psK, CAT], [PT])
                    V(lambda e, psQ=psQ, Wg=Wg, QKT=QKT: e.tensor_tensor(QKT[:, 0:Wg], psQ[0:64, 0:Wg], Dm[:, 0:Wg], ALU.mult), [psQ, Dm], [QKT])
                    tt3 = AP(TT, 0, [[W, 64], [64, G], [1, 64]]); pt3 = AP(PT, 0, [[W, 64], [64, G], [1, 64]])
                    V(lambda e, tt3=tt3, pt3=pt3, idb=idb: e.tensor_tensor(tt3, idb, pt3, ALU.subtract), [ident, PT], [TT])
                    for lev in range(5):
                        p1 = nps(); p2 = nps(); p3 = nps()
                        for j in range(G):
                            sl = slice(64 * j, 64 * j + 64)
                            T_(lambda e, p1=p1, sl=sl: e.matmul(p1[0:64, sl], PT[:, sl], Pm[:, sl], start=True, stop=True), [PT, Pm], [p1])
                        for j in range(G):
                            sl = slice(64 * j, 64 * j + 64)
                            T_(lambda e, p2=p2, sl=sl: e.matmul(p2[0:64, sl], Pm[:, sl], PT[:, sl], start=True, stop=True), [PT, Pm], [p2])
                        V(lambda e, p1=p1, Wg=Wg: e.tensor_copy(Pm[:, 0:Wg], p1[0:64, 0:Wg]), [p1], [Pm])
                        A_(lambda e, p2=p2, Wg=Wg: e.copy(PT[:, 0:Wg], p2[0:64, 0:Wg]), [p2], [PT])
                        for j in range(G):
                            sl = slice(64 * j, 64 * j + 64)
                            T_(lambda e, p3=p3, sl=sl: e.matmul(p3[0:64, sl], Pm[:, sl], TT[:, sl], start=True, stop=True), [Pm, TT], [p3])
                        V(lambda e, p3=p3, Wg=Wg: e.tensor_tensor(TT[:, 0:Wg], TT[:, 0:Wg], p3[0:64, 0:Wg], ALU.add), [p3, TT], [TT])
                    for h0 in range(0, G, 4):
                        gh = min(4, G - h0)
                        ps_ = nps()
                        for j in range(gh):
                            jj = h0 + j
                            T_(lambda e, ps_=ps_, j=j, jj=jj: e.matmul(ps_[0:64, j * 128:(j + 1) * 128], TT[:, 64 * jj:64 * jj + 64], BV[:, jj, :], start=True, stop=True),
                               [TT, BV], [ps_])
                        A_(lambda e, ps_=ps_, h0=h0, gh=gh, U=U: e.copy(U[:, h0:h0 + gh, :], AP(ps_, 0, [[512, 64], [128, gh], [1, 128]])), [ps_], [U])
                    ps_ = nps()
                    for j in range(G):
                        T_(lambda e, ps_=ps_, j=j: e.matmul(ps_[:, 64 * j:64 * j + 64], KB[:, j, :], TT[:, 64 * j:64 * j + 64], start=True, stop=True), [KB, TT], [ps_])
                    V(lambda e, ps_=ps_, Wg=Wg, WT=WT: e.tensor_copy(WT[:, 0:Wg], ps_[:, 0:Wg]), [ps_], [WT])
                    for c in grp:
                        j = c - c_lo
                        sl = slice(64 * j, 64 * j + 64)
                        p1, p2, p3 = pr
                        T_(lambda e, p1=p1, sl=sl, WT=WT: e.matmul(p1[0:64, :], WT[:, sl], S[:], start=True, stop=True), [WT, S], [p1])
                        V(lambda e, p1=p1, j=j, U=U: e.tensor_tensor(vnew[:], U[:, j, :], p1[0:64, :], ALU.subtract), [U, p1], [vnew])
                        T_(lambda e, p2=p2, sl=sl, QD=QD: e.matmul(p2[0:64, :], QD[:, sl], S[:], start=True, stop=False), [QD, S], [p2])
                        T_(lambda e, p2=p2, sl=sl, QKT=QKT: e.matmul(p2[0:64, :], QKT[:, sl], vnew[:], start=False, stop=True), [QKT, vnew], [p2])
                        T_(lambda e, p3=p3, j=j, KD=KD: e.matmul(p3[:, :], KD[:, j, :], vnew[:], start=True, stop=True), [KD, vnew], [p3])
                        V(lambda e, p3=p3, j=j, egl=egl: e.scalar_tensor_tensor(S[:], S[:], egl[:, j:j + 1], p3[:, :], ALU.mult, ALU.add), [S, egl, p3], [S])
                        if d == 0:
                            A_(lambda e, p2=p2, c=c: e.copy(Oacc[:, c, :], p2[0:64, :]), [p2], [Oacc])
                        else:
                            G_(lambda e: None, [], []) if False else None
                            V(lambda e, p2=p2, c=c: e.tensor_tensor(Oacc[:, c, :], Oacc[:, c, :], p2[0:64, :], ALU.add), [p2, Oacc], [Oacc])
            P.dma(Zt[:], AP(Z, bb * 64 * NCH * 128, [[NCH * 128, 64], [1, NCH * 128]]), reads=[Z], writes=[Zt])
            A_(lambda e: e.activation(Zt[:], Zt[:], AF.Silu), [Zt], [Zt])
            ssq = gt
            for h0 in range(0, NCH, 4):
                V(lambda e, h0=h0: e.tensor_tensor(KB[:, 0:4, :], Oacc[:, h0:h0 + 4, :], Oacc[:, h0:h0 + 4, :], ALU.mult), [Oacc], [KB])
                V(lambda e, h0=h0: e.tensor_reduce(ssq[:, 0, h0:h0 + 4], KB[:, 0:4, :], AX.X, ALU.add), [KB], [gt])
            A_(lambda e: e.activation(ssq[:, 0, :], ssq[:, 0, :], AF.Sqrt, bias=eps1[0:64, :], scale=1.0 / 128), [gt, eps1], [gt])
            V(lambda e: e.reciprocal(ssq[:, 0, :], ssq[:, 0, :]), [gt], [gt])
            V(lambda e: e.tensor_tensor(Oacc[:], Oacc[:], AP(gt, 0, [[2 * NCH, 64], [1, NCH], [0, 128]]), ALU.mult), [Oacc, gt], [Oacc])
            G_(lambda e: e.tensor_tensor(Oacc[:], Oacc[:], AP(nwb, 0, [[128, 64], [0, NCH], [1, 128]]), ALU.mult), [Oacc, nwb], [Oacc])
            V(lambda e: e.tensor_tensor(Oacc[:], Oacc[:], Zt[:], ALU.mult), [Oacc, Zt], [Oacc])
            P.dma(AP(Y, bb * 64 * NCH * 128, [[NCH * 128, 64], [1, NCH * 128]]), Oacc[:], reads=[Oacc], writes=[])
        P.finish()
        nc = P.emit()
    return nc


def gdn_masks():
    i = np.arange(64)
    M = np.zeros((64, 2, 5, 64), np.float32)
    for d in range(2):
        le = (i[:, None] <= i[None, :]) if d == 0 else (i[:, None] >= i[None, :])
        incl = (i[:, None] >= i[None, :]) if d == 0 else (i[:, None] <= i[None, :])
        strict = (i[:, None] > i[None, :]) if d == 0 else (i[:, None] < i[None, :])
        M[:, d, 0] = le; M[:, d, 1] = incl; M[:, d, 2] = strict; M[:, d, 3] = incl.T; M[:, d, 4] = strict.T
    return M


def gdn_inputs(h, pb_list, inp, L_):
    NB = len(pb_list)
    X = np.zeros((NB, 3, 128, 3, TG), np.float32)
    Zz = np.empty((NB, 64, NCH, 128), np.float32)
    AB = np.empty((NB, 64, 2, 2, NCH), np.float32)
    for bi, pb in enumerate(pb_list):
        seqs = [(pb[4096:4352], 0, 256), (pb[0:4096], 256, 4096)]
        for (ps_, off, L) in seqs:
            for s in range(3):
                x = ps_[:, s * 512 + h * 128:s * 512 + (h + 1) * 128]
                xp = np.pad(x, ((1, 1), (0, 0)))
                for k in range(3):
                    X[bi, s, :, k, off:off + L] = xp[k:k + L].T
        tok = np.concatenate([pb[4096:4352], pb[0:4096]], 0)
        z = tok[:, 1536 + h * 128:1536 + (h + 1) * 128]
        Zz[bi] = z.reshape(NCH, 64, 128).transpose(1, 0, 2)
        a = tok[:, 2048:2056].reshape(TG, 2, 4)[:, :, h]
        bt = tok[:, 2056:2064].reshape(TG, 2, 4)[:, :, h]
        AB[bi, :, 0] = a.reshape(NCH, 64, 2).transpose(1, 2, 0)
        AB[bi, :, 1] = bt.reshape(NCH, 64, 2).transpose(1, 2, 0)
    cw = np.ascontiguousarray(inp['gdn_conv_w'][L_].reshape(3, 3, 4, 128)[:, :, h, :].transpose(2, 1, 0))
    sc = np.empty((64, 2, 2), np.float32)
    sc[:, :, 0] = inp['gdn_a_log'][L_][:, h][None, :]
    sc[:, :, 1] = inp['gdn_dt_bias'][L_][:, h][None, :]
    return {'X': X, 'Z': Zz, 'AB': AB, 'cw': cw, 'sc': sc, 'nw': inp['gdn_norm_w'][L_], 'M': gdn_masks(), 'ident': np.eye(128, dtype=np.float32)}


def gdn_unpack(Y):
    t = Y.transpose(1, 0, 2).reshape(TG, 128)
    return t[256:], t[:256]


def build_l4():
    P = Prog()
    with ExitStack() as st:
        P._stack = st
        xT = P.dram('xT', [128, NKC, T3], F32, 'ExternalInput')
        fw = P.dram('fw', [128, NKC], F32, 'ExternalInput')
        oT = P.dram('oT', [128, NKC, T3], F32, 'ExternalOutput')
        fwt = P.sb('fwt', [128, NKC])
        P.dma(fwt[:], fw.ap(), reads=[fw], writes=[fwt])
        ones_bf = P.sb('ones_bf', [128, 128], BF16)
        P.op('vector', lambda e: e.memset(ones_bf[:], 1.0), writes=[ones_bf])
        eps_t = P.sb('eps_t', [128, 1])
        P.op('vector', lambda e: e.memset(eps_t[:], 1e-6), writes=[eps_t])
        SUB = 256
        xs = [P.sb('xs%d' % i, [128, NKC, SUB]) for i in range(2)]
        sq = P.sb('sq', [128, NKC, SUB], BF16)
        pss = P.ps('ps_ss', [128, SUB])
        rs = P.sb('rs', [128, SUB])
        tmp = P.sb('ntmp', [128, NKC, SUB])
        ot = [P.sb('ot%d' % i, [128, NKC, SUB]) for i in range(2)]
        nsub = (T3 + SUB - 1) // SUB
        for s in range(nsub):
            t0 = s * SUB
            n = min(SUB, T3 - t0)
            x_ = xs[s % 2]
            o_ = ot[s % 2]
            P.dma(x_[:, :, 0:n], AP(xT, t0, [[NKC * T3, 128], [T3, NKC], [1, n]]), reads=[xT], writes=[x_])
            P.op('scalar', lambda e, x_=x_, n=n: e.activation(sq[:, :, 0:n], x_[:, :, 0:n], AF.Square), reads=[x_], writes=[sq])
            for c in range(NKC):
                P.op('tensor', lambda e, c=c, n=n: e.matmul(pss[:, 0:n], ones_bf[:], sq[:, c, 0:n], start=(c == 0), stop=(c == NKC - 1)),
                     reads=[sq, ones_bf], writes=[pss])
            P.op('scalar', lambda e, n=n: e.activation(rs[:, 0:n], pss[:, 0:n], AF.Sqrt, bias=eps_t[:], scale=1.0 / D),
                 reads=[pss, eps_t], writes=[rs])
            P.op('vector', lambda e, n=n: e.reciprocal(rs[:, 0:n], rs[:, 0:n]), reads=[rs], writes=[rs])
            P.op('vector', lambda e, x_=x_, n=n: e.tensor_tensor(
                tmp[:, :, 0:n], x_[:, :, 0:n], AP(rs, 0, [[SUB, 128], [0, NKC], [1, n]]), ALU.mult), reads=[x_, rs], writes=[tmp])
            P.op('vector', lambda e, o_=o_, n=n: e.tensor_tensor(
                o_[:, :, 0:n], tmp[:, :, 0:n], AP(fwt, 0, [[NKC, 128], [1, NKC], [0, n]]), ALU.mult), reads=[tmp, fwt], writes=[o_])
            P.dma(AP(oT, t0, [[NKC * T3, 128], [T3, NKC], [1, n]]), o_[:, :, 0:n], reads=[o_], writes=[])
        P.finish()
        nc = P.emit()
    return nc


def _fm(a):
    return np.ascontiguousarray(a.T.reshape(16, 128, a.shape[0]).transpose(1, 0, 2))


def _unfm(a):
    return np.ascontiguousarray(a.transpose(2, 1, 0).reshape(a.shape[2], 2048))


def _rope_tables():
    n_freq = 16
    freqs = (10000.0 ** (-np.arange(n_freq, dtype=np.float32) / n_freq)).astype(np.float32)
    row = np.repeat(np.arange(64, dtype=np.float32), 64)
    col = np.tile(np.arange(64, dtype=np.float32), 64)
    ang = np.concatenate([row[:, None] * freqs, col[:, None] * freqs], -1)
    c = np.cos(ang).T.astype(np.float32)
    s = np.sin(ang).T.astype(np.float32)
    C = np.ones((64, TA), np.float32)
    S = np.zeros((64, TA), np.float32)
    C[:32, :4096] = c; C[32:, :4096] = c; S[:32, :4096] = -s; S[32:, :4096] = s
    rm = np.zeros((64, 64), np.float32)
    for m in range(64):
        rm[(m + 32) % 64, m] = 1
    return C, S, rm


def _run(nc, in_maps):
    res = run_bass_kernel_spmd(nc, in_maps, core_ids=list(range(8)))
    return res.results


def kernel(**inp):
    inp = {k: np.asarray(v) for k, v in inp.items()}
    x = inp['x']; ctx = inp['ctx']
    B = 4
    mod = run_l0(inp['c'], inp['c_ctx'], inp['w_ada'], inp['b_ada'])
    C, S, rm = _rope_tables()
    ident = np.eye(128, dtype=np.float32)
    nc1 = build_l1(); nc2 = build_l2a(); nc3 = build_l3(); nc4 = build_l4(); ncg = build_l2g(2); nch = build_l2h()
    xl = x.astype(np.float32); xc = ctx.astype(np.float32)
    for L in range(2):
        m = mod[L]

        def mrow(r, i):
            return vecT(m[r, i * 2048:(i + 1) * 2048])
        ims = []
        for core in range(8):
            b, hh = core // 2, core % 2
            xcat = np.concatenate([xl[b, hh * 2048:(hh + 1) * 2048], xc[b, hh * 128:(hh + 1) * 128]], 0)
            mv = np.stack([vecT(inp['norm1_w'][L]), mrow(b, 1), mrow(b, 0), mrow(4, 1), mrow(4, 0)])
            ims.append({'xT': _fm(xcat), 'mv': mv, 'W': inp['w_in'][L]})
        r1 = _run(nc1, ims)
        p = np.empty((B, TA, IN_DIM), np.float32)
        for core in range(8):
            b, hh = core // 2, core % 2
            pT = r1[core]['pT']
            p[b, hh * 2048:(hh + 1) * 2048] = pT[:, :2048].T
            p[b, 4096 + hh * 128:4096 + (hh + 1) * 128] = pT[:, 2048:].T
        del r1
        o = 2048 + 16
        nw = np.zeros((128, 8), np.float32)
        nw[:, 0:4] = inp['mla_q_norm_w'][L].reshape(4, 128).T
        nw[:, 4:6] = inp['mla_kv_norm_w'][L].reshape(2, 128).T
        nw[:64, 6] = inp['gqa_q_norm_w'][L]; nw[:64, 7] = inp['gqa_k_norm_w'][L]
        ims = []
        for core in range(8):
            b, hh = core // 2, core % 2
            pb = p[b]
            o2 = o + 832
            ims.append({'gq': np.ascontiguousarray(pb[:, o2 + hh * 256:o2 + (hh + 1) * 256].T),
                        'gk': np.ascontiguousarray(pb[:, o2 + 512 + hh * 64:o2 + 512 + (hh + 1) * 64].T),
                        'gv': np.ascontiguousarray(pb[:, o2 + 640 + hh * 64:o2 + 640 + (hh + 1) * 64]),
                        'cq': np.ascontiguousarray(pb[:, o:o + 512].T), 'ckv': np.ascontiguousarray(pb[:, o + 512:o + 768].T),
                        'kr': np.ascontiguousarray(pb[:, o + 768:o + 832].T),
                        'wuq': np.ascontiguousarray(inp['mla_w_uq'][L][:, 2 * hh:2 * hh + 2].reshape(512, 384)),
                        'wukv': np.ascontiguousarray(inp['mla_w_ukv'][L][:, 2 * hh:2 * hh + 2].reshape(256, 512)),
                        'nw': nw, 'cosd': C, 'sind': S, 'rmd': rm})
        r2 = _run(nc2, ims)
        br = np.zeros((B, TA, 4, 512), np.float32)
        for core in range(8):
            b, hh = core // 2, core % 2
            br[b, :, 1, hh * 256:(hh + 1) * 256] = r2[core]['om']
            br[b, :, 2, hh * 256:(hh + 1) * 256] = r2[core]['og']
        del r2
        ims = []
        for core in range(8):
            h, bp = core // 2, core % 2
            ims.append(gdn_inputs(h, [p[2 * bp], p[2 * bp + 1]], inp, L))
        rg = _run(ncg, ims)
        for core in range(8):
            h, bp = core // 2, core % 2
            for bi in range(2):
                lat, cx = gdn_unpack(rg[core]['Y'][bi])
                br[2 * bp + bi, :4096, 0, h * 128:(h + 1) * 128] = lat
                br[2 * bp + bi, 4096:, 0, h * 128:(h + 1) * 128] = cx
        del rg, ims
        pl_h = np.ascontiguousarray(p[:, :4096, 3664:5200]); pc_h = np.ascontiguousarray(p[:, 4096:, 3664:5200])
        ims = [hy_inputs(core, pl_h, pc_h, inp, L) for core in range(8)]
        rh = _run(nch, ims)
        for core in range(8):
            br[:, :4096, 3, core * 64:(core + 1) * 64] = hy_unpack(rh[core]['yL'], 4096)
            br[:, 4096:, 3, core * 64:(core + 1) * 64] = hy_unpack(rh[core]['yC'], 256)
        del rh, ims
        wbr = np.ascontiguousarray(inp['w_branch'][L].reshape(2048, 2048))
        wr = np.ascontiguousarray(inp['w_router'].reshape(16, 128, 16).transpose(1, 0, 2))
        nxl = np.empty_like(xl); nxc = np.empty_like(xc)
        for half in range(2):
            ims = []
            for core in range(8):
                sh = half * 8 + core
                b, q = sh // 4, sh % 4
                ls = slice(q * 1024, (q + 1) * 1024); cs_ = slice(q * 64, (q + 1) * 64)
                cs2 = slice(4096 + q * 64, 4096 + (q + 1) * 64)
                xcat = np.concatenate([xl[b, ls], xc[b, cs_]], 0)
                brc = np.concatenate([br[b, ls], br[b, cs2]], 0).reshape(T3, 2048)
                g = np.concatenate([p[b, ls, MIX_IN:], p[b, cs2, MIX_IN:]], 0)
                mv = np.stack([vecT(inp['norm2_w'][L]), mrow(b, 2), mrow(b, 3), mrow(b, 4), mrow(b, 5),
                               mrow(4, 2), mrow(4, 3), mrow(4, 4), mrow(4, 5)])
                ims.append({'xT': _fm(xcat), 'brT': np.ascontiguousarray(brc.T), 'gT': np.ascontiguousarray(g.T), 'mv': mv,
                            'wbr': wbr, 'wo': inp['w_out'][L], 'wr': wr, 'rb': inp['router_bias'],
                            'wg': inp['moe_w_gate'][L], 'wu': inp['moe_w_up'][L], 'wd': inp['moe_w_down'][L], 'ident': ident})
            r3 = _run(nc3, ims)
            for core in range(8):
                sh = half * 8 + core
                b, q = sh // 4, sh % 4
                x2 = _unfm(r3[core]['x2T'])
                nxl[b, q * 1024:(q + 1) * 1024] = x2[:1024]
                nxc[b, q * 64:(q + 1) * 64] = x2[1024:]
            del r3
        xl, xc = nxl, nxc
        del p, br
    out = np.empty_like(xl)
    fw = vecT(inp['final_norm_w'])
    for half in range(2):
        ims = []
        for core in range(8):
            sh = half * 8 + core
            b, q = sh // 4, sh % 4
            xcat = np.concatenate([xl[b, q * 1024:(q + 1) * 1024], xc[b, q * 64:(q + 1) * 64]], 0)
            ims.append({'xT': _fm(xcat), 'fw': fw})
        r4 = _run(nc4, ims)
        for core in range(8):
            sh = half * 8 + core
            b, q = sh // 4, sh % 4
            out[b, q * 1024:(q + 1) * 1024] = _unfm(r4[core]['oT'])[:1024]
    return out.astype(np.float32)
```

```python
import math
from contextlib import ExitStack
import types
import numpy as np
import concourse.bass as bass
import concourse.mybir as mybir
from concourse.bass_utils import run_bass_kernel_spmd

F32 = mybir.dt.float32
BF16 = mybir.dt.bfloat16
I32 = mybir.dt.int32
AF = mybir.ActivationFunctionType
ALU = mybir.AluOpType
AX = mybir.AxisListType

COMPUTE = ('tensor', 'vector', 'scalar', 'gpsimd')
NDMASEM = 24


def _freeze(fn):
    if not getattr(fn, '__closure__', None):
        return fn
    cells = []
    for c in fn.__closure__:
        try:
            cells.append(types.CellType(c.cell_contents))
        except ValueError:
            cells.append(c)
    g = types.FunctionType(fn.__code__, fn.__globals__, fn.__name__, fn.__defaults__, tuple(cells))
    g.__kwdefaults__ = fn.__kwdefaults__
    return g


class Prog:
    def __init__(self, name='k'):
        self.nc = bass.Bass('TRN2', target_bir_lowering=False)
        self.ops = {e: [] for e in COMPUTE + ('sync',)}
        self.cnt = {e: 0 for e in COMPUTE}
        self.waited = {e: {} for e in COMPUTE + ('sync',)}
        self.last_w = {}
        self.readers = {}
        self.dma_cnt = [0] * NDMASEM
        self.dma_rr = 0
        self.ctx = []
        self.sems = {}
        self.n_ops = 0
        self._stack = None
        self.safe = False
        self.pe_pending = {}

    def _enter(self, cm):
        return self._stack.enter_context(cm)

    def dram(self, name, shape, dt, kind):
        return self.nc.dram_tensor(name, list(shape), dt, kind=kind)

    def sb(self, name, shape, dt=F32):
        return self._enter(self.nc.sbuf_tensor(name, list(shape), dt))

    def ps(self, name, shape, dt=F32):
        return self._enter(self.nc.psum_tensor(name, list(shape), dt))

    @staticmethod
    def _key(t):
        if isinstance(t, str):
            return t
        if hasattr(t, 'tensor'):
            t = t.tensor
        return t.name

    def _deps(self, reads, writes):
        deps = []
        for r in reads:
            k = self._key(r)
            if k in self.last_w:
                deps.append(self.last_w[k])
        for w in writes:
            k = self._key(w)
            if k in self.last_w:
                deps.append(self.last_w[k])
            deps.extend(self.readers.get(k, []))
        return deps

    def _commit(self, ticket, reads, writes):
        for r in reads:
            self.readers.setdefault(self._key(r), []).append(ticket)
        for w in writes:
            k = self._key(w)
            self.last_w[k] = ticket
            self.readers[k] = []

    def _wait_list(self, eng, deps):
        need = {}
        for (kind, idx, val) in deps:
            if kind == 'eng' and idx == 'tensor' and eng == 'tensor':
                continue
            sk = (kind, idx)
            if self.waited[eng].get(sk, 0) >= val:
                continue
            if need.get(sk, 0) < val:
                need[sk] = val
        for sk, val in need.items():
            self.waited[eng][sk] = val
        return list(need.items())

    def op(self, eng, fn, reads=(), writes=()):
        deps = self._deps(reads, writes)
        waits = self._wait_list(eng, deps)
        self.cnt[eng] += 1
        ticket = ('eng', eng, self.cnt[eng])
        self._commit(ticket, reads, writes)
        if eng == 'tensor':
            for w in writes:
                self.pe_pending.setdefault(self._key(w), set()).update(self._key(r) for r in reads)
        else:
            for r in reads:
                pk = self._key(r)
                if pk in self.pe_pending:
                    for k in self.pe_pending.pop(pk):
                        self.readers.setdefault(k, []).append(ticket)
        self.ops[eng].append(('c', waits, _freeze(fn)))
        self.n_ops += 1
        return ticket

    def dma(self, out, in_, reads=(), writes=(), q='sync', **kw):
        deps = self._deps(reads, writes)
        si = self.dma_rr
        self.dma_rr = (self.dma_rr + 1) % NDMASEM
        if self.dma_cnt[si] > 0:
            deps.append(('dma', si, self.dma_cnt[si]))
        waits = self._wait_list(q, deps)
        self.dma_cnt[si] += 16
        ticket = ('dma', si, self.dma_cnt[si])
        self._commit(ticket, reads, writes)
        self.ops[q].append(('d', waits, (out, in_, si, kw)))
        self.n_ops += 1
        return ticket

    def finish(self, eng='sync'):
        deps = [('dma', i, c) for i, c in enumerate(self.dma_cnt) if c > 0]
        waits = self._wait_list(eng, deps)
        self.ops[eng].append(('w', waits, None))

    def emit(self):
        nc = self.nc
        esem = {e: self._enter(nc.semaphore('s_' + e)) for e in COMPUTE}
        dsem = [self._enter(nc.semaphore('d_%d' % i)) for i in range(NDMASEM)]

        def semof(sk):
            return esem[sk[1]] if sk[0] == 'eng' else dsem[sk[1]]

        ops = self.ops

        def run(engname):
            def body(e):
                for (kind, waits, payload) in ops[engname]:
                    for sk, val in waits:
                        e.wait_ge(semof(sk), val)
                    if kind == 'c':
                        ins = payload(e)
                        if self.safe and engname != 'tensor':
                            e.drain().then_inc(esem[engname], 1)
                        else:
                            ins.then_inc(esem[engname], 1)
                    elif kind == 'd':
                        out, in_, si, kw = payload
                        e.dma_start(out=out, in_=in_, **kw).then_inc(dsem[si], 16)
            return body

        with nc.Block() as block:
            block.sync(run('sync'))
            block.tensor(run('tensor'))
            block.vector(run('vector'))
            block.scalar(run('scalar'))
            block.gpsimd(run('gpsimd'))
        return nc


D = 2048
NKC = 16
IN_DIM = 13392
T1 = 2176


def AP(t, off, dims):
    return bass.AP(t, off, [list(d) for d in dims])


def build_l0():
    P = Prog()
    with ExitStack() as st:
        P._stack = st
        cT = P.dram('cT', [128, 16, 5], F32, 'ExternalInput')
        w = P.dram('w', [2, D, 1536], F32, 'ExternalInput')
        b = P.dram('b', [2, 1536], F32, 'ExternalInput')
        out = P.dram('mod', [2, 5, 1536], F32, 'ExternalOutput')
        ct = P.sb('ct', [128, 16, 5])
        cs = P.sb('cs', [128, 16, 5])
        P.dma(ct[:], cT.ap(), reads=[cT], writes=[ct])
        P.op('scalar', lambda e: e.activation(cs[:], ct[:], AF.Silu), reads=[ct], writes=[cs])
        wt = [P.sb('wt%d' % i, [128, 4, 512]) for i in range(2)]
        bt = P.sb('bt', [5, 2, 1536])
        P.dma(bt[:], AP(b, 0, [[0, 5], [1536, 2], [1, 1536]]), reads=[b], writes=[bt])
        ps = [P.ps('ps%d' % i, [5, 512]) for i in range(2)]
        ot = P.sb('ot', [5, 2, 1536])
        n = 0
        pi = 0
        for l in range(2):
            for cb in range(3):
                pst = ps[pi % 2]
                pi += 1
                for kg in range(4):
                    wtile = wt[n % 2]
                    n += 1
                    src = AP(w, l * D * 1536 + kg * 512 * 1536 + cb * 512, [[1536, 128], [128 * 1536, 4], [1, 512]])
                    P.dma(wtile[:], src, reads=[w], writes=[wtile])
                    for kk in range(4):
                        k = kg * 4 + kk
                        P.op('tensor', lambda e, k=k, kk=kk, wtile=wtile, pst=pst: e.matmul(
                            pst[:], cs[:, k, :], wtile[:, kk, :], start=(k == 0), stop=(k == 15)),
                            reads=[cs, wtile], writes=[pst])
                P.op('vector', lambda e, l=l, cb=cb, pst=pst: e.tensor_tensor(
                    ot[:, l, cb * 512:(cb + 1) * 512], pst[:], bt[:, l, cb * 512:(cb + 1) * 512], ALU.add),
                    reads=[pst, bt], writes=[ot])
        P.dma(AP(out, 0, [[1536, 5], [5 * 1536, 2], [1, 1536]]), ot[:], reads=[ot], writes=[out])
        P.finish()
        nc = P.emit()
    return nc


def run_l0(c, c_ctx, w_ada, b_ada):
    c5 = np.concatenate([c, c_ctx[None]], 0)
    cT = np.ascontiguousarray(c5.T.reshape(16, 128, 5).transpose(1, 0, 2))
    nc = build_l0()
    in_maps = []
    for i in range(8):
        sl = slice(i * 1536, (i + 1) * 1536)
        in_maps.append({'cT': cT, 'w': np.ascontiguousarray(w_ada[:, :, sl]), 'b': np.ascontiguousarray(b_ada[:, sl])})
    res = run_bass_kernel_spmd(nc, in_maps, core_ids=list(range(8)))
    mod = np.concatenate([r['mod'] for r in res.results], axis=2)
    return mod


def vecT(v):
    return np.ascontiguousarray(v.reshape(16, 128).T)


TOKB = [(0, 512), (512, 512), (1024, 512), (1536, 512), (2048, 128)]


def AP_slice(mvt, r):
    return mvt[:, r, :]


def norm_phase(P, xT, hT, gl, sl_, gc, sc_, ones_bf, eps_t, T, n_lat, x_reads=None, out_key='hT', SUB=256):
    xs = [P.sb('xs%d' % i, [128, NKC, SUB]) for i in range(2)]
    sq = P.sb('sq', [128, NKC, SUB], BF16)
    pss = P.ps('ps_ss', [128, SUB])
    rs = P.sb('rs', [128, SUB])
    tmp = P.sb('ntmp', [128, NKC, SUB])
    nsub = (T + SUB - 1) // SUB
    for s in range(nsub):
        t0 = s * SUB
        n = min(SUB, T - t0)
        x_ = xs[s % 2]
        P.dma(x_[:, :, 0:n], AP(xT, t0, [[NKC * T, 128], [T, NKC], [1, n]]), reads=(x_reads if x_reads is not None else [xT]), writes=[x_])
        P.op('scalar', lambda e, x_=x_, n=n: e.activation(sq[:, :, 0:n], x_[:, :, 0:n], AF.Square),
             reads=[x_], writes=[sq])
        for c in range(NKC):
            P.op('tensor', lambda e, c=c, n=n: e.matmul(pss[:, 0:n], ones_bf[:], sq[:, c, 0:n],
                                                          start=(c == 0), stop=(c == NKC - 1)),
                 reads=[sq, ones_bf], writes=[pss])
        P.op('scalar', lambda e, n=n: e.activation(rs[:, 0:n], pss[:, 0:n], AF.Sqrt, bias=eps_t[:], scale=1.0 / D),
             reads=[pss, eps_t], writes=[rs])
        P.op('vector', lambda e, n=n: e.reciprocal(rs[:, 0:n], rs[:, 0:n]), reads=[rs], writes=[rs])
        P.op('vector', lambda e, x_=x_, n=n: e.tensor_tensor(
            tmp[:, :, 0:n], x_[:, :, 0:n], AP(rs, 0, [[SUB, 128], [0, NKC], [1, n]]), ALU.mult),
            reads=[x_, rs], writes=[tmp])
        g_, s_ = (gl, sl_) if t0 < n_lat else (gc, sc_)
        for c in range(NKC):
            eng = 'vector' if c % 2 == 0 else 'gpsimd'
            P.op(eng, lambda e, c=c, n=n, g_=g_, s_=s_, t0=t0: e.tensor_scalar(
                hT[:, c, t0:t0 + n], tmp[:, c, 0:n], g_[:, c:c + 1], s_[:, c:c + 1], ALU.mult, ALU.add),
                reads=[tmp, g_, s_], writes=['%s:%d' % (out_key, c)])


def linear_fm(P, W, w_off, w_rs, K_chunks, n_out, rhs_fn, tokb, evac_fn, rhs_reads, tag='lin', grp=256):
    wst = [P.sb('%s_wst%d' % (tag, i), [128, K_chunks, grp]) for i in range(2)]
    wbf = [P.sb('%s_wbf%d' % (tag, i), [128, K_chunks, grp], BF16) for i in range(2)]
    pst = [P.ps('%s_ps%d' % (tag, i), [128, 512]) for i in range(4)]
    ng = (n_out + grp - 1) // grp
    pi = 0
    for g in range(ng):
        o0 = g * grp
        gw = min(grp, n_out - o0)
        ws, wb = wst[g % 2], wbf[g % 2]
        P.dma(ws[:, :, 0:gw], AP(W, w_off + o0, [[w_rs, 128], [128 * w_rs, K_chunks], [1, gw]]),
              reads=[W], writes=[ws])
        P.op('scalar', lambda e, ws=ws, wb=wb, gw=gw: e.copy(wb[:, :, 0:gw], ws[:, :, 0:gw]),
             reads=[ws], writes=[wb])
        for m0 in range(0, gw, 128):
            m = min(128, gw - m0)
            for (t0, n) in tokb:
                ps_ = pst[pi % 4]
                pi += 1
                for kc in range(K_chunks):
                    P.op('tensor', lambda e, ps_=ps_, wb=wb, kc=kc, m0=m0, m=m, t0=t0, n=n: e.matmul(
                        ps_[0:m, 0:n], wb[:, kc, m0:m0 + m], rhs_fn(kc, t0, n),
                        start=(kc == 0), stop=(kc == K_chunks - 1)),
                        reads=[wb] + rhs_reads(kc), writes=[ps_])
                evac_fn(o0 + m0, m, t0, n, ps_)


def build_l1():
    P = Prog()
    with ExitStack() as st:
        P._stack = st
        xT = P.dram('xT', [128, NKC, T1], F32, 'ExternalInput')
        mv = P.dram('mv', [5, 128, NKC], F32, 'ExternalInput')
        W = P.dram('W', [D, IN_DIM], F32, 'ExternalInput')
        pT = P.dram('pT', [IN_DIM, T1], F32, 'ExternalOutput')
        mvt = P.sb('mvt', [128, 5, NKC])
        P.dma(mvt[:], AP(mv, 0, [[NKC, 128], [128 * NKC, 5], [1, NKC]]), reads=[mv], writes=[mvt])
        gl = P.sb('gl', [128, NKC]); gc = P.sb('gc', [128, NKC])
        sl_ = P.sb('sl', [128, NKC]); sc_ = P.sb('sc', [128, NKC])
        P.op('vector', lambda e: e.scalar_tensor_tensor(gl[:], mvt[:, 1, :], 1.0, mvt[:, 0, :], ALU.add, ALU.mult),
             reads=[mvt], writes=[gl])
        P.op('vector', lambda e: e.scalar_tensor_tensor(gc[:], mvt[:, 3, :], 1.0, mvt[:, 0, :], ALU.add, ALU.mult),
             reads=[mvt], writes=[gc])
        P.op('vector', lambda e: e.tensor_copy(sl_[:], mvt[:, 2, :]), reads=[mvt], writes=[sl_])
        P.op('vector', lambda e: e.tensor_copy(sc_[:], mvt[:, 4, :]), reads=[mvt], writes=[sc_])
        ones_bf = P.sb('ones_bf', [128, 128], BF16)
        P.op('vector', lambda e: e.memset(ones_bf[:], 1.0), writes=[ones_bf])
        eps_t = P.sb('eps_t', [128, 1])
        P.op('vector', lambda e: e.memset(eps_t[:], 1e-6), writes=[eps_t])
        hT = P.sb('hT', [128, NKC, T1], BF16)
        norm_phase(P, xT, hT, gl, sl_, gc, sc_, ones_bf, eps_t, T1, 2048)
        osb = [P.sb('osb%d' % i, [128, 512]) for i in range(4)]
        cnt = [0]

        def evac(o0, m, t0, n, ps_):
            o_ = osb[cnt[0] % 4]
            eng = 'vector' if cnt[0] % 2 == 0 else 'scalar'
            cnt[0] += 1
            if eng == 'vector':
                P.op('vector', lambda e: e.tensor_copy(o_[0:m, 0:n], ps_[0:m, 0:n]), reads=[ps_], writes=[o_])
            else:
                P.op('scalar', lambda e: e.copy(o_[0:m, 0:n], ps_[0:m, 0:n]), reads=[ps_], writes=[o_])
            P.dma(AP(pT, o0 * T1 + t0, [[T1, m], [1, n]]), o_[0:m, 0:n], reads=[o_], writes=[], q='gpsimd')

        linear_fm(P, W, 0, IN_DIM, NKC, IN_DIM, lambda kc, t0, n: hT[:, kc, t0:t0 + n], TOKB, evac, lambda kc: ['hT:%d' % kc])
        P.finish()
        nc = P.emit()
    return nc


MIX_IN = 5200
T3 = 1088
TOKB3 = [(0, 512), (512, 512), (1024, 64)]
NLAT3 = 1024
BIG = 1.0e4


class Lin:
    def __init__(self, P, tag, kmax=16, grp=256, nps=2):
        self.P = P
        self.grp = grp
        self.wst = [P.sb('%s_wst%d' % (tag, i), [128, kmax, grp]) for i in range(2)]
        self.wbf = [P.sb('%s_wbf%d' % (tag, i), [128, kmax, grp], BF16) for i in range(2)]
        self.pst = [P.ps('%s_ps%d' % (tag, i), [128, 512]) for i in range(nps)]
        self.nps = nps
        self.g = 0
        self.pi = 0

    def run(self, W, w_off, w_rs, K_chunks, n_out, rhs_fn, tokb, evac_fn, rhs_reads, split=1):
        P = self.P
        grp = self.grp
        ng = (n_out + grp - 1) // grp
        kper = K_chunks // split
        for g in range(ng):
            o0 = g * grp
            gw = min(grp, n_out - o0)
            ws, wb = self.wst[self.g % 2], self.wbf[self.g % 2]
            P.dma(ws[:, 0:K_chunks, 0:gw], AP(W, w_off + o0, [[w_rs, 128], [128 * w_rs, K_chunks], [1, gw]]),
                  reads=[W], writes=[ws])
            self.g += 1
            P.op('scalar', lambda e, ws=ws, wb=wb, gw=gw: e.copy(wb[:, 0:K_chunks, 0:gw], ws[:, 0:K_chunks, 0:gw]),
                 reads=[ws], writes=[wb])
            for m0 in range(0, gw, 128):
                m = min(128, gw - m0)
                for (t0, n) in tokb:
                    tiles = []
                    for sp in range(split):
                        ps_ = self.pst[self.pi % self.nps]
                        self.pi += 1
                        tiles.append(ps_)
                        for kk in range(kper):
                            kc = sp * kper + kk
                            P.op('tensor', lambda e, ps_=ps_, wb=wb, kc=kc, kk=kk, m0=m0, m=m, t0=t0, n=n: e.matmul(
                                ps_[0:m, 0:n], wb[:, kc, m0:m0 + m], rhs_fn(kc, t0, n),
                                start=(kk == 0), stop=(kk == kper - 1)),
                                reads=[wb] + rhs_reads(kc), writes=[ps_])
                    evac_fn(o0 + m0, m, t0, n, tiles if split > 1 else tiles[0])


def build_l3():
    P = Prog()
    with ExitStack() as st:
        P._stack = st
        xT = P.dram('xT', [128, NKC, T3], F32, 'ExternalInput')
        brT = P.dram('brT', [2048, T3], F32, 'ExternalInput')
        gT = P.dram('gT', [8192, T3], F32, 'ExternalInput')
        mv = P.dram('mv', [9, 128, NKC], F32, 'ExternalInput')
        wbr = P.dram('wbr', [2048, D], F32, 'ExternalInput')
        wo = P.dram('wo', [D, D], F32, 'ExternalInput')
        wr = P.dram('wr', [128, NKC, 16], F32, 'ExternalInput')
        rb = P.dram('rb', [16], F32, 'ExternalInput')
        wg = P.dram('wg', [16, D, 512], F32, 'ExternalInput')
        wu = P.dram('wu', [16, D, 512], F32, 'ExternalInput')
        wd = P.dram('wd', [16, 512, D], F32, 'ExternalInput')
        ident_d = P.dram('ident', [128, 128], F32, 'ExternalInput')
        x1T = P.dram('x1T', [128, NKC, T3], F32, 'Internal')
        x2T = P.dram('x2T', [128, NKC, T3], F32, 'ExternalOutput')

        mvt = P.sb('mvt', [128, 9, NKC])
        P.dma(mvt[:], AP(mv, 0, [[NKC, 128], [128 * NKC, 9], [1, NKC]]), reads=[mv], writes=[mvt])
        gl = P.sb('gl', [128, NKC]); gc = P.sb('gc', [128, NKC])
        P.op('vector', lambda e: e.scalar_tensor_tensor(gl[:], mvt[:, 3, :], 1.0, mvt[:, 0, :], ALU.add, ALU.mult),
             reads=[mvt], writes=[gl])
        P.op('vector', lambda e: e.scalar_tensor_tensor(gc[:], mvt[:, 7, :], 1.0, mvt[:, 0, :], ALU.add, ALU.mult),
             reads=[mvt], writes=[gc])
        ones_bf = P.sb('ones_bf', [128, 128], BF16)
        P.op('vector', lambda e: e.memset(ones_bf[:], 1.0), writes=[ones_bf])
        eps_t = P.sb('eps_t', [128, 1])
        P.op('vector', lambda e: e.memset(eps_t[:], 1e-6), writes=[eps_t])
        ident = P.sb('identsb', [128, 128])
        P.dma(ident[:], ident_d.ap(), reads=[ident_d], writes=[ident])
        wrt = P.sb('wrt', [128, NKC, 16])
        P.dma(wrt[:], wr.ap(), reads=[wr], writes=[wrt])
        wrb = P.sb('wrb', [128, NKC, 16], BF16)
        P.op('vector', lambda e: e.tensor_copy(wrb[:], wrt[:]), reads=[wrt], writes=[wrb])
        rbt = P.sb('rbt', [128, 16])
        P.dma(rbt[:], AP(rb, 0, [[0, 128], [1, 16]]), reads=[rb], writes=[rbt])

        lin = Lin(P, 'lin', grp=128, nps=4)
        bigT = P.sb('bigT', [128, NKC, T3], BF16)
        RY = P.sb('RY', [128, NKC, T3])
        mrgT = AP(RY, 0, [[NKC * T3, 128], [1, NKC * T3 * 2]]).bitcast(BF16) if False else RY.bitcast(BF16)
        stg = [P.sb('stg%d' % i, [128, 512]) for i in range(3)]
        si = [0]

        def nxt():
            s = stg[si[0] % 3]
            si[0] += 1
            return s
        for c in range(NKC):
            for (t0, n) in TOKB3:
                s = nxt()
                P.dma(s[:, 0:n], AP(brT, c * 128 * T3 + t0, [[T3, 128], [1, n]]), reads=[brT], writes=[s])
                P.op('gpsimd', lambda e, s=s, c=c, t0=t0, n=n: e.tensor_copy(bigT[:, c, t0:t0 + n], s[:, 0:n]),
                     reads=[s], writes=['bigT:%d' % c])
        gst = [P.sb('gst%d' % i, [128, 4, 512]) for i in range(1)]
        acc = P.sb('macc', [128, 512])
        acc2 = P.sb('macc2', [128, 512])
        gi = [0]

        def evacA(o0, m, t0, n, tiles):
            c = o0 // 128
            g_ = gst[0]
            gi[0] += 1
            P.dma(g_[:, :, 0:n], AP(gT, o0 * T3 + t0, [[T3, 128], [2048 * T3, 4], [1, n]]), reads=[gT], writes=[g_])
            P.op('scalar', lambda e: e.activation(g_[:, :, 0:n], g_[:, :, 0:n], AF.Sigmoid), reads=[g_], writes=[g_])
            P.op('vector', lambda e: e.tensor_tensor(acc[:, 0:n], tiles[0][:, 0:n], g_[:, 0, 0:n], ALU.mult),
                 reads=[tiles[0], g_], writes=[acc])
            for j in range(1, 4):
                P.op('vector', lambda e, j=j: e.tensor_tensor(acc2[:, 0:n], tiles[j][:, 0:n], g_[:, j, 0:n], ALU.mult),
                     reads=[tiles[j], g_], writes=[acc2])
                if j < 3:
                    P.op('gpsimd', lambda e: e.tensor_tensor(acc[:, 0:n], acc[:, 0:n], acc2[:, 0:n], ALU.add),
                         reads=[acc, acc2], writes=[acc])
                else:
                    P.op('gpsimd', lambda e: e.tensor_tensor(mrgT[:, c, t0:t0 + n], acc[:, 0:n], acc2[:, 0:n], ALU.add),
                         reads=[acc, acc2], writes=['mrgT:%d' % c])
        lin.run(wbr, 0, D, 16, D, lambda kc, t0, n: bigT[:, kc, t0:t0 + n], TOKB3, evacA,
                lambda kc: ['bigT:%d' % kc], split=4)

        xo = [P.sb('xo%d' % i, [128, 512]) for i in range(2)]
        bi = [0]

        def evacB(o0, m, t0, n, ps_):
            c = o0 // 128
            s = nxt()
            P.dma(s[:, 0:n], AP(xT, c * T3 + t0, [[NKC * T3, 128], [1, n]]), reads=[xT], writes=[s])
            o_ = xo[bi[0] % 2]
            bi[0] += 1
            gm = mvt[:, 1, c:c + 1] if t0 < NLAT3 else mvt[:, 5, c:c + 1]
            P.op('vector', lambda e: e.scalar_tensor_tensor(o_[:, 0:n], ps_[:, 0:n], gm, s[:, 0:n], ALU.mult, ALU.add),
                 reads=[ps_, s, mvt], writes=[o_])
            P.dma(AP(x1T, c * T3 + t0, [[NKC * T3, 128], [1, n]]), o_[:, 0:n], reads=[o_], writes=['x1T:%d' % (t0 // 512)])
        lin.run(wo, 0, D, 16, D, lambda kc, t0, n: mrgT[:, kc, t0:t0 + n], TOKB3, evacB, lambda kc: ['mrgT:%d' % kc])

        class X1:
            pass
        norm_phase(P, x1T, bigT, gl, AP_slice(mvt, 2), gc, AP_slice(mvt, 6), ones_bf, eps_t, T3, NLAT3,
                   x_reads=['x1T:%d' % i for i in range(3)], out_key='h2T', SUB=64)

        psb = P.ps('psb', [128, 512])
        psr = psb[0:16, :]
        pssm = P.ps('pssm', [128, 128])
        pst_ = pssm[:, 0:16]
        lg = P.sb('lg', [16, 1152])
        P.op('vector', lambda e: e.memset(lg[:], 0.0), writes=[lg])
        gateT = P.sb('gateT', [16, T3], BF16)
        for (t0, n) in TOKB3:
            for kc in range(NKC):
                P.op('tensor', lambda e, kc=kc, t0=t0, n=n: e.matmul(psr[:, 0:n], wrb[:, kc, :], bigT[:, kc, t0:t0 + n],
                                                                    start=(kc == 0), stop=(kc == NKC - 1)),
                     reads=[wrb, 'h2T:%d' % kc], writes=[psr])
            P.op('scalar', lambda e, t0=t0, n=n: e.activation(lg[:, t0:t0 + n], psr[:, 0:n], AF.Sigmoid),
                 reads=[psr], writes=[lg])
        sc = P.sb('r_sc', [128, 16]); sel = P.sb('r_sel', [128, 16]); t4 = P.sb('r_t4', [128, 4]); t4b = P.sb('r_t4b', [128, 4])
        e16 = P.sb('r_e16', [128, 16]); s2 = P.sb('r_s2', [128, 16]); gs = P.sb('r_gs', [128, 4]); t1 = P.sb('r_t1', [128, 1])
        gm_ = P.sb('r_gm', [128, 4]); oh = P.sb('r_oh', [128, 16]); gt = P.sb('r_gt', [128, 16])
        psg = pssm[0:16, :]

        def V(fn, reads, writes):
            P.op('vector', fn, reads=reads, writes=writes)

        def bc4(t):
            return AP(t, 0, [[4, 128], [1, 4], [0, 4]])

        def v44(t):
            return AP(t, 0, [[16, 128], [4, 4], [1, 4]])
        for tt in range((T3 + 127) // 128):
            t0 = tt * 128
            nt = min(128, T3 - t0)
            P.op('tensor', lambda e, t0=t0: e.transpose(pst_[:], lg[:, t0:t0 + 128], ident[0:16, 0:16]),
                 reads=[lg, ident], writes=[pst_])
            V(lambda e: e.tensor_copy(sc[:], pst_[:]), [pst_], [sc])
            V(lambda e: e.tensor_tensor(sel[:], sc[:], rbt[:], ALU.add), [sc, rbt], [sel])
            V(lambda e: e.tensor_reduce(t4[:], v44(sel), AX.X, ALU.max), [sel], [t4])
            V(lambda e: e.tensor_tensor(v44(e16), v44(sel), bc4(t4), ALU.is_equal), [sel, t4], [e16])
            V(lambda e: e.scalar_tensor_tensor(s2[:], e16[:], -BIG, sel[:], ALU.mult, ALU.add), [e16, sel], [s2])
            V(lambda e: e.tensor_reduce(t4b[:], v44(s2), AX.X, ALU.max), [s2], [t4b])
            V(lambda e: e.tensor_tensor(gs[:], t4[:], t4b[:], ALU.add), [t4, t4b], [gs])
            V(lambda e: e.tensor_reduce(t1[:], gs[:], AX.X, ALU.max), [gs], [t1])
            V(lambda e: e.tensor_scalar(gm_[:], gs[:], t1[:, 0:1], None, ALU.is_equal), [gs, t1], [gm_])
            V(lambda e: e.tensor_scalar(gm_[:], gm_[:], -1.0, BIG, ALU.add, ALU.mult), [gm_], [gm_])
            V(lambda e: e.tensor_tensor(v44(s2), v44(sel), bc4(gm_), ALU.add), [sel, gm_], [s2])
            V(lambda e: e.tensor_reduce(t1[:], s2[:], AX.X, ALU.max), [s2], [t1])
            V(lambda e: e.tensor_scalar(oh[:], s2[:], t1[:, 0:1], None, ALU.is_equal), [s2, t1], [oh])
            V(lambda e: e.scalar_tensor_tensor(s2[:], oh[:], -4 * BIG, s2[:], ALU.mult, ALU.add), [oh, s2], [s2])
            V(lambda e: e.tensor_reduce(t1[:], s2[:], AX.X, ALU.max), [s2], [t1])
            V(lambda e: e.scalar_tensor_tensor(oh[:], s2[:], t1[:, 0:1], oh[:], ALU.is_equal, ALU.add), [s2, t1, oh], [oh])
            V(lambda e: e.tensor_tensor(gt[:], sc[:], oh[:], ALU.mult), [sc, oh], [gt])
            V(lambda e: e.tensor_reduce(t1[:], gt[:], AX.X, ALU.add), [gt], [t1])
            V(lambda e: e.reciprocal(t1[:], t1[:]), [t1], [t1])
            V(lambda e: e.tensor_scalar(gt[:], gt[:], t1[:, 0:1], None, ALU.mult), [gt, t1], [gt])
            P.op('tensor', lambda e: e.transpose(psg[:], gt[:], ident[:]), reads=[gt, ident], writes=[psg])
            P.op('scalar', lambda e, t0=t0, nt=nt: e.copy(gateT[:, t0:t0 + nt], psg[:, 0:nt]), reads=[psg], writes=[gateT])
        selm = P.sb('selm', [16, 16, 128], BF16)
        V(lambda e: e.tensor_copy(selm[:], AP(ident, 0, [[128, 16], [1, 16], [0, 128]])), [ident], [selm])

        yacc = RY
        gact = P.sb('gact', [128, 4, T3], BF16)
        actT = P.sb('actT', [128, 4, T3], BF16)
        gb = P.sb('gb', [128, T3])
        tmpm = P.sb('tmpm', [128, 512])
        for h in range(1):
            tokb = TOKB3
            hb = 0
            for ex in range(16):
                for (t0, n) in tokb:
                    P.op('tensor', lambda e, ex=ex, t0=t0, n=n: e.matmul(psb[:, 0:n], selm[:, ex, :], gateT[:, t0:t0 + n],
                                                                        start=True, stop=True),
                         reads=[selm, gateT], writes=[psb])
                    P.op('scalar', lambda e, t0=t0, n=n: e.copy(gb[:, t0 - hb:t0 - hb + n], psb[:, 0:n]),
                         reads=[psb], writes=[gb])

                def evG(o0, m, t0, n, ps_):
                    fc = o0 // 128
                    P.op('scalar', lambda e: e.activation(gact[:, fc, t0 - hb:t0 - hb + n], ps_[:, 0:n], AF.Silu),
                         reads=[ps_], writes=[gact])
                lin.run(wg, ex * D * 512, 512, 16, 512, lambda kc, t0, n: bigT[:, kc, t0:t0 + n], tokb, evG,
                        lambda kc: ['h2T:%d' % kc])

                def evU(o0, m, t0, n, ps_):
                    fc = o0 // 128
                    P.op('vector', lambda e: e.tensor_tensor(tmpm[:, 0:n], ps_[:, 0:n], gact[:, fc, t0 - hb:t0 - hb + n], ALU.mult),
                         reads=[ps_, gact], writes=[tmpm])
                    P.op('gpsimd', lambda e: e.tensor_tensor(actT[:, fc, t0 - hb:t0 - hb + n], tmpm[:, 0:n],
                                                             gb[:, t0 - hb:t0 - hb + n], ALU.mult),
                         reads=[tmpm, gb], writes=[actT])
                lin.run(wu, ex * D * 512, 512, 16, 512, lambda kc, t0, n: bigT[:, kc, t0:t0 + n], tokb, evU,
                        lambda kc: ['h2T:%d' % kc])

                def evD(o0, m, t0, n, ps_, ex=ex):
                    c = o0 // 128
                    if ex == 0:
                        P.op('scalar', lambda e: e.copy(yacc[:, c, t0 - hb:t0 - hb + n], ps_[:, 0:n]),
                             reads=[ps_], writes=['yacc:%d' % c])
                    else:
                        P.op('vector', lambda e: e.tensor_tensor(yacc[:, c, t0 - hb:t0 - hb + n], ps_[:, 0:n],
                                                                 yacc[:, c, t0 - hb:t0 - hb + n], ALU.add),
                             reads=[ps_, 'yacc:%d' % c], writes=['yacc:%d' % c])
                lin.run(wd, ex * 512 * D, D, 4, D, lambda kc, t0, n: actT[:, kc, t0 - hb:t0 - hb + n], tokb, evD,
                        lambda kc: [actT])
            for c in range(NKC):
                for (t0, n) in tokb:
                    s = nxt()
                    P.dma(s[:, 0:n], AP(x1T, c * T3 + t0, [[NKC * T3, 128], [1, n]]), reads=['x1T:%d' % (t0 // 512)], writes=[s])
                    o_ = xo[bi[0] % 2]
                    bi[0] += 1
                    gm = mvt[:, 4, c:c + 1] if t0 < NLAT3 else mvt[:, 8, c:c + 1]
                    P.op('vector', lambda e, s=s, o_=o_, gm=gm, c=c, t0=t0, n=n: e.scalar_tensor_tensor(
                        o_[:, 0:n], yacc[:, c, t0 - hb:t0 - hb + n], gm, s[:, 0:n], ALU.mult, ALU.add),
                        reads=['yacc:%d' % c, s, mvt], writes=[o_])
                    P.dma(AP(x2T, c * T3 + t0, [[NKC * T3, 128], [1, n]]), o_[:, 0:n], reads=[o_], writes=[])
        P.finish()
        nc = P.emit()
    return nc


DEBUG = False
TA = 4352
NKT = 34


def build_l2a(with_ctx=True):
    P = Prog()
    with ExitStack() as st:
        P._stack = st
        gq = P.dram('gq', [256, TA], F32, 'ExternalInput')
        gk = P.dram('gk', [64, TA], F32, 'ExternalInput')
        gv = P.dram('gv', [TA, 64], F32, 'ExternalInput')
        cq = P.dram('cq', [512, TA], F32, 'ExternalInput')
        ckv = P.dram('ckv', [256, TA], F32, 'ExternalInput')
        kr = P.dram('kr', [64, TA], F32, 'ExternalInput')
        wuq = P.dram('wuq', [512, 384], F32, 'ExternalInput')
        wukv = P.dram('wukv', [256, 512], F32, 'ExternalInput')
        nw = P.dram('nw', [128, 8], F32, 'ExternalInput')
        cosd = P.dram('cosd', [64, TA], F32, 'ExternalInput')
        sind = P.dram('sind', [64, TA], F32, 'ExternalInput')
        rmd = P.dram('rmd', [64, 64], F32, 'ExternalInput')
        og = P.dram('og', [TA, 256], F32, 'ExternalOutput')
        om = P.dram('om', [TA, 256], F32, 'ExternalOutput')

        pp = [P.ps('pp%d' % i, [128, 512]) for i in range(8)]
        ones = P.sb('ones', [128, 128])
        P.op('vector', lambda e: e.memset(ones[:], 1.0), writes=[ones])
        nwt = P.sb('nwt', [128, 8])
        P.dma(nwt[:], nw.ap(), reads=[nw], writes=[nwt])
        rm = P.sb('rm', [64, 64])
        P.dma(rm[:], rmd.ap(), reads=[rmd], writes=[rm])
        eps64 = P.sb('eps64', [128, 1])
        P.op('vector', lambda e: e.memset(eps64[:], 1e-6), writes=[eps64])
        wq_s = P.sb('wq_s', [128, 4, 384]); wq_b = P.sb('wq_b', [128, 4, 384], BF16)
        wqr_b = P.sb('wqr_b', [128, 4, 128], BF16)
        wkv_s = P.sb('wkv_s', [128, 2, 512]); wkv_b = P.sb('wkv_b', [128, 2, 512], BF16)
        P.dma(wq_s[:], AP(wuq, 0, [[384, 128], [128 * 384, 4], [1, 384]]), reads=[wuq], writes=[wq_s])
        P.dma(wkv_s[:], AP(wukv, 0, [[512, 128], [128 * 512, 2], [1, 512]]), reads=[wukv], writes=[wkv_s])
        P.op('vector', lambda e: e.tensor_copy(wq_b[:], wq_s[:]), reads=[wq_s], writes=[wq_b])
        P.op('vector', lambda e: e.tensor_copy(wkv_b[:], wkv_s[:]), reads=[wkv_s], writes=[wkv_b])
        for h in range(2):
            P.op('vector', lambda e, h=h: e.tensor_copy(wqr_b[:, :, h * 64:h * 64 + 32], wq_s[:, :, h * 192 + 160:h * 192 + 192]),
                 reads=[wq_s], writes=[wqr_b])
            P.op('vector', lambda e, h=h: e.tensor_copy(wqr_b[:, :, h * 64 + 32:h * 64 + 64], wq_s[:, :, h * 192 + 128:h * 192 + 160]),
                 reads=[wq_s], writes=[wqr_b])

        GQ = P.sb('GQ', [64, 4, TA], BF16)
        GK = P.sb('GK', [64, TA], BF16)
        GV = P.sb('GV', [128, NKT, 65], BF16)
        MQN = P.sb('MQN', [128, 2, TA], BF16)
        MQR = P.sb('MQR', [64, 2, TA], BF16)
        MKN = P.sb('MKN', [128, 2, TA], BF16)
        MKR = P.sb('MKR', [64, TA], BF16)
        MV = P.sb('MV', [128, NKT, 2, 129], BF16)
        P.op('vector', lambda e: e.memset(GV[:], 1.0), writes=[GV])
        P.op('vector', lambda e: e.memset(MV[:], 1.0), writes=[MV])
        gvs = P.sb('gvs', [128, NKT, 64])
        P.dma(gvs[:], AP(gv, 0, [[64, 128], [128 * 64, NKT], [1, 64]]), reads=[gv], writes=[gvs])
        P.op('vector', lambda e: e.tensor_copy(GV[:, :, 0:64], gvs[:]), reads=[gvs], writes=[GV])

        xs = [P.sb('xa%d' % i, [128, 4, 512]) for i in range(2)]
        sq = P.sb('sqa', [128, 4, 512])
        rs = P.sb('rsa', [128, 512])
        xn = P.sb('xna', [128, 512])
        xnb = P.sb('xnb', [128, 4, 512], BF16)
        xnk = P.sb('xnk', [128, 2, 512], BF16)
        cs = P.sb('csa', [64, 512]); sn = P.sb('sna', [64, 512])
        t1 = P.sb('t1a', [128, 512]); t2 = P.sb('t2a', [128, 512])
        ppi = [0]
        fence = P.sb('fence', [128, 8], BF16)

        def nps():
            p_ = pp[ppi[0] % 8]
            ppi[0] += 1
            return p_

        def rstd_of(x_, rows, nch, n, dim):
            P.op('scalar', lambda e: e.activation(sq[0:rows, 0:nch, 0:n], x_[0:rows, 0:nch, 0:n], AF.Square), reads=[x_], writes=[sq])
            ps_ = nps()
            for c in range(nch):
                P.op('tensor', lambda e, c=c: e.matmul(ps_[0:rows, 0:n], ones[0:rows, 0:rows], sq[0:rows, c, 0:n],
                                                       start=(c == 0), stop=(c == nch - 1)), reads=[ones, sq], writes=[ps_])
            P.op('scalar', lambda e: e.activation(rs[0:rows, 0:n], ps_[0:rows, 0:n], AF.Sqrt, bias=eps64[0:rows, :], scale=1.0 / dim),
                 reads=[ps_, eps64], writes=[rs])
            P.op('vector', lambda e: e.reciprocal(rs[0:rows, 0:n], rs[0:rows, 0:n]), reads=[rs], writes=[rs])

        def rope_to(dst, src, n):
            ps_ = nps()
            P.op('tensor', lambda e: e.matmul(ps_[0:64, 0:n], rm[:], src, start=True, stop=True), reads=[rm, src], writes=[ps_])
            P.op('vector', lambda e: e.tensor_tensor(t1[0:64, 0:n], ps_[0:64, 0:n], sn[:, 0:n], ALU.mult), reads=[ps_, sn], writes=[t1])
            P.op('gpsimd', lambda e: e.tensor_tensor(t2[0:64, 0:n], src, cs[:, 0:n], ALU.mult), reads=[src, cs], writes=[t2])
            P.op('vector', lambda e: e.tensor_tensor(dst, t1[0:64, 0:n], t2[0:64, 0:n], ALU.add), reads=[t1, t2], writes=[dst])

        for blk in range(9):
            t0 = blk * 512
            n = min(512, TA - t0)
            P.dma(cs[:, 0:n], AP(cosd, t0, [[TA, 64], [1, n]]), reads=[cosd], writes=[cs])
            P.dma(sn[:, 0:n], AP(sind, t0, [[TA, 64], [1, n]]), reads=[sind], writes=[sn])
            x_ = xs[0]
            P.dma(x_[0:64, :, 0:n], AP(gq, t0, [[TA, 64], [64 * TA, 4], [1, n]]), reads=[gq], writes=[x_])
            for h in range(4):
                P.op('scalar', lambda e, h=h: e.activation(sq[0:64, 0, 0:n], x_[0:64, h, 0:n], AF.Square), reads=[x_], writes=[sq])
                ps_ = nps()
                P.op('tensor', lambda e, ps_=ps_: e.matmul(ps_[0:64, 0:n], ones[0:64, 0:64], sq[0:64, 0, 0:n], start=True, stop=True),
                     reads=[ones, sq], writes=[ps_])
                P.op('scalar', lambda e, ps_=ps_: e.activation(rs[0:64, 0:n], ps_[0:64, 0:n], AF.Sqrt, bias=eps64[0:64, :], scale=1.0 / 64),
                     reads=[ps_, eps64], writes=[rs])
                P.op('vector', lambda e: e.reciprocal(rs[0:64, 0:n], rs[0:64, 0:n]), reads=[rs], writes=[rs])
                P.op('vector', lambda e, h=h: e.scalar_tensor_tensor(xn[0:64, 0:n], x_[0:64, h, 0:n], nwt[0:64, 6:7], rs[0:64, 0:n],
                                                                     ALU.mult, ALU.mult), reads=[x_, nwt, rs], writes=[xn])
                rope_to(GQ[:, h, t0:t0 + n], xn[0:64, 0:n], n)
            x2 = xs[1]
            P.dma(x2[0:64, 0, 0:n], AP(gk, t0, [[TA, 64], [1, n]]), reads=[gk], writes=[x2])
            rstd_of(x2, 64, 1, n, 64)
            P.op('vector', lambda e: e.scalar_tensor_tensor(xn[0:64, 0:n], x2[0:64, 0, 0:n], nwt[0:64, 7:8], rs[0:64, 0:n],
                                                            ALU.mult, ALU.mult), reads=[x2, nwt, rs], writes=[xn])
            rope_to(GK[:, t0:t0 + n], xn[0:64, 0:n], n)
            P.dma(x2[0:64, 1, 0:n], AP(kr, t0, [[TA, 64], [1, n]]), reads=[kr], writes=[x2])
            rope_to(MKR[:, t0:t0 + n], x2[0:64, 1, 0:n], n)
            P.dma(x_[:, :, 0:n], AP(cq, t0, [[TA, 128], [128 * TA, 4], [1, n]]), reads=[cq], writes=[x_])
            rstd_of(x_, 128, 4, n, 512)
            for c in range(4):
                P.op('vector', lambda e, c=c: e.scalar_tensor_tensor(xnb[:, c, 0:n], x_[:, c, 0:n], nwt[:, c:c + 1], rs[:, 0:n],
                                                                     ALU.mult, ALU.mult), reads=[x_, nwt, rs], writes=[xnb])
            P.op('vector', lambda e: e.drain(), reads=[xnb], writes=[xnb])
            if DEBUG and blk == 0:
                d1 = P.dram('d_xnb', [128, 4 * 512], BF16, 'ExternalOutput'); d2 = P.dram('d_rs', [128, 512], F32, 'ExternalOutput'); d3 = P.dram('d_x', [128, 4*512], F32, 'ExternalOutput')
                P.dma(d1.ap(), xnb[:], reads=[xnb], writes=[]); P.dma(d2.ap(), rs[:], reads=[rs], writes=[]); P.dma(d3.ap(), x_[:], reads=[x_], writes=[])
            for h in (0, 1):
                ps_ = nps()
                for c in range(4):
                    P.op('tensor', lambda e, ps_=ps_, c=c, h=h: e.matmul(ps_[:, 0:n], wq_b[:, c, h * 192:h * 192 + 128], xnb[:, c, 0:n],
                                                                         start=(c == 0), stop=(c == 3)), reads=[wq_b, xnb], writes=[ps_])
                P.op('vector', lambda e, t0=t0, ps_=ps_, h=h: e.tensor_copy(MQN[:, h, t0:t0 + n], ps_[:, 0:n]), reads=[ps_], writes=[MQN])
                pa = nps(); pb = nps()
                for c in range(4):
                    P.op('tensor', lambda e, pa=pa, c=c, h=h: e.matmul(pa[0:64, 0:n], wq_b[:, c, h * 192 + 128:h * 192 + 192], xnb[:, c, 0:n],
                                                                       start=(c == 0), stop=(c == 3)), reads=[wq_b, xnb], writes=[pa])
                for c in range(4):
                    P.op('tensor', lambda e, pb=pb, c=c, h=h: e.matmul(pb[0:64, 0:n], wqr_b[:, c, h * 64:h * 64 + 64], xnb[:, c, 0:n],
                                                                       start=(c == 0), stop=(c == 3)), reads=[wqr_b, xnb], writes=[pb])
                P.op('vector', lambda e, pa=pa: e.tensor_tensor(t1[0:64, 0:n], pa[0:64, 0:n], cs[:, 0:n], ALU.mult), reads=[pa, cs], writes=[t1])
                P.op('vector', lambda e, pb=pb: e.tensor_tensor(t2[0:64, 0:n], pb[0:64, 0:n], sn[:, 0:n], ALU.mult), reads=[pb, sn], writes=[t2])
                P.op('gpsimd', lambda e, t0=t0, h=h: e.tensor_tensor(MQR[:, h, t0:t0 + n], t1[0:64, 0:n], t2[0:64, 0:n], ALU.add), reads=[t1, t2], writes=[MQR])
            P.dma(x2[:, 2:4, 0:n], AP(ckv, t0, [[TA, 128], [128 * TA, 2], [1, n]]), reads=[ckv], writes=[x2])
            P.op('scalar', lambda e: e.activation(sq[:, 0:2, 0:n], x2[:, 2:4, 0:n], AF.Square), reads=[x2], writes=[sq])
            ps_ = nps()
            for c in range(2):
                P.op('tensor', lambda e, c=c, ps_=ps_: e.matmul(ps_[:, 0:n], ones[:], sq[:, c, 0:n], start=(c == 0), stop=(c == 1)),
                     reads=[ones, sq], writes=[ps_])
            P.op('scalar', lambda e, ps_=ps_: e.activation(rs[:, 0:n], ps_[:, 0:n], AF.Sqrt, bias=eps64[:], scale=1.0 / 256),
                 reads=[ps_, eps64], writes=[rs])
            P.op('vector', lambda e: e.reciprocal(rs[:, 0:n], rs[:, 0:n]), reads=[rs], writes=[rs])
            for c in range(2):
                P.op('vector', lambda e, c=c: e.scalar_tensor_tensor(xnk[:, c, 0:n], x2[:, 2 + c, 0:n], nwt[:, 4 + c:5 + c], rs[:, 0:n],
                                                                     ALU.mult, ALU.mult), reads=[x2, nwt, rs], writes=[xnk])
            P.op('vector', lambda e: e.drain(), reads=[xnk], writes=[xnk])
            for h in range(2):
                ps_ = nps()
                for c in range(2):
                    P.op('tensor', lambda e, ps_=ps_, c=c, h=h: e.matmul(ps_[:, 0:n], wkv_b[:, c, h * 256:h * 256 + 128], xnk[:, c, 0:n],
                                                                         start=(c == 0), stop=(c == 1)), reads=[wkv_b, xnk], writes=[ps_])
                P.op('scalar', lambda e, t0=t0, ps_=ps_, h=h: e.copy(MKN[:, h, t0:t0 + n], ps_[:, 0:n]), reads=[ps_], writes=[MKN])
                for j in range(n // 128):
                    ps2 = nps()
                    kt = (t0 + j * 128) // 128
                    for c in range(2):
                        P.op('tensor', lambda e, ps2=ps2, c=c, h=h, j=j: e.matmul(ps2[:, 0:128], xnk[:, c, j * 128:(j + 1) * 128],
                                                                                 wkv_b[:, c, h * 256 + 128:h * 256 + 256],
                                                                                 start=(c == 0), stop=(c == 1)), reads=[wkv_b, xnk], writes=[ps2])
                    P.op('vector', lambda e, ps2=ps2, h=h, kt=kt: e.tensor_copy(MV[:, kt, h, 0:128], ps2[:, 0:128]), reads=[ps2], writes=[MV])

        pT = [P.sb('pT%d' % i, [128, 512], BF16) for i in range(4)]
        osb = [P.sb('osb%d' % i, [128, 128]) for i in range(2)]
        rec = P.sb('rec', [128, 1])
        cnt = [0]
        DEPTH = 2

        def attn(qparts, kparts, vfn, dv, scale, t0, n, kts, out, ocol, ow):
            sps = [pp[0], pp[1], pp[6], pp[7]]
            ops_ = pp[2:6]
            nq = n // 128
            bufs = []
            nk = len(kts)
            for step in range(nk + DEPTH):
                if step < nk:
                    kt = kts[step]
                    s_ = sps[cnt[0] % 4]
                    p_ = pT[cnt[0] % 4]
                    cnt[0] += 1
                    bufs.append(p_)
                    for i, (qf, kf) in enumerate(zip(qparts, kparts)):
                        P.op('tensor', lambda e, s_=s_, qf=qf, kf=kf, i=i, kt=kt: e.matmul(
                            s_[:, 0:n], kf(kt), qf(t0, n), start=(i == 0), stop=(i == len(qparts) - 1)),
                            reads=[kf(kt), qf(t0, n)], writes=[s_])
                    P.op('scalar', lambda e, s_=s_, p_=p_: e.activation(p_[:, 0:n], s_[:, 0:n], AF.Exp, scale=scale), reads=[s_], writes=[p_])
                if step >= DEPTH:
                    ki = step - DEPTH
                    kt = kts[ki]
                    p_ = bufs[ki]
                    for qs in range(nq):
                        P.op('tensor', lambda e, p_=p_, qs=qs, kt=kt, ki=ki: e.matmul(
                            ops_[qs][:, 0:dv + 1], p_[:, qs * 128:(qs + 1) * 128], vfn(kt), start=(ki == 0), stop=(ki == nk - 1)),
                            reads=[p_, vfn(kt)], writes=[ops_[qs]])
            for qs in range(nq):
                o_ = osb[qs % 2]
                P.op('vector', lambda e, qs=qs: e.reciprocal(rec[:], ops_[qs][:, dv:dv + 1]), reads=[ops_[qs]], writes=[rec])
                P.op('vector', lambda e, qs=qs, o_=o_: e.tensor_scalar(o_[:, 0:dv], ops_[qs][:, 0:dv], rec[:, 0:1], None, ALU.mult),
                     reads=[ops_[qs], rec], writes=[o_])
                P.dma(AP(out, (t0 + qs * 128) * ow + ocol, [[ow, 128], [1, dv]]), o_[:, 0:dv], reads=[o_], writes=[])

        all_k = list(range(NKT))
        ctx_k = [32, 33]
        qblocks = [(i * 512, 512, all_k) for i in range(8)]
        if with_ctx:
            qblocks.append((4096, 256, ctx_k))
        for (t0, n, kts) in qblocks:
            for h in range(4):
                attn([lambda a, b, h=h: GQ[:, h, a:a + b]], [lambda kt: GK[:, kt * 128:(kt + 1) * 128]],
                     lambda kt: GV[:, kt, :], 64, 0.125, t0, n, kts, og, h * 64, 256)
            for h in range(2):
                attn([lambda a, b, h=h: MQN[:, h, a:a + b], lambda a, b, h=h: MQR[:, h, a:a + b]],
                     [lambda kt, h=h: MKN[:, h, kt * 128:(kt + 1) * 128], lambda kt: MKR[:, kt * 128:(kt + 1) * 128]],
                     lambda kt, h=h: MV[:, kt, h, :], 128, 192 ** -0.5, t0, n, kts, om, h * 128, 256)
        if DEBUG:
            for nm, t_, shp in [('d_GQ', GQ, [64, 4 * TA]), ('d_GK', GK, [64, TA]), ('d_MQR', MQR, [64, 2 * TA]), ('d_MKR', MKR, [64, TA]), ('d_MQN', MQN, [128, 2 * TA]), ('d_MKN', MKN, [128, 2*TA])]:
                dd = P.dram(nm, shp, BF16, 'ExternalOutput')
                P.dma(dd.ap(), t_[:], reads=[t_], writes=[])
        P.finish()
        nc = P.emit()
    return nc


HC = 64
TWO_PI = 2.0 * math.pi


def build_l2h():
    P = Prog()
    with ExitStack() as st:
        P._stack = st
        seqs = []
        for nm, L in (('L', 4096), ('C', 256)):
            nb = L // 128
            seqs.append(dict(
                nm=nm, L=L, nb=nb, NQ=128 * (2 * nb - 1), HPLEN=256 * nb, pad=L // 2 - 1,
                U=P.dram('U' + nm, [3, 3, 128, HC * 4 * nb], F32, 'ExternalInput'),
                feats=P.dram('feats' + nm, [33, L], F32, 'ExternalInput'),
                dec=P.dram('dec' + nm, [2, HC, L], F32, 'ExternalInput'),
                hp=P.dram('hp' + nm, [2, HC, 256 * nb], BF16, 'Internal'),
                out=P.dram('y' + nm, [128, HC * 4 * nb], F32, 'ExternalOutput')))
        cw = P.dram('cw', [3, 3, HC], F32, 'ExternalInput')
        w1 = P.dram('w1', [33, 64], F32, 'ExternalInput')
        w2 = P.dram('w2', [64, 64], F32, 'ExternalInput')
        w3 = P.dram('w3', [64, 2, HC], F32, 'ExternalInput')
        pv = P.dram('pv', [64, 4], F32, 'ExternalInput')
        hb = P.dram('hb', [2, HC], F32, 'ExternalInput')
        jm = P.dram('jm', [128, 128], F32, 'ExternalInput')

        F = 8192
        A1 = P.sb('A1', [128, F]); A2 = P.sb('A2', [128, F]); vN = P.sb('vN', [128, F]); xN = P.sb('xN', [128, F])
        zR = P.sb('zR', [128, F], BF16)
        toep = [P.sb('toep%d' % i, [128, 8064], BF16) for i in range(2)]
        wbc = P.sb('wbc', [128, 9 * HC]); hbc = P.sb('hbc', [128, 2 * HC])
        P.dma(wbc[:], AP(cw, 0, [[0, 128], [1, 9 * HC]]), reads=[cw], writes=[wbc])
        P.dma(hbc[:], AP(hb, 0, [[0, 128], [1, 2 * HC]]), reads=[hb], writes=[hbc])
        w1t = P.sb('w1t', [33, 64]); w2t = P.sb('w2t', [64, 64]); w3t = P.sb('w3t', [64, 2 * HC]); pvt = P.sb('pvt', [64, 4])
        jt = P.sb('jt', [128, 128])
        P.dma(w1t[:], w1.ap(), reads=[w1], writes=[w1t]); P.dma(w2t[:], w2.ap(), reads=[w2], writes=[w2t])
        P.dma(w3t[:], AP(w3, 0, [[2 * HC, 64], [1, 2 * HC]]), reads=[w3], writes=[w3t]); P.dma(pvt[:], pv.ap(), reads=[pv], writes=[pvt])
        P.dma(jt[:], jm.ap(), reads=[jm], writes=[jt])
        zero_bf = P.sb('zero_bf', [64, 2304], BF16)
        P.op('vector', lambda e: e.memset(zero_bf[:], 0.0), writes=[zero_bf])
        pp = [P.ps('pp%d' % i, [128, 512]) for i in range(6)]
        ppi = [0]

        def nps():
            p_ = pp[ppi[0] % 6]
            ppi[0] += 1
            return p_
        ft = [P.sb('ft%d' % i, [33, 256]) for i in range(2)]
        dct = [P.sb('dct%d' % i, [64, 2, 256]) for i in range(2)]
        arg = P.sb('arg', [64, 256]); ki = P.sb('ki', [64, 256], I32); kf = P.sb('kf', [64, 256])
        rsum = P.sb('rsum', [64, 2])

        def sin_to(dst, ps_, bcol, fcol):
            P.op('vector', lambda e: e.tensor_scalar(arg[:], ps_, pvt[:, bcol:bcol + 1], pvt[:, fcol:fcol + 1], ALU.add, ALU.mult),
                 reads=[ps_, pvt], writes=[arg])
            P.op('vector', lambda e: e.tensor_scalar(ki[:], arg[:], 1.0 / TWO_PI, None, ALU.mult), reads=[arg], writes=[ki])
            P.op('vector', lambda e: e.tensor_copy(kf[:], ki[:]), reads=[ki], writes=[kf])
            P.op('vector', lambda e: e.scalar_tensor_tensor(arg[:], kf[:], -TWO_PI, arg[:], ALU.mult, ALU.add), reads=[kf, arg], writes=[arg])
            P.op('vector', lambda e: e.tensor_scalar(kf[:], arg[:], math.pi, -TWO_PI, ALU.is_gt, ALU.mult), reads=[arg], writes=[kf])
            P.op('vector', lambda e: e.tensor_tensor(arg[:], arg[:], kf[:], ALU.add), reads=[arg, kf], writes=[arg])
            P.op('vector', lambda e: e.tensor_scalar(kf[:], arg[:], -math.pi, TWO_PI, ALU.is_lt, ALU.mult), reads=[arg], writes=[kf])
            P.op('vector', lambda e: e.tensor_tensor(arg[:], arg[:], kf[:], ALU.add), reads=[arg, kf], writes=[arg])
            P.op('vector', lambda e: e.tensor_scalar(arg[:], arg[:], 3.14159, -3.14159, ALU.min, ALU.max), reads=[arg], writes=[arg])
            P.op('scalar', lambda e: e.activation(dst, arg[:], AF.Sin), reads=[arg], writes=[dst])

        def filter_gen(S):
            L, hp, pad, HPLEN = S['L'], S['hp'], S['pad'], S['HPLEN']
            hid1 = A1[0:64, 0:L]; hid2 = A1[0:64, 4096:4096 + L]
            fr = [A2[0:64, 0:L], A2[0:64, 4096:4096 + L]]
            for bi, t0 in enumerate(range(0, L, 256)):
                f_ = ft[bi % 2]; d_ = dct[bi % 2]
                P.dma(f_[:], AP(S['feats'], t0, [[L, 33], [1, 256]]), reads=[S['feats']], writes=[f_])
                P.dma(d_[:], AP(S['dec'], t0, [[L, 64], [HC * L, 2], [1, 256]]), reads=[S['dec']], writes=[d_])
                ps_ = nps()
                P.op('tensor', lambda e, ps_=ps_, f_=f_: e.matmul(ps_[0:64, 0:256], w1t[:], f_[:], start=True, stop=True),
                     reads=[w1t, f_], writes=[ps_])
                sin_to(hid1[:, t0:t0 + 256], ps_[0:64, 0:256], 0, 2)
                ps_ = nps()
                P.op('tensor', lambda e, ps_=ps_, t0=t0: e.matmul(ps_[0:64, 0:256], w2t[:], hid1[:, t0:t0 + 256], start=True, stop=True),
                     reads=[w2t, A1], writes=[ps_])
                sin_to(hid2[:, t0:t0 + 256], ps_[0:64, 0:256], 1, 3)
                for o in range(2):
                    ps_ = nps()
                    P.op('tensor', lambda e, ps_=ps_, t0=t0, o=o: e.matmul(ps_[0:64, 0:256], w3t[:, o * HC:(o + 1) * HC], hid2[:, t0:t0 + 256],
                                                                          start=True, stop=True), reads=[w3t, A1], writes=[ps_])
                    P.op('vector', lambda e, ps_=ps_, t0=t0, o=o, d_=d_: e.tensor_tensor(fr[o][:, t0:t0 + 256], ps_[0:64, 0:256], d_[:, o, :], ALU.mult),
                         reads=[ps_, d_], writes=[A2])
            for o in range(2):
                P.op('vector', lambda e, o=o: e.tensor_reduce(rsum[:, o:o + 1], fr[o], AX.X, ALU.add, apply_absolute_value=True),
                     reads=[A2], writes=[rsum])
            P.op('vector', lambda e: e.reciprocal(rsum[:], rsum[:]), reads=[rsum], writes=[rsum])
            for o in range(2):
                fb = zR[0:64, o * 4096:o * 4096 + L]
                P.op('vector', lambda e, o=o, fb=fb: e.tensor_scalar(fb, fr[o], rsum[:, o:o + 1], None, ALU.mult), reads=[A2, rsum], writes=[zR])
                P.dma(AP(hp, o * HC * HPLEN + pad, [[HPLEN, 64], [1, L]]), fb, reads=[zR], writes=[hp])
                n1 = pad
                n2 = HPLEN - pad - L
                P.dma(AP(hp, o * HC * HPLEN, [[HPLEN, 64], [1, n1]]), zero_bf[:, 0:n1], reads=[zero_bf], writes=[hp])
                P.dma(AP(hp, o * HC * HPLEN + pad + L, [[HPLEN, 64], [1, n2]]), zero_bf[:, 0:n2], reads=[zero_bf], writes=[hp])

        def conv3(S, g, dest):
            n = HC * 4 * S['nb']
            for k in range(3):
                P.dma(A1[:, 0:n], AP(S['U'], (g * 3 + k) * 128 * n, [[n, 128], [1, n]]), reads=[S['U']], writes=[A1])
                wv = AP(wbc, (k * 3 + g) * HC, [[9 * HC, 128], [1, HC], [0, 4 * S['nb']]])
                a1v = AP(A1, 0, [[F, 128], [4 * S['nb'], HC], [1, 4 * S['nb']]])
                dv = AP(dest, 0, [[F, 128], [4 * S['nb'], HC], [1, 4 * S['nb']]])
                if k == 0:
                    P.op('vector', lambda e, a1v=a1v, wv=wv, dv=dv: e.tensor_tensor(dv, a1v, wv, ALU.mult), reads=[A1, wbc], writes=[dest])
                else:
                    P.op('vector', lambda e, a1v=a1v, wv=wv: e.tensor_tensor(a1v, a1v, wv, ALU.mult), reads=[A1, wbc], writes=[A1])
                    P.op('gpsimd', lambda e, n=n: e.tensor_tensor(dest[:, 0:n], dest[:, 0:n], A1[:, 0:n], ALU.add), reads=[dest, A1], writes=[dest])

        def reverse_to_zR(S, src):
            n = HC * 4 * S['nb']
            for i, c0 in enumerate(range(0, n, 256)):
                ps_ = nps()
                P.op('tensor', lambda e, ps_=ps_, c0=c0: e.matmul(ps_[:, 0:256], jt[:], src[:, c0:c0 + 256], start=True, stop=True),
                     reads=[jt, src], writes=[ps_])
                if i % 2 == 0:
                    P.op('vector', lambda e, ps_=ps_, c0=c0: e.tensor_copy(zR[:, c0:c0 + 256], ps_[:, 0:256]), reads=[ps_], writes=[zR])
                else:
                    P.op('scalar', lambda e, ps_=ps_, c0=c0: e.copy(zR[:, c0:c0 + 256], ps_[:, 0:256]), reads=[ps_], writes=[zR])

        tcnt = [0]

        def longconv(S, o):
            nb, NQ, HPLEN = S['nb'], S['NQ'], S['HPLEN']
            for c in range(HC):
                tp = toep[tcnt[0] % 2]
                q = 'sync' if tcnt[0] % 2 == 0 else 'gpsimd'
                tcnt[0] += 1
                P.dma(tp[:, 0:NQ], AP(S['hp'], (o * HC + c) * HPLEN, [[1, 128], [1, NQ]]), reads=[S['hp']], writes=[tp], q=q)
                ps_ = nps()
                ms = [0] + [m for m in range(-(nb - 1), nb) if m != 0]
                for mi, m in enumerate(ms):
                    a_lo, a_hi = max(0, m), min(nb, nb + m)
                    cnt = a_hi - a_lo
                    outv = AP(ps_, a_lo, [[512, 128], [nb, 4], [1, cnt]])
                    rhs = AP(zR, c * 4 * nb + (a_lo - m), [[F, 128], [nb, 4], [1, cnt]])
                    P.op('tensor', lambda e, outv=outv, rhs=rhs, tp=tp, m=m, mi=mi: e.matmul(
                        outv, tp[:, 128 * (m + nb - 1):128 * (m + nb)], rhs, start=(mi == 0), stop=(mi == len(ms) - 1), skip_group_check=True),
                        reads=[tp, zR], writes=[ps_])
                dst = A2[:, c * 4 * nb:(c + 1) * 4 * nb]
                if c % 2 == 0:
                    P.op('vector', lambda e, ps_=ps_, dst=dst: e.tensor_copy(dst, ps_[:, 0:4 * nb]), reads=[ps_], writes=[A2])
                else:
                    P.op('scalar', lambda e, ps_=ps_, dst=dst: e.copy(dst, ps_[:, 0:4 * nb]), reads=[ps_], writes=[A2])

        def gate(S, o, z, x):
            nb = S['nb']
            n = HC * 4 * nb
            bv = AP(hbc, o * HC, [[2 * HC, 128], [1, HC], [0, 4 * nb]])
            zv = AP(z, 0, [[F, 128], [4 * nb, HC], [1, 4 * nb]])
            P.op('vector', lambda e: e.tensor_tensor(zv, zv, bv, ALU.mult), reads=[z, hbc], writes=[z])
            P.op('gpsimd', lambda e: e.tensor_tensor(z[:, 0:n], z[:, 0:n], A2[:, 0:n], ALU.add), reads=[z, A2], writes=[z])
            P.op('vector', lambda e: e.tensor_tensor(z[:, 0:n], z[:, 0:n], x[:, 0:n], ALU.mult), reads=[z, x], writes=[z])

        for S in seqs:
            filter_gen(S)
            n = HC * 4 * S['nb']
            conv3(S, 0, vN)
            conv3(S, 1, xN)
            reverse_to_zR(S, vN)
            longconv(S, 0)
            gate(S, 0, vN, xN)
            conv3(S, 2, xN)
            reverse_to_zR(S, vN)
            longconv(S, 1)
            gate(S, 1, vN, xN)
            P.dma(S['out'].ap(), vN[:, 0:n], reads=[vN], writes=[])
        P.finish()
        nc = P.emit()
    return nc


def hy_consts(L):
    t = np.arange(L, dtype=np.float32)
    bands = 16
    f = np.linspace(1e-4, bands - 1, bands, dtype=np.float32)
    phase = (np.float32(2.0 * math.pi / L) * t[:, None] * f[None, :]).astype(np.float32)
    feats = np.concatenate([t[:, None] / np.float32(L - 1), np.cos(phase), -np.sin(phase)], -1).astype(np.float32)
    centre = L // 2
    dist = (np.abs(t - centre) / centre).astype(np.float32)
    deltas = np.abs(np.linspace(math.log(1e-2) / 1.5, math.log(1e-2) / 0.3, 1024, dtype=np.float32))
    dec = np.exp(-dist[:, None] * deltas[None, :]).astype(np.float32)
    return np.ascontiguousarray(feats.T), dec


def hy_inputs(core, pl_h, pc_h, inp, L_):
    c0 = core * HC
    d = {}
    for nm, u, L in (('L', pl_h, 4096), ('C', pc_h, 256)):
        nb = L // 128
        feats, dec = hy_consts(L)
        d['feats' + nm] = feats
        d['dec' + nm] = np.ascontiguousarray(dec.reshape(L, 2, 512)[:, :, c0:c0 + HC].transpose(1, 2, 0))
        ug = u.reshape(4, L, 3, 512)[:, :, :, c0:c0 + HC]
        up = np.pad(ug, ((0, 0), (1, 1), (0, 0), (0, 0)))
        U = np.empty((3, 3, 128, HC, 4, nb), np.float32)
        for k in range(3):
            s = up[:, k:k + L]
            U[:, k] = s.reshape(4, nb, 128, 3, HC).transpose(3, 2, 4, 0, 1)
        d['U' + nm] = U.reshape(3, 3, 128, HC * 4 * nb)
    d['cw'] = np.ascontiguousarray(inp['hy_conv_w'][L_].reshape(3, 3, 512)[:, :, c0:c0 + HC])
    d['w1'] = inp['hy_w1'][L_]; d['w2'] = inp['hy_w2'][L_]
    d['w3'] = np.ascontiguousarray(inp['hy_w3'][L_].reshape(64, 2, 512)[:, :, c0:c0 + HC])
    d['pv'] = np.ascontiguousarray(np.stack([inp['hy_b1'][L_], inp['hy_b2'][L_], inp['hy_sin_freq'][L_][0], inp['hy_sin_freq'][L_][1]], 1))
    d['hb'] = np.ascontiguousarray(inp['hy_bias'][L_][:, c0:c0 + HC])
    d['jm'] = np.ascontiguousarray(np.eye(128, dtype=np.float32)[::-1])
    return d


def hy_unpack(y, L):
    nb = L // 128
    return y.reshape(128, HC, 4, nb).transpose(2, 3, 0, 1).reshape(4, L, HC)


TG = 4352
F32R = mybir.dt.float32r
NCH = 68


def gdn_groups(d):
    ctx = [0, 1, 2, 3]
    lat = [list(range(4 + 8 * i, 12 + 8 * i)) for i in range(8)]
    if d == 0:
        return [ctx] + lat
    return [ctx[::-1]] + [g[::-1] for g in lat[::-1]]


def build_l2g(NB=2):
    P = Prog()
    with ExitStack() as st:
        P._stack = st
        X = P.dram('X', [NB, 3, 128, 3, TG], F32, 'ExternalInput')
        Z = P.dram('Z', [NB, 64, NCH, 128], F32, 'ExternalInput')
        AB = P.dram('AB', [NB, 64, 2, 2, NCH], F32, 'ExternalInput')
        cwd = P.dram('cw', [128, 3, 3], F32, 'ExternalInput')
        scd = P.dram('sc', [64, 2, 2], F32, 'ExternalInput')
        nwd = P.dram('nw', [128], F32, 'ExternalInput')
        Md = P.dram('M', [64, 2, 5, 64], F32, 'ExternalInput')
        identd = P.dram('ident', [128, 128], F32, 'ExternalInput')
        Y = P.dram('Y', [NB, 64, NCH, 128], F32, 'ExternalOutput')

        ident = P.sb('identsb', [128, 128]); P.dma(ident[:], identd.ap(), reads=[identd], writes=[ident])
        Mt = P.sb('Mt', [64, 2, 5, 64]); P.dma(Mt[:], Md.ap(), reads=[Md], writes=[Mt])
        cw = P.sb('cwt', [128, 9]); P.dma(cw[:], AP(cwd, 0, [[9, 128], [1, 9]]), reads=[cwd], writes=[cw])
        sc = P.sb('sct', [64, 4]); P.dma(sc[:], AP(scd, 0, [[4, 64], [1, 4]]), reads=[scd], writes=[sc])
        nwb = P.sb('nwb', [64, 128]); P.dma(nwb[:], AP(nwd, 0, [[0, 64], [1, 128]]), reads=[nwd], writes=[nwb])
        ones = P.sb('ones', [128, 128]); P.op('vector', lambda e: e.memset(ones[:], 1.0), writes=[ones])
        one1 = P.sb('one1', [128, 1]); P.op('vector', lambda e: e.memset(one1[:], 1.0), writes=[one1])
        eps1 = P.sb('eps1', [128, 1]); P.op('vector', lambda e: e.memset(eps1[:], 1e-6), writes=[eps1])
        nea = P.sb('nea', [64, 2])
        for d in range(2):
            P.op('scalar', lambda e, d=d: e.activation(nea[:, d:d + 1], sc[:, 2 * d:2 * d + 1], AF.Exp), reads=[sc], writes=[nea])
        P.op('vector', lambda e: e.tensor_scalar(nea[:], nea[:], -1.0, None, ALU.mult), reads=[nea], writes=[nea])

        pp = [P.ps('pp%d' % i, [128, 512]) for i in range(5)]
        pr = [P.ps('pr%d' % i, [128, 128]) for i in range(3)]
        ppi = [0]

        def nps():
            p_ = pp[ppi[0] % 5]
            ppi[0] += 1
            return p_

        Qf = P.sb('Qf', [128, TG]); Kf = P.sb('Kf', [128, TG]); Vf = P.sb('Vf', [128, TG])
        Oacc = P.sb('Oacc', [64, NCH, 128])
        Zt = P.sb('Zt', [64, NCH, 128])
        stg = P.sb('stg', [128, 3, 256]); cacc = P.sb('cacc', [128, 256]); sq = P.sb('sqg', [128, 256]); rsb = P.sb('rsb', [128, 256])
        abt = P.sb('abt', [64, 2, 2, NCH])
        gt = P.sb('gt', [64, 2, NCH]); bet = P.sb('bet', [64, 2, NCH])
        S = P.sb('S', [128, 128]); S1 = P.sb('S1', [128, 128])
        W = 512
        gc = P.sb('gc', [64, 8]); egc = P.sb('egc', [64, 8]); ckb = P.sb('ckb', [64, 8]); ckd = P.sb('ckd', [64, 8])
        E3a = P.sb('E3a', [64, W]); E3b = P.sb('E3b', [64, W])
        Dm = P.sb('Dm', [64, W]); CA = P.sb('CA', [64, W]); CAT = P.sb('CAT', [64, W]); tmpw = P.sb('tmpw', [64, W])
        Pm = P.sb('Pm', [64, W]); PT = P.sb('PTm', [64, W]); TT = P.sb('TTm', [64, W])
        KB = P.sb('KB', [64, 8, 128]); BV = P.sb('BV', [64, 8, 128])
        dbl = []
        for i in range(2):
            dbl.append(dict(U=P.sb('U%d' % i, [64, 8, 128]), KD=P.sb('KD%d' % i, [64, 8, 128]), WT=P.sb('WT%d' % i, [128, W]),
                            QD=P.sb('QD%d' % i, [128, W]), QKT=P.sb('QKT%d' % i, [64, W]), egl=P.sb('egl%d' % i, [128, 8])))
        vnew = P.sb('vnew', [64, 128])
        otmp = P.sb('otmp', [64, 128])

        def V(fn, reads, writes):
            P.op('vector', fn, reads=reads, writes=writes)

        def G_(fn, reads, writes):
            P.op('gpsimd', fn, reads=reads, writes=writes)

        def A_(fn, reads, writes):
            P.op('scalar', fn, reads=reads, writes=writes)

        def T_(fn, reads, writes):
            P.op('tensor', fn, reads=reads, writes=writes)

        def mm256(out_fn, lhsT, rhs_fn, n, reads, ps_):
            for c0 in range(0, n, 256):
                m = min(256, n - c0)
                T_(lambda e, c0=c0, m=m: e.matmul(out_fn(c0, m), lhsT, rhs_fn(c0, m), start=True, stop=True), reads, [ps_])

        gcount = [0]
        for bb in range(NB):
            for s, dst in enumerate((Qf, Kf, Vf)):
                for t0 in range(0, TG, 256):
                    P.dma(stg[:], AP(X, ((bb * 3 + s) * 128) * 3 * TG + t0, [[3 * TG, 128], [TG, 3], [1, 256]]), reads=[X], writes=[stg])
                    V(lambda e, s=s: e.tensor_scalar(cacc[:], stg[:, 0, :], cw[:, s * 3:s * 3 + 1], None, ALU.mult), [stg, cw], [cacc])
                    V(lambda e, s=s: e.scalar_tensor_tensor(cacc[:], stg[:, 1, :], cw[:, s * 3 + 1:s * 3 + 2], cacc[:], ALU.mult, ALU.add), [stg, cw, cacc], [cacc])
                    V(lambda e, s=s: e.scalar_tensor_tensor(cacc[:], stg[:, 2, :], cw[:, s * 3 + 2:s * 3 + 3], cacc[:], ALU.mult, ALU.add), [stg, cw, cacc], [cacc])
                    if s == 2:
                        A_(lambda e, t0=t0, dst=dst: e.activation(dst[:, t0:t0 + 256], cacc[:], AF.Silu), [cacc], [dst])
                    else:
                        A_(lambda e: e.activation(cacc[:], cacc[:], AF.Silu), [cacc], [cacc])
                        A_(lambda e: e.activation(sq[:], cacc[:], AF.Square), [cacc], [sq])
                        ps_ = nps()
                        T_(lambda e, ps_=ps_: e.matmul(ps_[:, 0:256], ones[:], sq[:], start=True, stop=True), [ones, sq], [ps_])
                        A_(lambda e, ps_=ps_: e.activation(rsb[:], ps_[:, 0:256], AF.Sqrt, bias=eps1[:], scale=1.0), [ps_, eps1], [rsb])
                        V(lambda e: e.reciprocal(rsb[:], rsb[:]), [rsb], [rsb])
                        scl = 128 ** -0.5 if s == 0 else 1.0
                        V(lambda e, t0=t0, dst=dst, scl=scl: e.scalar_tensor_tensor(dst[:, t0:t0 + 256], cacc[:], scl, rsb[:], ALU.mult, ALU.mult),
                          [cacc, rsb], [dst])
            P.dma(abt[:], AP(AB, bb * 64 * 4 * NCH, [[4 * NCH, 64], [1, 4 * NCH]]), reads=[AB], writes=[abt])
            for d in range(2):
                V(lambda e, d=d: e.tensor_scalar(gt[:, d, :], abt[:, 0, d, :], sc[:, 2 * d + 1:2 * d + 2], None, ALU.add), [abt, sc], [gt])
                A_(lambda e, d=d: e.activation(gt[:, d, :], gt[:, d, :], AF.Exp), [gt], [gt])
                A_(lambda e, d=d: e.activation(gt[:, d, :], gt[:, d, :], AF.Ln, bias=one1[0:64, :], scale=1.0), [gt, one1], [gt])
                V(lambda e, d=d: e.tensor_scalar(gt[:, d, :], gt[:, d, :], nea[:, d:d + 1], None, ALU.mult), [gt, nea], [gt])
                A_(lambda e, d=d: e.activation(bet[:, d, :], abt[:, 1, d, :], AF.Sigmoid), [abt], [bet])
            Sd = [S, S1]
            V(lambda e: e.memset(S[:], 0.0), [], [S])
            V(lambda e: e.memset(S1[:], 0.0), [], [S1])
            G_(lambda e: e.memset(Oacc[:], 0.0), [], [Oacc])

            def pre(d, grp, B_):
                tri = Mt[:, d, 0, :]

                def mask(i, G, d=d):
                    return AP(Mt, (d * 5 + i) * 64, [[640, 64], [0, G], [1, 64]])
                G = len(grp)
                c_lo = min(grp)
                Wg = 64 * G
                col0 = 64 * c_lo
                U, KD, WT, QD, QKT, egl = B_['U'], B_['KD'], B_['WT'], B_['QD'], B_['QKT'], B_['egl']
                gsl = gt[:, d, c_lo:c_lo + G]
                bsl = bet[:, d, c_lo:c_lo + G]
                ps_ = nps()
                T_(lambda e, ps_=ps_, gsl=gsl, tri=tri: e.matmul(ps_[0:64, 0:G], tri, gsl, start=True, stop=True), [Mt, gt], [ps_])
                T_(lambda e, ps_=ps_, gsl=gsl: e.matmul(ps_[:, 8:8 + G], ones[0:64, :], gsl, start=True, stop=True), [ones, gt], [ps_])
                V(lambda e, ps_=ps_, G=G: e.tensor_copy(gc[:, 0:G], ps_[0:64, 0:G]), [ps_], [gc])
                A_(lambda e, ps_=ps_, G=G, egl=egl: e.activation(egl[:, 0:G], ps_[:, 8:8 + G], AF.Exp), [ps_], [egl])
                A_(lambda e, G=G: e.activation(egc[:, 0:G], gc[:, 0:G], AF.Exp), [gc], [egc])
                V(lambda e, G=G, bsl=bsl: e.tensor_tensor(ckb[:, 0:G], egc[:, 0:G], bsl, ALU.mult), [egc, bet], [ckb])
                V(lambda e, ps_=ps_, G=G: e.tensor_tensor(ckd[:, 0:G], ps_[0:64, 8:8 + G], gc[:, 0:G], ALU.subtract), [ps_, gc], [ckd])
                A_(lambda e, G=G: e.activation(ckd[:, 0:G], ckd[:, 0:G], AF.Exp), [ckd], [ckd])
                yield
                for (src, outs) in ((Kf, ((KB, ckb), (KD, ckd))), (Vf, ((BV, None),))):
                    for h0 in range(0, G, 4):
                        gh = min(4, G - h0)
                        ps_ = nps()
                        for j in range(gh):
                            c = c_lo + h0 + j
                            T_(lambda e, ps_=ps_, j=j, c=c, src=src: e.transpose(ps_[0:64, j * 128:(j + 1) * 128], src[:, 64 * c:64 * c + 64], ident[:]),
                               [src, ident], [ps_])
                        pv = AP(ps_, 0, [[512, 64], [128, gh], [1, 128]])
                        for (dst, coef) in outs:
                            dv = dst[:, h0:h0 + gh, :]
                            if coef is None:
                                cf = AP(bet, d * NCH + c_lo + h0, [[2 * NCH, 64], [1, gh], [0, 128]])
                                V(lambda e, dv=dv, pv=pv, cf=cf: e.tensor_tensor(dv, pv, cf, ALU.mult), [ps_, bet], [dst])
                            else:
                                cf = AP(coef, h0, [[8, 64], [1, gh], [0, 128]])
                                V(lambda e, dv=dv, pv=pv, cf=cf: e.tensor_tensor(dv, pv, cf, ALU.mult), [ps_, coef], [dst])
                yield
                idb = AP(ident, 0, [[128, 64], [0, G], [1, 64]])
                e3a = AP(E3a, 0, [[W, 64], [64, G], [1, 64]]); e3b = AP(E3b, 0, [[W, 64], [64, G], [1, 64]])
                V(lambda e, e3a=e3a, idb=idb, G=G: e.tensor_tensor(e3a, idb, AP(gc, 0, [[8, 64], [1, G], [0, 64]]), ALU.mult), [ident, gc], [E3a])
                V(lambda e, e3b=e3b, idb=idb, G=G, c_lo=c_lo: e.tensor_tensor(e3b, idb, AP(bet, d * NCH + c_lo, [[2 * NCH, 64], [1, G], [0, 64]]), ALU.mult),
                  [ident, bet], [E3b])
                psA = nps(); psB = nps(); psC = nps()
                mm256(lambda c0, m, psA=psA: psA[0:64, c0:c0 + m], ones[0:64, 0:64], lambda c0, m: E3a[:, c0:c0 + m], Wg, [ones, E3a], psA)
                mm256(lambda c0, m, psB=psB: psB[0:64, c0:c0 + m], ones[0:64, 0:64], lambda c0, m: E3b[:, c0:c0 + m], Wg, [ones, E3b], psB)
                mm256(lambda c0, m, psC=psC: psC[:, c0:c0 + m], ones[0:64, :], lambda c0, m: E3a[:, c0:c0 + m], Wg, [ones, E3a], psC)
                A_(lambda e, psC=psC, Wg=Wg, QD=QD: e.activation(QD[:, 0:Wg], psC[:, 0:Wg], AF.Exp), [psC], [QD])
                V(lambda e, Wg=Wg, QD=QD, col0=col0: e.tensor_tensor(QD[:, 0:Wg], QD[:, 0:Wg], Qf[:, col0:col0 + Wg], ALU.mult), [QD, Qf], [QD])
                yield
                dm3 = AP(Dm, 0, [[W, 64], [64, G], [1, 64]])
                V(lambda e, psA=psA, dm3=dm3, G=G: e.scalar_tensor_tensor(dm3, AP(psA, 0, [[512, 64], [64, G], [1, 64]]), -1.0,
                                                                          AP(gc, 0, [[8, 64], [1, G], [0, 64]]), ALU.mult, ALU.add), [psA, gc], [Dm])
                V(lambda e, Wg=Wg: e.tensor_scalar(tmpw[:, 0:Wg], Dm[:, 0:Wg], 0.0, None, ALU.min), [Dm], [tmpw])
                A_(lambda e, Wg=Wg: e.activation(tmpw[:, 0:Wg], tmpw[:, 0:Wg], AF.Exp), [tmpw], [tmpw])
                ca3 = AP(CA, 0, [[W, 64], [64, G], [1, 64]]); tw3 = AP(tmpw, 0, [[W, 64], [64, G], [1, 64]])
                V(lambda e, ca3=ca3, tw3=tw3, G=G, c_lo=c_lo: e.tensor_tensor(ca3, tw3, AP(bet, d * NCH + c_lo, [[2 * NCH, 64], [1, G], [0, 64]]), ALU.mult),
                  [tmpw, bet], [CA])
                G_(lambda e, ca3=ca3, G=G: e.tensor_tensor(ca3, ca3, mask(2, G), ALU.mult), [CA, Mt], [CA])
                V(lambda e, Wg=Wg: e.tensor_scalar(tmpw[:, 0:Wg], Dm[:, 0:Wg], -1.0, 0.0, ALU.mult, ALU.min), [Dm], [tmpw])
                A_(lambda e, Wg=Wg: e.activation(tmpw[:, 0:Wg], tmpw[:, 0:Wg], AF.Exp), [tmpw], [tmpw])
                cat3 = AP(CAT, 0, [[W, 64], [64, G], [1, 64]])
                V(lambda e, psB=psB, Wg=Wg: e.tensor_tensor(CAT[:, 0:Wg], tmpw[:, 0:Wg], psB[0:64, 0:Wg], ALU.mult), [tmpw, psB], [CAT])
                G_(lambda e, cat3=cat3, G=G: e.tensor_tensor(cat3, cat3, mask(4, G), ALU.mult), [CAT, Mt], [CAT])
                G_(lambda e, dm3=dm3, tw3=tw3, G=G: e.tensor_tensor(dm3, tw3, mask(3, G), ALU.mult), [tmpw, Mt], [Dm])
                yield
                psK = nps(); psQ = nps()
                for j in range(G):
                    c = c_lo + j
                    T_(lambda e, psK=psK, j=j, c=c: e.matmul(psK[0:64, 64 * j:64 * j + 64], Kf[:, 64 * c:64 * c + 64], Kf[:, 64 * c:64 * c + 64], start=True, stop=True),
                       [Kf], [psK])
                for j in range(G):
                    c = c_lo + j
                    T_(lambda e, psQ=psQ, j=j, c=c: e.matmul(psQ[0:64, 64 * j:64 * j + 64], Kf[:, 64 * c:64 * c + 64], Qf[:, 64 * c:64 * c + 64], start=True, stop=True),
                       [Kf, Qf], [psQ])
                V(lambda e, psK=psK, Wg=Wg: e.tensor_tensor(Pm[:, 0:Wg].bitcast(F32R), psK[0:64, 0:Wg], CA[:, 0:Wg], ALU.mult), [psK, CA], [Pm])
                V(lambda e, psK=psK, Wg=Wg: e.tensor_tensor(PT[:, 0:Wg].bitcast(F32R), psK[0:64, 0:Wg], CAT[:, 0:Wg], ALU.mult), [psK, CAT], [PT])
                V(lambda e, psQ=psQ, Wg=Wg, QKT=QKT: e.tensor_tensor(QKT[:, 0:Wg], psQ[0:64, 0:Wg], Dm[:, 0:Wg], ALU.mult), [psQ, Dm], [QKT])
                yield
                tt3 = AP(TT, 0, [[W, 64], [64, G], [1, 64]]); pt3 = AP(PT, 0, [[W, 64], [64, G], [1, 64]])
                V(lambda e, tt3=tt3, pt3=pt3, idb=idb: e.tensor_tensor(tt3.bitcast(F32R), idb, pt3, ALU.subtract), [ident, PT], [TT])
                for lev in range(5):
                    yield
                    p1 = nps(); p2 = nps(); p3 = nps()
                    for j in range(G):
                        sl = slice(64 * j, 64 * j + 64)
                        T_(lambda e, p1=p1, sl=sl: e.matmul(p1[0:64, sl], PT[:, sl].bitcast(F32R), Pm[:, sl].bitcast(F32R), start=True, stop=True), [PT, Pm], [p1])
                    for j in range(G):
                        sl = slice(64 * j, 64 * j + 64)
                        T_(lambda e, p2=p2, sl=sl: e.matmul(p2[0:64, sl], Pm[:, sl].bitcast(F32R), PT[:, sl].bitcast(F32R), start=True, stop=True), [PT, Pm], [p2])
                    V(lambda e, p1=p1, Wg=Wg: e.tensor_copy(Pm[:, 0:Wg].bitcast(F32R), p1[0:64, 0:Wg]), [p1], [Pm])
                    A_(lambda e, p2=p2, Wg=Wg: e.copy(PT[:, 0:Wg].bitcast(F32R), p2[0:64, 0:Wg]), [p2], [PT])
                    for j in range(G):
                        sl = slice(64 * j, 64 * j + 64)
                        T_(lambda e, p3=p3, sl=sl: e.matmul(p3[0:64, sl], Pm[:, sl].bitcast(F32R), TT[:, sl].bitcast(F32R), start=True, stop=True), [Pm, TT], [p3])
                    V(lambda e, p3=p3, Wg=Wg: e.tensor_tensor(TT[:, 0:Wg].bitcast(F32R), TT[:, 0:Wg], p3[0:64, 0:Wg], ALU.add), [p3, TT], [TT])
                yield
                for h0 in range(0, G, 4):
                    gh = min(4, G - h0)
                    ps_ = nps()
                    for j in range(gh):
                        jj = h0 + j
                        T_(lambda e, ps_=ps_, j=j, jj=jj: e.matmul(ps_[0:64, j * 128:(j + 1) * 128], TT[:, 64 * jj:64 * jj + 64], BV[:, jj, :], start=True, stop=True),
                           [TT, BV], [ps_])
                    A_(lambda e, ps_=ps_, h0=h0, gh=gh, U=U: e.copy(U[:, h0:h0 + gh, :], AP(ps_, 0, [[512, 64], [128, gh], [1, 128]])), [ps_], [U])
                ps_ = nps()
                for j in range(G):
                    T_(lambda e, ps_=ps_, j=j: e.matmul(ps_[:, 64 * j:64 * j + 64], KB[:, j, :], TT[:, 64 * j:64 * j + 64], start=True, stop=True), [KB, TT], [ps_])
                V(lambda e, ps_=ps_, Wg=Wg, WT=WT: e.tensor_copy(WT[:, 0:Wg], ps_[:, 0:Wg]), [ps_], [WT])
                yield

            def rec(d, grp, B_):
                S_ = Sd[d]
                c_lo = min(grp)
                U, KD, WT, QD, QKT, egl = B_['U'], B_['KD'], B_['WT'], B_['QD'], B_['QKT'], B_['egl']
                for c in grp:
                    j = c - c_lo
                    sl = slice(64 * j, 64 * j + 64)
                    p1, p2, p3 = pr
                    T_(lambda e, p1=p1, sl=sl, WT=WT: e.matmul(p1[0:64, :], WT[:, sl], S_[:], start=True, stop=True), [WT, S_], [p1])
                    V(lambda e, p1=p1, j=j, U=U: e.tensor_tensor(vnew[:], U[:, j, :], p1[0:64, :], ALU.subtract), [U, p1], [vnew])
                    yield
                    T_(lambda e, p2=p2, sl=sl, QD=QD: e.matmul(p2[0:64, :], QD[:, sl], S_[:], start=True, stop=False), [QD, S_], [p2])
                    T_(lambda e, p2=p2, sl=sl, QKT=QKT: e.matmul(p2[0:64, :], QKT[:, sl], vnew[:], start=False, stop=True), [QKT, vnew], [p2])
                    T_(lambda e, p3=p3, j=j, KD=KD: e.matmul(p3[:, :], KD[:, j, :], vnew[:], start=True, stop=True), [KD, vnew], [p3])
                    V(lambda e, p3=p3, j=j, egl=egl: e.scalar_tensor_tensor(S_[:], S_[:], egl[:, j:j + 1], p3[:, :], ALU.mult, ALU.add), [S_, egl, p3], [S_])
                    V(lambda e, p2=p2, c=c: e.tensor_tensor(Oacc[:, c, :], Oacc[:, c, :], p2[0:64, :], ALU.add), [p2, Oacc], [Oacc])
                    yield

            def drain(g):
                for _ in g:
                    pass

            def interleave(g1, g2):
                a1 = a2 = True
                while a1 or a2:
                    if a1:
                        try:
                            next(g1)
                        except StopIteration:
                            a1 = False
                    if a2:
                        try:
                            next(g2)
                        except StopIteration:
                            a2 = False

            groups = [gdn_groups(0), gdn_groups(1)]
            order = [(d, gi) for gi in range(9) for d in range(2)]
            prev = None
            for idx, (d, gi) in enumerate(order):
                B_ = dbl[idx % 2]
                gp = pre(d, groups[d][gi], B_)
                if prev is None:
                    drain(gp)
                else:
                    interleave(prev, gp)
                prev = rec(d, groups[d][gi], B_)
            drain(prev)
            P.dma(Zt[:], AP(Z, bb * 64 * NCH * 128, [[NCH * 128, 64], [1, NCH * 128]]), reads=[Z], writes=[Zt])
            A_(lambda e: e.activation(Zt[:], Zt[:], AF.Silu), [Zt], [Zt])
            ssq = gt
            for h0 in range(0, NCH, 4):
                V(lambda e, h0=h0: e.tensor_tensor(KB[:, 0:4, :], Oacc[:, h0:h0 + 4, :], Oacc[:, h0:h0 + 4, :], ALU.mult), [Oacc], [KB])
                V(lambda e, h0=h0: e.tensor_reduce(ssq[:, 0, h0:h0 + 4], KB[:, 0:4, :], AX.X, ALU.add), [KB], [gt])
            A_(lambda e: e.activation(ssq[:, 0, :], ssq[:, 0, :], AF.Sqrt, bias=eps1[0:64, :], scale=1.0 / 128), [gt, eps1], [gt])
            V(lambda e: e.reciprocal(ssq[:, 0, :], ssq[:, 0, :]), [gt], [gt])
            V(lambda e: e.tensor_tensor(Oacc[:], Oacc[:], AP(gt, 0, [[2 * NCH, 64], [1, NCH], [0, 128]]), ALU.mult), [Oacc, gt], [Oacc])
            G_(lambda e: e.tensor_tensor(Oacc[:], Oacc[:], AP(nwb, 0, [[128, 64], [0, NCH], [1, 128]]), ALU.mult), [Oacc, nwb], [Oacc])
            V(lambda e: e.tensor_tensor(Oacc[:], Oacc[:], Zt[:], ALU.mult), [Oacc, Zt], [Oacc])
            P.dma(AP(Y, bb * 64 * NCH * 128, [[NCH * 128, 64], [1, NCH * 128]]), Oacc[:], reads=[Oacc], writes=[])
        P.finish()
        nc = P.emit()
    return nc


def gdn_masks():
    i = np.arange(64)
    M = np.zeros((64, 2, 5, 64), np.float32)
    for d in range(2):
        le = (i[:, None] <= i[None, :]) if d == 0 else (i[:, None] >= i[None, :])
        incl = (i[:, None] >= i[None, :]) if d == 0 else (i[:, None] <= i[None, :])
        strict = (i[:, None] > i[None, :]) if d == 0 else (i[:, None] < i[None, :])
        M[:, d, 0] = le; M[:, d, 1] = incl; M[:, d, 2] = strict; M[:, d, 3] = incl.T; M[:, d, 4] = strict.T
    return M


def gdn_inputs(h, pb_list, inp, L_):
    NB = len(pb_list)
    X = np.zeros((NB, 3, 128, 3, TG), np.float32)
    Zz = np.empty((NB, 64, NCH, 128), np.float32)
    AB = np.empty((NB, 64, 2, 2, NCH), np.float32)
    for bi, pb in enumerate(pb_list):
        seqs = [(pb[4096:4352], 0, 256), (pb[0:4096], 256, 4096)]
        for (ps_, off, L) in seqs:
            for s in range(3):
                x = ps_[:, s * 512 + h * 128:s * 512 + (h + 1) * 128]
                xp = np.pad(x, ((1, 1), (0, 0)))
                for k in range(3):
                    X[bi, s, :, k, off:off + L] = xp[k:k + L].T
        tok = np.concatenate([pb[4096:4352], pb[0:4096]], 0)
        z = tok[:, 1536 + h * 128:1536 + (h + 1) * 128]
        Zz[bi] = z.reshape(NCH, 64, 128).transpose(1, 0, 2)
        a = tok[:, 2048:2056].reshape(TG, 2, 4)[:, :, h]
        bt = tok[:, 2056:2064].reshape(TG, 2, 4)[:, :, h]
        AB[bi, :, 0] = a.reshape(NCH, 64, 2).transpose(1, 2, 0)
        AB[bi, :, 1] = bt.reshape(NCH, 64, 2).transpose(1, 2, 0)
    cw = np.ascontiguousarray(inp['gdn_conv_w'][L_].reshape(3, 3, 4, 128)[:, :, h, :].transpose(2, 1, 0))
    sc = np.empty((64, 2, 2), np.float32)
    sc[:, :, 0] = inp['gdn_a_log'][L_][:, h][None, :]
    sc[:, :, 1] = inp['gdn_dt_bias'][L_][:, h][None, :]
    return {'X': X, 'Z': Zz, 'AB': AB, 'cw': cw, 'sc': sc, 'nw': inp['gdn_norm_w'][L_], 'M': gdn_masks(), 'ident': np.eye(128, dtype=np.float32)}


def gdn_unpack(Y):
    t = Y.transpose(1, 0, 2).reshape(TG, 128)
    return t[256:], t[:256]


def build_l4():
    P = Prog()
    with ExitStack() as st:
        P._stack = st
        xT = P.dram('xT', [128, NKC, T3], F32, 'ExternalInput')
        fw = P.dram('fw', [128, NKC], F32, 'ExternalInput')
        oT = P.dram('oT', [128, NKC, T3], F32, 'ExternalOutput')
        fwt = P.sb('fwt', [128, NKC])
        P.dma(fwt[:], fw.ap(), reads=[fw], writes=[fwt])
        ones_bf = P.sb('ones_bf', [128, 128], BF16)
        P.op('vector', lambda e: e.memset(ones_bf[:], 1.0), writes=[ones_bf])
        eps_t = P.sb('eps_t', [128, 1])
        P.op('vector', lambda e: e.memset(eps_t[:], 1e-6), writes=[eps_t])
        SUB = 256
        xs = [P.sb('xs%d' % i, [128, NKC, SUB]) for i in range(2)]
        sq = P.sb('sq', [128, NKC, SUB], BF16)
        pss = P.ps('ps_ss', [128, SUB])
        rs = P.sb('rs', [128, SUB])
        tmp = P.sb('ntmp', [128, NKC, SUB])
        ot = [P.sb('ot%d' % i, [128, NKC, SUB]) for i in range(2)]
        nsub = (T3 + SUB - 1) // SUB
        for s in range(nsub):
            t0 = s * SUB
            n = min(SUB, T3 - t0)
            x_ = xs[s % 2]
            o_ = ot[s % 2]
            P.dma(x_[:, :, 0:n], AP(xT, t0, [[NKC * T3, 128], [T3, NKC], [1, n]]), reads=[xT], writes=[x_])
            P.op('scalar', lambda e, x_=x_, n=n: e.activation(sq[:, :, 0:n], x_[:, :, 0:n], AF.Square), reads=[x_], writes=[sq])
            for c in range(NKC):
                P.op('tensor', lambda e, c=c, n=n: e.matmul(pss[:, 0:n], ones_bf[:], sq[:, c, 0:n], start=(c == 0), stop=(c == NKC - 1)),
                     reads=[sq, ones_bf], writes=[pss])
            P.op('scalar', lambda e, n=n: e.activation(rs[:, 0:n], pss[:, 0:n], AF.Sqrt, bias=eps_t[:], scale=1.0 / D),
                 reads=[pss, eps_t], writes=[rs])
            P.op('vector', lambda e, n=n: e.reciprocal(rs[:, 0:n], rs[:, 0:n]), reads=[rs], writes=[rs])
            P.op('vector', lambda e, x_=x_, n=n: e.tensor_tensor(
                tmp[:, :, 0:n], x_[:, :, 0:n], AP(rs, 0, [[SUB, 128], [0, NKC], [1, n]]), ALU.mult), reads=[x_, rs], writes=[tmp])
            P.op('vector', lambda e, o_=o_, n=n: e.tensor_tensor(
                o_[:, :, 0:n], tmp[:, :, 0:n], AP(fwt, 0, [[NKC, 128], [1, NKC], [0, n]]), ALU.mult), reads=[tmp, fwt], writes=[o_])
            P.dma(AP(oT, t0, [[NKC * T3, 128], [T3, NKC], [1, n]]), o_[:, :, 0:n], reads=[o_], writes=[])
        P.finish()
        nc = P.emit()
    return nc


def _fm(a):
    return np.ascontiguousarray(a.T.reshape(16, 128, a.shape[0]).transpose(1, 0, 2))


def _unfm(a):
    return np.ascontiguousarray(a.transpose(2, 1, 0).reshape(a.shape[2], 2048))


def _rope_tables():
    n_freq = 16
    freqs = (10000.0 ** (-np.arange(n_freq, dtype=np.float32) / n_freq)).astype(np.float32)
    row = np.repeat(np.arange(64, dtype=np.float32), 64)
    col = np.tile(np.arange(64, dtype=np.float32), 64)
    ang = np.concatenate([row[:, None] * freqs, col[:, None] * freqs], -1)
    c = np.cos(ang).T.astype(np.float32)
    s = np.sin(ang).T.astype(np.float32)
    C = np.ones((64, TA), np.float32)
    S = np.zeros((64, TA), np.float32)
    C[:32, :4096] = c; C[32:, :4096] = c; S[:32, :4096] = -s; S[32:, :4096] = s
    rm = np.zeros((64, 64), np.float32)
    for m in range(64):
        rm[(m + 32) % 64, m] = 1
    return C, S, rm


def _run(nc, in_maps):
    res = run_bass_kernel_spmd(nc, in_maps, core_ids=list(range(8)))
    return res.results


def kernel(**inp):
    inp = {k: np.asarray(v) for k, v in inp.items()}
    x = inp['x']; ctx = inp['ctx']
    B = 4
    mod = run_l0(inp['c'], inp['c_ctx'], inp['w_ada'], inp['b_ada'])
    C, S, rm = _rope_tables()
    ident = np.eye(128, dtype=np.float32)
    nc1 = build_l1(); nc2 = build_l2a(); nc3 = build_l3(); nc4 = build_l4(); ncg = build_l2g(2); nch = build_l2h()
    xl = x.astype(np.float32); xc = ctx.astype(np.float32)
    for L in range(2):
        m = mod[L]

        def mrow(r, i):
            return vecT(m[r, i * 2048:(i + 1) * 2048])
        ims = []
        for core in range(8):
            b, hh = core // 2, core % 2
            xcat = np.concatenate([xl[b, hh * 2048:(hh + 1) * 2048], xc[b, hh * 128:(hh + 1) * 128]], 0)
            mv = np.stack([vecT(inp['norm1_w'][L]), mrow(b, 1), mrow(b, 0), mrow(4, 1), mrow(4, 0)])
            ims.append({'xT': _fm(xcat), 'mv': mv, 'W': inp['w_in'][L]})
        r1 = _run(nc1, ims)
        p = np.empty((B, TA, IN_DIM), np.float32)
        for core in range(8):
            b, hh = core // 2, core % 2
            pT = r1[core]['pT']
            p[b, hh * 2048:(hh + 1) * 2048] = pT[:, :2048].T
            p[b, 4096 + hh * 128:4096 + (hh + 1) * 128] = pT[:, 2048:].T
        del r1
        o = 2048 + 16
        nw = np.zeros((128, 8), np.float32)
        nw[:, 0:4] = inp['mla_q_norm_w'][L].reshape(4, 128).T
        nw[:, 4:6] = inp['mla_kv_norm_w'][L].reshape(2, 128).T
        nw[:64, 6] = inp['gqa_q_norm_w'][L]; nw[:64, 7] = inp['gqa_k_norm_w'][L]
        ims = []
        for core in range(8):
            b, hh = core // 2, core % 2
            pb = p[b]
            o2 = o + 832
            ims.append({'gq': np.ascontiguousarray(pb[:, o2 + hh * 256:o2 + (hh + 1) * 256].T),
                        'gk': np.ascontiguousarray(pb[:, o2 + 512 + hh * 64:o2 + 512 + (hh + 1) * 64].T),
                        'gv': np.ascontiguousarray(pb[:, o2 + 640 + hh * 64:o2 + 640 + (hh + 1) * 64]),
                        'cq': np.ascontiguousarray(pb[:, o:o + 512].T), 'ckv': np.ascontiguousarray(pb[:, o + 512:o + 768].T),
                        'kr': np.ascontiguousarray(pb[:, o + 768:o + 832].T),
                        'wuq': np.ascontiguousarray(inp['mla_w_uq'][L][:, 2 * hh:2 * hh + 2].reshape(512, 384)),
                        'wukv': np.ascontiguousarray(inp['mla_w_ukv'][L][:, 2 * hh:2 * hh + 2].reshape(256, 512)),
                        'nw': nw, 'cosd': C, 'sind': S, 'rmd': rm})
        r2 = _run(nc2, ims)
        br = np.zeros((B, TA, 4, 512), np.float32)
        for core in range(8):
            b, hh = core // 2, core % 2
            br[b, :, 1, hh * 256:(hh + 1) * 256] = r2[core]['om']
            br[b, :, 2, hh * 256:(hh + 1) * 256] = r2[core]['og']
        del r2
        ims = []
        for core in range(8):
            h, bp = core // 2, core % 2
            ims.append(gdn_inputs(h, [p[2 * bp], p[2 * bp + 1]], inp, L))
        rg = _run(ncg, ims)
        for core in range(8):
            h, bp = core // 2, core % 2
            for bi in range(2):
                lat, cx = gdn_unpack(rg[core]['Y'][bi])
                br[2 * bp + bi, :4096, 0, h * 128:(h + 1) * 128] = lat
                br[2 * bp + bi, 4096:, 0, h * 128:(h + 1) * 128] = cx
        del rg, ims
        pl_h = np.ascontiguousarray(p[:, :4096, 3664:5200]); pc_h = np.ascontiguousarray(p[:, 4096:, 3664:5200])
        ims = [hy_inputs(core, pl_h, pc_h, inp, L) for core in range(8)]
        rh = _run(nch, ims)
        for core in range(8):
            br[:, :4096, 3, core * 64:(core + 1) * 64] = hy_unpack(rh[core]['yL'], 4096)
            br[:, 4096:, 3, core * 64:(core + 1) * 64] = hy_unpack(rh[core]['yC'], 256)
        del rh, ims
        wbr = np.ascontiguousarray(inp['w_branch'][L].reshape(2048, 2048))
        wr = np.ascontiguousarray(inp['w_router'].reshape(16, 128, 16).transpose(1, 0, 2))
        nxl = np.empty_like(xl); nxc = np.empty_like(xc)
        for half in range(2):
            ims = []
            for core in range(8):
                sh = half * 8 + core
                b, q = sh // 4, sh % 4
                ls = slice(q * 1024, (q + 1) * 1024); cs_ = slice(q * 64, (q + 1) * 64)
                cs2 = slice(4096 + q * 64, 4096 + (q + 1) * 64)
                xcat = np.concatenate([xl[b, ls], xc[b, cs_]], 0)
                brc = np.concatenate([br[b, ls], br[b, cs2]], 0).reshape(T3, 2048)
                g = np.concatenate([p[b, ls, MIX_IN:], p[b, cs2, MIX_IN:]], 0)
                mv = np.stack([vecT(inp['norm2_w'][L]), mrow(b, 2), mrow(b, 3), mrow(b, 4), mrow(b, 5),
                               mrow(4, 2), mrow(4, 3), mrow(4, 4), mrow(4, 5)])
                ims.append({'xT': _fm(xcat), 'brT': np.ascontiguousarray(brc.T), 'gT': np.ascontiguousarray(g.T), 'mv': mv,
                            'wbr': wbr, 'wo': inp['w_out'][L], 'wr': wr, 'rb': inp['router_bias'],
                            'wg': inp['moe_w_gate'][L], 'wu': inp['moe_w_up'][L], 'wd': inp['moe_w_down'][L], 'ident': ident})
            r3 = _run(nc3, ims)
            for core in range(8):
                sh = half * 8 + core
                b, q = sh // 4, sh % 4
                x2 = _unfm(r3[core]['x2T'])
                nxl[b, q * 1024:(q + 1) * 1024] = x2[:1024]
                nxc[b, q * 64:(q + 1) * 64] = x2[1024:]
            del r3
        xl, xc = nxl, nxc
        del p, br
    out = np.empty_like(xl)
    fw = vecT(inp['final_norm_w'])
    for half in range(2):
        ims = []
        for core in range(8):
            sh = half * 8 + core
            b, q = sh // 4, sh % 4
            xcat = np.concatenate([xl[b, q * 1024:(q + 1) * 1024], xc[b, q * 64:(q + 1) * 64]], 0)
            ims.append({'xT': _fm(xcat), 'fw': fw})
        r4 = _run(nc4, ims)
        for core in range(8):
            sh = half * 8 + core
            b, q = sh // 4, sh % 4
            out[b, q * 1024:(q + 1) * 1024] = _unfm(r4[core]['oT'])[:1024]
    return out.astype(np.float32)
```

```python
import math
from contextlib import ExitStack
import types
import numpy as np
import concourse.bass as bass
import concourse.mybir as mybir
from concourse.bass_utils import run_bass_kernel_spmd

F32 = mybir.dt.float32
BF16 = mybir.dt.bfloat16
I32 = mybir.dt.int32
AF = mybir.ActivationFunctionType
ALU = mybir.AluOpType
AX = mybir.AxisListType

COMPUTE = ('tensor', 'vector', 'scalar', 'gpsimd')
NDMASEM = 24


def _freeze(fn):
    if not getattr(fn, '__closure__', None):
        return fn
    cells = []
    for c in fn.__closure__:
        try:
            cells.append(types.CellType(c.cell_contents))
        except ValueError:
            cells.append(c)
    g = types.FunctionType(fn.__code__, fn.__globals__, fn.__name__, fn.__defaults__, tuple(cells))
    g.__kwdefaults__ = fn.__kwdefaults__
    return g


class Prog:
    def __init__(self, name='k'):
        self.nc = bass.Bass('TRN2', target_bir_lowering=False)
        self.ops = {e: [] for e in COMPUTE + ('sync',)}
        self.cnt = {e: 0 for e in COMPUTE}
        self.waited = {e: {} for e in COMPUTE + ('sync',)}
        self.last_w = {}
        self.readers = {}
        self.dma_cnt = [0] * NDMASEM
        self.dma_rr = 0
        self.ctx = []
        self.sems = {}
        self.n_ops = 0
        self._stack = None
        self.safe = False
        self.pe_pending = {}

    def _enter(self, cm):
        return self._stack.enter_context(cm)

    def dram(self, name, shape, dt, kind):
        return self.nc.dram_tensor(name, list(shape), dt, kind=kind)

    def sb(self, name, shape, dt=F32):
        return self._enter(self.nc.sbuf_tensor(name, list(shape), dt))

    def ps(self, name, shape, dt=F32):
        return self._enter(self.nc.psum_tensor(name, list(shape), dt))

    @staticmethod
    def _key(t):
        if isinstance(t, str):
            return t
        if hasattr(t, 'tensor'):
            t = t.tensor
        return t.name

    def _deps(self, reads, writes):
        deps = []
        for r in reads:
            k = self._key(r)
            if k in self.last_w:
                deps.append(self.last_w[k])
        for w in writes:
            k = self._key(w)
            if k in self.last_w:
                deps.append(self.last_w[k])
            deps.extend(self.readers.get(k, []))
        return deps

    def _commit(self, ticket, reads, writes):
        for r in reads:
            self.readers.setdefault(self._key(r), []).append(ticket)
        for w in writes:
            k = self._key(w)
            self.last_w[k] = ticket
            self.readers[k] = []

    def _wait_list(self, eng, deps):
        need = {}
        for (kind, idx, val) in deps:
            if kind == 'eng' and idx == 'tensor' and eng == 'tensor':
                continue
            sk = (kind, idx)
            if self.waited[eng].get(sk, 0) >= val:
                continue
            if need.get(sk, 0) < val:
                need[sk] = val
        for sk, val in need.items():
            self.waited[eng][sk] = val
        return list(need.items())

    def op(self, eng, fn, reads=(), writes=()):
        deps = self._deps(reads, writes)
        waits = self._wait_list(eng, deps)
        self.cnt[eng] += 1
        ticket = ('eng', eng, self.cnt[eng])
        self._commit(ticket, reads, writes)
        if eng == 'tensor':
            for w in writes:
                self.pe_pending.setdefault(self._key(w), set()).update(self._key(r) for r in reads)
        else:
            for r in reads:
                pk = self._key(r)
                if pk in self.pe_pending:
                    for k in self.pe_pending.pop(pk):
                        self.readers.setdefault(k, []).append(ticket)
        self.ops[eng].append(('c', waits, _freeze(fn)))
        self.n_ops += 1
        return ticket

    def dma(self, out, in_, reads=(), writes=(), q='sync', **kw):
        deps = self._deps(reads, writes)
        si = self.dma_rr
        self.dma_rr = (self.dma_rr + 1) % NDMASEM
        if self.dma_cnt[si] > 0:
            deps.append(('dma', si, self.dma_cnt[si]))
        waits = self._wait_list(q, deps)
        self.dma_cnt[si] += 16
        ticket = ('dma', si, self.dma_cnt[si])
        self._commit(ticket, reads, writes)
        self.ops[q].append(('d', waits, (out, in_, si, kw)))
        self.n_ops += 1
        return ticket

    def finish(self, eng='sync'):
        deps = [('dma', i, c) for i, c in enumerate(self.dma_cnt) if c > 0]
        waits = self._wait_list(eng, deps)
        self.ops[eng].append(('w', waits, None))

    def emit(self):
        nc = self.nc
        esem = {e: self._enter(nc.semaphore('s_' + e)) for e in COMPUTE}
        dsem = [self._enter(nc.semaphore('d_%d' % i)) for i in range(NDMASEM)]

        def semof(sk):
            return esem[sk[1]] if sk[0] == 'eng' else dsem[sk[1]]

        ops = self.ops

        def run(engname):
            def body(e):
                for (kind, waits, payload) in ops[engname]:
                    for sk, val in waits:
                        e.wait_ge(semof(sk), val)
                    if kind == 'c':
                        ins = payload(e)
                        if self.safe and engname != 'tensor':
                            e.drain().then_inc(esem[engname], 1)
                        else:
                            ins.then_inc(esem[engname], 1)
                    elif kind == 'd':
                        out, in_, si, kw = payload
                        e.dma_start(out=out, in_=in_, **kw).then_inc(dsem[si], 16)
            return body

        with nc.Block() as block:
            block.sync(run('sync'))
            block.tensor(run('tensor'))
            block.vector(run('vector'))
            block.scalar(run('scalar'))
            block.gpsimd(run('gpsimd'))
        return nc


D = 2048
NKC = 16
IN_DIM = 13392
T1 = 2176


def AP(t, off, dims):
    return bass.AP(t, off, [list(d) for d in dims])


def build_l0():
    P = Prog()
    with ExitStack() as st:
        P._stack = st
        cT = P.dram('cT', [128, 16, 5], F32, 'ExternalInput')
        w = P.dram('w', [2, D, 1536], F32, 'ExternalInput')
        b = P.dram('b', [2, 1536], F32, 'ExternalInput')
        out = P.dram('mod', [2, 5, 1536], F32, 'ExternalOutput')
        ct = P.sb('ct', [128, 16, 5])
        cs = P.sb('cs', [128, 16, 5])
        P.dma(ct[:], cT.ap(), reads=[cT], writes=[ct])
        P.op('scalar', lambda e: e.activation(cs[:], ct[:], AF.Silu), reads=[ct], writes=[cs])
        wt = [P.sb('wt%d' % i, [128, 4, 512]) for i in range(2)]
        bt = P.sb('bt', [5, 2, 1536])
        P.dma(bt[:], AP(b, 0, [[0, 5], [1536, 2], [1, 1536]]), reads=[b], writes=[bt])
        ps = [P.ps('ps%d' % i, [5, 512]) for i in range(2)]
        ot = P.sb('ot', [5, 2, 1536])
        n = 0
        pi = 0
        for l in range(2):
            for cb in range(3):
                pst = ps[pi % 2]
                pi += 1
                for kg in range(4):
                    wtile = wt[n % 2]
                    n += 1
                    src = AP(w, l * D * 1536 + kg * 512 * 1536 + cb * 512, [[1536, 128], [128 * 1536, 4], [1, 512]])
                    P.dma(wtile[:], src, reads=[w], writes=[wtile])
                    for kk in range(4):
                        k = kg * 4 + kk
                        P.op('tensor', lambda e, k=k, kk=kk, wtile=wtile, pst=pst: e.matmul(
                            pst[:], cs[:, k, :], wtile[:, kk, :], start=(k == 0), stop=(k == 15)),
                            reads=[cs, wtile], writes=[pst])
                P.op('vector', lambda e, l=l, cb=cb, pst=pst: e.tensor_tensor(
                    ot[:, l, cb * 512:(cb + 1) * 512], pst[:], bt[:, l, cb * 512:(cb + 1) * 512], ALU.add),
                    reads=[pst, bt], writes=[ot])
        P.dma(AP(out, 0, [[1536, 5], [5 * 1536, 2], [1, 1536]]), ot[:], reads=[ot], writes=[out])
        P.finish()
        nc = P.emit()
    return nc


def run_l0(c, c_ctx, w_ada, b_ada):
    c5 = np.concatenate([c, c_ctx[None]], 0)
    cT = np.ascontiguousarray(c5.T.reshape(16, 128, 5).transpose(1, 0, 2))
    nc = build_l0()
    in_maps = []
    for i in range(8):
        sl = slice(i * 1536, (i + 1) * 1536)
        in_maps.append({'cT': cT, 'w': np.ascontiguousarray(w_ada[:, :, sl]), 'b': np.ascontiguousarray(b_ada[:, sl])})
    res = run_bass_kernel_spmd(nc, in_maps, core_ids=list(range(8)))
    mod = np.concatenate([r['mod'] for r in res.results], axis=2)
    return mod


def vecT(v):
    return np.ascontiguousarray(v.reshape(16, 128).T)


TOKB = [(0, 512), (512, 512), (1024, 512), (1536, 512), (2048, 128)]


def AP_slice(mvt, r):
    return mvt[:, r, :]


def norm_phase(P, xT, hT, gl, sl_, gc, sc_, ones_bf, eps_t, T, n_lat, x_reads=None, out_key='hT', SUB=256):
    xs = [P.sb('xs%d' % i, [128, NKC, SUB]) for i in range(2)]
    sq = P.sb('sq', [128, NKC, SUB], BF16)
    pss = P.ps('ps_ss', [128, SUB])
    rs = P.sb('rs', [128, SUB])
    tmp = P.sb('ntmp', [128, NKC, SUB])
    nsub = (T + SUB - 1) // SUB
    for s in range(nsub):
        t0 = s * SUB
        n = min(SUB, T - t0)
        x_ = xs[s % 2]
        P.dma(x_[:, :, 0:n], AP(xT, t0, [[NKC * T, 128], [T, NKC], [1, n]]), reads=(x_reads if x_reads is not None else [xT]), writes=[x_])
        P.op('scalar', lambda e, x_=x_, n=n: e.activation(sq[:, :, 0:n], x_[:, :, 0:n], AF.Square),
             reads=[x_], writes=[sq])
        for c in range(NKC):
            P.op('tensor', lambda e, c=c, n=n: e.matmul(pss[:, 0:n], ones_bf[:], sq[:, c, 0:n],
                                                          start=(c == 0), stop=(c == NKC - 1)),
                 reads=[sq, ones_bf], writes=[pss])
        P.op('scalar', lambda e, n=n: e.activation(rs[:, 0:n], pss[:, 0:n], AF.Sqrt, bias=eps_t[:], scale=1.0 / D),
             reads=[pss, eps_t], writes=[rs])
        P.op('vector', lambda e, n=n: e.reciprocal(rs[:, 0:n], rs[:, 0:n]), reads=[rs], writes=[rs])
        P.op('vector', lambda e, x_=x_, n=n: e.tensor_tensor(
            tmp[:, :, 0:n], x_[:, :, 0:n], AP(rs, 0, [[SUB, 128], [0, NKC], [1, n]]), ALU.mult),
            reads=[x_, rs], writes=[tmp])
        g_, s_ = (gl, sl_) if t0 < n_lat else (gc, sc_)
        for c in range(NKC):
            eng = 'vector' if c % 2 == 0 else 'gpsimd'
            P.op(eng, lambda e, c=c, n=n, g_=g_, s_=s_, t0=t0: e.tensor_scalar(
                hT[:, c, t0:t0 + n], tmp[:, c, 0:n], g_[:, c:c + 1], s_[:, c:c + 1], ALU.mult, ALU.add),
                reads=[tmp, g_, s_], writes=['%s:%d' % (out_key, c)])


def linear_fm(P, W, w_off, w_rs, K_chunks, n_out, rhs_fn, tokb, evac_fn, rhs_reads, tag='lin', grp=256):
    wst = [P.sb('%s_wst%d' % (tag, i), [128, K_chunks, grp]) for i in range(2)]
    wbf = [P.sb('%s_wbf%d' % (tag, i), [128, K_chunks, grp], BF16) for i in range(2)]
    pst = [P.ps('%s_ps%d' % (tag, i), [128, 512]) for i in range(4)]
    ng = (n_out + grp - 1) // grp
    pi = 0
    for g in range(ng):
        o0 = g * grp
        gw = min(grp, n_out - o0)
        ws, wb = wst[g % 2], wbf[g % 2]
        P.dma(ws[:, :, 0:gw], AP(W, w_off + o0, [[w_rs, 128], [128 * w_rs, K_chunks], [1, gw]]),
              reads=[W], writes=[ws])
        P.op('scalar', lambda e, ws=ws, wb=wb, gw=gw: e.copy(wb[:, :, 0:gw], ws[:, :, 0:gw]),
             reads=[ws], writes=[wb])
        for m0 in range(0, gw, 128):
            m = min(128, gw - m0)
            for (t0, n) in tokb:
                ps_ = pst[pi % 4]
                pi += 1
                for kc in range(K_chunks):
                    P.op('tensor', lambda e, ps_=ps_, wb=wb, kc=kc, m0=m0, m=m, t0=t0, n=n: e.matmul(
                        ps_[0:m, 0:n], wb[:, kc, m0:m0 + m], rhs_fn(kc, t0, n),
                        start=(kc == 0), stop=(kc == K_chunks - 1)),
                        reads=[wb] + rhs_reads(kc), writes=[ps_])
                evac_fn(o0 + m0, m, t0, n, ps_)


def build_l1():
    P = Prog()
    with ExitStack() as st:
        P._stack = st
        xT = P.dram('xT', [128, NKC, T1], F32, 'ExternalInput')
        mv = P.dram('mv', [5, 128, NKC], F32, 'ExternalInput')
        W = P.dram('W', [D, IN_DIM], F32, 'ExternalInput')
        pT = P.dram('pT', [IN_DIM, T1], F32, 'ExternalOutput')
        mvt = P.sb('mvt', [128, 5, NKC])
        P.dma(mvt[:], AP(mv, 0, [[NKC, 128], [128 * NKC, 5], [1, NKC]]), reads=[mv], writes=[mvt])
        gl = P.sb('gl', [128, NKC]); gc = P.sb('gc', [128, NKC])
        sl_ = P.sb('sl', [128, NKC]); sc_ = P.sb('sc', [128, NKC])
        P.op('vector', lambda e: e.scalar_tensor_tensor(gl[:], mvt[:, 1, :], 1.0, mvt[:, 0, :], ALU.add, ALU.mult),
             reads=[mvt], writes=[gl])
        P.op('vector', lambda e: e.scalar_tensor_tensor(gc[:], mvt[:, 3, :], 1.0, mvt[:, 0, :], ALU.add, ALU.mult),
             reads=[mvt], writes=[gc])
        P.op('vector', lambda e: e.tensor_copy(sl_[:], mvt[:, 2, :]), reads=[mvt], writes=[sl_])
        P.op('vector', lambda e: e.tensor_copy(sc_[:], mvt[:, 4, :]), reads=[mvt], writes=[sc_])
        ones_bf = P.sb('ones_bf', [128, 128], BF16)
        P.op('vector', lambda e: e.memset(ones_bf[:], 1.0), writes=[ones_bf])
        eps_t = P.sb('eps_t', [128, 1])
        P.op('vector', lambda e: e.memset(eps_t[:], 1e-6), writes=[eps_t])
        hT = P.sb('hT', [128, NKC, T1], BF16)
        norm_phase(P, xT, hT, gl, sl_, gc, sc_, ones_bf, eps_t, T1, 2048)
        osb = [P.sb('osb%d' % i, [128, 512]) for i in range(4)]
        cnt = [0]

        def evac(o0, m, t0, n, ps_):
            o_ = osb[cnt[0] % 4]
            eng = 'vector' if cnt[0] % 2 == 0 else 'scalar'
            cnt[0] += 1
            if eng == 'vector':
                P.op('vector', lambda e: e.tensor_copy(o_[0:m, 0:n], ps_[0:m, 0:n]), reads=[ps_], writes=[o_])
            else:
                P.op('scalar', lambda e: e.copy(o_[0:m, 0:n], ps_[0:m, 0:n]), reads=[ps_], writes=[o_])
            P.dma(AP(pT, o0 * T1 + t0, [[T1, m], [1, n]]), o_[0:m, 0:n], reads=[o_], writes=[], q='gpsimd')

        linear_fm(P, W, 0, IN_DIM, NKC, IN_DIM, lambda kc, t0, n: hT[:, kc, t0:t0 + n], TOKB, evac, lambda kc: ['hT:%d' % kc])
        P.finish()
        nc = P.emit()
    return nc


MIX_IN = 5200
T3 = 1088
TOKB3 = [(0, 512), (512, 512), (1024, 64)]
NLAT3 = 1024
BIG = 1.0e4


class Lin:
    def __init__(self, P, tag, kmax=16, grp=256, nps=2):
        self.P = P
        self.grp = grp
        self.wst = [P.sb('%s_wst%d' % (tag, i), [128, kmax, grp]) for i in range(2)]
        self.wbf = [P.sb('%s_wbf%d' % (tag, i), [128, kmax, grp], BF16) for i in range(2)]
        self.pst = [P.ps('%s_ps%d' % (tag, i), [128, 512]) for i in range(nps)]
        self.nps = nps
        self.g = 0
        self.pi = 0

    def run(self, W, w_off, w_rs, K_chunks, n_out, rhs_fn, tokb, evac_fn, rhs_reads, split=1):
        P = self.P
        grp = self.grp
        ng = (n_out + grp - 1) // grp
        kper = K_chunks // split
        for g in range(ng):
            o0 = g * grp
            gw = min(grp, n_out - o0)
            ws, wb = self.wst[self.g % 2], self.wbf[self.g % 2]
            P.dma(ws[:, 0:K_chunks, 0:gw], AP(W, w_off + o0, [[w_rs, 128], [128 * w_rs, K_chunks], [1, gw]]),
                  reads=[W], writes=[ws])
            self.g += 1
            P.op('scalar', lambda e, ws=ws, wb=wb, gw=gw: e.copy(wb[:, 0:K_chunks, 0:gw], ws[:, 0:K_chunks, 0:gw]),
                 reads=[ws], writes=[wb])
            for m0 in range(0, gw, 128):
                m = min(128, gw - m0)
                for (t0, n) in tokb:
                    tiles = []
                    for sp in range(split):
                        ps_ = self.pst[self.pi % self.nps]
                        self.pi += 1
                        tiles.append(ps_)
                        for kk in range(kper):
                            kc = sp * kper + kk
                            P.op('tensor', lambda e, ps_=ps_, wb=wb, kc=kc, kk=kk, m0=m0, m=m, t0=t0, n=n: e.matmul(
                                ps_[0:m, 0:n], wb[:, kc, m0:m0 + m], rhs_fn(kc, t0, n),
                                start=(kk == 0), stop=(kk == kper - 1)),
                                reads=[wb] + rhs_reads(kc), writes=[ps_])
                    evac_fn(o0 + m0, m, t0, n, tiles if split > 1 else tiles[0])


def build_l3():
    P = Prog()
    with ExitStack() as st:
        P._stack = st
        xT = P.dram('xT', [128, NKC, T3], F32, 'ExternalInput')
        brT = P.dram('brT', [2048, T3], F32, 'ExternalInput')
        gT = P.dram('gT', [8192, T3], F32, 'ExternalInput')
        mv = P.dram('mv', [9, 128, NKC], F32, 'ExternalInput')
        wbr = P.dram('wbr', [2048, D], F32, 'ExternalInput')
        wo = P.dram('wo', [D, D], F32, 'ExternalInput')
        wr = P.dram('wr', [128, NKC, 16], F32, 'ExternalInput')
        rb = P.dram('rb', [16], F32, 'ExternalInput')
        wg = P.dram('wg', [16, D, 512], F32, 'ExternalInput')
        wu = P.dram('wu', [16, D, 512], F32, 'ExternalInput')
        wd = P.dram('wd', [16, 512, D], F32, 'ExternalInput')
        ident_d = P.dram('ident', [128, 128], F32, 'ExternalInput')
        x1T = P.dram('x1T', [128, NKC, T3], F32, 'Internal')
        x2T = P.dram('x2T', [128, NKC, T3], F32, 'ExternalOutput')

        mvt = P.sb('mvt', [128, 9, NKC])
        P.dma(mvt[:], AP(mv, 0, [[NKC, 128], [128 * NKC, 9], [1, NKC]]), reads=[mv], writes=[mvt])
        gl = P.sb('gl', [128, NKC]); gc = P.sb('gc', [128, NKC])
        P.op('vector', lambda e: e.scalar_tensor_tensor(gl[:], mvt[:, 3, :], 1.0, mvt[:, 0, :], ALU.add, ALU.mult),
             reads=[mvt], writes=[gl])
        P.op('vector', lambda e: e.scalar_tensor_tensor(gc[:], mvt[:, 7, :], 1.0, mvt[:, 0, :], ALU.add, ALU.mult),
             reads=[mvt], writes=[gc])
        ones_bf = P.sb('ones_bf', [128, 128], BF16)
        P.op('vector', lambda e: e.memset(ones_bf[:], 1.0), writes=[ones_bf])
        eps_t = P.sb('eps_t', [128, 1])
        P.op('vector', lambda e: e.memset(eps_t[:], 1e-6), writes=[eps_t])
        ident = P.sb('identsb', [128, 128])
        P.dma(ident[:], ident_d.ap(), reads=[ident_d], writes=[ident])
        wrt = P.sb('wrt', [128, NKC, 16])
        P.dma(wrt[:], wr.ap(), reads=[wr], writes=[wrt])
        wrb = P.sb('wrb', [128, NKC, 16], BF16)
        P.op('vector', lambda e: e.tensor_copy(wrb[:], wrt[:]), reads=[wrt], writes=[wrb])
        rbt = P.sb('rbt', [128, 16])
        P.dma(rbt[:], AP(rb, 0, [[0, 128], [1, 16]]), reads=[rb], writes=[rbt])

        lin = Lin(P, 'lin', grp=128, nps=4)
        bigT = P.sb('bigT', [128, NKC, T3], BF16)
        RY = P.sb('RY', [128, NKC, T3])
        mrgT = AP(RY, 0, [[NKC * T3, 128], [1, NKC * T3 * 2]]).bitcast(BF16) if False else RY.bitcast(BF16)
        stg = [P.sb('stg%d' % i, [128, 512]) for i in range(3)]
        si = [0]

        def nxt():
            s = stg[si[0] % 3]
            si[0] += 1
            return s
        for c in range(NKC):
            for (t0, n) in TOKB3:
                s = nxt()
                P.dma(s[:, 0:n], AP(brT, c * 128 * T3 + t0, [[T3, 128], [1, n]]), reads=[brT], writes=[s])
                P.op('gpsimd', lambda e, s=s, c=c, t0=t0, n=n: e.tensor_copy(bigT[:, c, t0:t0 + n], s[:, 0:n]),
                     reads=[s], writes=['bigT:%d' % c])
        gst = [P.sb('gst%d' % i, [128, 4, 512]) for i in range(1)]
        acc = P.sb('macc', [128, 512])
        acc2 = P.sb('macc2', [128, 512])
        gi = [0]

        def evacA(o0, m, t0, n, tiles):
            c = o0 // 128
            g_ = gst[0]
            gi[0] += 1
            P.dma(g_[:, :, 0:n], AP(gT, o0 * T3 + t0, [[T3, 128], [2048 * T3, 4], [1, n]]), reads=[gT], writes=[g_])
            P.op('scalar', lambda e: e.activation(g_[:, :, 0:n], g_[:, :, 0:n], AF.Sigmoid), reads=[g_], writes=[g_])
            P.op('vector', lambda e: e.tensor_tensor(acc[:, 0:n], tiles[0][:, 0:n], g_[:, 0, 0:n], ALU.mult),
                 reads=[tiles[0], g_], writes=[acc])
            for j in range(1, 4):
                P.op('vector', lambda e, j=j: e.tensor_tensor(acc2[:, 0:n], tiles[j][:, 0:n], g_[:, j, 0:n], ALU.mult),
                     reads=[tiles[j], g_], writes=[acc2])
                if j < 3:
                    P.op('gpsimd', lambda e: e.tensor_tensor(acc[:, 0:n], acc[:, 0:n], acc2[:, 0:n], ALU.add),
                         reads=[acc, acc2], writes=[acc])
                else:
                    P.op('gpsimd', lambda e: e.tensor_tensor(mrgT[:, c, t0:t0 + n], acc[:, 0:n], acc2[:, 0:n], ALU.add),
                         reads=[acc, acc2], writes=['mrgT:%d' % c])
        lin.run(wbr, 0, D, 16, D, lambda kc, t0, n: bigT[:, kc, t0:t0 + n], TOKB3, evacA,
                lambda kc: ['bigT:%d' % kc], split=4)

        xo = [P.sb('xo%d' % i, [128, 512]) for i in range(2)]
        bi = [0]

        def evacB(o0, m, t0, n, ps_):
            c = o0 // 128
            s = nxt()
            P.dma(s[:, 0:n], AP(xT, c * T3 + t0, [[NKC * T3, 128], [1, n]]), reads=[xT], writes=[s])
            o_ = xo[bi[0] % 2]
            bi[0] += 1
            gm = mvt[:, 1, c:c + 1] if t0 < NLAT3 else mvt[:, 5, c:c + 1]
            P.op('vector', lambda e: e.scalar_tensor_tensor(o_[:, 0:n], ps_[:, 0:n], gm, s[:, 0:n], ALU.mult, ALU.add),
                 reads=[ps_, s, mvt], writes=[o_])
            P.dma(AP(x1T, c * T3 + t0, [[NKC * T3, 128], [1, n]]), o_[:, 0:n], reads=[o_], writes=['x1T:%d' % (t0 // 512)])
        lin.run(wo, 0, D, 16, D, lambda kc, t0, n: mrgT[:, kc, t0:t0 + n], TOKB3, evacB, lambda kc: ['mrgT:%d' % kc])

        class X1:
            pass
        norm_phase(P, x1T, bigT, gl, AP_slice(mvt, 2), gc, AP_slice(mvt, 6), ones_bf, eps_t, T3, NLAT3,
                   x_reads=['x1T:%d' % i for i in range(3)], out_key='h2T', SUB=64)

        psb = P.ps('psb', [128, 512])
        psr = psb[0:16, :]
        pssm = P.ps('pssm', [128, 128])
        pst_ = pssm[:, 0:16]
        lg = P.sb('lg', [16, 1152])
        P.op('vector', lambda e: e.memset(lg[:], 0.0), writes=[lg])
        gateT = P.sb('gateT', [16, T3], BF16)
        for (t0, n) in TOKB3:
            for kc in range(NKC):
                P.op('tensor', lambda e, kc=kc, t0=t0, n=n: e.matmul(psr[:, 0:n], wrb[:, kc, :], bigT[:, kc, t0:t0 + n],
                                                                    start=(kc == 0), stop=(kc == NKC - 1)),
                     reads=[wrb, 'h2T:%d' % kc], writes=[psr])
            P.op('scalar', lambda e, t0=t0, n=n: e.activation(lg[:, t0:t0 + n], psr[:, 0:n], AF.Sigmoid),
                 reads=[psr], writes=[lg])
        sc = P.sb('r_sc', [128, 16]); sel = P.sb('r_sel', [128, 16]); t4 = P.sb('r_t4', [128, 4]); t4b = P.sb('r_t4b', [128, 4])
        e16 = P.sb('r_e16', [128, 16]); s2 = P.sb('r_s2', [128, 16]); gs = P.sb('r_gs', [128, 4]); t1 = P.sb('r_t1', [128, 1])
        gm_ = P.sb('r_gm', [128, 4]); oh = P.sb('r_oh', [128, 16]); gt = P.sb('r_gt', [128, 16])
        psg = pssm[0:16, :]

        def V(fn, reads, writes):
            P.op('vector', fn, reads=reads, writes=writes)

        def bc4(t):
            return AP(t, 0, [[4, 128], [1, 4], [0, 4]])

        def v44(t):
            return AP(t, 0, [[16, 128], [4, 4], [1, 4]])
        for tt in range((T3 + 127) // 128):
            t0 = tt * 128
            nt = min(128, T3 - t0)
            P.op('tensor', lambda e, t0=t0: e.transpose(pst_[:], lg[:, t0:t0 + 128], ident[0:16, 0:16]),
                 reads=[lg, ident], writes=[pst_])
            V(lambda e: e.tensor_copy(sc[:], pst_[:]), [pst_], [sc])
            V(lambda e: e.tensor_tensor(sel[:], sc[:], rbt[:], ALU.add), [sc, rbt], [sel])
            V(lambda e: e.tensor_reduce(t4[:], v44(sel), AX.X, ALU.max), [sel], [t4])
            V(lambda e: e.tensor_tensor(v44(e16), v44(sel), bc4(t4), ALU.is_equal), [sel, t4], [e16])
            V(lambda e: e.scalar_tensor_tensor(s2[:], e16[:], -BIG, sel[:], ALU.mult, ALU.add), [e16, sel], [s2])
            V(lambda e: e.tensor_reduce(t4b[:], v44(s2), AX.X, ALU.max), [s2], [t4b])
            V(lambda e: e.tensor_tensor(gs[:], t4[:], t4b[:], ALU.add), [t4, t4b], [gs])
            V(lambda e: e.tensor_reduce(t1[:], gs[:], AX.X, ALU.max), [gs], [t1])
            V(lambda e: e.tensor_scalar(gm_[:], gs[:], t1[:, 0:1], None, ALU.is_equal), [gs, t1], [gm_])
            V(lambda e: e.tensor_scalar(gm_[:], gm_[:], -1.0, BIG, ALU.add, ALU.mult), [gm_], [gm_])
            V(lambda e: e.tensor_tensor(v44(s2), v44(sel), bc4(gm_), ALU.add), [sel, gm_], [s2])
            V(lambda e: e.tensor_reduce(t1[:], s2[:], AX.X, ALU.max), [s2], [t1])
            V(lambda e: e.tensor_scalar(oh[:], s2[:], t1[:, 0:1], None, ALU.is_equal), [s2, t1], [oh])
            V(lambda e: e.scalar_tensor_tensor(s2[:], oh[:], -4 * BIG, s2[:], ALU.mult, ALU.add), [oh, s2], [s2])
            V(lambda e: e.tensor_reduce(t1[:], s2[:], AX.X, ALU.max), [s2], [t1])
            V(lambda e: e.scalar_tensor_tensor(oh[:], s2[:], t1[:, 0:1], oh[:], ALU.is_equal, ALU.add), [s2, t1, oh], [oh])
            V(lambda e: e.tensor_tensor(gt[:], sc[:], oh[:], ALU.mult), [sc, oh], [gt])
            V(lambda e: e.tensor_reduce(t1[:], gt[:], AX.X, ALU.add), [gt], [t1])
            V(lambda e: e.reciprocal(t1[:], t1[:]), [t1], [t1])
            V(lambda e: e.tensor_scalar(gt[:], gt[:], t1[:, 0:1], None, ALU.mult), [gt, t1], [gt])
            P.op('tensor', lambda e: e.transpose(psg[:], gt[:], ident[:]), reads=[gt, ident], writes=[psg])
            P.op('scalar', lambda e, t0=t0, nt=nt: e.copy(gateT[:, t0:t0 + nt], psg[:, 0:nt]), reads=[psg], writes=[gateT])
        selm = P.sb('selm', [16, 16, 128], BF16)
        V(lambda e: e.tensor_copy(selm[:], AP(ident, 0, [[128, 16], [1, 16], [0, 128]])), [ident], [selm])

        yacc = RY
        gact = P.sb('gact', [128, 4, T3], BF16)
        actT = P.sb('actT', [128, 4, T3], BF16)
        gb = P.sb('gb', [128, T3])
        tmpm = P.sb('tmpm', [128, 512])
        for h in range(1):
            tokb = TOKB3
            hb = 0
            for ex in range(16):
                for (t0, n) in tokb:
                    P.op('tensor', lambda e, ex=ex, t0=t0, n=n: e.matmul(psb[:, 0:n], selm[:, ex, :], gateT[:, t0:t0 + n],
                                                                        start=True, stop=True),
                         reads=[selm, gateT], writes=[psb])
                    P.op('scalar', lambda e, t0=t0, n=n: e.copy(gb[:, t0 - hb:t0 - hb + n], psb[:, 0:n]),
                         reads=[psb], writes=[gb])

                def evG(o0, m, t0, n, ps_):
                    fc = o0 // 128
                    P.op('scalar', lambda e: e.activation(gact[:, fc, t0 - hb:t0 - hb + n], ps_[:, 0:n], AF.Silu),
                         reads=[ps_], writes=[gact])
                lin.run(wg, ex * D * 512, 512, 16, 512, lambda kc, t0, n: bigT[:, kc, t0:t0 + n], tokb, evG,
                        lambda kc: ['h2T:%d' % kc])

                def evU(o0, m, t0, n, ps_):
                    fc = o0 // 128
                    P.op('vector', lambda e: e.tensor_tensor(tmpm[:, 0:n], ps_[:, 0:n], gact[:, fc, t0 - hb:t0 - hb + n], ALU.mult),
                         reads=[ps_, gact], writes=[tmpm])
                    P.op('gpsimd', lambda e: e.tensor_tensor(actT[:, fc, t0 - hb:t0 - hb + n], tmpm[:, 0:n],
                                                             gb[:, t0 - hb:t0 - hb + n], ALU.mult),
                         reads=[tmpm, gb], writes=[actT])
                lin.run(wu, ex * D * 512, 512, 16, 512, lambda kc, t0, n: bigT[:, kc, t0:t0 + n], tokb, evU,
                        lambda kc: ['h2T:%d' % kc])

                def evD(o0, m, t0, n, ps_, ex=ex):
                    c = o0 // 128
                    if ex == 0:
                        P.op('scalar', lambda e: e.copy(yacc[:, c, t0 - hb:t0 - hb + n], ps_[:, 0:n]),
                             reads=[ps_], writes=['yacc:%d' % c])
                    else:
                        P.op('vector', lambda e: e.tensor_tensor(yacc[:, c, t0 - hb:t0 - hb + n], ps_[:, 0:n],
                                                                 yacc[:, c, t0 - hb:t0 - hb + n], ALU.add),
                             reads=[ps_, 'yacc:%d' % c], writes=['yacc:%d' % c])
                lin.run(wd, ex * 512 * D, D, 4, D, lambda kc, t0, n: actT[:, kc, t0 - hb:t0 - hb + n], tokb, evD,
                        lambda kc: [actT])
            for c in range(NKC):
                for (t0, n) in tokb:
                    s = nxt()
                    P.dma(s[:, 0:n], AP(x1T, c * T3 + t0, [[NKC * T3, 128], [1, n]]), reads=['x1T:%d' % (t0 // 512)], writes=[s])
                    o_ = xo[bi[0] % 2]
                    bi[0] += 1
                    gm = mvt[:, 4, c:c + 1] if t0 < NLAT3 else mvt[:, 8, c:c + 1]
                    P.op('vector', lambda e, s=s, o_=o_, gm=gm, c=c, t0=t0, n=n: e.scalar_tensor_tensor(
                        o_[:, 0:n], yacc[:, c, t0 - hb:t0 - hb + n], gm, s[:, 0:n], ALU.mult, ALU.add),
                        reads=['yacc:%d' % c, s, mvt], writes=[o_])
                    P.dma(AP(x2T, c * T3 + t0, [[NKC * T3, 128], [1, n]]), o_[:, 0:n], reads=[o_], writes=[])
        P.finish()
        nc = P.emit()
    return nc


DEBUG = False
TA = 4352
NKT = 34


def build_l2a(with_ctx=True):
    P = Prog()
    with ExitStack() as st:
        P._stack = st
        gq = P.dram('gq', [256, TA], F32, 'ExternalInput')
        gk = P.dram('gk', [64, TA], F32, 'ExternalInput')
        gv = P.dram('gv', [TA, 64], F32, 'ExternalInput')
        cq = P.dram('cq', [512, TA], F32, 'ExternalInput')
        ckv = P.dram('ckv', [256, TA], F32, 'ExternalInput')
        kr = P.dram('kr', [64, TA], F32, 'ExternalInput')
        wuq = P.dram('wuq', [512, 384], F32, 'ExternalInput')
        wukv = P.dram('wukv', [256, 512], F32, 'ExternalInput')
        nw = P.dram('nw', [128, 8], F32, 'ExternalInput')
        cosd = P.dram('cosd', [64, TA], F32, 'ExternalInput')
        sind = P.dram('sind', [64, TA], F32, 'ExternalInput')
        rmd = P.dram('rmd', [64, 64], F32, 'ExternalInput')
        og = P.dram('og', [TA, 256], F32, 'ExternalOutput')
        om = P.dram('om', [TA, 256], F32, 'ExternalOutput')

        pp = [P.ps('pp%d' % i, [128, 512]) for i in range(8)]
        ones = P.sb('ones', [128, 128])
        P.op('vector', lambda e: e.memset(ones[:], 1.0), writes=[ones])
        nwt = P.sb('nwt', [128, 8])
        P.dma(nwt[:], nw.ap(), reads=[nw], writes=[nwt])
        rm = P.sb('rm', [64, 64])
        P.dma(rm[:], rmd.ap(), reads=[rmd], writes=[rm])
        eps64 = P.sb('eps64', [128, 1])
        P.op('vector', lambda e: e.memset(eps64[:], 1e-6), writes=[eps64])
        wq_s = P.sb('wq_s', [128, 4, 384]); wq_b = P.sb('wq_b', [128, 4, 384], BF16)
        wqr_b = P.sb('wqr_b', [128, 4, 128], BF16)
        wkv_s = P.sb('wkv_s', [128, 2, 512]); wkv_b = P.sb('wkv_b', [128, 2, 512], BF16)
        P.dma(wq_s[:], AP(wuq, 0, [[384, 128], [128 * 384, 4], [1, 384]]), reads=[wuq], writes=[wq_s])
        P.dma(wkv_s[:], AP(wukv, 0, [[512, 128], [128 * 512, 2], [1, 512]]), reads=[wukv], writes=[wkv_s])
        P.op('vector', lambda e: e.tensor_copy(wq_b[:], wq_s[:]), reads=[wq_s], writes=[wq_b])
        P.op('vector', lambda e: e.tensor_copy(wkv_b[:], wkv_s[:]), reads=[wkv_s], writes=[wkv_b])
        for h in range(2):
            P.op('vector', lambda e, h=h: e.tensor_copy(wqr_b[:, :, h * 64:h * 64 + 32], wq_s[:, :, h * 192 + 160:h * 192 + 192]),
                 reads=[wq_s], writes=[wqr_b])
            P.op('vector', lambda e, h=h: e.tensor_copy(wqr_b[:, :, h * 64 + 32:h * 64 + 64], wq_s[:, :, h * 192 + 128:h * 192 + 160]),
                 reads=[wq_s], writes=[wqr_b])

        GQ = P.sb('GQ', [64, 4, TA], BF16)
        GK = P.sb('GK', [64, TA], BF16)
        GV = P.sb('GV', [128, NKT, 65], BF16)
        MQN = P.sb('MQN', [128, 2, TA], BF16)
        MQR = P.sb('MQR', [64, 2, TA], BF16)
        MKN = P.sb('MKN', [128, 2, TA], BF16)
        MKR = P.sb('MKR', [64, TA], BF16)
        MV = P.sb('MV', [128, NKT, 2, 129], BF16)
        P.op('vector', lambda e: e.memset(GV[:], 1.0), writes=[GV])
        P.op('vector', lambda e: e.memset(MV[:], 1.0), writes=[MV])
        gvs = P.sb('gvs', [128, NKT, 64])
        P.dma(gvs[:], AP(gv, 0, [[64, 128], [128 * 64, NKT], [1, 64]]), reads=[gv], writes=[gvs])
        P.op('vector', lambda e: e.tensor_copy(GV[:, :, 0:64], gvs[:]), reads=[gvs], writes=[GV])

        xs = [P.sb('xa%d' % i, [128, 4, 512]) for i in range(2)]
        sq = P.sb('sqa', [128, 4, 512])
        rs = P.sb('rsa', [128, 512])
        xn = P.sb('xna', [128, 512])
        xnb = P.sb('xnb', [128, 4, 512], BF16)
        xnk = P.sb('xnk', [128, 2, 512], BF16)
        cs = P.sb('csa', [64, 512]); sn = P.sb('sna', [64, 512])
        t1 = P.sb('t1a', [128, 512]); t2 = P.sb('t2a', [128, 512])
        ppi = [0]
        fence = P.sb('fence', [128, 8], BF16)

        def nps():
            p_ = pp[ppi[0] % 8]
            ppi[0] += 1
            return p_

        def rstd_of(x_, rows, nch, n, dim):
            P.op('scalar', lambda e: e.activation(sq[0:rows, 0:nch, 0:n], x_[0:rows, 0:nch, 0:n], AF.Square), reads=[x_], writes=[sq])
            ps_ = nps()
            for c in range(nch):
                P.op('tensor', lambda e, c=c: e.matmul(ps_[0:rows, 0:n], ones[0:rows, 0:rows], sq[0:rows, c, 0:n],
                                                       start=(c == 0), stop=(c == nch - 1)), reads=[ones, sq], writes=[ps_])
            P.op('scalar', lambda e: e.activation(rs[0:rows, 0:n], ps_[0:rows, 0:n], AF.Sqrt, bias=eps64[0:rows, :], scale=1.0 / dim),
                 reads=[ps_, eps64], writes=[rs])
            P.op('vector', lambda e: e.reciprocal(rs[0:rows, 0:n], rs[0:rows, 0:n]), reads=[rs], writes=[rs])

        def rope_to(dst, src, n):
            ps_ = nps()
            P.op('tensor', lambda e: e.matmul(ps_[0:64, 0:n], rm[:], src, start=True, stop=True), reads=[rm, src], writes=[ps_])
            P.op('vector', lambda e: e.tensor_tensor(t1[0:64, 0:n], ps_[0:64, 0:n], sn[:, 0:n], ALU.mult), reads=[ps_, sn], writes=[t1])
            P.op('gpsimd', lambda e: e.tensor_tensor(t2[0:64, 0:n], src, cs[:, 0:n], ALU.mult), reads=[src, cs], writes=[t2])
            P.op('vector', lambda e: e.tensor_tensor(dst, t1[0:64, 0:n], t2[0:64, 0:n], ALU.add), reads=[t1, t2], writes=[dst])

        for blk in range(9):
            t0 = blk * 512
            n = min(512, TA - t0)
            P.dma(cs[:, 0:n], AP(cosd, t0, [[TA, 64], [1, n]]), reads=[cosd], writes=[cs])
            P.dma(sn[:, 0:n], AP(sind, t0, [[TA, 64], [1, n]]), reads=[sind], writes=[sn])
            x_ = xs[0]
            P.dma(x_[0:64, :, 0:n], AP(gq, t0, [[TA, 64], [64 * TA, 4], [1, n]]), reads=[gq], writes=[x_])
            for h in range(4):
                P.op('scalar', lambda e, h=h: e.activation(sq[0:64, 0, 0:n], x_[0:64, h, 0:n], AF.Square), reads=[x_], writes=[sq])
                ps_ = nps()
                P.op('tensor', lambda e, ps_=ps_: e.matmul(ps_[0:64, 0:n], ones[0:64, 0:64], sq[0:64, 0, 0:n], start=True, stop=True),
                     reads=[ones, sq], writes=[ps_])
                P.op('scalar', lambda e, ps_=ps_: e.activation(rs[0:64, 0:n], ps_[0:64, 0:n], AF.Sqrt, bias=eps64[0:64, :], scale=1.0 / 64),
                     reads=[ps_, eps64], writes=[rs])
                P.op('vector', lambda e: e.reciprocal(rs[0:64, 0:n], rs[0:64, 0:n]), reads=[rs], writes=[rs])
                P.op('vector', lambda e, h=h: e.scalar_tensor_tensor(xn[0:64, 0:n], x_[0:64, h, 0:n], nwt[0:64, 6:7], rs[0:64, 0:n],
                                                                     ALU.mult, ALU.mult), reads=[x_, nwt, rs], writes=[xn])
                rope_to(GQ[:, h, t0:t0 + n], xn[0:64, 0:n], n)
            x2 = xs[1]
            P.dma(x2[0:64, 0, 0:n], AP(gk, t0, [[TA, 64], [1, n]]), reads=[gk], writes=[x2])
            rstd_of(x2, 64, 1, n, 64)
            P.op('vector', lambda e: e.scalar_tensor_tensor(xn[0:64, 0:n], x2[0:64, 0, 0:n], nwt[0:64, 7:8], rs[0:64, 0:n],
                                                            ALU.mult, ALU.mult), reads=[x2, nwt, rs], writes=[xn])
            rope_to(GK[:, t0:t0 + n], xn[0:64, 0:n], n)
            P.dma(x2[0:64, 1, 0:n], AP(kr, t0, [[TA, 64], [1, n]]), reads=[kr], writes=[x2])
            rope_to(MKR[:, t0:t0 + n], x2[0:64, 1, 0:n], n)
            P.dma(x_[:, :, 0:n], AP(cq, t0, [[TA, 128], [128 * TA, 4], [1, n]]), reads=[cq], writes=[x_])
            rstd_of(x_, 128, 4, n, 512)
            for c in range(4):
                P.op('vector', lambda e, c=c: e.scalar_tensor_tensor(xnb[:, c, 0:n], x_[:, c, 0:n], nwt[:, c:c + 1], rs[:, 0:n],
                                                                     ALU.mult, ALU.mult), reads=[x_, nwt, rs], writes=[xnb])
            P.op('vector', lambda e: e.drain(), reads=[xnb], writes=[xnb])
            if DEBUG and blk == 0:
                d1 = P.dram('d_xnb', [128, 4 * 512], BF16, 'ExternalOutput'); d2 = P.dram('d_rs', [128, 512], F32, 'ExternalOutput'); d3 = P.dram('d_x', [128, 4*512], F32, 'ExternalOutput')
                P.dma(d1.ap(), xnb[:], reads=[xnb], writes=[]); P.dma(d2.ap(), rs[:], reads=[rs], writes=[]); P.dma(d3.ap(), x_[:], reads=[x_], writes=[])
            for h in (0, 1):
                ps_ = nps()
                for c in range(4):
                    P.op('tensor', lambda e, ps_=ps_, c=c, h=h: e.matmul(ps_[:, 0:n], wq_b[:, c, h * 192:h * 192 + 128], xnb[:, c, 0:n],
                                                                         start=(c == 0), stop=(c == 3)), reads=[wq_b, xnb], writes=[ps_])
                P.op('vector', lambda e, t0=t0, ps_=ps_, h=h: e.tensor_copy(MQN[:, h, t0:t0 + n], ps_[:, 0:n]), reads=[ps_], writes=[MQN])
                pa = nps(); pb = nps()
                for c in range(4):
                    P.op('tensor', lambda e, pa=pa, c=c, h=h: e.matmul(pa[0:64, 0:n], wq_b[:, c, h * 192 + 128:h * 192 + 192], xnb[:, c, 0:n],
                                                                       start=(c == 0), stop=(c == 3)), reads=[wq_b, xnb], writes=[pa])
                for c in range(4):
                    P.op('tensor', lambda e, pb=pb, c=c, h=h: e.matmul(pb[0:64, 0:n], wqr_b[:, c, h * 64:h * 64 + 64], xnb[:, c, 0:n],
                                                                       start=(c == 0), stop=(c == 3)), reads=[wqr_b, xnb], writes=[pb])
                P.op('vector', lambda e, pa=pa: e.tensor_tensor(t1[0:64, 0:n], pa[0:64, 0:n], cs[:, 0:n], ALU.mult), reads=[pa, cs], writes=[t1])
                P.op('vector', lambda e, pb=pb: e.tensor_tensor(t2[0:64, 0:n], pb[0:64, 0:n], sn[:, 0:n], ALU.mult), reads=[pb, sn], writes=[t2])
                P.op('gpsimd', lambda e, t0=t0, h=h: e.tensor_tensor(MQR[:, h, t0:t0 + n], t1[0:64, 0:n], t2[0:64, 0:n], ALU.add), reads=[t1, t2], writes=[MQR])
            P.dma(x2[:, 2:4, 0:n], AP(ckv, t0, [[TA, 128], [128 * TA, 2], [1, n]]), reads=[ckv], writes=[x2])
            P.op('scalar', lambda e: e.activation(sq[:, 0:2, 0:n], x2[:, 2:4, 0:n], AF.Square), reads=[x2], writes=[sq])
            ps_ = nps()
            for c in range(2):
                P.op('tensor', lambda e, c=c, ps_=ps_: e.matmul(ps_[:, 0:n], ones[:], sq[:, c, 0:n], start=(c == 0), stop=(c == 1)),
                     reads=[ones, sq], writes=[ps_])
            P.op('scalar', lambda e, ps_=ps_: e.activation(rs[:, 0:n], ps_[:, 0:n], AF.Sqrt, bias=eps64[:], scale=1.0 / 256),
                 reads=[ps_, eps64], writes=[rs])
            P.op('vector', lambda e: e.reciprocal(rs[:, 0:n], rs[:, 0:n]), reads=[rs], writes=[rs])
            for c in range(2):
                P.op('vector', lambda e, c=c: e.scalar_tensor_tensor(xnk[:, c, 0:n], x2[:, 2 + c, 0:n], nwt[:, 4 + c:5 + c], rs[:, 0:n],
                                                                     ALU.mult, ALU.mult), reads=[x2, nwt, rs], writes=[xnk])
            P.op('vector', lambda e: e.drain(), reads=[xnk], writes=[xnk])
            for h in range(2):
                ps_ = nps()
                for c in range(2):
                    P.op('tensor', lambda e, ps_=ps_, c=c, h=h: e.matmul(ps_[:, 0:n], wkv_b[:, c, h * 256:h * 256 + 128], xnk[:, c, 0:n],
                                                                         start=(c == 0), stop=(c == 1)), reads=[wkv_b, xnk], writes=[ps_])
                P.op('scalar', lambda e, t0=t0, ps_=ps_, h=h: e.copy(MKN[:, h, t0:t0 + n], ps_[:, 0:n]), reads=[ps_], writes=[MKN])
                for j in range(n // 128):
                    ps2 = nps()
                    kt = (t0 + j * 128) // 128
                    for c in range(2):
                        P.op('tensor', lambda e, ps2=ps2, c=c, h=h, j=j: e.matmul(ps2[:, 0:128], xnk[:, c, j * 128:(j + 1) * 128],
                                                                                 wkv_b[:, c, h * 256 + 128:h * 256 + 256],
                                                                                 start=(c == 0), stop=(c == 1)), reads=[wkv_b, xnk], writes=[ps2])
                    P.op('vector', lambda e, ps2=ps2, h=h, kt=kt: e.tensor_copy(MV[:, kt, h, 0:128], ps2[:, 0:128]), reads=[ps2], writes=[MV])

        pT = [P.sb('pT%d' % i, [128, 512], BF16) for i in range(4)]
        osb = [P.sb('osb%d' % i, [128, 128]) for i in range(2)]
        rec = P.sb('rec', [128, 1])
        cnt = [0]
        DEPTH = 2

        def attn(qparts, kparts, vfn, dv, scale, t0, n, kts, out, ocol, ow):
            sps = [pp[0], pp[1], pp[6], pp[7]]
            ops_ = pp[2:6]
            nq = n // 128
            bufs = []
            nk = len(kts)
            for step in range(nk + DEPTH):
                if step < nk:
                    kt = kts[step]
                    s_ = sps[cnt[0] % 4]
                    p_ = pT[cnt[0] % 4]
                    cnt[0] += 1
                    bufs.append(p_)
                    for i, (qf, kf) in enumerate(zip(qparts, kparts)):
                        P.op('tensor', lambda e, s_=s_, qf=qf, kf=kf, i=i, kt=kt: e.matmul(
                            s_[:, 0:n], kf(kt), qf(t0, n), start=(i == 0), stop=(i == len(qparts) - 1)),
                            reads=[kf(kt), qf(t0, n)], writes=[s_])
                    P.op('scalar', lambda e, s_=s_, p_=p_: e.activation(p_[:, 0:n], s_[:, 0:n], AF.Exp, scale=scale), reads=[s_], writes=[p_])
                if step >= DEPTH:
                    ki = step - DEPTH
                    kt = kts[ki]
                    p_ = bufs[ki]
                    for qs in range(nq):
                        P.op('tensor', lambda e, p_=p_, qs=qs, kt=kt, ki=ki: e.matmul(
                            ops_[qs][:, 0:dv + 1], p_[:, qs * 128:(qs + 1) * 128], vfn(kt), start=(ki == 0), stop=(ki == nk - 1)),
                            reads=[p_, vfn(kt)], writes=[ops_[qs]])
            for qs in range(nq):
                o_ = osb[qs % 2]
                P.op('vector', lambda e, qs=qs: e.reciprocal(rec[:], ops_[qs][:, dv:dv + 1]), reads=[ops_[qs]], writes=[rec])
                P.op('vector', lambda e, qs=qs, o_=o_: e.tensor_scalar(o_[:, 0:dv], ops_[qs][:, 0:dv], rec[:, 0:1], None, ALU.mult),
                     reads=[ops_[qs], rec], writes=[o_])
                P.dma(AP(out, (t0 + qs * 128) * ow + ocol, [[ow, 128], [1, dv]]), o_[:, 0:dv], reads=[o_], writes=[])

        all_k = list(range(NKT))
        ctx_k = [32, 33]
        qblocks = [(i * 512, 512, all_k) for i in range(8)]
        if with_ctx:
            qblocks.append((4096, 256, ctx_k))
        for (t0, n, kts) in qblocks:
            for h in range(4):
                attn([lambda a, b, h=h: GQ[:, h, a:a + b]], [lambda kt: GK[:, kt * 128:(kt + 1) * 128]],
                     lambda kt: GV[:, kt, :], 64, 0.125, t0, n, kts, og, h * 64, 256)
            for h in range(2):
                attn([lambda a, b, h=h: MQN[:, h, a:a + b], lambda a, b, h=h: MQR[:, h, a:a + b]],
                     [lambda kt, h=h: MKN[:, h, kt * 128:(kt + 1) * 128], lambda kt: MKR[:, kt * 128:(kt + 1) * 128]],
                     lambda kt, h=h: MV[:, kt, h, :], 128, 192 ** -0.5, t0, n, kts, om, h * 128, 256)
        if DEBUG:
            for nm, t_, shp in [('d_GQ', GQ, [64, 4 * TA]), ('d_GK', GK, [64, TA]), ('d_MQR', MQR, [64, 2 * TA]), ('d_MKR', MKR, [64, TA]), ('d_MQN', MQN, [128, 2 * TA]), ('d_MKN', MKN, [128, 2*TA])]:
                dd = P.dram(nm, shp, BF16, 'ExternalOutput')
                P.dma(dd.ap(), t_[:], reads=[t_], writes=[])
        P.finish()
        nc = P.emit()
    return nc


HC = 64
TWO_PI = 2.0 * math.pi


def build_l2h():
    P = Prog()
    with ExitStack() as st:
        P._stack = st
        seqs = []
        for nm, L in (('L', 4096), ('C', 256)):
            nb = L // 128
            seqs.append(dict(
                nm=nm, L=L, nb=nb, NQ=128 * (2 * nb - 1), HPLEN=256 * nb, pad=L // 2 - 1,
                U=P.dram('U' + nm, [3, 3, 128, HC * 4 * nb], F32, 'ExternalInput'),
                feats=P.dram('feats' + nm, [33, L], F32, 'ExternalInput'),
                dec=P.dram('dec' + nm, [2, HC, L], F32, 'ExternalInput'),
                hp=P.dram('hp' + nm, [2, HC, 256 * nb], BF16, 'Internal'),
                out=P.dram('y' + nm, [128, HC * 4 * nb], F32, 'ExternalOutput')))
        cw = P.dram('cw', [3, 3, HC], F32, 'ExternalInput')
        w1 = P.dram('w1', [33, 64], F32, 'ExternalInput')
        w2 = P.dram('w2', [64, 64], F32, 'ExternalInput')
        w3 = P.dram('w3', [64, 2, HC], F32, 'ExternalInput')
        pv = P.dram('pv', [64, 4], F32, 'ExternalInput')
        hb = P.dram('hb', [2, HC], F32, 'ExternalInput')
        jm = P.dram('jm', [128, 128], F32, 'ExternalInput')

        F = 8192
        A1 = P.sb('A1', [128, F]); A2 = P.sb('A2', [128, F]); vN = P.sb('vN', [128, F]); xN = P.sb('xN', [128, F])
        zR = P.sb('zR', [128, F], BF16)
        toep = [P.sb('toep%d' % i, [128, 8064], BF16) for i in range(2)]
        wbc = P.sb('wbc', [128, 9 * HC]); hbc = P.sb('hbc', [128, 2 * HC])
        P.dma(wbc[:], AP(cw, 0, [[0, 128], [1, 9 * HC]]), reads=[cw], writes=[wbc])
        P.dma(hbc[:], AP(hb, 0, [[0, 128], [1, 2 * HC]]), reads=[hb], writes=[hbc])
        w1t = P.sb('w1t', [33, 64]); w2t = P.sb('w2t', [64, 64]); w3t = P.sb('w3t', [64, 2 * HC]); pvt = P.sb('pvt', [64, 4])
        jt = P.sb('jt', [128, 128])
        P.dma(w1t[:], w1.ap(), reads=[w1], writes=[w1t]); P.dma(w2t[:], w2.ap(), reads=[w2], writes=[w2t])
        P.dma(w3t[:], AP(w3, 0, [[2 * HC, 64], [1, 2 * HC]]), reads=[w3], writes=[w3t]); P.dma(pvt[:], pv.ap(), reads=[pv], writes=[pvt])
        P.dma(jt[:], jm.ap(), reads=[jm], writes=[jt])
        zero_bf = P.sb('zero_bf', [64, 2304], BF16)
        P.op('vector', lambda e: e.memset(zero_bf[:], 0.0), writes=[zero_bf])
        pp = [P.ps('pp%d' % i, [128, 512]) for i in range(6)]
        ppi = [0]

        def nps():
            p_ = pp[ppi[0] % 6]
            ppi[0] += 1
            return p_
        ft = [P.sb('ft%d' % i, [33, 256]) for i in range(2)]
        dct = [P.sb('dct%d' % i, [64, 2, 256]) for i in range(2)]
        arg = P.sb('arg', [64, 256]); ki = P.sb('ki', [64, 256], I32); kf = P.sb('kf', [64, 256])
        rsum = P.sb('rsum', [64, 2])

        def sin_to(dst, ps_, bcol, fcol):
            P.op('vector', lambda e: e.tensor_scalar(arg[:], ps_, pvt[:, bcol:bcol + 1], pvt[:, fcol:fcol + 1], ALU.add, ALU.mult),
                 reads=[ps_, pvt], writes=[arg])
            P.op('vector', lambda e: e.tensor_scalar(ki[:], arg[:], 1.0 / TWO_PI, None, ALU.mult), reads=[arg], writes=[ki])
            P.op('vector', lambda e: e.tensor_copy(kf[:], ki[:]), reads=[ki], writes=[kf])
            P.op('vector', lambda e: e.scalar_tensor_tensor(arg[:], kf[:], -TWO_PI, arg[:], ALU.mult, ALU.add), reads=[kf, arg], writes=[arg])
            P.op('vector', lambda e: e.tensor_scalar(kf[:], arg[:], math.pi, -TWO_PI, ALU.is_gt, ALU.mult), reads=[arg], writes=[kf])
            P.op('vector', lambda e: e.tensor_tensor(arg[:], arg[:], kf[:], ALU.add), reads=[arg, kf], writes=[arg])
            P.op('vector', lambda e: e.tensor_scalar(kf[:], arg[:], -math.pi, TWO_PI, ALU.is_lt, ALU.mult), reads=[arg], writes=[kf])
            P.op('vector', lambda e: e.tensor_tensor(arg[:], arg[:], kf[:], ALU.add), reads=[arg, kf], writes=[arg])
            P.op('vector', lambda e: e.tensor_scalar(arg[:], arg[:], 3.14159, -3.14159, ALU.min, ALU.max), reads=[arg], writes=[arg])
            P.op('scalar', lambda e: e.activation(dst, arg[:], AF.Sin), reads=[arg], writes=[dst])

        def filter_gen(S):
            L, hp, pad, HPLEN = S['L'], S['hp'], S['pad'], S['HPLEN']
            hid1 = A1[0:64, 0:L]; hid2 = A1[0:64, 4096:4096 + L]
            fr = [A2[0:64, 0:L], A2[0:64, 4096:4096 + L]]
            for bi, t0 in enumerate(range(0, L, 256)):
                f_ = ft[bi % 2]; d_ = dct[bi % 2]
                P.dma(f_[:], AP(S['feats'], t0, [[L, 33], [1, 256]]), reads=[S['feats']], writes=[f_])
                P.dma(d_[:], AP(S['dec'], t0, [[L, 64], [HC * L, 2], [1, 256]]), reads=[S['dec']], writes=[d_])
                ps_ = nps()
                P.op('tensor', lambda e, ps_=ps_, f_=f_: e.matmul(ps_[0:64, 0:256], w1t[:], f_[:], start=True, stop=True),
                     reads=[w1t, f_], writes=[ps_])
                sin_to(hid1[:, t0:t0 + 256], ps_[0:64, 0:256], 0, 2)
                ps_ = nps()
                P.op('tensor', lambda e, ps_=ps_, t0=t0: e.matmul(ps_[0:64, 0:256], w2t[:], hid1[:, t0:t0 + 256], start=True, stop=True),
                     reads=[w2t, A1], writes=[ps_])
                sin_to(hid2[:, t0:t0 + 256], ps_[0:64, 0:256], 1, 3)
                for o in range(2):
                    ps_ = nps()
                    P.op('tensor', lambda e, ps_=ps_, t0=t0, o=o: e.matmul(ps_[0:64, 0:256], w3t[:, o * HC:(o + 1) * HC], hid2[:, t0:t0 + 256],
                                                                          start=True, stop=True), reads=[w3t, A1], writes=[ps_])
                    P.op('vector', lambda e, ps_=ps_, t0=t0, o=o, d_=d_: e.tensor_tensor(fr[o][:, t0:t0 + 256], ps_[0:64, 0:256], d_[:, o, :], ALU.mult),
                         reads=[ps_, d_], writes=[A2])
            for o in range(2):
                P.op('vector', lambda e, o=o: e.tensor_reduce(rsum[:, o:o + 1], fr[o], AX.X, ALU.add, apply_absolute_value=True),
                     reads=[A2], writes=[rsum])
            P.op('vector', lambda e: e.reciprocal(rsum[:], rsum[:]), reads=[rsum], writes=[rsum])
            for o in range(2):
                fb = zR[0:64, o * 4096:o * 4096 + L]
                P.op('vector', lambda e, o=o, fb=fb: e.tensor_scalar(fb, fr[o], rsum[:, o:o + 1], None, ALU.mult), reads=[A2, rsum], writes=[zR])
                P.dma(AP(hp, o * HC * HPLEN + pad, [[HPLEN, 64], [1, L]]), fb, reads=[zR], writes=[hp])
                n1 = pad
                n2 = HPLEN - pad - L
                P.dma(AP(hp, o * HC * HPLEN, [[HPLEN, 64], [1, n1]]), zero_bf[:, 0:n1], reads=[zero_bf], writes=[hp])
                P.dma(AP(hp, o * HC * HPLEN + pad + L, [[HPLEN, 64], [1, n2]]), zero_bf[:, 0:n2], reads=[zero_bf], writes=[hp])

        def conv3(S, g, dest):
            n = HC * 4 * S['nb']
            for k in range(3):
                P.dma(A1[:, 0:n], AP(S['U'], (g * 3 + k) * 128 * n, [[n, 128], [1, n]]), reads=[S['U']], writes=[A1])
                wv = AP(wbc, (k * 3 + g) * HC, [[9 * HC, 128], [1, HC], [0, 4 * S['nb']]])
                a1v = AP(A1, 0, [[F, 128], [4 * S['nb'], HC], [1, 4 * S['nb']]])
                dv = AP(dest, 0, [[F, 128], [4 * S['nb'], HC], [1, 4 * S['nb']]])
                if k == 0:
                    P.op('vector', lambda e, a1v=a1v, wv=wv, dv=dv: e.tensor_tensor(dv, a1v, wv, ALU.mult), reads=[A1, wbc], writes=[dest])
                else:
                    P.op('vector', lambda e, a1v=a1v, wv=wv: e.tensor_tensor(a1v, a1v, wv, ALU.mult), reads=[A1, wbc], writes=[A1])
                    P.op('gpsimd', lambda e, n=n: e.tensor_tensor(dest[:, 0:n], dest[:, 0:n], A1[:, 0:n], ALU.add), reads=[dest, A1], writes=[dest])

        def reverse_to_zR(S, src):
            n = HC * 4 * S['nb']
            for i, c0 in enumerate(range(0, n, 256)):
                ps_ = nps()
                P.op('tensor', lambda e, ps_=ps_, c0=c0: e.matmul(ps_[:, 0:256], jt[:], src[:, c0:c0 + 256], start=True, stop=True),
                     reads=[jt, src], writes=[ps_])
                if i % 2 == 0:
                    P.op('vector', lambda e, ps_=ps_, c0=c0: e.tensor_copy(zR[:, c0:c0 + 256], ps_[:, 0:256]), reads=[ps_], writes=[zR])
                else:
                    P.op('scalar', lambda e, ps_=ps_, c0=c0: e.copy(zR[:, c0:c0 + 256], ps_[:, 0:256]), reads=[ps_], writes=[zR])

        tcnt = [0]

        def longconv(S, o):
            nb, NQ, HPLEN = S['nb'], S['NQ'], S['HPLEN']
            for c in range(HC):
                tp = toep[tcnt[0] % 2]
                q = 'sync' if tcnt[0] % 2 == 0 else 'gpsimd'
                tcnt[0] += 1
                P.dma(tp[:, 0:NQ], AP(S['hp'], (o * HC + c) * HPLEN, [[1, 128], [1, NQ]]), reads=[S['hp']], writes=[tp], q=q)
                ps_ = nps()
                ms = [0] + [m for m in range(-(nb - 1), nb) if m != 0]
                for mi, m in enumerate(ms):
                    a_lo, a_hi = max(0, m), min(nb, nb + m)
                    cnt = a_hi - a_lo
                    outv = AP(ps_, a_lo, [[512, 128], [nb, 4], [1, cnt]])
                    rhs = AP(zR, c * 4 * nb + (a_lo - m), [[F, 128], [nb, 4], [1, cnt]])
                    P.op('tensor', lambda e, outv=outv, rhs=rhs, tp=tp, m=m, mi=mi: e.matmul(
                        outv, tp[:, 128 * (m + nb - 1):128 * (m + nb)], rhs, start=(mi == 0), stop=(mi == len(ms) - 1), skip_group_check=True),
                        reads=[tp, zR], writes=[ps_])
                dst = A2[:, c * 4 * nb:(c + 1) * 4 * nb]
                if c % 2 == 0:
                    P.op('vector', lambda e, ps_=ps_, dst=dst: e.tensor_copy(dst, ps_[:, 0:4 * nb]), reads=[ps_], writes=[A2])
                else:
                    P.op('scalar', lambda e, ps_=ps_, dst=dst: e.copy(dst, ps_[:, 0:4 * nb]), reads=[ps_], writes=[A2])

        def gate(S, o, z, x):
            nb = S['nb']
            n = HC * 4 * nb
            bv = AP(hbc, o * HC, [[2 * HC, 128], [1, HC], [0, 4 * nb]])
            zv = AP(z, 0, [[F, 128], [4 * nb, HC], [1, 4 * nb]])
            P.op('vector', lambda e: e.tensor_tensor(zv, zv, bv, ALU.mult), reads=[z, hbc], writes=[z])
            P.op('gpsimd', lambda e: e.tensor_tensor(z[:, 0:n], z[:, 0:n], A2[:, 0:n], ALU.add), reads=[z, A2], writes=[z])
            P.op('vector', lambda e: e.tensor_tensor(z[:, 0:n], z[:, 0:n], x[:, 0:n], ALU.mult), reads=[z, x], writes=[z])

        for S in seqs:
            filter_gen(S)
            n = HC * 4 * S['nb']
            conv3(S, 0, vN)
            conv3(S, 1, xN)
            reverse_to_zR(S, vN)
            longconv(S, 0)
            gate(S, 0, vN, xN)
            conv3(S, 2, xN)
            reverse_to_zR(S, vN)
            longconv(S, 1)
            gate(S, 1, vN, xN)
            P.dma(S['out'].ap(), vN[:, 0:n], reads=[vN], writes=[])
        P.finish()
        nc = P.emit()
    return nc


def hy_consts(L):
    t = np.arange(L, dtype=np.float32)
    bands = 16
    f = np.linspace(1e-4, bands - 1, bands, dtype=np.float32)
    phase = (np.float32(2.0 * math.pi / L) * t[:, None] * f[None, :]).astype(np.float32)
    feats = np.concatenate([t[:, None] / np.float32(L - 1), np.cos(phase), -np.sin(phase)], -1).astype(np.float32)
    centre = L // 2
    dist = (np.abs(t - centre) / centre).astype(np.float32)
    deltas = np.abs(np.linspace(math.log(1e-2) / 1.5, math.log(1e-2) / 0.3, 1024, dtype=np.float32))
    dec = np.exp(-dist[:, None] * deltas[None, :]).astype(np.float32)
    return np.ascontiguousarray(feats.T), dec


def hy_inputs(core, pl_h, pc_h, inp, L_):
    c0 = core * HC
    d = {}
    for nm, u, L in (('L', pl_h, 4096), ('C', pc_h, 256)):
        nb = L // 128
        feats, dec = hy_consts(L)
        d['feats' + nm] = feats
        d['dec' + nm] = np.ascontiguousarray(dec.reshape(L, 2, 512)[:, :, c0:c0 + HC].transpose(1, 2, 0))
        ug = u.reshape(4, L, 3, 512)[:, :, :, c0:c0 + HC]
        up = np.pad(ug, ((0, 0), (1, 1), (0, 0), (0, 0)))
        U = np.empty((3, 3, 128, HC, 4, nb), np.float32)
        for k in range(3):
            s = up[:, k:k + L]
            U[:, k] = s.reshape(4, nb, 128, 3, HC).transpose(3, 2, 4, 0, 1)
        d['U' + nm] = U.reshape(3, 3, 128, HC * 4 * nb)
    d['cw'] = np.ascontiguousarray(inp['hy_conv_w'][L_].reshape(3, 3, 512)[:, :, c0:c0 + HC])
    d['w1'] = inp['hy_w1'][L_]; d['w2'] = inp['hy_w2'][L_]
    d['w3'] = np.ascontiguousarray(inp['hy_w3'][L_].reshape(64, 2, 512)[:, :, c0:c0 + HC])
    d['pv'] = np.ascontiguousarray(np.stack([inp['hy_b1'][L_], inp['hy_b2'][L_], inp['hy_sin_freq'][L_][0], inp['hy_sin_freq'][L_][1]], 1))
    d['hb'] = np.ascontiguousarray(inp['hy_bias'][L_][:, c0:c0 + HC])
    d['jm'] = np.ascontiguousarray(np.eye(128, dtype=np.float32)[::-1])
    return d


def hy_unpack(y, L):
    nb = L // 128
    return y.reshape(128, HC, 4, nb).transpose(2, 3, 0, 1).reshape(4, L, HC)


TG = 4352
F32R = mybir.dt.float32r
NCH = 68


def gdn_groups(d):
    ctx = [0, 1, 2, 3]
    lat = [list(range(4 + 8 * i, 12 + 8 * i)) for i in range(8)]
    if d == 0:
        return [ctx] + lat
    return [ctx[::-1]] + [g[::-1] for g in lat[::-1]]


def build_l2g(NB=2):
    P = Prog()
    with ExitStack() as st:
        P._stack = st
        X = P.dram('X', [NB, 3, 128, 3, TG], F32, 'ExternalInput')
        Z = P.dram('Z', [NB, 64, NCH, 128], F32, 'ExternalInput')
        AB = P.dram('AB', [NB, 64, 2, 2, NCH], F32, 'ExternalInput')
        cwd = P.dram('cw', [128, 3, 3], F32, 'ExternalInput')
        scd = P.dram('sc', [64, 2, 2], F32, 'ExternalInput')
        nwd = P.dram('nw', [128], F32, 'ExternalInput')
        Md = P.dram('M', [64, 2, 5, 64], F32, 'ExternalInput')
        identd = P.dram('ident', [128, 128], F32, 'ExternalInput')
        Y = P.dram('Y', [NB, 64, NCH, 128], F32, 'ExternalOutput')

        ident = P.sb('identsb', [128, 128]); P.dma(ident[:], identd.ap(), reads=[identd], writes=[ident])
        Mt = P.sb('Mt', [64, 2, 5, 64]); P.dma(Mt[:], Md.ap(), reads=[Md], writes=[Mt])
        cw = P.sb('cwt', [128, 9]); P.dma(cw[:], AP(cwd, 0, [[9, 128], [1, 9]]), reads=[cwd], writes=[cw])
        sc = P.sb('sct', [64, 4]); P.dma(sc[:], AP(scd, 0, [[4, 64], [1, 4]]), reads=[scd], writes=[sc])
        nwb = P.sb('nwb', [64, 128]); P.dma(nwb[:], AP(nwd, 0, [[0, 64], [1, 128]]), reads=[nwd], writes=[nwb])
        ones = P.sb('ones', [128, 128]); P.op('vector', lambda e: e.memset(ones[:], 1.0), writes=[ones])
        one1 = P.sb('one1', [128, 1]); P.op('vector', lambda e: e.memset(one1[:], 1.0), writes=[one1])
        eps1 = P.sb('eps1', [128, 1]); P.op('vector', lambda e: e.memset(eps1[:], 1e-6), writes=[eps1])
        nea = P.sb('nea', [64, 2])
        for d in range(2):
            P.op('scalar', lambda e, d=d: e.activation(nea[:, d:d + 1], sc[:, 2 * d:2 * d + 1], AF.Exp), reads=[sc], writes=[nea])
        P.op('vector', lambda e: e.tensor_scalar(nea[:], nea[:], -1.0, None, ALU.mult), reads=[nea], writes=[nea])

        pp = [P.ps('pp%d' % i, [128, 512]) for i in range(5)]
        pr = [P.ps('pr%d' % i, [128, 128]) for i in range(3)]
        ppi = [0]

        def nps():
            p_ = pp[ppi[0] % 5]
            ppi[0] += 1
            return p_

        Qf = P.sb('Qf', [128, TG]); Kf = P.sb('Kf', [128, TG]); Vf = P.sb('Vf', [128, TG])
        Oacc = P.sb('Oacc', [64, NCH, 128])
        Zt = P.sb('Zt', [64, NCH, 128])
        stg = P.sb('stg', [128, 3, 256]); cacc = P.sb('cacc', [128, 256]); sq = P.sb('sqg', [128, 256]); rsb = P.sb('rsb', [128, 256])
        abt = P.sb('abt', [64, 2, 2, NCH])
        gt = P.sb('gt', [64, 2, NCH]); bet = P.sb('bet', [64, 2, NCH])
        S = P.sb('S', [128, 128]); S1 = P.sb('S1', [128, 128])
        W = 512
        gc = P.sb('gc', [64, 8]); egc = P.sb('egc', [64, 8]); ckb = P.sb('ckb', [64, 8]); ckd = P.sb('ckd', [64, 8])
        E3a = P.sb('E3a', [64, W]); E3b = P.sb('E3b', [64, W])
        Dm = P.sb('Dm', [64, W]); CA = P.sb('CA', [64, W]); CAT = P.sb('CAT', [64, W]); tmpw = P.sb('tmpw', [64, W])
        Pm = P.sb('Pm', [64, W]); PT = P.sb('PTm', [64, W]); TT = P.sb('TTm', [64, W])
        KB = P.sb('KB', [64, 8, 128]); BV = P.sb('BV', [64, 8, 128])
        dbl = []
        for i in range(2):
            dbl.append(dict(U=P.sb('U%d' % i, [64, 8, 128]), KD=P.sb('KD%d' % i, [64, 8, 128]), WT=P.sb('WT%d' % i, [128, W]),
                            QD=P.sb('QD%d' % i, [128, W]), QKT=P.sb('QKT%d' % i, [64, W]), egl=P.sb('egl%d' % i, [128, 8])))
        vnew = P.sb('vnew', [64, 128])
        otmp = P.sb('otmp', [64, 128])

        def V(fn, reads, writes):
            P.op('vector', fn, reads=reads, writes=writes)

        def G_(fn, reads, writes):
            P.op('gpsimd', fn, reads=reads, writes=writes)

        def A_(fn, reads, writes):
            P.op('scalar', fn, reads=reads, writes=writes)

        def T_(fn, reads, writes):
            P.op('tensor', fn, reads=reads, writes=writes)

        def mm256(out_fn, lhsT, rhs_fn, n, reads, ps_):
            for c0 in range(0, n, 256):
                m = min(256, n - c0)
                T_(lambda e, c0=c0, m=m: e.matmul(out_fn(c0, m), lhsT, rhs_fn(c0, m), start=True, stop=True), reads, [ps_])

        gcount = [0]
        for bb in range(NB):
            for s, dst in enumerate((Qf, Kf, Vf)):
                for t0 in range(0, TG, 256):
                    P.dma(stg[:], AP(X, ((bb * 3 + s) * 128) * 3 * TG + t0, [[3 * TG, 128], [TG, 3], [1, 256]]), reads=[X], writes=[stg])
                    V(lambda e, s=s: e.tensor_scalar(cacc[:], stg[:, 0, :], cw[:, s * 3:s * 3 + 1], None, ALU.mult), [stg, cw], [cacc])
                    V(lambda e, s=s: e.scalar_tensor_tensor(cacc[:], stg[:, 1, :], cw[:, s * 3 + 1:s * 3 + 2], cacc[:], ALU.mult, ALU.add), [stg, cw, cacc], [cacc])
                    V(lambda e, s=s: e.scalar_tensor_tensor(cacc[:], stg[:, 2, :], cw[:, s * 3 + 2:s * 3 + 3], cacc[:], ALU.mult, ALU.add), [stg, cw, cacc], [cacc])
                    if s == 2:
                        A_(lambda e, t0=t0, dst=dst: e.activation(dst[:, t0:t0 + 256], cacc[:], AF.Silu), [cacc], [dst])
                    else:
                        A_(lambda e: e.activation(cacc[:], cacc[:], AF.Silu), [cacc], [cacc])
                        A_(lambda e: e.activation(sq[:], cacc[:], AF.Square), [cacc], [sq])
                        ps_ = nps()
                        T_(lambda e, ps_=ps_: e.matmul(ps_[:, 0:256], ones[:], sq[:], start=True, stop=True), [ones, sq], [ps_])
                        A_(lambda e, ps_=ps_: e.activation(rsb[:], ps_[:, 0:256], AF.Sqrt, bias=eps1[:], scale=1.0), [ps_, eps1], [rsb])
                        V(lambda e: e.reciprocal(rsb[:], rsb[:]), [rsb], [rsb])
                        scl = 128 ** -0.5 if s == 0 else 1.0
                        V(lambda e, t0=t0, dst=dst, scl=scl: e.scalar_tensor_tensor(dst[:, t0:t0 + 256], cacc[:], scl, rsb[:], ALU.mult, ALU.mult),
                          [cacc, rsb], [dst])
            P.dma(abt[:], AP(AB, bb * 64 * 4 * NCH, [[4 * NCH, 64], [1, 4 * NCH]]), reads=[AB], writes=[abt])
            for d in range(2):
                V(lambda e, d=d: e.tensor_scalar(gt[:, d, :], abt[:, 0, d, :], sc[:, 2 * d + 1:2 * d + 2], None, ALU.add), [abt, sc], [gt])
                A_(lambda e, d=d: e.activation(gt[:, d, :], gt[:, d, :], AF.Exp), [gt], [gt])
                A_(lambda e, d=d: e.activation(gt[:, d, :], gt[:, d, :], AF.Ln, bias=one1[0:64, :], scale=1.0), [gt, one1], [gt])
                V(lambda e, d=d: e.tensor_scalar(gt[:, d, :], gt[:, d, :], nea[:, d:d + 1], None, ALU.mult), [gt, nea], [gt])
                A_(lambda e, d=d: e.activation(bet[:, d, :], abt[:, 1, d, :], AF.Sigmoid), [abt], [bet])
            Sd = [S, S1]
            V(lambda e: e.memset(S[:], 0.0), [], [S])
            V(lambda e: e.memset(S1[:], 0.0), [], [S1])
            G_(lambda e: e.memset(Oacc[:], 0.0), [], [Oacc])

            def pre(d, grp, B_):
                tri = Mt[:, d, 0, :]

                def mask(i, G, d=d):
                    return AP(Mt, (d * 5 + i) * 64, [[640, 64], [0, G], [1, 64]])
                G = len(grp)
                c_lo = min(grp)
                Wg = 64 * G
                col0 = 64 * c_lo
                U, KD, WT, QD, QKT, egl = B_['U'], B_['KD'], B_['WT'], B_['QD'], B_['QKT'], B_['egl']
                gsl = gt[:, d, c_lo:c_lo + G]
                bsl = bet[:, d, c_lo:c_lo + G]
                ps_ = nps()
                T_(lambda e, ps_=ps_, gsl=gsl, tri=tri: e.matmul(ps_[0:64, 0:G], tri, gsl, start=True, stop=True), [Mt, gt], [ps_])
                T_(lambda e, ps_=ps_, gsl=gsl: e.matmul(ps_[:, 8:8 + G], ones[0:64, :], gsl, start=True, stop=True), [ones, gt], [ps_])
                V(lambda e, ps_=ps_, G=G: e.tensor_copy(gc[:, 0:G], ps_[0:64, 0:G]), [ps_], [gc])
                A_(lambda e, ps_=ps_, G=G, egl=egl: e.activation(egl[:, 0:G], ps_[:, 8:8 + G], AF.Exp), [ps_], [egl])
                A_(lambda e, G=G: e.activation(egc[:, 0:G], gc[:, 0:G], AF.Exp), [gc], [egc])
                V(lambda e, G=G, bsl=bsl: e.tensor_tensor(ckb[:, 0:G], egc[:, 0:G], bsl, ALU.mult), [egc, bet], [ckb])
                V(lambda e, ps_=ps_, G=G: e.tensor_tensor(ckd[:, 0:G], ps_[0:64, 8:8 + G], gc[:, 0:G], ALU.subtract), [ps_, gc], [ckd])
                A_(lambda e, G=G: e.activation(ckd[:, 0:G], ckd[:, 0:G], AF.Exp), [ckd], [ckd])
                yield
                for (src, outs) in ((Kf, ((KB, ckb), (KD, ckd))), (Vf, ((BV, None),))):
                    for h0 in range(0, G, 4):
                        gh = min(4, G - h0)
                        ps_ = nps()
                        for j in range(gh):
                            c = c_lo + h0 + j
                            T_(lambda e, ps_=ps_, j=j, c=c, src=src: e.transpose(ps_[0:64, j * 128:(j + 1) * 128], src[:, 64 * c:64 * c + 64], ident[:]),
                               [src, ident], [ps_])
                        pv = AP(ps_, 0, [[512, 64], [128, gh], [1, 128]])
                        for (dst, coef) in outs:
                            dv = dst[:, h0:h0 + gh, :]
                            if coef is None:
                                cf = AP(bet, d * NCH + c_lo + h0, [[2 * NCH, 64], [1, gh], [0, 128]])
                                V(lambda e, dv=dv, pv=pv, cf=cf: e.tensor_tensor(dv, pv, cf, ALU.mult), [ps_, bet], [dst])
                            else:
                                cf = AP(coef, h0, [[8, 64], [1, gh], [0, 128]])
                                V(lambda e, dv=dv, pv=pv, cf=cf: e.tensor_tensor(dv, pv, cf, ALU.mult), [ps_, coef], [dst])
                yield
                idb = AP(ident, 0, [[128, 64], [0, G], [1, 64]])
                e3a = AP(E3a, 0, [[W, 64], [64, G], [1, 64]]); e3b = AP(E3b, 0, [[W, 64], [64, G], [1, 64]])
                V(lambda e, e3a=e3a, idb=idb, G=G: e.tensor_tensor(e3a, idb, AP(gc, 0, [[8, 64], [1, G], [0, 64]]), ALU.mult), [ident, gc], [E3a])
                V(lambda e, e3b=e3b, idb=idb, G=G, c_lo=c_lo: e.tensor_tensor(e3b, idb, AP(bet, d * NCH + c_lo, [[2 * NCH, 64], [1, G], [0, 64]]), ALU.mult),
                  [ident, bet], [E3b])
                psA = nps(); psB = nps(); psC = nps()
                mm256(lambda c0, m, psA=psA: psA[0:64, c0:c0 + m], ones[0:64, 0:64], lambda c0, m: E3a[:, c0:c0 + m], Wg, [ones, E3a], psA)
                mm256(lambda c0, m, psB=psB: psB[0:64, c0:c0 + m], ones[0:64, 0:64], lambda c0, m: E3b[:, c0:c0 + m], Wg, [ones, E3b], psB)
                mm256(lambda c0, m, psC=psC: psC[:, c0:c0 + m], ones[0:64, :], lambda c0, m: E3a[:, c0:c0 + m], Wg, [ones, E3a], psC)
                A_(lambda e, psC=psC, Wg=Wg, QD=QD: e.activation(QD[:, 0:Wg], psC[:, 0:Wg], AF.Exp), [psC], [QD])
                V(lambda e, Wg=Wg, QD=QD, col0=col0: e.tensor_tensor(QD[:, 0:Wg], QD[:, 0:Wg], Qf[:, col0:col0 + Wg], ALU.mult), [QD, Qf], [QD])
                yield
                dm3 = AP(Dm, 0, [[W, 64], [64, G], [1, 64]])
                V(lambda e, psA=psA, dm3=dm3, G=G: e.scalar_tensor_tensor(dm3, AP(psA, 0, [[512, 64], [64, G], [1, 64]]), -1.0,
                                                                          AP(gc, 0, [[8, 64], [1, G], [0, 64]]), ALU.mult, ALU.add), [psA, gc], [Dm])
                V(lambda e, Wg=Wg: e.tensor_scalar(tmpw[:, 0:Wg], Dm[:, 0:Wg], 0.0, None, ALU.min), [Dm], [tmpw])
                A_(lambda e, Wg=Wg: e.activation(tmpw[:, 0:Wg], tmpw[:, 0:Wg], AF.Exp), [tmpw], [tmpw])
                ca3 = AP(CA, 0, [[W, 64], [64, G], [1, 64]]); tw3 = AP(tmpw, 0, [[W, 64], [64, G], [1, 64]])
                V(lambda e, ca3=ca3, tw3=tw3, G=G, c_lo=c_lo: e.tensor_tensor(ca3, tw3, AP(bet, d * NCH + c_lo, [[2 * NCH, 64], [1, G], [0, 64]]), ALU.mult),
                  [tmpw, bet], [CA])
                G_(lambda e, ca3=ca3, G=G: e.tensor_tensor(ca3, ca3, mask(2, G), ALU.mult), [CA, Mt], [CA])
                V(lambda e, Wg=Wg: e.tensor_scalar(tmpw[:, 0:Wg], Dm[:, 0:Wg], -1.0, 0.0, ALU.mult, ALU.min), [Dm], [tmpw])
                A_(lambda e, Wg=Wg: e.activation(tmpw[:, 0:Wg], tmpw[:, 0:Wg], AF.Exp), [tmpw], [tmpw])
                cat3 = AP(CAT, 0, [[W, 64], [64, G], [1, 64]])
                V(lambda e, psB=psB, Wg=Wg: e.tensor_tensor(CAT[:, 0:Wg], tmpw[:, 0:Wg], psB[0:64, 0:Wg], ALU.mult), [tmpw, psB], [CAT])
                G_(lambda e, cat3=cat3, G=G: e.tensor_tensor(cat3, cat3, mask(4, G), ALU.mult), [CAT, Mt], [CAT])
                G_(lambda e, dm3=dm3, tw3=tw3, G=G: e.tensor_tensor(dm3, tw3, mask(3, G), ALU.mult), [tmpw, Mt], [Dm])
                yield
                psK = nps(); psQ = nps()
                for j in range(G):
                    c = c_lo + j
                    T_(lambda e, psK=psK, j=j, c=c: e.matmul(psK[0:64, 64 * j:64 * j + 64], Kf[:, 64 * c:64 * c + 64], Kf[:, 64 * c:64 * c + 64], start=True, stop=True),
                       [Kf], [psK])
                for j in range(G):
                    c = c_lo + j
                    T_(lambda e, psQ=psQ, j=j, c=c: e.matmul(psQ[0:64, 64 * j:64 * j + 64], Kf[:, 64 * c:64 * c + 64], Qf[:, 64 * c:64 * c + 64], start=True, stop=True),
                       [Kf, Qf], [psQ])
                V(lambda e, psK=psK, Wg=Wg: e.tensor_tensor(Pm[:, 0:Wg].bitcast(F32R), psK[0:64, 0:Wg], CA[:, 0:Wg], ALU.mult), [psK, CA], [Pm])
                V(lambda e, psK=psK, Wg=Wg: e.tensor_tensor(PT[:, 0:Wg].bitcast(F32R), psK[0:64, 0:Wg], CAT[:, 0:Wg], ALU.mult), [psK, CAT], [PT])
                V(lambda e, psQ=psQ, Wg=Wg, QKT=QKT: e.tensor_tensor(QKT[:, 0:Wg], psQ[0:64, 0:Wg], Dm[:, 0:Wg], ALU.mult), [psQ, Dm], [QKT])
                yield
                tt3 = AP(TT, 0, [[W, 64], [64, G], [1, 64]]); pt3 = AP(PT, 0, [[W, 64], [64, G], [1, 64]])
                V(lambda e, tt3=tt3, pt3=pt3, idb=idb: e.tensor_tensor(tt3.bitcast(F32R), idb, pt3, ALU.subtract), [ident, PT], [TT])
                for lev in range(5):
                    yield
                    p1 = nps(); p2 = nps(); p3 = nps()
                    for j in range(G):
                        sl = slice(64 * j, 64 * j + 64)
                        T_(lambda e, p1=p1, sl=sl: e.matmul(p1[0:64, sl], PT[:, sl].bitcast(F32R), Pm[:, sl].bitcast(F32R), start=True, stop=True), [PT, Pm], [p1])
                    for j in range(G):
                        sl = slice(64 * j, 64 * j + 64)
                        T_(lambda e, p2=p2, sl=sl: e.matmul(p2[0:64, sl], Pm[:, sl].bitcast(F32R), PT[:, sl].bitcast(F32R), start=True, stop=True), [PT, Pm], [p2])
                    V(lambda e, p1=p1, Wg=Wg: e.tensor_copy(Pm[:, 0:Wg].bitcast(F32R), p1[0:64, 0:Wg]), [p1], [Pm])
                    A_(lambda e, p2=p2, Wg=Wg: e.copy(PT[:, 0:Wg].bitcast(F32R), p2[0:64, 0:Wg]), [p2], [PT])
                    for j in range(G):
                        sl = slice(64 * j, 64 * j + 64)
                        T_(lambda e, p3=p3, sl=sl: e.matmul(p3[0:64, sl], Pm[:, sl].bitcast(F32R), TT[:, sl].bitcast(F32R), start=True, stop=True), [Pm, TT], [p3])
                    V(lambda e, p3=p3, Wg=Wg: e.tensor_tensor(TT[:, 0:Wg].bitcast(F32R), TT[:, 0:Wg], p3[0:64, 0:Wg], ALU.add), [p3, TT], [TT])
                yield
                for h0 in range(0, G, 4):
                    gh = min(4, G - h0)
                    ps_ = nps()
                    for j in range(gh):
                        jj = h0 + j
                        T_(lambda e, ps_=ps_, j=j, jj=jj: e.matmul(ps_[0:64, j * 128:(j + 1) * 128], TT[:, 64 * jj:64 * jj + 64], BV[:, jj, :], start=True, stop=True),
                           [TT, BV], [ps_])
                    A_(lambda e, ps_=ps_, h0=h0, gh=gh, U=U: e.copy(U[:, h0:h0 + gh, :], AP(ps_, 0, [[512, 64], [128, gh], [1, 128]])), [ps_], [U])
                ps_ = nps()
                for j in range(G):
                    T_(lambda e, ps_=ps_, j=j: e.matmul(ps_[:, 64 * j:64 * j + 64], KB[:, j, :], TT[:, 64 * j:64 * j + 64], start=True, stop=True), [KB, TT], [ps_])
                V(lambda e, ps_=ps_, Wg=Wg, WT=WT: e.tensor_copy(WT[:, 0:Wg], ps_[:, 0:Wg]), [ps_], [WT])
                yield

            def rec(d, grp, B_):
                S_ = Sd[d]
                c_lo = min(grp)
                U, KD, WT, QD, QKT, egl = B_['U'], B_['KD'], B_['WT'], B_['QD'], B_['QKT'], B_['egl']
                for c in grp:
                    j = c - c_lo
                    sl = slice(64 * j, 64 * j + 64)
                    p1, p2, p3 = pr
                    T_(lambda e, p1=p1, sl=sl, WT=WT: e.matmul(p1[0:64, :], WT[:, sl], S_[:], start=True, stop=True), [WT, S_], [p1])
                    V(lambda e, p1=p1, j=j, U=U: e.tensor_tensor(vnew[:], U[:, j, :], p1[0:64, :], ALU.subtract), [U, p1], [vnew])
                    yield
                    T_(lambda e, p2=p2, sl=sl, QD=QD: e.matmul(p2[0:64, :], QD[:, sl], S_[:], start=True, stop=False), [QD, S_], [p2])
                    T_(lambda e, p2=p2, sl=sl, QKT=QKT: e.matmul(p2[0:64, :], QKT[:, sl], vnew[:], start=False, stop=True), [QKT, vnew], [p2])
                    T_(lambda e, p3=p3, j=j, KD=KD: e.matmul(p3[:, :], KD[:, j, :], vnew[:], start=True, stop=True), [KD, vnew], [p3])
                    V(lambda e, p3=p3, j=j, egl=egl: e.scalar_tensor_tensor(S_[:], S_[:], egl[:, j:j + 1], p3[:, :], ALU.mult, ALU.add), [S_, egl, p3], [S_])
                    V(lambda e, p2=p2, c=c: e.tensor_tensor(Oacc[:, c, :], Oacc[:, c, :], p2[0:64, :], ALU.add), [p2, Oacc], [Oacc])
                    yield

            def drain(g):
                for _ in g:
                    pass

            def interleave(g1, g2):
                a1 = a2 = True
                while a1 or a2:
                    if a1:
                        try:
                            next(g1)
                        except StopIteration:
                            a1 = False
                    if a2:
                        try:
                            next(g2)
                        except StopIteration:
                            a2 = False

            groups = [gdn_groups(0), gdn_groups(1)]
            order = [(d, gi) for gi in range(9) for d in range(2)]
            prev = None
            for idx, (d, gi) in enumerate(order):
                B_ = dbl[idx % 2]
                gp = pre(d, groups[d][gi], B_)
                if prev is None:
                    drain(gp)
                else:
                    interleave(prev, gp)
                prev = rec(d, groups[d][gi], B_)
            drain(prev)
            P.dma(Zt[:], AP(Z, bb * 64 * NCH * 128, [[NCH * 128, 64], [1, NCH * 128]]), reads=[Z], writes=[Zt])
            A_(lambda e: e.activation(Zt[:], Zt[:], AF.Silu), [Zt], [Zt])
            ssq = gt
            for h0 in range(0, NCH, 4):
                V(lambda e, h0=h0: e.tensor_tensor(KB[:, 0:4, :], Oacc[:, h0:h0 + 4, :], Oacc[:, h0:h0 + 4, :], ALU.mult), [Oacc], [KB])
                V(lambda e, h0=h0: e.tensor_reduce(ssq[:, 0, h0:h0 + 4], KB[:, 0:4, :], AX.X, ALU.add), [KB], [gt])
            A_(lambda e: e.activation(ssq[:, 0, :], ssq[:, 0, :], AF.Sqrt, bias=eps1[0:64, :], scale=1.0 / 128), [gt, eps1], [gt])
            V(lambda e: e.reciprocal(ssq[:, 0, :], ssq[:, 0, :]), [gt], [gt])
            V(lambda e: e.tensor_tensor(Oacc[:], Oacc[:], AP(gt, 0, [[2 * NCH, 64], [1, NCH], [0, 128]]), ALU.mult), [Oacc, gt], [Oacc])
            G_(lambda e: e.tensor_tensor(Oacc[:], Oacc[:], AP(nwb, 0, [[128, 64], [0, NCH], [1, 128]]), ALU.mult), [Oacc, nwb], [Oacc])
            V(lambda e: e.tensor_tensor(Oacc[:], Oacc[:], Zt[:], ALU.mult), [Oacc, Zt], [Oacc])
            P.dma(AP(Y, bb * 64 * NCH * 128, [[NCH * 128, 64], [1, NCH * 128]]), Oacc[:], reads=[Oacc], writes=[])
        P.finish()
        nc = P.emit()
    return nc


def gdn_masks():
    i = np.arange(64)
    M = np.zeros((64, 2, 5, 64), np.float32)
    for d in range(2):
        le = (i[:, None] <= i[None, :]) if d == 0 else (i[:, None] >= i[None, :])
        incl = (i[:, None] >= i[None, :]) if d == 0 else (i[:, None] <= i[None, :])
        strict = (i[:, None] > i[None, :]) if d == 0 else (i[:, None] < i[None, :])
        M[:, d, 0] = le; M[:, d, 1] = incl; M[:, d, 2] = strict; M[:, d, 3] = incl.T; M[:, d, 4] = strict.T
    return M


def gdn_inputs(h, pb_list, inp, L_):
    NB = len(pb_list)
    X = np.zeros((NB, 3, 128, 3, TG), np.float32)
    Zz = np.empty((NB, 64, NCH, 128), np.float32)
    AB = np.empty((NB, 64, 2, 2, NCH), np.float32)
    for bi, pb in enumerate(pb_list):
        seqs = [(pb[4096:4352], 0, 256), (pb[0:4096], 256, 4096)]
        for (ps_, off, L) in seqs:
            for s in range(3):
                x = ps_[:, s * 512 + h * 128:s * 512 + (h + 1) * 128]
                xp = np.pad(x, ((1, 1), (0, 0)))
                for k in range(3):
                    X[bi, s, :, k, off:off + L] = xp[k:k + L].T
        tok = np.concatenate([pb[4096:4352], pb[0:4096]], 0)
        z = tok[:, 1536 + h * 128:1536 + (h + 1) * 128]
        Zz[bi] = z.reshape(NCH, 64, 128).transpose(1, 0, 2)
        a = tok[:, 2048:2056].reshape(TG, 2, 4)[:, :, h]
        bt = tok[:, 2056:2064].reshape(TG, 2, 4)[:, :, h]
        AB[bi, :, 0] = a.reshape(NCH, 64, 2).transpose(1, 2, 0)
        AB[bi, :, 1] = bt.reshape(NCH, 64, 2).transpose(1, 2, 0)
    cw = np.ascontiguousarray(inp['gdn_conv_w'][L_].reshape(3, 3, 4, 128)[:, :, h, :].transpose(2, 1, 0))
    sc = np.empty((64, 2, 2), np.float32)
    sc[:, :, 0] = inp['gdn_a_log'][L_][:, h][None, :]
    sc[:, :, 1] = inp['gdn_dt_bias'][L_][:, h][None, :]
    return {'X': X, 'Z': Zz, 'AB': AB, 'cw': cw, 'sc': sc, 'nw': inp['gdn_norm_w'][L_], 'M': gdn_masks(), 'ident': np.eye(128, dtype=np.float32)}


def gdn_unpack(Y):
    t = Y.transpose(1, 0, 2).reshape(TG, 128)
    return t[256:], t[:256]


T4 = 2176


def build_l4():
    P = Prog()
    with ExitStack() as st:
        P._stack = st
        xT = P.dram('xT', [128, NKC, T4], F32, 'ExternalInput')
        fw = P.dram('fw', [128, NKC], F32, 'ExternalInput')
        oT = P.dram('oT', [128, NKC, T4], F32, 'ExternalOutput')
        fwt = P.sb('fwt', [128, NKC])
        P.dma(fwt[:], fw.ap(), reads=[fw], writes=[fwt])
        ones_bf = P.sb('ones_bf', [128, 128], BF16)
        P.op('vector', lambda e: e.memset(ones_bf[:], 1.0), writes=[ones_bf])
        eps_t = P.sb('eps_t', [128, 1])
        P.op('vector', lambda e: e.memset(eps_t[:], 1e-6), writes=[eps_t])
        SUB = 256
        xs = [P.sb('xs%d' % i, [128, NKC, SUB]) for i in range(2)]
        sq = P.sb('sq', [128, NKC, SUB], BF16)
        pss = P.ps('ps_ss', [128, SUB])
        rs = P.sb('rs', [128, SUB])
        tmp = P.sb('ntmp', [128, NKC, SUB])
        ot = [P.sb('ot%d' % i, [128, NKC, SUB]) for i in range(2)]
        nsub = (T4 + SUB - 1) // SUB
        for s in range(nsub):
            t0 = s * SUB
            n = min(SUB, T4 - t0)
            x_ = xs[s % 2]
            o_ = ot[s % 2]
            P.dma(x_[:, :, 0:n], AP(xT, t0, [[NKC * T4, 128], [T4, NKC], [1, n]]), reads=[xT], writes=[x_])
            P.op('scalar', lambda e, x_=x_, n=n: e.activation(sq[:, :, 0:n], x_[:, :, 0:n], AF.Square), reads=[x_], writes=[sq])
            for c in range(NKC):
                P.op('tensor', lambda e, c=c, n=n: e.matmul(pss[:, 0:n], ones_bf[:], sq[:, c, 0:n], start=(c == 0), stop=(c == NKC - 1)),
                     reads=[sq, ones_bf], writes=[pss])
            P.op('scalar', lambda e, n=n: e.activation(rs[:, 0:n], pss[:, 0:n], AF.Sqrt, bias=eps_t[:], scale=1.0 / D),
                 reads=[pss, eps_t], writes=[rs])
            P.op('vector', lambda e, n=n: e.reciprocal(rs[:, 0:n], rs[:, 0:n]), reads=[rs], writes=[rs])
            P.op('vector', lambda e, x_=x_, n=n: e.tensor_tensor(
                tmp[:, :, 0:n], x_[:, :, 0:n], AP(rs, 0, [[SUB, 128], [0, NKC], [1, n]]), ALU.mult), reads=[x_, rs], writes=[tmp])
            P.op('vector', lambda e, o_=o_, n=n: e.tensor_tensor(
                o_[:, :, 0:n], tmp[:, :, 0:n], AP(fwt, 0, [[NKC, 128], [1, NKC], [0, n]]), ALU.mult), reads=[tmp, fwt], writes=[o_])
            P.dma(AP(oT, t0, [[NKC * T4, 128], [T4, NKC], [1, n]]), o_[:, :, 0:n], reads=[o_], writes=[])
        P.finish()
        nc = P.emit()
    return nc


def _fm(a):
    return np.ascontiguousarray(a.T.reshape(16, 128, a.shape[0]).transpose(1, 0, 2))


def _unfm(a):
    return np.ascontiguousarray(a.transpose(2, 1, 0).reshape(a.shape[2], 2048))


def _rope_tables():
    n_freq = 16
    freqs = (10000.0 ** (-np.arange(n_freq, dtype=np.float32) / n_freq)).astype(np.float32)
    row = np.repeat(np.arange(64, dtype=np.float32), 64)
    col = np.tile(np.arange(64, dtype=np.float32), 64)
    ang = np.concatenate([row[:, None] * freqs, col[:, None] * freqs], -1)
    c = np.cos(ang).T.astype(np.float32)
    s = np.sin(ang).T.astype(np.float32)
    C = np.ones((64, TA), np.float32)
    S = np.zeros((64, TA), np.float32)
    C[:32, :4096] = c; C[32:, :4096] = c; S[:32, :4096] = -s; S[32:, :4096] = s
    rm = np.zeros((64, 64), np.float32)
    for m in range(64):
        rm[(m + 32) % 64, m] = 1
    return C, S, rm


def _run(nc, in_maps):
    res = run_bass_kernel_spmd(nc, in_maps, core_ids=list(range(8)))
    return res.results


def kernel(**inp):
    inp = {k: np.asarray(v) for k, v in inp.items()}
    x = inp['x']; ctx = inp['ctx']
    B = 4
    mod = run_l0(inp['c'], inp['c_ctx'], inp['w_ada'], inp['b_ada'])
    C, S, rm = _rope_tables()
    ident = np.eye(128, dtype=np.float32)
    nc1 = build_l1(); nc2 = build_l2a(); nc3 = build_l3(); nc4 = build_l4(); ncg = build_l2g(2); nch = build_l2h()
    xl = x.astype(np.float32); xc = ctx.astype(np.float32)
    for L in range(2):
        m = mod[L]

        def mrow(r, i):
            return vecT(m[r, i * 2048:(i + 1) * 2048])
        ims = []
        for core in range(8):
            b, hh = core // 2, core % 2
            xcat = np.concatenate([xl[b, hh * 2048:(hh + 1) * 2048], xc[b, hh * 128:(hh + 1) * 128]], 0)
            mv = np.stack([vecT(inp['norm1_w'][L]), mrow(b, 1), mrow(b, 0), mrow(4, 1), mrow(4, 0)])
            ims.append({'xT': _fm(xcat), 'mv': mv, 'W': inp['w_in'][L]})
        r1 = _run(nc1, ims)
        p = np.empty((B, TA, IN_DIM), np.float32)
        for core in range(8):
            b, hh = core // 2, core % 2
            pT = r1[core]['pT']
            p[b, hh * 2048:(hh + 1) * 2048] = pT[:, :2048].T
            p[b, 4096 + hh * 128:4096 + (hh + 1) * 128] = pT[:, 2048:].T
        del r1
        o = 2048 + 16
        nw = np.zeros((128, 8), np.float32)
        nw[:, 0:4] = inp['mla_q_norm_w'][L].reshape(4, 128).T
        nw[:, 4:6] = inp['mla_kv_norm_w'][L].reshape(2, 128).T
        nw[:64, 6] = inp['gqa_q_norm_w'][L]; nw[:64, 7] = inp['gqa_k_norm_w'][L]
        ims = []
        for core in range(8):
            b, hh = core // 2, core % 2
            pb = p[b]
            o2 = o + 832
            ims.append({'gq': np.ascontiguousarray(pb[:, o2 + hh * 256:o2 + (hh + 1) * 256].T),
                        'gk': np.ascontiguousarray(pb[:, o2 + 512 + hh * 64:o2 + 512 + (hh + 1) * 64].T),
                        'gv': np.ascontiguousarray(pb[:, o2 + 640 + hh * 64:o2 + 640 + (hh + 1) * 64]),
                        'cq': np.ascontiguousarray(pb[:, o:o + 512].T), 'ckv': np.ascontiguousarray(pb[:, o + 512:o + 768].T),
                        'kr': np.ascontiguousarray(pb[:, o + 768:o + 832].T),
                        'wuq': np.ascontiguousarray(inp['mla_w_uq'][L][:, 2 * hh:2 * hh + 2].reshape(512, 384)),
                        'wukv': np.ascontiguousarray(inp['mla_w_ukv'][L][:, 2 * hh:2 * hh + 2].reshape(256, 512)),
                        'nw': nw, 'cosd': C, 'sind': S, 'rmd': rm})
        r2 = _run(nc2, ims)
        br = np.zeros((B, TA, 4, 512), np.float32)
        for core in range(8):
            b, hh = core // 2, core % 2
            br[b, :, 1, hh * 256:(hh + 1) * 256] = r2[core]['om']
            br[b, :, 2, hh * 256:(hh + 1) * 256] = r2[core]['og']
        del r2
        ims = []
        for core in range(8):
            h, bp = core // 2, core % 2
            ims.append(gdn_inputs(h, [p[2 * bp], p[2 * bp + 1]], inp, L))
        rg = _run(ncg, ims)
        for core in range(8):
            h, bp = core // 2, core % 2
            for bi in range(2):
                lat, cx = gdn_unpack(rg[core]['Y'][bi])
                br[2 * bp + bi, :4096, 0, h * 128:(h + 1) * 128] = lat
                br[2 * bp + bi, 4096:, 0, h * 128:(h + 1) * 128] = cx
        del rg, ims
        pl_h = np.ascontiguousarray(p[:, :4096, 3664:5200]); pc_h = np.ascontiguousarray(p[:, 4096:, 3664:5200])
        ims = [hy_inputs(core, pl_h, pc_h, inp, L) for core in range(8)]
        rh = _run(nch, ims)
        for core in range(8):
            br[:, :4096, 3, core * 64:(core + 1) * 64] = hy_unpack(rh[core]['yL'], 4096)
            br[:, 4096:, 3, core * 64:(core + 1) * 64] = hy_unpack(rh[core]['yC'], 256)
        del rh, ims
        wbr = np.ascontiguousarray(inp['w_branch'][L].reshape(2048, 2048))
        wr = np.ascontiguousarray(inp['w_router'].reshape(16, 128, 16).transpose(1, 0, 2))
        nxl = np.empty_like(xl); nxc = np.empty_like(xc)
        for half in range(2):
            ims = []
            for core in range(8):
                sh = half * 8 + core
                b, q = sh // 4, sh % 4
                ls = slice(q * 1024, (q + 1) * 1024); cs_ = slice(q * 64, (q + 1) * 64)
                cs2 = slice(4096 + q * 64, 4096 + (q + 1) * 64)
                xcat = np.concatenate([xl[b, ls], xc[b, cs_]], 0)
                brc = np.concatenate([br[b, ls], br[b, cs2]], 0).reshape(T3, 2048)
                g = np.concatenate([p[b, ls, MIX_IN:], p[b, cs2, MIX_IN:]], 0)
                mv = np.stack([vecT(inp['norm2_w'][L]), mrow(b, 2), mrow(b, 3), mrow(b, 4), mrow(b, 5),
                               mrow(4, 2), mrow(4, 3), mrow(4, 4), mrow(4, 5)])
                ims.append({'xT': _fm(xcat), 'brT': np.ascontiguousarray(brc.T), 'gT': np.ascontiguousarray(g.T), 'mv': mv,
                            'wbr': wbr, 'wo': inp['w_out'][L], 'wr': wr, 'rb': inp['router_bias'],
                            'wg': inp['moe_w_gate'][L], 'wu': inp['moe_w_up'][L], 'wd': inp['moe_w_down'][L], 'ident': ident})
            r3 = _run(nc3, ims)
            for core in range(8):
                sh = half * 8 + core
                b, q = sh // 4, sh % 4
                x2 = _unfm(r3[core]['x2T'])
                nxl[b, q * 1024:(q + 1) * 1024] = x2[:1024]
                nxc[b, q * 64:(q + 1) * 64] = x2[1024:]
            del r3
        xl, xc = nxl, nxc
        del p, br
    out = np.empty_like(xl)
    fw = vecT(inp['final_norm_w'])
    ims = []
    for core in range(8):
        b, hh = core // 2, core % 2
        xcat = np.concatenate([xl[b, hh * 2048:(hh + 1) * 2048], xc[b, hh * 128:(hh + 1) * 128]], 0)
        ims.append({'xT': _fm(xcat), 'fw': fw})
    r4 = _run(nc4, ims)
    for core in range(8):
        b, hh = core // 2, core % 2
        out[b, hh * 2048:(hh + 1) * 2048] = _unfm(r4[core]['oT'])[:2048]
    return out.astype(np.float32)
```
